# Optimizing a Trainium2 kernel written in Bass

```python
import jax, jax.numpy as jnp
from jax import lax
import numpy as np

D_MODEL = 2048
BATCH = 8
SEQ = 2048
DEPTH = 1

RET_HEADS = 4
RET_DK = 256
RET_DV = 256
RET_CHUNK = 128
RET_WIDTH = RET_HEADS * RET_DV
NSA_HEADS = 8
NSA_KV_GROUPS = 2
NSA_HPG = NSA_HEADS // NSA_KV_GROUPS
NSA_DK = 128
NSA_DV = 128
NSA_WIDTH = NSA_HEADS * NSA_DV
MIX_WIDTH = RET_WIDTH + NSA_WIDTH
CMP_BLOCK = 32
CMP_STRIDE = 16
CMP_HIDDEN = 256
SEL_BLOCK = 64
SEL_COUNT = 16
WIN = 512
WIN_QBLOCK = 128
SEL_QCHUNK = 64
N_GROUPS = 8
EXP_PER_GROUP = 8
N_EXPERTS = N_GROUPS * EXP_PER_GROUP
TOP_K_IN_GROUP = 2
D_EXPERT = 512
MOE_BLOCK = 128
ROPE_BASE = 10000.0
EPS = 1e-6
NEG = -1e30
FORCE_BONUS = 1e4
PROJ_SIZES = (
    RET_HEADS * RET_DK, RET_HEADS * RET_DK, RET_WIDTH, RET_WIDTH,
    NSA_HEADS * NSA_DK,
    NSA_KV_GROUPS * NSA_DK, NSA_KV_GROUPS * NSA_DV,
    NSA_KV_GROUPS * NSA_DK, NSA_KV_GROUPS * NSA_DV,
    NSA_KV_GROUPS * NSA_DK, NSA_KV_GROUPS * NSA_DV,
    NSA_HEADS * 3,
)
PROJ_TOTAL = sum(PROJ_SIZES)

kernel_name = 'hybrid_retention_nsa_hmoe_block'


def split_cols(a):
    outs, off = [], 0
    for s in PROJ_SIZES:
        outs.append(a[..., off:off + s])
        off += s
    return outs


def rms_norm(x, g):
    xf = x.astype(jnp.float32)
    y = xf * lax.rsqrt(jnp.mean(xf * xf, axis=-1, keepdims=True) + EPS)
    return (y * g).astype(x.dtype)


def masked_softmax(s, mask):
    s = jnp.where(mask, s.astype(jnp.float32), NEG)
    p = jax.nn.softmax(s, axis=-1)
    return jnp.where(mask, p, 0.0)


def rotary(x, pos):
    half = x.shape[-1] // 2
    inv = ROPE_BASE ** (-jnp.arange(half, dtype=jnp.float32) / half)
    ang = pos.astype(jnp.float32)[:, None] * inv[None, :]
    cos = jnp.cos(ang)[None, :, None, :]
    sin = jnp.sin(ang)[None, :, None, :]
    x1, x2 = x[..., :half], x[..., half:]
    return jnp.concatenate([x1 * cos - x2 * sin, x1 * sin + x2 * cos], axis=-1).astype(x.dtype)


def retention(q, k, v, g, gn_g):
    B, T, H, dk = q.shape
    dv = v.shape[-1]
    C = RET_CHUNK
    NC = T // C
    pos = jnp.arange(T)
    q = rotary(q, pos)
    k = rotary(k, pos) * (dk ** -0.5)
    log_gamma = jnp.log1p(-jnp.exp2(-5.0 - jnp.arange(H, dtype=jnp.float32)))
    idx = jnp.arange(C, dtype=jnp.float32)
    rel = idx[:, None] - idx[None, :]
    decay_in = jnp.where(rel >= 0, jnp.exp(log_gamma[:, None, None] * jnp.maximum(rel, 0.0)), 0.0)
    zeta = jnp.exp(log_gamma[:, None] * (C - 1 - idx)[None, :])
    q_decay = jnp.exp(log_gamma[:, None] * (idx + 1)[None, :])
    chunk_decay = jnp.exp(log_gamma * C)

    def to_chunks(a):
        return a.reshape(B, NC, C, H, a.shape[-1]).transpose(0, 3, 1, 2, 4)

    qc, kc, vc = to_chunks(q), to_chunks(k), to_chunks(v)
    s = jnp.einsum('bhncd,bhnmd->bhncm', qc, kc) * decay_in[None, :, None]
    inner = jnp.einsum('bhncm,bhnme->bhnce', s, vc)
    kv = jnp.einsum('bhnmd,bhnme->nbhde', kc * zeta[None, :, None, :, None], vc).astype(jnp.float32)

    def step(S, kv_n):
        return chunk_decay[None, :, None, None] * S + kv_n, S

    _, S_prev = lax.scan(step, jnp.zeros((B, H, dk, dv), jnp.float32), kv)
    cross = jnp.einsum('bhncd,nbhde->bhnce', qc.astype(jnp.float32), S_prev) * q_decay[None, :, None, :, None]
    o = (inner + cross).astype(jnp.float32).transpose(0, 2, 3, 1, 4).reshape(B, T, H, dv)
    mu = jnp.mean(o, axis=-1, keepdims=True)
    var = jnp.mean(jnp.square(o - mu), axis=-1, keepdims=True)
    o = ((o - mu) * lax.rsqrt(var + EPS)).reshape(B, T, H * dv) * gn_g
    return o * jax.nn.silu(g.astype(jnp.float32))


def nsa(q, k_cmp, v_cmp, k_slc, v_slc, k_win, v_win, gate_logits,
        pos_k, w1_k, w2_k, pos_v, w1_v, w2_v):
    B, T = q.shape[:2]
    G, Hg, d = NSA_KV_GROUPS, NSA_HPG, NSA_DK
    qg = (q * (d ** -0.5)).reshape(B, T, G, Hg, d).transpose(0, 2, 3, 1, 4)
    t_pos = jnp.arange(T)

    def kv_heads(a):
        return a.reshape(B, T, G, -1).transpose(0, 2, 1, 3)

    Nc = (T - CMP_BLOCK) // CMP_STRIDE + 1
    blk_idx = jnp.arange(Nc)[:, None] * CMP_STRIDE + jnp.arange(CMP_BLOCK)[None, :]

    def compress(a, pe, w1, w2):
        blocks = kv_heads(a)[:, :, blk_idx] + pe
        flat = blocks.reshape(B, G, Nc, -1)
        return jax.nn.silu(flat @ w1) @ w2

    kc = compress(k_cmp, pos_k, w1_k, w2_k)
    vc = compress(v_cmp, pos_v, w1_v, w2_v)
    cmp_end = jnp.arange(Nc) * CMP_STRIDE + CMP_BLOCK - 1
    cmask = cmp_end[None, :] <= t_pos[:, None]
    p_cmp = masked_softmax(jnp.einsum('bghtd,bgcd->bghtc', qg, kc), cmask)
    o_cmp = jnp.einsum('bghtc,bgcd->bghtd', p_cmp.astype(vc.dtype), vc)

    Ns = T // SEL_BLOCK
    n_sel = min(SEL_COUNT, Ns)
    cmp_start = jnp.arange(Nc) * CMP_STRIDE
    sel_start = jnp.arange(Ns) * SEL_BLOCK
    overlap = ((cmp_start[:, None] < sel_start[None, :] + SEL_BLOCK)
               & (cmp_start[:, None] + CMP_BLOCK > sel_start[None, :])).astype(jnp.float32)
    imp = jnp.einsum('bghtc,cs->bgts', p_cmp, overlap)
    cur = t_pos // SEL_BLOCK
    sblk = jnp.arange(Ns)
    valid = sblk[None, :] <= cur[:, None]
    forced = (sblk[None, :] == 0) | (sblk[None, :] == cur[:, None]) | (sblk[None, :] == cur[:, None] - 1)
    score = jnp.where(valid, imp + jnp.where(forced, FORCE_BONUS, 0.0), -1.0)
    _, sel_idx = lax.top_k(score, n_sel)

    ks_blocks = kv_heads(k_slc).reshape(B, G, Ns, SEL_BLOCK, -1)
    vs_blocks = kv_heads(v_slc).reshape(B, G, Ns, SEL_BLOCK, -1)
    QC = min(SEL_QCHUNK, T)
    NQ = T // QC
    q_ch = qg.reshape(B, G, Hg, NQ, QC, d).transpose(3, 0, 1, 2, 4, 5)
    idx_ch = sel_idx.reshape(B, G, NQ, QC, n_sel).transpose(2, 0, 1, 3, 4)
    t_ch = t_pos.reshape(NQ, QC)
    b_ix = jnp.arange(B)[:, None, None, None]
    g_ix = jnp.arange(G)[None, :, None, None]

    def sel_chunk(args):
        qq, ii, tt = args
        kk = ks_blocks[b_ix, g_ix, ii]
        vv = vs_blocks[b_ix, g_ix, ii]
        kpos = ii[..., None] * SEL_BLOCK + jnp.arange(SEL_BLOCK)
        mask = (kpos <= tt[None, None, :, None, None]).reshape(B, G, 1, QC, -1)
        s = jnp.einsum('bghqd,bgqnld->bghqnl', qq, kk).reshape(B, G, Hg, QC, -1)
        p = masked_softmax(s, mask).reshape(B, G, Hg, QC, n_sel, SEL_BLOCK)
        return jnp.einsum('bghqnl,bgqnld->bghqd', p.astype(vv.dtype), vv)

    o_slc = lax.map(sel_chunk, (q_ch, idx_ch, t_ch))
    o_slc = o_slc.transpose(1, 2, 3, 0, 4, 5).reshape(B, G, Hg, T, -1)

    WB = WIN_QBLOCK
    NB = T // WB
    nprev = WIN // WB

    def windows(a):
        ap = jnp.pad(kv_heads(a), ((0, 0), (0, 0), (WIN, 0), (0, 0))).reshape(B, G, NB + nprev, WB, -1)
        return jnp.stack([ap[:, :, i:i + NB] for i in range(nprev + 1)], axis=3).reshape(B, G, NB, (nprev + 1) * WB, -1)

    kw, vw = windows(k_win), windows(v_win)
    qpos = t_pos.reshape(NB, WB)
    kpos = (jnp.arange(NB) * WB - WIN)[:, None] + jnp.arange((nprev + 1) * WB)[None, :]
    delta = qpos[:, :, None] - kpos[:, None, :]
    wmask = (delta >= 0) & (delta < WIN) & (kpos[:, None, :] >= 0)
    qb = qg.reshape(B, G, Hg, NB, WB, d)
    p_win = masked_softmax(jnp.einsum('bghnqd,bgnkd->bghnqk', qb, kw), wmask)
    o_win = jnp.einsum('bghnqk,bgnkd->bghnqd', p_win.astype(vw.dtype), vw).reshape(B, G, Hg, T, -1)

    gt = jax.nn.sigmoid(gate_logits.astype(jnp.float32)).reshape(B, T, G, Hg, 3).transpose(0, 2, 3, 1, 4)
    o = gt[..., 0:1] * o_cmp + gt[..., 1:2] * o_slc + gt[..., 2:3] * o_win
    return o.transpose(0, 3, 1, 2, 4).reshape(B, T, NSA_HEADS * NSA_DV)


def hier_moe(h, w_grp, b_grp, w_exp, b_exp, w_gate, w_up, w_down):
    B, T, D = h.shape
    N = B * T
    K = TOP_K_IN_GROUP
    hf = h.reshape(N, D)
    pg = jax.nn.softmax((hf @ w_grp).astype(jnp.float32) + b_grp, axis=-1)
    pg_top, grp = lax.top_k(pg, 1)
    le = ((hf @ w_exp).astype(jnp.float32) + b_exp).reshape(N, N_GROUPS, EXP_PER_GROUP)
    le_g = le[jnp.arange(N), grp[:, 0]]
    pe_top, loc = lax.top_k(jax.nn.softmax(le_g, axis=-1), K)
    wts = pg_top * pe_top / jnp.sum(pe_top, axis=-1, keepdims=True)
    eid = grp * EXP_PER_GROUP + loc

    A = N * K
    e_flat = eid.reshape(A)
    tok = jnp.repeat(jnp.arange(N, dtype=jnp.int32), K)
    w_flat = wts.reshape(A)
    order = jnp.argsort(e_flat)
    e_sorted = e_flat[order]
    counts = jnp.bincount(e_flat, length=N_EXPERTS)
    padded = (counts + MOE_BLOCK - 1) // MOE_BLOCK * MOE_BLOCK
    pad_end = jnp.cumsum(padded)
    pad_start = pad_end - padded
    start = jnp.cumsum(counts) - counts
    dest = pad_start[e_sorted] + (jnp.arange(A) - start[e_sorted])
    n_blocks = -(-A // MOE_BLOCK) + N_EXPERTS
    P = n_blocks * MOE_BLOCK
    row_tok = jnp.zeros((P,), jnp.int32).at[dest].set(tok[order])
    row_w = jnp.zeros((P,), jnp.float32).at[dest].set(w_flat[order])
    blk_exp = jnp.minimum(jnp.searchsorted(pad_end, jnp.arange(n_blocks) * MOE_BLOCK, side='right'), N_EXPERTS - 1)
    xs = hf[row_tok].reshape(n_blocks, MOE_BLOCK, D)

    def expert_block(args):
        xb, e = args
        a = jax.nn.silu(xb @ w_gate[e]) * (xb @ w_up[e])
        return a @ w_down[e]

    ys = lax.map(expert_block, (xs, blk_exp)).reshape(P, D)
    out = jnp.zeros((N, D), jnp.float32).at[row_tok].add(ys.astype(jnp.float32) * row_w[:, None])
    return out.reshape(B, T, D).astype(h.dtype)


def setup_inputs(seed: int = 0) -> dict:
    key = jax.random.key(seed)
    ks = jax.random.split(key, 24)
    L, D = DEPTH, D_MODEL

    def nrm(k, shape, scale):
        return jax.random.normal(k, shape, jnp.float32) * scale

    return {
        'x': nrm(ks[0], (BATCH, SEQ, D), 1.0),
        'c': nrm(ks[1], (BATCH, D), 1.0),
        'w_ada': nrm(ks[2], (L, D, 6 * D), 0.5 * D ** -0.5),
        'b_ada': nrm(ks[3], (L, 6 * D), 0.02),
        'norm1_g': 1.0 + nrm(ks[4], (L, D), 0.02),
        'norm2_g': 1.0 + nrm(ks[5], (L, D), 0.02),
        'final_g': 1.0 + nrm(ks[6], (D,), 0.02),
        'w_in': nrm(ks[7], (L, D, PROJ_TOTAL), D ** -0.5),
        'ret_gn_g': 1.0 + nrm(ks[8], (L, RET_WIDTH), 0.02),
        'cmp_pos_k': nrm(ks[9], (L, CMP_BLOCK, NSA_DK), 0.1),
        'cmp_w1_k': nrm(ks[10], (L, CMP_BLOCK * NSA_DK, CMP_HIDDEN), (CMP_BLOCK * NSA_DK) ** -0.5),
        'cmp_w2_k': nrm(ks[11], (L, CMP_HIDDEN, NSA_DK), CMP_HIDDEN ** -0.5),
        'cmp_pos_v': nrm(ks[12], (L, CMP_BLOCK, NSA_DV), 0.1),
        'cmp_w1_v': nrm(ks[13], (L, CMP_BLOCK * NSA_DV, CMP_HIDDEN), (CMP_BLOCK * NSA_DV) ** -0.5),
        'cmp_w2_v': nrm(ks[14], (L, CMP_HIDDEN, NSA_DV), CMP_HIDDEN ** -0.5),
        'w_out': nrm(ks[15], (L, MIX_WIDTH, D), MIX_WIDTH ** -0.5),
        'w_grp': nrm(ks[16], (L, D, N_GROUPS), D ** -0.5),
        'b_grp': nrm(ks[17], (L, N_GROUPS), 0.01),
        'w_exp': nrm(ks[18], (L, D, N_EXPERTS), D ** -0.5),
        'b_exp': nrm(ks[19], (L, N_EXPERTS), 0.01),
        'w_gate': nrm(ks[20], (L, N_EXPERTS, D, D_EXPERT), D ** -0.5),
        'w_up': nrm(ks[21], (L, N_EXPERTS, D, D_EXPERT), D ** -0.5),
        'w_down': nrm(ks[22], (L, N_EXPERTS, D_EXPERT, D), D_EXPERT ** -0.5),
    }


def reference(x, c, w_ada, b_ada, norm1_g, norm2_g, final_g, w_in, ret_gn_g,
              cmp_pos_k, cmp_w1_k, cmp_w2_k, cmp_pos_v, cmp_w1_v, cmp_w2_v,
              w_out, w_grp, b_grp, w_exp, b_exp, w_gate, w_up, w_down):
    B, T, D = x.shape
    c_act = jax.nn.silu(c)
    for l in range(DEPTH):
        mod = c_act @ w_ada[l] + b_ada[l]
        sh1, sc1, g1, sh2, sc2, g2 = jnp.split(mod[:, None, :], 6, axis=-1)
        h = rms_norm(x, norm1_g[l]) * (1.0 + sc1) + sh1
        rq, rk, rv, rg, nq, kc, vc, ks_, vs_, kw, vw, ng = split_cols(h @ w_in[l])
        o_ret = retention(rq.reshape(B, T, RET_HEADS, RET_DK), rk.reshape(B, T, RET_HEADS, RET_DK),
                          rv.reshape(B, T, RET_HEADS, RET_DV), rg, ret_gn_g[l])
        o_nsa = nsa(nq, kc, vc, ks_, vs_, kw, vw, ng,
                    cmp_pos_k[l], cmp_w1_k[l], cmp_w2_k[l], cmp_pos_v[l], cmp_w1_v[l], cmp_w2_v[l])
        mix = jnp.concatenate([o_ret.astype(x.dtype), o_nsa.astype(x.dtype)], axis=-1) @ w_out[l]
        x = x + g1 * mix
        h2 = rms_norm(x, norm2_g[l]) * (1.0 + sc2) + sh2
        x = x + g2 * hier_moe(h2, w_grp[l], b_grp[l], w_exp[l], b_exp[l], w_gate[l], w_up[l], w_down[l])
    return rms_norm(x, final_g)
```

```python
import os
from contextlib import ExitStack
import numpy as np
import ml_dtypes
import concourse.bass as bass
import concourse.mybir as mybir
from concourse.bass_utils import run_bass_kernel_spmd

F32 = mybir.dt.float32
BF16 = mybir.dt.bfloat16
I32 = mybir.dt.int32
AF = mybir.ActivationFunctionType
ALU = mybir.AluOpType
AX = mybir.AxisListType

D = 2048
T = 2048
NT = 16
KC = 16
PROJ = 6680
CAP = 256
RB = CAP // 128
NEG = -30000.0
EPS = 1e-6

ENGS = ("pe", "act", "dve", "pool", "sp")
SAME_ENGINE_IN_ORDER = False


_UNIQ = [0]


def _namers(nc):
    def sb(name, shp, dt):
        _UNIQ[0] += 1
        return nc.sbuf_tensor("s%d_%s" % (_UNIQ[0], name), shp, dt)

    def ps(name, shp, dt):
        _UNIQ[0] += 1
        return nc.psum_tensor("p%d_%s" % (_UNIQ[0], name), shp, dt)
    return sb, ps


class Sched:
    NDMA = 24

    def __init__(self, nc):
        self.nc = nc
        self.handles = dict(pe=nc.tensor, act=nc.scalar, dve=nc.vector, pool=nc.gpsimd, sp=nc.sync)
        self.cnt = {e: 0 for e in ENGS}
        self.waited = {e: {} for e in ENGS}
        self.lastw = {}
        self.readers = {}
        self.sems = {}
        self.dma_sems = []
        self.dma_n = 0
        self.dma_q = [0, 0, 0]
        self.bg_list = []
        self.bg_pos = 0
        self.dma_last = {}
        self._stack = []

    def open(self):
        for e in ENGS:
            cm = self.nc.semaphore("sem_" + e)
            self.sems[e] = cm.__enter__()
            self._stack.append(cm)
        for i in range(self.NDMA):
            cm = self.nc.semaphore("semd%d" % i)
            self.dma_sems.append(cm.__enter__())
            self._stack.append(cm)

    def close(self):
        for cm in reversed(self._stack):
            cm.__exit__(None, None, None)

    def _need(self, eng, tok, waits):
        if tok is None:
            return
        sem_id, val, src = tok
        if src == eng and (eng == "pe" or SAME_ENGINE_IN_ORDER):
            return
        w = self.waited[eng]
        if w.get(sem_id, 0) >= val:
            return
        w[sem_id] = val
        waits.append((sem_id, val))

    def _deps(self, eng, reads, writes):
        waits = []
        for b in reads:
            self._need(eng, self.lastw.get(b), waits)
        for b in writes:
            self._need(eng, self.lastw.get(b), waits)
            for t in self.readers.get(b, ()):
                self._need(eng, t, waits)
        return waits

    def _commit(self, tok, reads, writes):
        for b in reads:
            self.readers.setdefault(b, []).append(tok)
        for b in writes:
            self.lastw[b] = tok
            self.readers[b] = []

    def op(self, eng, fn, reads=(), writes=()):
        waits = self._deps(eng, reads, writes)
        self.cnt[eng] += 1
        tok = ("E" + eng, self.cnt[eng], eng)
        self._emit1(eng, waits, fn, ("E" + eng, 1))
        self._commit(tok, reads, writes)
        return tok

    def dma(self, eng, fn, reads=(), writes=(), grp=None):
        qi = grp if grp is not None else (0 if eng == "pool" else 1)
        base, size = ((0, 8), (8, 12), (20, 4))[qi]
        j = self.dma_q[qi]
        self.dma_q[qi] += 1
        s = base + (j % size)
        sid = "D%d" % s
        prev = self.dma_last.get(s, 0)
        waits = self._deps(eng, reads, writes)
        if prev and self.waited[eng].get(sid, 0) < prev:
            self.waited[eng][sid] = prev
            waits.append((sid, prev))
        val = prev + 16
        self.dma_last[s] = val
        tok = (sid, val, "dma")
        self._emit1(eng, waits, fn, (sid, 16))
        self._commit(tok, reads, writes)
        return tok

    def _emit1(self, engname, waits, fn, inc):
        e = self.handles[engname]
        if os.environ.get("K_TRACE"):
            print("TR", engname, self.cnt[engname], waits, inc, flush=True)
        for sid, val in waits:
            e.wait_ge(self._sem(sid), val)
        if fn is not None:
            ins = fn(e)
            ins.then_inc(self._sem(inc[0]), inc[1])

    def bg(self, n=1):
        for _ in range(n):
            if self.bg_pos >= len(self.bg_list):
                return
            fn, key = self.bg_list[self.bg_pos]
            self.bg_pos += 1
            self.dma("pool", fn, writes=[key], grp=2)

    def _sem(self, sid):
        if sid[0] == "E":
            return self.sems[sid[1:]]
        return self.dma_sems[int(sid[1:])]

    def barrier(self, engs=ENGS, with_bg=False):
        toks = [("E" + e, self.cnt[e], e) for e in ENGS if self.cnt[e] > 0]
        toks += [("D%d" % s, v, "dma") for s, v in self.dma_last.items() if with_bg or s < 20]
        for e in engs:
            waits = []
            for t in toks:
                self._need(e, t, waits)
            self._emit1(e, waits, None, None)


def make_consts():
    bf = ml_dtypes.bfloat16
    c = {}
    half = 128
    inv = (10000.0 ** (-np.arange(half, dtype=np.float32) / half)).astype(np.float32)
    pos = np.arange(T, dtype=np.float32)
    ang = (inv[:, None] * pos[None, :]).astype(np.float32)
    c["cos"] = np.cos(ang).astype(np.float32)
    c["sin"] = np.sin(ang).astype(np.float32)
    c["identb"] = np.eye(128, dtype=np.float32).astype(bf)
    c["identf"] = np.eye(128, dtype=np.float32)
    H = 4
    lg = np.log1p(-np.exp2(-5.0 - np.arange(H, dtype=np.float64)))
    m = np.arange(128, dtype=np.float64)
    dm = np.zeros((128, H, 128), np.float32)
    for h in range(H):
        val = np.exp(lg[h] * (-m - 1.0))
        dm[:, h, :] = np.where(m[None, :] >= m[:, None], val[:, None], 0.0)
    c["dm"] = dm
    c["zeta"] = np.exp(lg[None, :] * (127.0 - m)[:, None]).astype(np.float32)
    c["qdec"] = np.exp(lg[None, :] * (m + 1.0)[:, None]).astype(np.float32)
    c["_cd"] = [float(np.exp(lg[h] * 128.0)) for h in range(H)]
    j = np.arange(128)
    caus = np.where(j[:, None] <= j[None, :], 0.0, NEG).astype(np.float32)
    anti = np.where(j[:, None] > j[None, :], 0.0, NEG).astype(np.float32)
    c["caus"] = caus.astype(bf)
    c["anti"] = anti.astype(bf)
    cc = np.arange(128)
    tt = np.arange(T)
    c["cmask"] = np.where(16 * cc[:, None] + 31 <= tt[None, :], 0.0, NEG).astype(np.float32).astype(bf)
    ss = np.arange(32)
    ov = ((16 * cc[:, None] < 64 * ss[None, :] + 64) & (16 * cc[:, None] + 32 > 64 * ss[None, :])).astype(np.float32)
    c["overlap"] = ov.astype(bf)
    es = np.zeros((32, 16, 128), np.float32)
    for kt in range(16):
        for p in range(128):
            es[2 * kt + p // 64, kt, p] = 1.0
    c["esel"] = es.astype(bf)
    cur = tt // 64
    valid = (ss[None, :] <= cur[:, None])
    forced = (ss[None, :] == 0) | (ss[None, :] == cur[:, None]) | (ss[None, :] == cur[:, None] - 1)
    ctab = np.where(valid, np.where(forced, 1e4, 0.0), -1.0).astype(np.float32)
    c["valid"] = np.ascontiguousarray(valid.astype(np.float32).reshape(16, 128, 32).transpose(1, 0, 2))
    c["ctab"] = np.ascontiguousarray(ctab.reshape(16, 128, 32).transpose(1, 0, 2))
    c["lstrict"] = (j[:, None] < j[None, :]).astype(np.float32).astype(bf)
    c["ones"] = np.ones((128, 128), np.float32).astype(bf)
    c["ebase"] = np.tile((np.arange(64, dtype=np.float32) * CAP + 1.0)[None, :], (128, 1))
    return c


CONST_SPECS = [
    ("cos", [128, T], F32), ("sin", [128, T], F32), ("identb", [128, 128], BF16), ("identf", [128, 128], F32),
    ("dm", [128, 4, 128], F32), ("zeta", [128, 4], F32), ("qdec", [128, 4], F32),
    ("caus", [128, 128], BF16), ("anti", [128, 128], BF16), ("cmask", [128, T], BF16), ("overlap", [128, 32], BF16),
    ("esel", [32, 16, 128], BF16), ("valid", [128, 16, 32], F32), ("ctab", [128, 16, 32], F32),
    ("lstrict", [128, 128], BF16), ("ones", [128, 128], BF16), ("ebase", [128, 64], F32),
]

IN_SPECS = [
    ("x", [T, D], F32), ("cT", [128, 16], F32), ("w_ada", [D, 6 * D], F32), ("b_adaT", [128, 96], F32),
    ("n1gT", [128, 16], F32), ("n2gT", [128, 16], F32), ("fg_row", [128, D], F32), ("w_in", [D, PROJ], F32),
    ("gng_row", [128, 1024], F32), ("peT_k", [128, 32], F32), ("w1_k", [4096, 256], F32), ("w2_k", [256, 128], F32),
    ("peT_v", [128, 32], F32), ("w1_v", [4096, 256], F32), ("w2_v", [256, 128], F32), ("w_out", [D, D], F32),
    ("w_rt", [D, 72], F32), ("b_rt", [128, 72], F32), ("w_gate", [64, D, 512], F32), ("w_up", [64, D, 512], F32),
    ("w_down", [64, 512, D], F32),
]


def build_nc(stop=99, dbg=()):
    CST = make_consts()
    nc = bass.Bass("TRN2", target_bir_lowering=False)
    io = {}
    for name, shp, dt in IN_SPECS + CONST_SPECS:
        if stop < 8 and name in ("w_gate", "w_up", "w_down"):
            shp = [1] + shp[1:]
        io[name] = nc.dram_tensor(name, shp, dt, kind="ExternalInput").ap()
    out = nc.dram_tensor("out", [T, D], F32, kind="ExternalOutput").ap()

    def scratch(name, shp, dt):
        kind = "ExternalOutput" if name in dbg else "Internal"
        return nc.dram_tensor(name, shp, dt, kind=kind).ap()

    PFM = scratch("PFM", [32, 128, T], BF16)
    PTM = scratch("PTM", [T, 2560], BF16)
    X1 = scratch("X1", [T, D], F32)
    XS = scratch("XS", [64 * CAP, D], BF16)
    YS = scratch("YS", [64 * CAP, D], F32)
    NEX = 64 if stop >= 8 else 1
    WGB = scratch("WGB", [NEX, D, 512], BF16)
    WUB = scratch("WUB", [NEX, D, 512], BF16)
    WDB = scratch("WDB", [NEX, 512, D], BF16)
    dbgout = {}
    for name, shp, dt in [("d_modT", [128, 96], F32), ("d_hT", [128, 16, T], BF16), ("d_ocatT", [128, 16, T], BF16),
                          ("d_ngs", [128, 16, 24], F32), ("d_slots", [128, 16, 2], I32), ("d_wab", [128, 16, 2], F32),
                          ("d_misc", [128, 16, 64], F32)]:
        if name in dbg:
            dbgout[name] = nc.dram_tensor(name, shp, dt, kind="ExternalOutput").ap()

    S = Sched(nc)
    S.open()
    sb, ps = _namers(nc)
    final_toks = []
    if stop >= 8:
        for ex in range(64):
            for nm, dst, src in (("wgb", WGB, "w_gate"), ("wub", WUB, "w_up"), ("wdb", WDB, "w_down")):
                S.bg_list.append((lambda e, ex=ex, dst=dst, src=src: e.dma_start(out=dst[ex], in_=io[src][ex]), (nm, ex)))

    def dbgdump(name, ap, key):
        if name in dbgout:
            final_toks.append(S.dma("sp", lambda e: e.dma_start(out=dbgout[name], in_=ap), reads=[key], writes=["dbg_" + name]))

    with ExitStack() as _es:
        modT = _es.enter_context(sb("modT", [128, 96], F32))
        A1 = _es.enter_context(sb("A1", [128, 16], F32))
        A2 = _es.enter_context(sb("A2", [128, 16], F32))
        identb = _es.enter_context(sb("identb", [128, 128], BF16))
        identf = _es.enter_context(sb("identf", [128, 128], F32))
        ngs = _es.enter_context(sb("ngs", [128, 16, 24], F32))
        slots = _es.enter_context(sb("slots", [128, 16, 2], I32))
        wab = _es.enter_context(sb("wab", [128, 16, 2], F32))
        global epsc
        epsc = _es.enter_context(sb("epsc", [128, 1], F32))
        S.op("dve", lambda e: e.memset(epsc[:], EPS), writes=["epsc"])
        S.dma("sp", lambda e: e.dma_start(out=identb[:], in_=io["identb"]), writes=["identb"])
        S.dma("sp", lambda e: e.dma_start(out=identf[:], in_=io["identf"]), writes=["identf"])

        with ExitStack() as _es:
            cT = _es.enter_context(sb("cT", [128, 16], F32))
            cact = _es.enter_context(sb("cact", [128, 16], BF16))
            badaT = _es.enter_context(sb("badaT", [128, 96], F32))
            n1gT = _es.enter_context(sb("n1gT", [128, 16], F32))
            n2gT = _es.enter_context(sb("n2gT", [128, 16], F32))
            wa = _es.enter_context(sb("wa", [128, 2, 16, 512], BF16))
            modps = _es.enter_context(ps("modps", [128, 96], F32))
            S.dma("sp", lambda e: e.dma_start(out=cT[:], in_=io["cT"]), writes=["cT"])
            S.dma("sp", lambda e: e.dma_start(out=badaT[:], in_=io["b_adaT"]), writes=["badaT"])
            S.dma("sp", lambda e: e.dma_start(out=n1gT[:], in_=io["n1gT"]), writes=["n1gT"])
            S.dma("sp", lambda e: e.dma_start(out=n2gT[:], in_=io["n2gT"]), writes=["n2gT"])
            S.op("act", lambda e: e.activation(out=cact[:], in_=cT[:], func=AF.Silu), reads=["cT"], writes=["cact"])
            if stop >= 7:
                zt = _es.enter_context(sb("zt", [128, RB, D], BF16))
                S.op("dve", lambda e: e.memset(zt[:], 0.0), writes=["zt"])
                for ex in range(64):
                    S.dma("sp", lambda e, ex=ex: e.dma_start(out=XS[ex * CAP:(ex + 1) * CAP, :].rearrange("(r p) d -> p r d", p=128), in_=zt[:]),
                          reads=["zt"], writes=["XS"])
            wada = io["w_ada"].rearrange("(k p) n -> p k n", p=128)
            for blk in range(24 if not os.environ.get("K_SKIPABC") else 0):
                b = blk % 2
                if blk % 2 == 1:
                    S.bg(1)
                S.dma("pool", lambda e, b=b, blk=blk: e.dma_start(out=wa[:, b], in_=wada[:, :, blk * 512:(blk + 1) * 512]),
                      writes=[("wa", b)])
                for j in range(4):
                    col = blk * 4 + j
                    for k in range(16):
                        S.op("pe", lambda e, b=b, j=j, k=k, col=col: e.matmul(
                            modps[:, col:col + 1], lhsT=wa[:, b, k, j * 128:(j + 1) * 128], rhs=cact[:, k:k + 1],
                            start=(k == 0), stop=(k == 15)), reads=[("wa", b), "cact"], writes=["modps"])
            S.op("dve", lambda e: e.tensor_tensor(out=modT[:], in0=modps[:], in1=badaT[:], op=ALU.add),
                 reads=["modps", "badaT"], writes=["modT"])
            S.op("dve", lambda e: e.scalar_tensor_tensor(out=A1[:], in0=modT[:, 16:32], scalar=1.0, in1=n1gT[:],
                                                         op0=ALU.add, op1=ALU.mult), reads=["modT", "n1gT"], writes=["A1"])
            S.op("dve", lambda e: e.scalar_tensor_tensor(out=A2[:], in0=modT[:, 64:80], scalar=1.0, in1=n2gT[:],
                                                         op0=ALU.add, op1=ALU.mult), reads=["modT", "n2gT"], writes=["A2"])
            dbgdump("d_modT", modT[:], "modT")
            S.barrier()

        def rowbcast(dst, col0, key):
            with sb("rb_l", [128, 2, 128], F32) as rbl, ps("rb_ps", [128, 2, 512], F32) as rbps:
                for k in range(16):
                    lb = k % 2
                    S.op("dve", lambda e, k=k, lb=lb: e.tensor_copy(out=rbl[:, lb, :], in_=modT[:, col0 + k:col0 + k + 1].to_broadcast([128, 128])),
                         reads=["modT"], writes=[("rbl", lb)])
                    S.op("pe", lambda e, k=k, lb=lb: e.matmul(rbps[:, (k // 4) % 2, (k % 4) * 128:(k % 4 + 1) * 128], lhsT=rbl[:, lb, :], rhs=identf[:],
                                                              start=True, stop=True), reads=[("rbl", lb), "identf"], writes=[("rbps", (k // 4) % 2)])
                    if k % 4 == 3:
                        q = k // 4
                        S.op("act", lambda e, q=q: e.copy(out=dst[:, q * 512:(q + 1) * 512], in_=rbps[:, q % 2, :]),
                             reads=[("rbps", q % 2)], writes=[key])
                S.barrier()

        if stop >= 2 and not os.environ.get("K_SKIPABC"):
          with sb("hT", [128, 16, T], BF16) as hT:
            with ExitStack() as _es:
                xt = _es.enter_context(sb("xt", [128, 2, D], F32))
                xn = _es.enter_context(sb("xn", [128, 4, D], BF16))
                junk = _es.enter_context(sb("junk", [128, D], BF16))
                ss = _es.enter_context(sb("ss", [128, 16], F32))
                rstd = _es.enter_context(sb("rstd", [128, 16], F32))
                tp = _es.enter_context(ps("tpB", [128, 2, 1024], BF16))
                S.op("dve", lambda e: e.memset(ss[:], 0.0), writes=["ss"])
                for tg in range(4):
                    for i4 in range(4):
                        i = tg * 4 + i4
                        b = i % 2
                        S.dma("sp", lambda e, i=i, b=b: e.dma_start(out=xt[:, b], in_=io["x"][i * 128:(i + 1) * 128, :]), writes=[("xt", b)])
                        S.op("act", lambda e, i=i, b=b: e.activation(out=junk[:], in_=xt[:, b], func=AF.Square, accum_out=ss[:, i:i + 1]),
                             reads=[("xt", b), "ss"], writes=["junk", ("ss", i)])
                        S.op("act", lambda e, i=i: e.activation(out=rstd[:, i:i + 1], in_=ss[:, i:i + 1], func=AF.Sqrt, bias=epsc[:, 0:1], scale=1.0 / D),
                             reads=[("ss", i), "epsc"], writes=[("rstd", i)])
                        S.op("dve", lambda e, i=i: e.reciprocal(out=rstd[:, i:i + 1], in_=rstd[:, i:i + 1]), reads=[("rstd", i)], writes=[("rstd", i)])
                        S.op("dve", lambda e, i=i, i4=i4, b=b: e.tensor_scalar(out=xn[:, i4], in0=xt[:, b], scalar1=rstd[:, i:i + 1], scalar2=None,
                                                                               op0=ALU.mult), reads=[("xt", b), ("rstd", i)], writes=[("xn", i4)])
                    for k in range(16):
                        pb = k % 2
                        for i4 in range(4):
                            S.op("pe", lambda e, k=k, pb=pb, i4=i4: e.transpose(tp[:, pb, i4 * 128:(i4 + 1) * 128], xn[:, i4, k * 128:(k + 1) * 128], identb[:]),
                                 reads=[("xn", i4), "identb"], writes=[("tp", pb)])
                        S.op("act", lambda e, k=k, pb=pb, tg=tg: e.activation(out=hT[:, k, tg * 512:(tg + 1) * 512], in_=tp[:, pb, 0:512], func=AF.Identity,
                                                                              scale=A1[:, k:k + 1], bias=modT[:, k:k + 1]),
                             reads=[("tp", pb), "A1", "modT"], writes=[("hT", tg)])
                dbgdump("d_hT", hT[:], ("hT", 3))
                S.barrier()

            if stop >= 3:
                stage_C(nc, S, io, hT, PFM, PTM, ngs, dbgdump)
            S.barrier()

        if stop >= 4:
          with sb("ocatT", [128, 16, T], BF16) as ocatT:
            if not os.environ.get("K_SKIPD"):
                stage_D(nc, S, io, CST, PFM, PTM, ocatT, identb)
            if stop >= 5:
                stage_E(nc, S, io, PFM, PTM, ocatT, identb, ngs)
            dbgdump("d_ocatT", ocatT[:], "ocatT")
            if stop >= 6:
                with sb("g1row", [128, D], F32) as g1row:
                    rowbcast(g1row, 32, "g1row")
                    stage_F(nc, S, io, ocatT, g1row, X1)
            S.barrier()

        if stop >= 7:
            with sb("a2row", [128, D], F32) as a2row, sb("sh2row", [128, D], F32) as sh2row:
                S.op("dve", lambda e: e.tensor_copy(out=modT[:, 64:80], in_=A2[:]), reads=["A2", "modT"], writes=["modT"])
                rowbcast(a2row, 64, "a2row")
                rowbcast(sh2row, 48, "sh2row")
                stage_G(nc, S, io, X1, XS, a2row, sh2row, identf, slots, wab, dbgout, final_toks)
            S.barrier()
            dbgdump("d_slots", slots[:], "slots")
            dbgdump("d_wab", wab[:], "wab")
        if stop >= 8:
            S.bg(1000)
            stage_H(nc, S, io, XS, YS, identb, WGB, WUB, WDB)
            S.barrier()
        if stop >= 9:
            with sb("g2row", [128, D], F32) as g2row:
                rowbcast(g2row, 80, "g2row")
                final_toks += stage_I(nc, S, io, X1, YS, g2row, slots, wab, out)
        S.barrier(with_bg=True)
        e = S.handles["sp"]
        waits = []
        for t in final_toks:
            S._need("sp", t, waits)
        for sid, val in waits:
            e.wait_ge(S._sem(sid), val)
    S.close()
    return nc


def stage_C(nc, S, io, hT, PFM, PTM, ngs, dbgdump):
    sb, ps = _namers(nc)
    win = io["w_in"].rearrange("(k p) n -> p k n", p=128)
    units = []
    for h in range(4):
        units.append((h * 256, "rotq", 2 * h))
    for h in range(4):
        units.append((1024 + h * 256, "rotk", 8 + 2 * h))
    for u in range(4):
        units.append((4096 + u * 256, "scale", 16 + 2 * u))
    units += [(5120, "copy", 24), (5376, "copy", 26), (5632, "copy", 28), (6144, "copy", 30)]
    with ExitStack() as _es:
        cos = _es.enter_context(sb("cos", [128, T], F32))
        sin = _es.enter_context(sb("sin", [128, T], F32))
        wf = _es.enter_context(sb("wf", [128, 2, 16, 256], BF16))
        stg = _es.enter_context(sb("stg", [128, 2, 2, T], BF16))
        f12 = _es.enter_context(sb("f12", [128, 2, 512], F32))
        tt = _es.enter_context(sb("tt", [128, 4, 512], F32))
        pfm = _es.enter_context(ps("pfm", [128, 2, 2, 512], F32))
        S.dma("sp", lambda e: e.dma_start(out=cos[:], in_=io["cos"]), writes=["cos"])
        S.dma("sp", lambda e: e.dma_start(out=sin[:], in_=io["sin"]), writes=["sin"])
        n = 0
        for ui, (c0, kind, ch) in enumerate(units):
            wb = ui % 2
            S.dma("pool", lambda e, wb=wb, c0=c0: e.dma_start(out=wf[:, wb], in_=win[:, :, c0:c0 + 256]), writes=[("wf", wb)])
            for tg in range(4):
                pb = n % 2
                n += 1
                if n % 2 == 0:
                    S.bg(1)
                for half in range(2):
                    for k in range(16):
                        S.op("pe", lambda e, wb=wb, pb=pb, half=half, k=k, tg=tg: e.matmul(
                            pfm[:, pb, half, :], lhsT=wf[:, wb, k, half * 128:(half + 1) * 128], rhs=hT[:, k, tg * 512:(tg + 1) * 512],
                            start=(k == 0), stop=(k == 15)), reads=[("wf", wb), ("hT", tg)], writes=[("pfm", pb)])
                tsl = slice(tg * 512, (tg + 1) * 512)
                if kind in ("copy", "scale"):
                    sc = 1.0 if kind == "copy" else 128.0 ** -0.5
                    for half in range(2):
                        S.op("act", lambda e, pb=pb, wb=wb, tsl=tsl, sc=sc, half=half: e.activation(out=stg[:, wb, half, tsl], in_=pfm[:, pb, half, :], func=AF.Copy, scale=sc),
                             reads=[("pfm", pb)], writes=[("stg", wb)])
                else:
                    sc = 1.0 if kind == "rotq" else 1.0 / 16.0
                    for half in range(2):
                        S.op("act", lambda e, pb=pb, sc=sc, half=half: e.activation(out=f12[:, half, :], in_=pfm[:, pb, half, :], func=AF.Copy, scale=sc),
                             reads=[("pfm", pb)], writes=["f12"])
                    S.op("dve", lambda e, tsl=tsl: e.tensor_tensor(out=tt[:, 0], in0=f12[:, 0], in1=cos[:, tsl], op=ALU.mult), reads=["f12", "cos"], writes=["tt0"])
                    S.op("dve", lambda e, tsl=tsl: e.tensor_tensor(out=tt[:, 1], in0=f12[:, 1], in1=sin[:, tsl], op=ALU.mult), reads=["f12", "sin"], writes=["tt1"])
                    S.op("pool", lambda e, tsl=tsl: e.tensor_tensor(out=tt[:, 2], in0=f12[:, 0], in1=sin[:, tsl], op=ALU.mult), reads=["f12", "sin"], writes=["tt2"])
                    S.op("pool", lambda e, tsl=tsl: e.tensor_tensor(out=tt[:, 3], in0=f12[:, 1], in1=cos[:, tsl], op=ALU.mult), reads=["f12", "cos"], writes=["tt3"])
                    S.op("dve", lambda e, wb=wb, tsl=tsl: e.tensor_tensor(out=stg[:, wb, 0, tsl], in0=tt[:, 0], in1=tt[:, 1], op=ALU.subtract),
                         reads=["tt0", "tt1"], writes=[("stg", wb)])
                    S.op("pool", lambda e, wb=wb, tsl=tsl: e.tensor_tensor(out=stg[:, wb, 1, tsl], in0=tt[:, 2], in1=tt[:, 3], op=ALU.add),
                         reads=["tt2", "tt3"], writes=[("stg", wb)])
            S.dma("sp", lambda e, wb=wb, ch=ch: e.dma_start(out=PFM[ch:ch + 2].rearrange("c p t -> p c t"), in_=stg[:, wb]),
                  reads=[("stg", wb)], writes=["PFM"])
        S.barrier()
    tunits = [([(2048, 512)], 0), ([(2560, 512)], 512), ([(3072, 512)], 1024), ([(3584, 512)], 1536),
              ([(5888, 256), (6400, 256)], 2048), ([(6656, 24)], None)]
    ptm_v = PTM.rearrange("(i p) c -> p i c", p=128)
    with ExitStack() as _es:
        wt = _es.enter_context(sb("wt", [128, 2, 16, 512], BF16))
        stgt = _es.enter_context(sb("stgt", [128, 2, 16, 512], BF16))
        ptm = _es.enter_context(ps("ptm", [128, 2, 512], F32))
        n = 0
        for ui, (srcs, dcol) in enumerate(tunits):
            wb = ui % 2
            off = 0
            for (c0, w) in srcs:
                S.dma("pool", lambda e, wb=wb, c0=c0, w=w, off=off: e.dma_start(out=wt[:, wb, :, off:off + w], in_=win[:, :, c0:c0 + w]),
                      writes=[("wt", wb)])
                off += w
            ncol = off
            for i in range(16):
                pb = n % 2
                n += 1
                if n % 6 == 0:
                    S.bg(1)
                for k in range(16):
                    S.op("pe", lambda e, wb=wb, pb=pb, i=i, k=k, ncol=ncol: e.matmul(
                        ptm[:, pb, 0:ncol], lhsT=hT[:, k, i * 128:(i + 1) * 128], rhs=wt[:, wb, k, 0:ncol],
                        start=(k == 0), stop=(k == 15)), reads=[("wt", wb), ("hT", i // 4)], writes=[("ptm", pb)])
                if dcol is None:
                    S.op("dve", lambda e, pb=pb, i=i: e.tensor_copy(out=ngs[:, i, :], in_=ptm[:, pb, 0:24]), reads=[("ptm", pb)], writes=["ngs"])
                else:
                    eng = "act" if i % 2 == 0 else "dve"
                    if eng == "act":
                        S.op("act", lambda e, pb=pb, wb=wb, i=i: e.copy(out=stgt[:, wb, i, :], in_=ptm[:, pb, :]), reads=[("ptm", pb)], writes=[("stgt", wb)])
                    else:
                        S.op("dve", lambda e, pb=pb, wb=wb, i=i: e.tensor_copy(out=stgt[:, wb, i, :], in_=ptm[:, pb, :]), reads=[("ptm", pb)], writes=[("stgt", wb)])
            if dcol is not None:
                S.dma("sp", lambda e, wb=wb, dcol=dcol: e.dma_start(out=ptm_v[:, :, dcol:dcol + 512], in_=stgt[:, wb]),
                      reads=[("stgt", wb)], writes=["PTM"])
        dbgdump("d_ngs", ngs[:], "ngs")
        S.barrier()


def stage_D(nc, S, io, CST, PFM, PTM, ocatT, identb):
    sb, ps = _namers(nc)
    ptm_v = PTM.rearrange("(i p) c -> p i c", p=128)
    with ExitStack() as _es:
        qT = _es.enter_context(sb("qT", [128, 2, T], BF16))
        kT = _es.enter_context(sb("kT", [128, 2, T], BF16))
        kz = _es.enter_context(sb("kz", [128, 16, 256], BF16))
        v = _es.enter_context(sb("v", [128, 16, 256], BF16))
        g = _es.enter_context(sb("g", [128, 16, 256], BF16))
        dm = _es.enter_context(sb("dm", [128, 4, 128], F32))
        zeta = _es.enter_context(sb("zeta", [128, 4], F32))
        qdec = _es.enter_context(sb("qdec", [128, 4], F32))
        gng = _es.enter_context(sb("gng", [128, 1024], F32))
        Sf = _es.enter_context(sb("Sf", [128, 2, 256], F32))
        Sb = _es.enter_context(sb("Sb", [128, 2, 256], BF16))
        sTm = _es.enter_context(sb("sTm", [128, 2, 128], BF16))
        o = _es.enter_context(sb("o", [128, 2, 256], F32))
        on = _es.enter_context(sb("on", [128, 2, 256], F32))
        sg = _es.enter_context(sb("sg", [128, 2, 256], F32))
        ob = _es.enter_context(sb("ob", [128, 2, 256], BF16))
        st = _es.enter_context(sb("st", [128, 2, 6], F32))
        junkd = _es.enter_context(sb("junkd", [128, 256], BF16))
        mv = _es.enter_context(sb("mv", [128, 2, 2], F32))
        rg = _es.enter_context(sb("rg", [128, 2], F32))
        sT_ps = _es.enter_context(ps("sT_ps", [128, 2, 512], F32))
        o_ps = _es.enter_context(ps("o_ps", [128, 2, 512], F32))
        kv_ps = _es.enter_context(ps("kv_ps", [128, 2, 256], F32))
        tpk = _es.enter_context(ps("tpk", [128, 2, 1024], BF16))
        S.dma("sp", lambda e: e.dma_start(out=dm[:], in_=io["dm"]), writes=["dm"])
        S.dma("sp", lambda e: e.dma_start(out=zeta[:], in_=io["zeta"]), writes=["zeta"])
        S.dma("sp", lambda e: e.dma_start(out=qdec[:], in_=io["qdec"]), writes=["qdec"])
        S.dma("sp", lambda e: e.dma_start(out=gng[:], in_=io["gng_row"]), writes=["gng"])
        for h in range(4):
            cd = CST["_cd"][h]
            S.dma("sp", lambda e, h=h: e.dma_start(out=qT[:], in_=PFM[2 * h:2 * h + 2].rearrange("c p t -> p c t")), reads=["PFM"], writes=["qT"])
            S.dma("sp", lambda e, h=h: e.dma_start(out=kT[:], in_=PFM[8 + 2 * h:10 + 2 * h].rearrange("c p t -> p c t")), reads=["PFM"], writes=["kT"])
            S.dma("sp", lambda e, h=h: e.dma_start(out=v[:], in_=ptm_v[:, :, h * 256:(h + 1) * 256]), reads=["PTM"], writes=["v"])
            S.dma("sp", lambda e, h=h: e.dma_start(out=g[:], in_=ptm_v[:, :, 1024 + h * 256:1024 + (h + 1) * 256]), reads=["PTM"], writes=["g"])
            for i in range(16):
                pb = i % 2
                for half in range(2):
                    S.op("pe", lambda e, i=i, pb=pb, half=half: e.transpose(tpk[:, pb, half * 128:(half + 1) * 128], kT[:, half, i * 128:(i + 1) * 128], identb[:]),
                         reads=["kT", "identb"], writes=[("tpk", pb)])
                S.op("act", lambda e, i=i, pb=pb, h=h: e.activation(out=kz[:, i, :].rearrange("p (a b) -> p a b", a=2), in_=tpk[:, pb, 0:256].rearrange("p (a b) -> p a b", a=2), func=AF.Identity,
                                                                    scale=zeta[:, h:h + 1]), reads=[("tpk", pb), "zeta"], writes=["kz"])
            S.op("dve", lambda e: e.memset(Sf[:], 0.0), writes=["Sf"])
            S.op("dve", lambda e: e.memset(Sb[:], 0.0), writes=["Sb"])
            for n in range(16):
                b = n % 2
                csl = slice(n * 128, (n + 1) * 128)
                if n % 2 == 0:
                    S.bg(1)
                for half in range(2):
                    S.op("pe", lambda e, b=b, half=half, csl=csl: e.matmul(sT_ps[:, b, 0:128], lhsT=kT[:, half, csl], rhs=qT[:, half, csl],
                                                                           start=(half == 0), stop=(half == 1)), reads=["kT", "qT"], writes=[("sT_ps", b)])
                S.op("dve", lambda e, b=b, h=h: e.tensor_tensor(out=sTm[:, b, :], in0=sT_ps[:, b, 0:128], in1=dm[:, h, :], op=ALU.mult),
                     reads=[("sT_ps", b), "dm"], writes=[("sTm", b)])
                S.op("pe", lambda e, b=b, n=n: e.matmul(o_ps[:, b, 0:256], lhsT=sTm[:, b, :], rhs=v[:, n, :], start=True, stop=(n == 0)),
                     reads=[("sTm", b), "v"], writes=[("o_ps", b)])
                if n > 0:
                    for half in range(2):
                        S.op("pe", lambda e, b=b, half=half, csl=csl: e.matmul(o_ps[:, b, 0:256], lhsT=qT[:, half, csl], rhs=Sb[:, half, :],
                                                                               start=False, stop=(half == 1)), reads=["qT", "Sb"], writes=[("o_ps", b)])
                if n < 15 and "state" not in os.environ.get("K_DSKIP", ""):
                    for half in range(2):
                        S.op("pe", lambda e, n=n, half=half: e.matmul(kv_ps[:, half, :], lhsT=kz[:, n, half * 128:(half + 1) * 128], rhs=v[:, n, :],
                                                                      start=True, stop=True), reads=["kz", "v"], writes=["kv_ps"])
                    S.op("dve", lambda e, cd=cd: e.scalar_tensor_tensor(out=Sf[:], in0=Sf[:], scalar=cd, in1=kv_ps[:], op0=ALU.mult, op1=ALU.add),
                         reads=["Sf", "kv_ps"], writes=["Sf"])
                    S.op("act", lambda e: e.copy(out=Sb[:], in_=Sf[:]), reads=["Sf"], writes=["Sb"])
                if "epi" in os.environ.get("K_DSKIP", ""):
                    continue
                S.op("dve", lambda e, b=b, h=h: e.tensor_scalar(out=o[:, b, :], in0=o_ps[:, b, 0:256], scalar1=qdec[:, h:h + 1], scalar2=None, op0=ALU.mult),
                     reads=[("o_ps", b), "qdec"], writes=[("o", b)])
                S.op("dve", lambda e, b=b: e.tensor_reduce(out=mv[:, b, 0:1], in_=o[:, b, :], axis=AX.X, op=ALU.add), reads=[("o", b)], writes=[("mv", b)])
                S.op("dve", lambda e, b=b: e.tensor_scalar(out=mv[:, b, 1:2], in0=mv[:, b, 0:1], scalar1=-1.0 / 256.0, scalar2=None, op0=ALU.mult),
                     reads=[("mv", b)], writes=[("mv1", b)])
                S.op("act", lambda e, b=b: e.activation(out=on[:, b, :], in_=o[:, b, :], func=AF.Identity, bias=mv[:, b, 1:2], scale=1.0),
                     reads=[("o", b), ("mv1", b)], writes=[("on", b)])
                S.op("act", lambda e, b=b: e.activation(out=junkd[:], in_=on[:, b, :], func=AF.Square, accum_out=st[:, b, 0:1]),
                     reads=[("on", b)], writes=["junkd", ("st", b)])
                S.op("act", lambda e, b=b: e.activation(out=rg[:, b:b + 1], in_=st[:, b, 0:1], func=AF.Sqrt, bias=epsc[:, 0:1], scale=1.0 / 256.0),
                     reads=[("st", b), "epsc"], writes=[("rg", b)])
                S.op("dve", lambda e, b=b: e.reciprocal(out=rg[:, b:b + 1], in_=rg[:, b:b + 1]), reads=[("rg", b)], writes=[("rg", b)])
                S.op("dve", lambda e, b=b, h=h: e.scalar_tensor_tensor(out=o[:, b, :], in0=on[:, b, :], scalar=rg[:, b:b + 1], in1=gng[:, h * 256:(h + 1) * 256],
                                                                       op0=ALU.mult, op1=ALU.mult), reads=[("on", b), ("rg", b), "gng"], writes=[("o", b)])
                S.op("act", lambda e, b=b, n=n: e.activation(out=sg[:, b, :], in_=g[:, n, :], func=AF.Silu), reads=["g"], writes=[("sg", b)])
                S.op("pool", lambda e, b=b: e.tensor_tensor(out=ob[:, b, :], in0=o[:, b, :], in1=sg[:, b, :], op=ALU.mult),
                     reads=[("o", b), ("sg", b)], writes=[("ob", b)])
                for half in range(2):
                    S.op("pe", lambda e, b=b, half=half: e.transpose(tpk[:, b, half * 128:(half + 1) * 128], ob[:, b, half * 128:(half + 1) * 128], identb[:]),
                         reads=[("ob", b), "identb"], writes=[("tpk", b)])
                S.op("act", lambda e, b=b, h=h, csl=csl: e.copy(out=ocatT[:, 2 * h:2 * h + 2, csl], in_=tpk[:, b, 0:256].rearrange("p (a b) -> p a b", a=2)), reads=[("tpk", b)], writes=["ocatT"])
        S.barrier()


def stage_E(nc, S, io, PFM, PTM, ocatT, identb, ngs):
    sb, ps = _namers(nc)
    ptm_v = PTM.rearrange("(i p) c -> p i c", p=128)
    with ExitStack() as _es:
        w1k = _es.enter_context(sb("w1k", [128, 32, 256], BF16))
        w1v = _es.enter_context(sb("w1v", [128, 32, 256], BF16))
        w2k = _es.enter_context(sb("w2k", [128, 2, 128], BF16))
        w2v = _es.enter_context(sb("w2v", [128, 2, 128], BF16))
        pek = _es.enter_context(sb("pek", [128, 32], BF16))
        pev = _es.enter_context(sb("pev", [128, 32], BF16))
        hb = _es.enter_context(sb("hb", [128, 2, 2], F32))
        caus = _es.enter_context(sb("caus", [128, 128], BF16))
        anti = _es.enter_context(sb("anti", [128, 128], BF16))
        cmask = _es.enter_context(sb("cmask", [128, T], BF16))
        overlap = _es.enter_context(sb("overlap", [128, 32], BF16))
        esel = _es.enter_context(sb("esel", [32, 16, 128], BF16))
        valid = _es.enter_context(sb("valid", [128, 16, 32], F32))
        ctab = _es.enter_context(sb("ctab", [128, 16, 32], F32))
        qT4 = _es.enter_context(sb("qT4", [128, 4, T], BF16))
        kcT2 = _es.enter_context(sb("kcT2", [128, 2, T], BF16))
        vcT2 = _es.enter_context(sb("vcT2", [128, 2, T], BF16))
        ksT = _es.enter_context(sb("ksT", [128, T], BF16))
        kwT = _es.enter_context(sb("kwT", [128, T], BF16))
        vsa = _es.enter_context(sb("vsa", [128, 16, 130], BF16))
        vwa = _es.enter_context(sb("vwa", [128, 16, 130], BF16))
        hid = _es.enter_context(sb("hid", [128, 4, 2, 128], BF16))
        kc16 = _es.enter_context(sb("kc16", [128, 2, 16, 130], BF16))
        kcmpT2 = _es.enter_context(sb("kcmpT2", [128, 2, 128], BF16))
        vcaug2 = _es.enter_context(sb("vcaug2", [128, 2, 162], BF16))
        ocmp = _es.enter_context(sb("ocmp", [128, 16, 4, 128], BF16))
        gts = _es.enter_context(sb("gts", [128, 16, 12], F32))
        selnegT = _es.enter_context(sb("selnegT", [32, T], BF16))
        _sk = os.environ.get("K_EPRO", "")
        if "c" not in _sk:
            for nm, t in [("caus", caus), ("anti", anti), ("cmask", cmask), ("overlap", overlap), ("esel", esel), ("valid", valid), ("ctab", ctab)]:
                S.dma("sp", lambda e, nm=nm, t=t: e.dma_start(out=t[:], in_=io[nm]), writes=[nm])
        if "w" not in _sk:
            for lh in range(2):
                S.dma("pool", lambda e, lh=lh: e.dma_start(out=w1k[:, lh * 16:(lh + 1) * 16, :], in_=io["w1_k"].rearrange("(l p) n -> p l n", p=128)[:, lh * 16:(lh + 1) * 16, :]), writes=["w1k"])
                S.dma("pool", lambda e, lh=lh: e.dma_start(out=w1v[:, lh * 16:(lh + 1) * 16, :], in_=io["w1_v"].rearrange("(l p) n -> p l n", p=128)[:, lh * 16:(lh + 1) * 16, :]), writes=["w1v"])
            S.dma("pool", lambda e: e.dma_start(out=w2k[:], in_=io["w2_k"].rearrange("(c p) n -> p c n", p=128)), writes=["w2k"])
            S.dma("pool", lambda e: e.dma_start(out=w2v[:], in_=io["w2_v"].rearrange("(c p) n -> p c n", p=128)), writes=["w2v"])
            S.dma("pool", lambda e: e.dma_start(out=pek[:], in_=io["peT_k"]), writes=["pek"])
            S.dma("pool", lambda e: e.dma_start(out=pev[:], in_=io["peT_v"]), writes=["pev"])

        with ps("hb_ps", [128, 2, 2], F32) as hb_ps:
            for kv, (w1, pe) in enumerate([(w1k, pek), (w1v, pev)]):
                for hc in range(2):
                    for l in range(32):
                        S.op("pe", lambda e, kv=kv, hc=hc, l=l, w1=w1, pe=pe: e.matmul(hb_ps[:, kv, hc:hc + 1], lhsT=w1[:, l, hc * 128:(hc + 1) * 128],
                                                                                     rhs=pe[:, l:l + 1], start=(l == 0), stop=(l == 31)),
                             reads=["w1k", "w1v", "pek", "pev"], writes=["hb_ps"])
            if "h" not in _sk:
                S.op("dve", lambda e: e.tensor_copy(out=hb[:], in_=hb_ps[:]), reads=["hb_ps"], writes=["hb"])
            S.barrier()

        for _ in range(int(os.environ.get("K_PENOP", "0"))):
            nc.tensor.wait_ge(S.sems["dve"], 0)
        S.op("dve", lambda e: e.memset(kc16[:], 0.0), writes=[("kc16", 0), ("kc16", 1)])
        S.op("dve", lambda e: e.memset(vcaug2[:], 0.0), writes=["vcaug"])
        with ExitStack() as _es2:
            hid_ps = _es2.enter_context(ps("hid_ps", [128, 4, 2, 128], F32))
            cmp_ps = _es2.enter_context(ps("cmp_ps", [128, 2, 512], F32))
            S.dma("sp", lambda e: e.dma_start(out=kcT2[:], in_=PFM[24:26].rearrange("c p t -> p c t")), reads=["PFM"], writes=["kcT2"])
            S.dma("sp", lambda e: e.dma_start(out=vcT2[:], in_=PFM[26:28].rearrange("c p t -> p c t")), reads=["PFM"], writes=["vcT2"])
            for u in range(4):
                kv, gg = u // 2, u % 2
                w1 = w1k if kv == 0 else w1v
                src = kcT2 if kv == 0 else vcT2
                nm = "kcT2" if kv == 0 else "vcT2"
                kb = u % 2
                S.op("dve" if u % 2 == 0 else "pool", lambda e, kb=kb, src=src, gg=gg: e.tensor_copy(
                    out=kc16[:, kb, :, 0:128], in_=src[:, gg, :].rearrange("p (c r) -> p r c", r=16)), reads=[nm], writes=[("kc16", kb)])
                for hc in range(2):
                    for l in range(32):
                        S.op("pe", lambda e, u=u, kb=kb, hc=hc, l=l, w1=w1: e.matmul(
                            hid_ps[:, u, hc, :], lhsT=w1[:, l, hc * 128:(hc + 1) * 128], rhs=kc16[:, kb, l % 16, (l // 16):(l // 16) + 128],
                            start=(l == 0), stop=(l == 31)), reads=["w1k", "w1v", ("kc16", kb)], writes=[("hid_ps", u // 2)])
                    S.op("act", lambda e, u=u, kv=kv, hc=hc: e.activation(out=hid[:, u, hc, :], in_=hid_ps[:, u, hc, :], func=AF.Silu,
                                                                     bias=hb[:, kv, hc:hc + 1]), reads=[("hid_ps", u // 2), "hb"], writes=["hid"])
            for gg in range(2):
                for hc in range(2):
                    S.op("pe", lambda e, gg=gg, hc=hc: e.matmul(cmp_ps[:, 0, gg * 128:(gg + 1) * 128], lhsT=w2k[:, hc, :], rhs=hid[:, gg, hc, :], start=(hc == 0), stop=(hc == 1)),
                         reads=["w2k", "hid"], writes=["cmp_ps0"])
            for gg in range(2):
                for hc in range(2):
                    S.op("pe", lambda e, gg=gg, hc=hc: e.matmul(cmp_ps[:, 1, gg * 128:(gg + 1) * 128], lhsT=hid[:, 2 + gg, hc, :], rhs=w2v[:, hc, :], start=(hc == 0), stop=(hc == 1)),
                         reads=["w2v", "hid"], writes=["cmp_ps1"])
            _cs = os.environ.get("K_CONS", "")
            if "A" not in _cs:
                S.op("act", lambda e: e.copy(out=kcmpT2[:], in_=cmp_ps[:, 0, 0:256].rearrange("p (a b) -> p a b", a=2)), reads=["cmp_ps0"], writes=["kcmpT"])
            if "V" not in _cs:
                S.op("dve", lambda e: e.tensor_copy(out=vcaug2[:, :, 0:128], in_=cmp_ps[:, 1, 0:256].rearrange("p (a b) -> p a b", a=2)), reads=["cmp_ps1", "vcaug"], writes=["vcaug"])
            if "M" not in _cs:
                S.op("dve", lambda e: e.memset(vcaug2[:, :, 128:129], 1.0), reads=["vcaug"], writes=["vcaug"])
            for gg in range(2 if "O" not in _cs else 0):
                S.op("dve", lambda e, gg=gg: e.tensor_copy(out=vcaug2[:, gg, 129:161], in_=overlap[:]), reads=["overlap", "vcaug"], writes=["vcaug"])
            S.barrier()
        if "cmp" in os.environ.get("K_ESKIP", ""):
            return
        for g in ([int(c) for c in os.environ["K_GSEL"]] if os.environ.get("K_GSEL") else range(2)):
            _ld = os.environ.get("K_ELD", "")
            if "q" not in _ld:
                S.dma("sp", lambda e, g=g: e.dma_start(out=qT4[:], in_=PFM[16 + 4 * g:20 + 4 * g].rearrange("c p t -> p c t")), reads=["PFM"], writes=["qT4"])
            for nm, t, ch in [("ksT", ksT, 28), ("kwT", kwT, 30)]:
                S.dma("sp", lambda e, t=t, ch=ch, g=g: e.dma_start(out=t[:], in_=PFM[ch + g]), reads=["PFM"], writes=[nm])
            if "a" not in _ld:
                S.dma("sp", lambda e, g=g: e.dma_start(out=vsa[:, :, 0:128], in_=ptm_v[:, :, 2048 + g * 128:2048 + (g + 1) * 128]), reads=["PTM"], writes=["vsa"])
                S.dma("sp", lambda e, g=g: e.dma_start(out=vwa[:, :, 0:128], in_=ptm_v[:, :, 2304 + g * 128:2304 + (g + 1) * 128]), reads=["PTM"], writes=["vwa"])
            if "m" not in _sk:
                S.op("dve", lambda e: e.memset(vsa[:, :, 128:130], 1.0), reads=["vsa"], writes=["vsa"])
                S.op("dve", lambda e: e.memset(vwa[:, :, 128:130], 1.0), reads=["vwa"], writes=["vwa"])
            if "e2a" in os.environ.get("K_ESKIP", ""):
                continue
            with ExitStack() as _es:
                sc_ps = _es.enter_context(ps("sc_ps", [128, 2, 512], F32))
                oc_ps = _es.enter_context(ps("oc_ps", [128, 4, 256], F32))
                tps = _es.enter_context(ps("tps", [32, 2, 128], BF16))
                pc = _es.enter_context(sb("pc", [128, 2, 512], BF16))
                rc = _es.enter_context(sb("rc", [128, 4], F32))
                imp = _es.enter_context(sb("imp", [128, 32], F32))
                score = _es.enter_context(sb("score", [128, 32], F32))
                sc2 = _es.enter_context(sb("sc2", [128, 32], F32))
                m8 = _es.enter_context(sb("m8", [128, 16], F32))
                selneg = _es.enter_context(sb("selneg", [128, 32], BF16))
                coef = _es.enter_context(sb("coef", [128, 4], F32))
                for qt in range(16):
                    b = qt % 2
                    qsl = slice(qt * 128, (qt + 1) * 128)
                    S.bg(1)
                    S.op("pe", lambda e, b=b, qsl=qsl: e.matmul(sc_ps[:, b, :].rearrange("p (h t) -> p h t", h=4), lhsT=kcmpT2[:, g, :], rhs=qT4[:, :, qsl],
                                                                start=True, stop=False), reads=["kcmpT", "qT4"], writes=[("sc_ps", b)])
                    S.op("pe", lambda e, b=b, qsl=qsl: e.matmul(sc_ps[:, b, :].rearrange("p (h t) -> p h t", h=4), lhsT=identb[:, :],
                                                                rhs=cmask[:, qsl].unsqueeze(1).to_broadcast([128, 4, 128]), start=False, stop=True),
                         reads=["identb", "cmask"], writes=[("sc_ps", b)])
                    S.op("act", lambda e, b=b: e.activation(out=pc[:, b, :], in_=sc_ps[:, b, :], func=AF.Exp), reads=[("sc_ps", b)], writes=[("pc", b)])
                    for h in range(4):
                        S.op("pe", lambda e, b=b, h=h: e.matmul(oc_ps[:, h, 0:162], lhsT=pc[:, b, h * 128:(h + 1) * 128], rhs=vcaug2[:, g, 0:162],
                                                                start=True, stop=True), reads=[("pc", b), "vcaug"], writes=["oc_ps"])
                    for hh in (0, 2):
                        S.op("dve", lambda e, hh=hh: e.tensor_scalar(out=rc[:, hh:hh + 2], in0=oc_ps[:, hh:hh + 2, 128], scalar1=1e-30, scalar2=None, op0=ALU.max),
                             reads=["oc_ps"], writes=["rc"])
                    S.op("dve", lambda e: e.reciprocal(out=rc[:], in_=rc[:]), reads=["rc"], writes=["rc"])
                    S.op("act", lambda e, qt=qt, g=g: e.activation(out=gts[:, qt, :], in_=ngs[:, qt, g * 12:(g + 1) * 12], func=AF.Sigmoid), reads=["ngs"], writes=["gts"])
                    for h in range(4):
                        if h == 0:
                            S.op("dve", lambda e: e.tensor_scalar(out=imp[:], in0=oc_ps[:, 0, 129:161], scalar1=rc[:, 0:1], scalar2=None, op0=ALU.mult),
                                 reads=["oc_ps", "rc"], writes=["imp"])
                        else:
                            S.op("dve", lambda e, h=h: e.scalar_tensor_tensor(out=imp[:], in0=oc_ps[:, h, 129:161], scalar=rc[:, h:h + 1], in1=imp[:],
                                                                              op0=ALU.mult, op1=ALU.add), reads=["oc_ps", "rc", "imp"], writes=["imp"])
                    S.op("dve", lambda e, qt=qt: e.tensor_tensor(out=coef[:], in0=rc[:], in1=gts[:, qt, 0:12:3], op=ALU.mult), reads=["rc", "gts"], writes=["coef"])
                    for h in range(4):
                        S.op("dve", lambda e, h=h, qt=qt: e.tensor_scalar(out=ocmp[:, qt, h, :], in0=oc_ps[:, h, 0:128], scalar1=coef[:, h:h + 1], scalar2=None,
                                                                          op0=ALU.mult), reads=["oc_ps", "coef"], writes=["ocmp"])
                    S.op("dve", lambda e, qt=qt: e.tensor_tensor(out=score[:], in0=imp[:], in1=valid[:, qt, :], op=ALU.mult), reads=["imp", "valid"], writes=["score"])
                    S.op("dve", lambda e, qt=qt: e.tensor_tensor(out=score[:], in0=score[:], in1=ctab[:, qt, :], op=ALU.add), reads=["score", "ctab"], writes=["score"])
                    S.op("dve", lambda e: e.max(out=m8[:, 0:8], in_=score[:]), reads=["score"], writes=["m8a"])
                    S.op("dve", lambda e: e.match_replace(out=sc2[:], in_to_replace=m8[:, 0:8], in_values=score[:], imm_value=-2.0),
                         reads=["score", "m8a"], writes=["sc2"])
                    S.op("dve", lambda e: e.max(out=m8[:, 8:16], in_=sc2[:]), reads=["sc2"], writes=["m8b"])
                    S.op("dve", lambda e: e.tensor_scalar(out=selneg[:], in0=score[:], scalar1=m8[:, 15:16], scalar2=NEG, op0=ALU.is_lt, op1=ALU.mult),
                         reads=["score", "m8b"], writes=["selneg"])
                    S.op("pe", lambda e, b=b: e.transpose(tps[:, 0, :], selneg[:], identb[:]), reads=["selneg", "identb"], writes=["tps"])
                    S.op("act", lambda e, b=b, qsl=qsl: e.copy(out=selnegT[:, qsl], in_=tps[:, 0, :]), reads=["tps"], writes=["selnegT"])
                S.barrier()
            if "e2b" in os.environ.get("K_ESKIP", ""):
                continue
            with ExitStack() as _es:
                ss_ps = _es.enter_context(ps("ss_ps", [128, 2, 512], F32))
                os_ps = _es.enter_context(ps("os_ps", [128, 4, 256], F32))
                ow_ps = _es.enter_context(ps("ow_ps", [128, 4, 256], F32))
                tpo = _es.enter_context(ps("tpo", [128, 4, 128], BF16))
                pp = _es.enter_context(sb("pp", [128, 2, 512], BF16))
                rs = _es.enter_context(sb("rs", [128, 4], F32))
                rw = _es.enter_context(sb("rw", [128, 4], F32))
                acc = _es.enter_context(sb("acc", [128, 4, 128], F32))
                ob4 = _es.enter_context(sb("ob4", [128, 4, 128], BF16))
                n = 0
                for qt in range(16):
                    qsl = slice(qt * 128, (qt + 1) * 128)
                    S.bg(1)
                    for br, (kTt, knm, va, vnm, o_ps, kts) in enumerate([
                            (ksT, "ksT", vsa, "vsa", os_ps, list(range(0, qt + 1))),
                            (kwT, "kwT", vwa, "vwa", ow_ps, list(range(max(0, qt - 4), qt + 1)))]):
                        opk = "os_ps" if br == 0 else "ow_ps"
                        for kt in kts:
                            b = n % 2
                            n += 1
                            ksl = slice(kt * 128, (kt + 1) * 128)
                            extra = []
                            if br == 0:
                                extra.append((esel[:, kt, :], selnegT[:, qsl].unsqueeze(1).to_broadcast([32, 4, 128]), ["esel", "selnegT"]))
                            if kt == qt:
                                extra.append((identb[:], caus[:].unsqueeze(1).to_broadcast([128, 4, 128]), ["identb", "caus"]))
                            if br == 1 and kt == qt - 4:
                                extra.append((identb[:], anti[:].unsqueeze(1).to_broadcast([128, 4, 128]), ["identb", "anti"]))
                            outv = ss_ps[:, b, :].rearrange("p (h t) -> p h t", h=4)
                            S.op("pe", lambda e, outv=outv, kTt=kTt, ksl=ksl, qsl=qsl, last=(len(extra) == 0): e.matmul(
                                outv, lhsT=kTt[:, ksl], rhs=qT4[:, :, qsl], start=True, stop=last), reads=[knm, "qT4"], writes=[("ss_ps", b)])
                            for xi, (l_, r_, rd) in enumerate(extra):
                                S.op("pe", lambda e, outv=outv, l_=l_, r_=r_, last=(xi == len(extra) - 1): e.matmul(outv, lhsT=l_, rhs=r_, start=False, stop=last),
                                     reads=rd, writes=[("ss_ps", b)])
                            S.op("act", lambda e, b=b: e.activation(out=pp[:, b, :], in_=ss_ps[:, b, :], func=AF.Exp), reads=[("ss_ps", b)], writes=[("pp", b)])
                            for h in range(4):
                                S.op("pe", lambda e, b=b, h=h, kt=kt, va=va, o_ps=o_ps, first=(kt == kts[0]), lastk=(kt == kts[-1]): e.matmul(
                                    o_ps[:, h, 0:130], lhsT=pp[:, b, h * 128:(h + 1) * 128], rhs=va[:, kt, :], start=(first and h % 2 == 0), stop=lastk, skip_group_check=True),
                                    reads=[("pp", b), vnm], writes=[opk])
                    for hh in (0, 2):
                        S.op("dve", lambda e, hh=hh: e.reciprocal(out=rs[:, hh:hh + 2], in_=os_ps[:, hh:hh + 2, 128]), reads=["os_ps"], writes=["rs"])
                        S.op("dve", lambda e, hh=hh: e.reciprocal(out=rw[:, hh:hh + 2], in_=ow_ps[:, hh:hh + 2, 128]), reads=["ow_ps"], writes=["rw"])
                    S.op("dve", lambda e, qt=qt: e.tensor_tensor(out=rs[:], in0=rs[:], in1=gts[:, qt, 1:12:3], op=ALU.mult), reads=["rs", "gts"], writes=["rs"])
                    S.op("dve", lambda e, qt=qt: e.tensor_tensor(out=rw[:], in0=rw[:], in1=gts[:, qt, 2:12:3], op=ALU.mult), reads=["rw", "gts"], writes=["rw"])
                    for h in range(4):
                        S.op("dve", lambda e, h=h: e.tensor_scalar(out=acc[:, h, :], in0=os_ps[:, h, 0:128], scalar1=rs[:, h:h + 1], scalar2=None, op0=ALU.mult),
                             reads=["os_ps", "rs"], writes=["acc"])
                        S.op("dve", lambda e, h=h: e.scalar_tensor_tensor(out=acc[:, h, :], in0=ow_ps[:, h, 0:128], scalar=rw[:, h:h + 1], in1=acc[:, h, :],
                                                                          op0=ALU.mult, op1=ALU.add), reads=["ow_ps", "rw", "acc"], writes=["acc"])
                    S.op("pool", lambda e, qt=qt: e.tensor_tensor(out=ob4[:], in0=acc[:], in1=ocmp[:, qt], op=ALU.add), reads=["acc", "ocmp"], writes=["ob4"])
                    for h in range(4):
                        S.op("pe", lambda e, h=h: e.transpose(tpo[:, h, :], ob4[:, h, :], identb[:]), reads=["ob4", "identb"], writes=["tpo"])
                    S.op("act", lambda e, g=g, qsl=qsl: e.copy(out=ocatT[:, 8 + 4 * g:12 + 4 * g, qsl], in_=tpo[:]), reads=["tpo"], writes=["ocatT"])
                S.barrier()
        S.barrier()


def stage_F(nc, S, io, ocatT, g1row, X1):
    sb, ps = _namers(nc)
    wout = io["w_out"].rearrange("(k p) n -> p k n", p=128)
    with ExitStack() as _es:
        wo = _es.enter_context(sb("wo", [128, 2, 16, 512], BF16))
        xr = _es.enter_context(sb("xr", [128, 2, 512], F32))
        x1t = _es.enter_context(sb("x1t", [128, 2, 512], F32))
        mx_ps = _es.enter_context(ps("mx_ps", [128, 2, 512], F32))
        n = 0
        for cb in range(4):
            wb = cb % 2
            csl = slice(cb * 512, (cb + 1) * 512)
            S.dma("pool", lambda e, wb=wb, csl=csl: e.dma_start(out=wo[:, wb], in_=wout[:, :, csl]), writes=[("wo", wb)])
            for i in range(16):
                b = n % 2
                n += 1
                if n % 4 == 0:
                    S.bg(1)
                isl = slice(i * 128, (i + 1) * 128)
                S.dma("sp", lambda e, b=b, isl=isl, csl=csl: e.dma_start(out=xr[:, b, :], in_=io["x"][isl, csl]), writes=[("xr", b)])
                for k in range(16):
                    S.op("pe", lambda e, b=b, wb=wb, k=k, isl=isl: e.matmul(mx_ps[:, b, :], lhsT=ocatT[:, k, isl], rhs=wo[:, wb, k, :],
                                                                            start=(k == 0), stop=(k == 15)), reads=["ocatT", ("wo", wb)], writes=[("mx_ps", b)])
                S.op("dve", lambda e, b=b, csl=csl: e.tensor_tensor(out=x1t[:, b, :], in0=mx_ps[:, b, :], in1=g1row[:, csl], op=ALU.mult),
                     reads=[("mx_ps", b), "g1row"], writes=[("x1t", b)])
                S.op("pool", lambda e, b=b: e.tensor_tensor(out=x1t[:, b, :], in0=x1t[:, b, :], in1=xr[:, b, :], op=ALU.add),
                     reads=[("x1t", b), ("xr", b)], writes=[("x1t", b)])
                S.dma("sp", lambda e, b=b, isl=isl, csl=csl: e.dma_start(out=X1[isl, csl], in_=x1t[:, b, :]), reads=[("x1t", b)], writes=["X1"])
        S.barrier()


def stage_G(nc, S, io, X1, XS, a2row, sh2row, identf, slots, wab, dbgout, final_toks):
    sb, ps = _namers(nc)
    with ExitStack() as _es:
        x1 = _es.enter_context(sb("x1", [128, 2, D], F32))
        h2f = _es.enter_context(sb("h2f", [128, D], F32))
        h2b = _es.enter_context(sb("h2b", [128, 2, D], BF16))
        junk = _es.enter_context(sb("junk2", [128, D], BF16))
        h2Tf = _es.enter_context(sb("h2Tf", [128, 16, 128], F32))
        wr = _es.enter_context(sb("wr", [128, 16, 72], F32))
        brt = _es.enter_context(sb("brt", [128, 72], F32))
        lstrict = _es.enter_context(sb("lstrict", [128, 128], BF16))
        ones = _es.enter_context(sb("ones", [128, 128], BF16))
        ebase = _es.enter_context(sb("ebase", [128, 64], F32))
        ss2 = _es.enter_context(sb("ss2", [128, 16], F32))
        rstd2 = _es.enter_context(sb("rstd2", [128, 16], F32))
        lg = _es.enter_context(sb("lg", [128, 72], F32))
        gm = _es.enter_context(sb("gm", [128, 4], F32))
        eg = _es.enter_context(sb("eg", [128, 8], F32))
        onehot = _es.enter_context(sb("onehot", [128, 8], F32))
        tmp88 = _es.enter_context(sb("tmp88", [128, 8, 8], F32))
        leg = _es.enter_context(sb("leg", [128, 8], F32))
        m8r = _es.enter_context(sb("m8r", [128, 8], F32))
        selloc = _es.enter_context(sb("selloc", [128, 8], F32))
        wl = _es.enter_context(sb("wl", [128, 8], F32))
        wfull = _es.enter_context(sb("wfull", [128, 64], F32))
        Ab = _es.enter_context(sb("Ab", [128, 64], BF16))
        Af = _es.enter_context(sb("Af", [128, 64], F32))
        tot = _es.enter_context(sb("tot", [128, 64], F32))
        cnt = _es.enter_context(sb("cnt", [128, 64], F32))
        key = _es.enter_context(sb("key", [128, 64], F32))
        m8k = _es.enter_context(sb("m8k", [128, 8], F32))
        slf = _es.enter_context(sb("slf", [128, 2], F32))
        eq = _es.enter_context(sb("eq", [128, 64], F32))
        tpf = _es.enter_context(ps("tpf", [128, 2, 4, 128], F32))
        lg_ps = _es.enter_context(ps("lg_ps", [128, 72], F32))
        cnt_ps = _es.enter_context(ps("cnt_ps", [128, 2, 64], F32))
        S.dma("sp", lambda e: e.dma_start(out=wr[:], in_=io["w_rt"].rearrange("(k p) n -> p k n", p=128)), writes=["wr"])
        for nm, t in [("b_rt", brt), ("lstrict", lstrict), ("ones", ones), ("ebase", ebase)]:
            S.dma("sp", lambda e, nm=nm, t=t: e.dma_start(out=t[:], in_=io[nm]), writes=[nm])
        S.op("dve", lambda e: e.memset(ss2[:], 0.0), writes=["ss2"])
        S.op("dve", lambda e: e.memset(tot[:], 0.0), writes=["tot"])
        S.op("dve", lambda e: e.memset(gm[:], 0.0), writes=["gm"])
        for i in range(16):
            b = i % 2
            isl = slice(i * 128, (i + 1) * 128)
            S.bg(1)
            S.dma("sp", lambda e, b=b, isl=isl: e.dma_start(out=x1[:, b], in_=X1[isl, :]), reads=["X1"], writes=[("x1", b)])
            S.op("act", lambda e, b=b, i=i: e.activation(out=junk[:], in_=x1[:, b], func=AF.Square, accum_out=ss2[:, i:i + 1]),
                 reads=[("x1", b), "ss2"], writes=["junk", ("ss2", i)])
            S.op("act", lambda e, i=i: e.activation(out=rstd2[:, i:i + 1], in_=ss2[:, i:i + 1], func=AF.Sqrt, bias=epsc[:, 0:1], scale=1.0 / D),
                 reads=[("ss2", i), "epsc"], writes=[("rstd2", i)])
            S.op("dve", lambda e, i=i: e.reciprocal(out=rstd2[:, i:i + 1], in_=rstd2[:, i:i + 1]), reads=[("rstd2", i)], writes=[("rstd2", i)])
            S.op("dve", lambda e, b=b, i=i: e.scalar_tensor_tensor(out=h2f[:], in0=x1[:, b], scalar=rstd2[:, i:i + 1], in1=a2row[:], op0=ALU.mult, op1=ALU.mult),
                 reads=[("x1", b), ("rstd2", i), "a2row"], writes=["h2f"])
            S.op("pool", lambda e: e.tensor_tensor(out=h2f[:], in0=h2f[:], in1=sh2row[:], op=ALU.add), reads=["h2f", "sh2row"], writes=["h2f"])
            S.op("act", lambda e, b=b: e.copy(out=h2b[:, b], in_=h2f[:]), reads=["h2f"], writes=[("h2b", b)])
            for k4 in range(4):
                pb = k4 % 2
                for kk in range(4):
                    k = k4 * 4 + kk
                    S.op("pe", lambda e, pb=pb, kk=kk, k=k: e.transpose(tpf[:, pb, kk, :], h2f[:, k * 128:(k + 1) * 128], identf[:]),
                         reads=["h2f", "identf"], writes=[("tpf", pb)])
                S.op("dve", lambda e, pb=pb, k4=k4: e.tensor_copy(out=h2Tf[:, k4 * 4:(k4 + 1) * 4, :], in_=tpf[:, pb]), reads=[("tpf", pb)], writes=["h2Tf"])
            for k in range(16):
                S.op("pe", lambda e, k=k: e.matmul(lg_ps[:], lhsT=h2Tf[:, k, :], rhs=wr[:, k, :], start=(k == 0), stop=(k == 15)),
                     reads=["h2Tf", "wr"], writes=["lg_ps"])
            V = lambda fn, r, w: S.op("dve", fn, reads=r, writes=w)
            V(lambda e: e.tensor_tensor(out=lg[:], in0=lg_ps[:], in1=brt[:], op=ALU.add), ["lg_ps", "b_rt"], ["lg"])
            V(lambda e: e.tensor_reduce(out=gm[:, 0:1], in_=lg[:, 0:8], axis=AX.X, op=ALU.max), ["lg"], ["gm0"])
            V(lambda e: e.tensor_scalar(out=gm[:, 1:2], in0=gm[:, 0:1], scalar1=-1.0, scalar2=None, op0=ALU.mult), ["gm0"], ["gm1"])
            S.op("act", lambda e: e.activation(out=eg[:], in_=lg[:, 0:8], func=AF.Exp, bias=gm[:, 1:2], scale=1.0), reads=["lg", "gm1"], writes=["eg"])
            V(lambda e: e.tensor_reduce(out=gm[:, 2:3], in_=eg[:], axis=AX.X, op=ALU.add), ["eg"], ["gm2"])
            V(lambda e: e.tensor_scalar(out=onehot[:], in0=lg[:, 0:8], scalar1=gm[:, 0:1], scalar2=None, op0=ALU.is_ge), ["lg", "gm0"], ["onehot"])
            V(lambda e: e.tensor_tensor(out=tmp88[:], in0=lg[:, 8:72].rearrange("p (g j) -> p g j", g=8),
                                        in1=onehot[:].unsqueeze(2).to_broadcast([128, 8, 8]), op=ALU.mult), ["lg", "onehot"], ["tmp88"])
            V(lambda e: e.tensor_reduce(out=leg[:], in_=tmp88[:].rearrange("p g j -> p j g"), axis=AX.X, op=ALU.add), ["tmp88"], ["leg"])
            V(lambda e: e.max(out=m8r[:], in_=leg[:]), ["leg"], ["m8r"])
            V(lambda e: e.tensor_scalar(out=selloc[:], in0=leg[:], scalar1=m8r[:, 1:2], scalar2=None, op0=ALU.is_ge), ["leg", "m8r"], ["selloc"])
            V(lambda e: e.tensor_scalar(out=gm[:, 3:4], in0=m8r[:, 0:1], scalar1=-1.0, scalar2=None, op0=ALU.mult), ["m8r"], ["gm3"])
            S.op("act", lambda e: e.activation(out=wl[:], in_=leg[:], func=AF.Exp, bias=gm[:, 3:4], scale=1.0), reads=["leg", "gm3"], writes=["wl"])
            V(lambda e: e.tensor_tensor(out=wl[:], in0=wl[:], in1=selloc[:], op=ALU.mult), ["wl", "selloc"], ["wl"])
            V(lambda e: e.tensor_reduce(out=gm[:, 1:2], in_=wl[:], axis=AX.X, op=ALU.add), ["wl", "eg"], ["gm1"])
            V(lambda e: e.tensor_tensor(out=gm[:, 1:2], in0=gm[:, 1:2], in1=gm[:, 2:3], op=ALU.mult), ["gm1", "gm2"], ["gm1"])
            V(lambda e: e.reciprocal(out=gm[:, 1:2], in_=gm[:, 1:2]), ["gm1"], ["gm1"])
            V(lambda e: e.tensor_scalar(out=wl[:], in0=wl[:], scalar1=gm[:, 1:2], scalar2=None, op0=ALU.mult), ["wl", "gm1"], ["wl"])
            V(lambda e: e.tensor_tensor(out=wfull[:].rearrange("p (g j) -> p g j", g=8), in0=onehot[:].unsqueeze(2).to_broadcast([128, 8, 8]),
                                        in1=wl[:].unsqueeze(1).to_broadcast([128, 8, 8]), op=ALU.mult), ["onehot", "wl"], ["wfull"])
            V(lambda e: e.tensor_scalar(out=Af[:], in0=wfull[:], scalar1=0.0, scalar2=None, op0=ALU.is_gt), ["wfull"], ["Af"])
            V(lambda e: e.tensor_copy(out=Ab[:], in_=Af[:]), ["Af"], ["Ab"])
            S.op("pe", lambda e: e.matmul(cnt_ps[:, 0, :], lhsT=lstrict[:], rhs=Ab[:], start=True, stop=True), reads=["lstrict", "Ab"], writes=["cnt_ps"])
            S.op("pe", lambda e: e.matmul(cnt_ps[:, 1, :], lhsT=ones[:], rhs=Ab[:], start=True, stop=True), reads=["ones", "Ab"], writes=["cnt_ps"])
            V(lambda e: e.tensor_tensor(out=cnt[:], in0=cnt_ps[:, 0, :], in1=tot[:], op=ALU.add), ["cnt_ps", "tot"], ["cnt"])
            V(lambda e: e.tensor_tensor(out=tot[:], in0=cnt_ps[:, 1, :], in1=tot[:], op=ALU.add), ["cnt_ps", "tot"], ["tot"])
            V(lambda e: e.tensor_scalar(out=cnt[:], in0=cnt[:], scalar1=float(CAP - 1), scalar2=None, op0=ALU.min), ["cnt"], ["cnt"])
            V(lambda e: e.tensor_tensor(out=key[:], in0=cnt[:], in1=ebase[:], op=ALU.add), ["cnt", "ebase"], ["key"])
            V(lambda e: e.tensor_tensor(out=key[:], in0=key[:], in1=Af[:], op=ALU.mult), ["key", "Af"], ["key"])
            V(lambda e: e.max(out=m8k[:], in_=key[:]), ["key"], ["m8k"])
            V(lambda e: e.tensor_scalar(out=slf[:], in0=m8k[:, 0:2], scalar1=-1.0, scalar2=None, op0=ALU.add), ["m8k"], ["slf"])
            V(lambda e, i=i: e.tensor_copy(out=slots[:, i, :], in_=slf[:]), ["slf"], ["slots", ("slots", i)])
            for j in range(2):
                V(lambda e, j=j: e.tensor_scalar(out=eq[:], in0=key[:], scalar1=m8k[:, j:j + 1], scalar2=None, op0=ALU.is_equal), ["key", "m8k"], ["eq"])
                V(lambda e: e.tensor_tensor(out=eq[:], in0=eq[:], in1=wfull[:], op=ALU.mult), ["eq", "wfull"], ["eq"])
                V(lambda e, i=i, j=j: e.tensor_reduce(out=wab[:, i, j:j + 1], in_=eq[:], axis=AX.X, op=ALU.add), ["eq"], ["wab"])
            if "d_misc" in dbgout:
                final_toks.append(S.dma("sp", lambda e, i=i: e.dma_start(out=dbgout["d_misc"][:, i, :], in_=wfull[:]), reads=["wfull"], writes=["dbg_misc"]))
            for j in range(2):
                S.dma("pool", lambda e, b=b, i=i, j=j: e.indirect_dma_start(
                    out=XS, out_offset=bass.IndirectOffsetOnAxis(ap=slots[:, i, j:j + 1], axis=0), in_=h2b[:, b, :], in_offset=None),
                    reads=[("h2b", b), ("slots", i)], writes=["XS"])
        S.barrier()


def stage_H(nc, S, io, XS, YS, identb, WGB, WUB, WDB):
    sb, ps = _namers(nc)
    with ExitStack() as _es:
        wg = _es.enter_context(sb("wg", [128, 2, 16, 512], BF16))
        wu = _es.enter_context(sb("wu", [128, 2, 16, 512], BF16))
        wd = _es.enter_context(sb("wd", [128, 2, 4, D], BF16))
        xs = _es.enter_context(sb("xs", [128, 2, RB, D], BF16))
        xsT = _es.enter_context(sb("xsT", [128, 16, CAP], BF16))
        sgh = _es.enter_context(sb("sgh", [128, 2, CAP], F32))
        aT = _es.enter_context(sb("aT", [128, 4, CAP], BF16))
        ysb = _es.enter_context(sb("ysb", [128, 2, D], F32))
        tpx = _es.enter_context(ps("tpx", [128, 2, 1024], BF16))
        gu_ps = _es.enter_context(ps("gu_ps", [128, 2, 2, CAP], F32))
        y_ps = _es.enter_context(ps("y_ps", [128, 2, 512], F32))
        n = 0
        ny = 0
        for ex in range(64):
            wb = ex % 2
            S.dma("sp", lambda e, wb=wb, ex=ex: e.dma_start(out=wg[:, wb], in_=WGB[ex].rearrange("(k p) n -> p k n", p=128)), reads=[("wgb", ex)], writes=[("wg", wb)])
            S.dma("pool", lambda e, wb=wb, ex=ex: e.dma_start(out=wu[:, wb], in_=WUB[ex].rearrange("(k p) n -> p k n", p=128)), reads=[("wub", ex)], writes=[("wu", wb)])
            S.dma("pool", lambda e, wb=wb, ex=ex: e.dma_start(out=wd[:, wb], in_=WDB[ex].rearrange("(c p) n -> p c n", p=128)), reads=[("wdb", ex)], writes=[("wd", wb)])
            S.dma("sp", lambda e, wb=wb, ex=ex: e.dma_start(out=xs[:, wb], in_=XS[ex * CAP:(ex + 1) * CAP, :].rearrange("(r p) d -> p r d", p=128)),
                  reads=["XS"], writes=[("xs", wb)])
            for r in range(RB):
                for k4 in range(4):
                    pb = n % 2
                    n += 1
                    for kk in range(4):
                        k = k4 * 4 + kk
                        S.op("pe", lambda e, wb=wb, r=r, pb=pb, kk=kk, k=k: e.transpose(tpx[:, pb, kk * 128:(kk + 1) * 128], xs[:, wb, r, k * 128:(k + 1) * 128], identb[:]),
                             reads=[("xs", wb), "identb"], writes=[("tpx", pb)])
                    eng = "act" if n % 2 == 0 else "dve"
                    if eng == "act":
                        S.op("act", lambda e, pb=pb, k4=k4, r=r: e.copy(out=xsT[:, k4 * 4:(k4 + 1) * 4, r * 128:(r + 1) * 128], in_=tpx[:, pb, 0:512].rearrange("p (a b) -> p a b", a=4)),
                             reads=[("tpx", pb)], writes=["xsT"])
                    else:
                        S.op("dve", lambda e, pb=pb, k4=k4, r=r: e.tensor_copy(out=xsT[:, k4 * 4:(k4 + 1) * 4, r * 128:(r + 1) * 128], in_=tpx[:, pb, 0:512].rearrange("p (a b) -> p a b", a=4)),
                             reads=[("tpx", pb)], writes=["xsT"])
            for hc in range(4):
                gb = hc % 2
                for which, w in enumerate([wg, wu]):
                    for k in range(16):
                        S.op("pe", lambda e, gb=gb, which=which, w=w, wb=wb, k=k, hc=hc: e.matmul(
                            gu_ps[:, gb, which, :], lhsT=w[:, wb, k, hc * 128:(hc + 1) * 128], rhs=xsT[:, k, :], start=(k == 0), stop=(k == 15)),
                            reads=[("wg", wb), ("wu", wb), "xsT"], writes=[("gu_ps", gb)])
                S.op("act", lambda e, gb=gb: e.activation(out=sgh[:, gb, :], in_=gu_ps[:, gb, 0, :], func=AF.Silu), reads=[("gu_ps", gb)], writes=[("sgh", gb)])
                S.op("dve", lambda e, gb=gb, hc=hc: e.tensor_tensor(out=aT[:, hc, :], in0=sgh[:, gb, :], in1=gu_ps[:, gb, 1, :], op=ALU.mult),
                     reads=[("sgh", gb), ("gu_ps", gb)], writes=["aT"])
            for r in range(RB):
                yb = ny % 2
                ny += 1
                for cb in range(4):
                    pb = cb % 2
                    for hc in range(4):
                        S.op("pe", lambda e, pb=pb, hc=hc, r=r, wb=wb, cb=cb: e.matmul(
                            y_ps[:, pb, :], lhsT=aT[:, hc, r * 128:(r + 1) * 128], rhs=wd[:, wb, hc, cb * 512:(cb + 1) * 512], start=(hc == 0), stop=(hc == 3)),
                            reads=["aT", ("wd", wb)], writes=[("y_ps", pb)])
                    if cb % 2 == 0:
                        S.op("act", lambda e, pb=pb, yb=yb, cb=cb: e.copy(out=ysb[:, yb, cb * 512:(cb + 1) * 512], in_=y_ps[:, pb, :]),
                             reads=[("y_ps", pb)], writes=[("ysb", yb)])
                    else:
                        S.op("dve", lambda e, pb=pb, yb=yb, cb=cb: e.tensor_copy(out=ysb[:, yb, cb * 512:(cb + 1) * 512], in_=y_ps[:, pb, :]),
                             reads=[("y_ps", pb)], writes=[("ysb", yb)])
                S.dma("sp", lambda e, yb=yb, ex=ex, r=r: e.dma_start(out=YS[ex * CAP + r * 128:ex * CAP + (r + 1) * 128, :], in_=ysb[:, yb]),
                      reads=[("ysb", yb)], writes=["YS"])
        S.barrier()


def stage_I(nc, S, io, X1, YS, g2row, slots, wab, out):
    sb, ps = _namers(nc)
    toks = []
    with ExitStack() as _es:
        ya = _es.enter_context(sb("ya", [128, 2, D], F32))
        yb_ = _es.enter_context(sb("yb_", [128, 2, D], F32))
        x1i = _es.enter_context(sb("x1i", [128, 2, D], F32))
        mo = _es.enter_context(sb("mo", [128, D], F32))
        ot = _es.enter_context(sb("ot", [128, 2, D], F32))
        fg = _es.enter_context(sb("fg", [128, D], F32))
        junk = _es.enter_context(sb("junk3", [128, D], BF16))
        ss3 = _es.enter_context(sb("ss3", [128, 16], F32))
        rstd3 = _es.enter_context(sb("rstd3", [128, 16], F32))
        S.dma("sp", lambda e: e.dma_start(out=fg[:], in_=io["fg_row"]), writes=["fg"])
        S.op("dve", lambda e: e.memset(ss3[:], 0.0), writes=["ss3"])
        for i in range(16):
            b = i % 2
            isl = slice(i * 128, (i + 1) * 128)
            S.dma("pool", lambda e, b=b, i=i: e.indirect_dma_start(out=ya[:, b, :], out_offset=None, in_=YS,
                                                                   in_offset=bass.IndirectOffsetOnAxis(ap=slots[:, i, 0:1], axis=0)),
                  reads=["YS", "slots"], writes=[("ya", b)])
            S.dma("pool", lambda e, b=b, i=i: e.indirect_dma_start(out=yb_[:, b, :], out_offset=None, in_=YS,
                                                                   in_offset=bass.IndirectOffsetOnAxis(ap=slots[:, i, 1:2], axis=0)),
                  reads=["YS", "slots"], writes=[("yb", b)])
            S.dma("sp", lambda e, b=b, isl=isl: e.dma_start(out=x1i[:, b], in_=X1[isl, :]), reads=["X1"], writes=[("x1i", b)])
            S.op("dve", lambda e, b=b, i=i: e.tensor_scalar(out=mo[:], in0=ya[:, b], scalar1=wab[:, i, 0:1], scalar2=None, op0=ALU.mult),
                 reads=[("ya", b), "wab"], writes=["mo"])
            S.op("dve", lambda e, b=b, i=i: e.scalar_tensor_tensor(out=mo[:], in0=yb_[:, b], scalar=wab[:, i, 1:2], in1=mo[:], op0=ALU.mult, op1=ALU.add),
                 reads=[("yb", b), "wab", "mo"], writes=["mo"])
            S.op("pool", lambda e: e.tensor_tensor(out=mo[:], in0=mo[:], in1=g2row[:], op=ALU.mult), reads=["mo", "g2row"], writes=["mo"])
            S.op("pool", lambda e, b=b: e.tensor_tensor(out=mo[:], in0=mo[:], in1=x1i[:, b], op=ALU.add), reads=["mo", ("x1i", b)], writes=["mo"])
            S.op("act", lambda e, i=i: e.activation(out=junk[:], in_=mo[:], func=AF.Square, accum_out=ss3[:, i:i + 1]), reads=["mo", "ss3"], writes=["junk", ("ss3", i)])
            S.op("act", lambda e, i=i: e.activation(out=rstd3[:, i:i + 1], in_=ss3[:, i:i + 1], func=AF.Sqrt, bias=epsc[:, 0:1], scale=1.0 / D),
                 reads=[("ss3", i), "epsc"], writes=[("rstd3", i)])
            S.op("dve", lambda e, i=i: e.reciprocal(out=rstd3[:, i:i + 1], in_=rstd3[:, i:i + 1]), reads=[("rstd3", i)], writes=[("rstd3", i)])
            S.op("dve", lambda e, b=b, i=i: e.scalar_tensor_tensor(out=ot[:, b], in0=mo[:], scalar=rstd3[:, i:i + 1], in1=fg[:], op0=ALU.mult, op1=ALU.mult),
                 reads=["mo", ("rstd3", i), "fg"], writes=[("ot", b)])
            toks.append(S.dma("sp", lambda e, b=b, isl=isl: e.dma_start(out=out[isl, :], in_=ot[:, b]), reads=[("ot", b)], writes=["out"]))
        S.barrier()
    return toks


def host_inputs(inp, b):
    f = lambda a: np.ascontiguousarray(a, dtype=np.float32)
    m = {}
    m["x"] = f(inp["x"][b])
    m["cT"] = f(inp["c"][b].reshape(16, 128).T)
    m["w_ada"] = f(inp["w_ada"][0])
    m["b_adaT"] = f(inp["b_ada"][0].reshape(96, 128).T)
    m["n1gT"] = f(inp["norm1_g"][0].reshape(16, 128).T)
    m["n2gT"] = f(inp["norm2_g"][0].reshape(16, 128).T)
    m["fg_row"] = f(np.broadcast_to(inp["final_g"][None, :], (128, D)))
    m["w_in"] = f(inp["w_in"][0])
    m["gng_row"] = f(np.broadcast_to(inp["ret_gn_g"][0][None, :], (128, 1024)))
    m["peT_k"] = f(inp["cmp_pos_k"][0].T)
    m["w1_k"] = f(inp["cmp_w1_k"][0])
    m["w2_k"] = f(inp["cmp_w2_k"][0])
    m["peT_v"] = f(inp["cmp_pos_v"][0].T)
    m["w1_v"] = f(inp["cmp_w1_v"][0])
    m["w2_v"] = f(inp["cmp_w2_v"][0])
    m["w_out"] = f(inp["w_out"][0])
    m["w_rt"] = f(np.concatenate([inp["w_grp"][0], inp["w_exp"][0]], axis=1))
    m["b_rt"] = f(np.broadcast_to(np.concatenate([inp["b_grp"][0], inp["b_exp"][0]])[None, :], (128, 72)))
    m["w_gate"] = f(inp["w_gate"][0])
    m["w_up"] = f(inp["w_up"][0])
    m["w_down"] = f(inp["w_down"][0])
    return m


def kernel(**inputs):
    inp = {k: np.asarray(v) for k, v in inputs.items()}
    nc = build_nc()
    consts = {k: v for k, v in make_consts().items() if not k.startswith("_")}
    shared = None
    in_maps = []
    for b in range(8):
        m = host_inputs(inp, b)
        if shared is None:
            shared = {k: m[k] for k in m if k not in ("x", "cT")}
        else:
            for k in shared:
                m[k] = shared[k]
        m.update(consts)
        in_maps.append(m)
    res = run_bass_kernel_spmd(nc, in_maps, core_ids=list(range(8)))
    return np.stack([np.asarray(r["out"], dtype=np.float32).reshape(T, D) for r in res.results], axis=0)
```

```python
import os
from contextlib import ExitStack
import numpy as np
import ml_dtypes
import concourse.bass as bass
import concourse.mybir as mybir
from concourse.bass_utils import run_bass_kernel_spmd

F32 = mybir.dt.float32
BF16 = mybir.dt.bfloat16
I32 = mybir.dt.int32
AF = mybir.ActivationFunctionType
ALU = mybir.AluOpType
AX = mybir.AxisListType

D = 2048
T = 2048
NT = 16
KC = 16
PROJ = 6680
CAP = 256
RB = CAP // 128
NEG = -30000.0
EPS = 1e-6

ENGS = ("pe", "act", "dve", "pool", "sp")


_UNIQ = [0]


def _namers(nc):
    def sb(name, shp, dt):
        _UNIQ[0] += 1
        return nc.sbuf_tensor("s%d_%s" % (_UNIQ[0], name), shp, dt)

    def ps(name, shp, dt):
        _UNIQ[0] += 1
        return nc.psum_tensor("p%d_%s" % (_UNIQ[0], name), shp, dt)
    return sb, ps


class Sched:
    NDMA = 24

    def __init__(self, nc):
        self.nc = nc
        self.handles = dict(pe=nc.tensor, act=nc.scalar, dve=nc.vector, pool=nc.gpsimd, sp=nc.sync)
        self.cnt = {e: 0 for e in ENGS}
        self.waited = {e: {} for e in ENGS}
        self.lastw = {}
        self.readers = {}
        self.sems = {}
        self.dma_sems = []
        self.dma_n = 0
        self.dma_q = [0, 0]
        self.dma_last = {}
        self._stack = []

    def open(self):
        for e in ENGS:
            cm = self.nc.semaphore("sem_" + e)
            self.sems[e] = cm.__enter__()
            self._stack.append(cm)
        for i in range(self.NDMA):
            cm = self.nc.semaphore("semd%d" % i)
            self.dma_sems.append(cm.__enter__())
            self._stack.append(cm)

    def close(self):
        for cm in reversed(self._stack):
            cm.__exit__(None, None, None)

    def _need(self, eng, tok, waits):
        if tok is None:
            return
        sem_id, val, src = tok
        if src == "pe" and eng == "pe":
            return
        w = self.waited[eng]
        if w.get(sem_id, 0) >= val:
            return
        w[sem_id] = val
        waits.append((sem_id, val))

    def _deps(self, eng, reads, writes):
        waits = []
        for b in reads:
            self._need(eng, self.lastw.get(b), waits)
        for b in writes:
            self._need(eng, self.lastw.get(b), waits)
            for t in self.readers.get(b, ()):
                self._need(eng, t, waits)
        return waits

    def _commit(self, tok, reads, writes):
        for b in reads:
            self.readers.setdefault(b, []).append(tok)
        for b in writes:
            self.lastw[b] = tok
            self.readers[b] = []

    def op(self, eng, fn, reads=(), writes=()):
        waits = self._deps(eng, reads, writes)
        self.cnt[eng] += 1
        tok = ("E" + eng, self.cnt[eng], eng)
        self._emit1(eng, waits, fn, ("E" + eng, 1))
        self._commit(tok, reads, writes)
        return tok

    def dma(self, eng, fn, reads=(), writes=()):
        half = self.NDMA // 2
        qi = 0 if eng == "pool" else 1
        j = self.dma_q[qi]
        self.dma_q[qi] += 1
        s = qi * half + (j % half)
        sid = "D%d" % s
        prev = self.dma_last.get(s, 0)
        waits = self._deps(eng, reads, writes)
        if prev and self.waited[eng].get(sid, 0) < prev:
            self.waited[eng][sid] = prev
            waits.append((sid, prev))
        val = prev + 16
        self.dma_last[s] = val
        tok = (sid, val, "dma")
        self._emit1(eng, waits, fn, (sid, 16))
        self._commit(tok, reads, writes)
        return tok

    def _emit1(self, engname, waits, fn, inc):
        e = self.handles[engname]
        if os.environ.get("K_TRACE"):
            print("TR", engname, self.cnt[engname], waits, inc, flush=True)
        for sid, val in waits:
            e.wait_ge(self._sem(sid), val)
        if fn is not None:
            ins = fn(e)
            ins.then_inc(self._sem(inc[0]), inc[1])

    def _sem(self, sid):
        if sid[0] == "E":
            return self.sems[sid[1:]]
        return self.dma_sems[int(sid[1:])]

    def barrier(self, engs=ENGS):
        toks = [("E" + e, self.cnt[e], e) for e in ENGS if self.cnt[e] > 0]
        toks += [("D%d" % s, v, "dma") for s, v in self.dma_last.items()]
        for e in engs:
            waits = []
            for t in toks:
                self._need(e, t, waits)
            self._emit1(e, waits, None, None)


def make_consts():
    bf = ml_dtypes.bfloat16
    c = {}
    half = 128
    inv = (10000.0 ** (-np.arange(half, dtype=np.float32) / half)).astype(np.float32)
    pos = np.arange(T, dtype=np.float32)
    ang = (inv[:, None] * pos[None, :]).astype(np.float32)
    c["cos"] = np.cos(ang).astype(np.float32)
    c["sin"] = np.sin(ang).astype(np.float32)
    c["identb"] = np.eye(128, dtype=np.float32).astype(bf)
    c["identf"] = np.eye(128, dtype=np.float32)
    H = 4
    lg = np.log1p(-np.exp2(-5.0 - np.arange(H, dtype=np.float64)))
    m = np.arange(128, dtype=np.float64)
    dm = np.zeros((128, H, 128), np.float32)
    for h in range(H):
        val = np.exp(lg[h] * (-m - 1.0))
        dm[:, h, :] = np.where(m[None, :] >= m[:, None], val[:, None], 0.0)
    c["dm"] = dm
    c["zeta"] = np.exp(lg[None, :] * (127.0 - m)[:, None]).astype(np.float32)
    c["qdec"] = np.exp(lg[None, :] * (m + 1.0)[:, None]).astype(np.float32)
    c["_cd"] = [float(np.exp(lg[h] * 128.0)) for h in range(H)]
    j = np.arange(128)
    caus = np.where(j[:, None] <= j[None, :], 0.0, NEG).astype(np.float32)
    anti = np.where(j[:, None] > j[None, :], 0.0, NEG).astype(np.float32)
    c["caus"] = caus.astype(bf)
    c["anti"] = anti.astype(bf)
    cc = np.arange(128)
    tt = np.arange(T)
    c["cmask"] = np.where(16 * cc[:, None] + 31 <= tt[None, :], 0.0, NEG).astype(np.float32).astype(bf)
    ss = np.arange(32)
    ov = ((16 * cc[:, None] < 64 * ss[None, :] + 64) & (16 * cc[:, None] + 32 > 64 * ss[None, :])).astype(np.float32)
    c["overlap"] = ov.astype(bf)
    es = np.zeros((32, 16, 128), np.float32)
    for kt in range(16):
        for p in range(128):
            es[2 * kt + p // 64, kt, p] = 1.0
    c["esel"] = es.astype(bf)
    cur = tt // 64
    valid = (ss[None, :] <= cur[:, None])
    forced = (ss[None, :] == 0) | (ss[None, :] == cur[:, None]) | (ss[None, :] == cur[:, None] - 1)
    ctab = np.where(valid, np.where(forced, 1e4, 0.0), -1.0).astype(np.float32)
    c["valid"] = np.ascontiguousarray(valid.astype(np.float32).reshape(16, 128, 32).transpose(1, 0, 2))
    c["ctab"] = np.ascontiguousarray(ctab.reshape(16, 128, 32).transpose(1, 0, 2))
    c["lstrict"] = (j[:, None] < j[None, :]).astype(np.float32).astype(bf)
    c["ones"] = np.ones((128, 128), np.float32).astype(bf)
    c["ebase"] = np.tile((np.arange(64, dtype=np.float32) * CAP + 1.0)[None, :], (128, 1))
    return c


CONST_SPECS = [
    ("cos", [128, T], F32), ("sin", [128, T], F32), ("identb", [128, 128], BF16), ("identf", [128, 128], F32),
    ("dm", [128, 4, 128], F32), ("zeta", [128, 4], F32), ("qdec", [128, 4], F32),
    ("caus", [128, 128], BF16), ("anti", [128, 128], BF16), ("cmask", [128, T], BF16), ("overlap", [128, 32], BF16),
    ("esel", [32, 16, 128], BF16), ("valid", [128, 16, 32], F32), ("ctab", [128, 16, 32], F32),
    ("lstrict", [128, 128], BF16), ("ones", [128, 128], BF16), ("ebase", [128, 64], F32),
]

IN_SPECS = [
    ("x", [T, D], F32), ("cT", [128, 16], F32), ("w_ada", [D, 6 * D], F32), ("b_adaT", [128, 96], F32),
    ("n1gT", [128, 16], F32), ("n2gT", [128, 16], F32), ("fg_row", [128, D], F32), ("w_in", [D, PROJ], F32),
    ("gng_row", [128, 1024], F32), ("peT_k", [128, 32], F32), ("w1_k", [4096, 256], F32), ("w2_k", [256, 128], F32),
    ("peT_v", [128, 32], F32), ("w1_v", [4096, 256], F32), ("w2_v", [256, 128], F32), ("w_out", [D, D], F32),
    ("w_rt", [D, 72], F32), ("b_rt", [128, 72], F32), ("w_gate", [64, D, 512], F32), ("w_up", [64, D, 512], F32),
    ("w_down", [64, 512, D], F32),
]


def build_nc(stop=99, dbg=()):
    CST = make_consts()
    nc = bass.Bass("TRN2", target_bir_lowering=False)
    io = {}
    for name, shp, dt in IN_SPECS + CONST_SPECS:
        if stop < 8 and name in ("w_gate", "w_up", "w_down"):
            shp = [1] + shp[1:]
        io[name] = nc.dram_tensor(name, shp, dt, kind="ExternalInput").ap()
    out = nc.dram_tensor("out", [T, D], F32, kind="ExternalOutput").ap()

    def scratch(name, shp, dt):
        kind = "ExternalOutput" if name in dbg else "Internal"
        return nc.dram_tensor(name, shp, dt, kind=kind).ap()

    PFM = scratch("PFM", [32, 128, T], BF16)
    PTM = scratch("PTM", [T, 2560], BF16)
    X1 = scratch("X1", [T, D], F32)
    XS = scratch("XS", [64 * CAP, D], BF16)
    YS = scratch("YS", [64 * CAP, D], BF16)
    dbgout = {}
    for name, shp, dt in [("d_modT", [128, 96], F32), ("d_hT", [128, 16, T], BF16), ("d_ocatT", [128, 16, T], BF16),
                          ("d_ngs", [128, 16, 24], F32), ("d_slots", [128, 16, 2], I32), ("d_wab", [128, 16, 2], F32),
                          ("d_misc", [128, 16, 64], F32)]:
        if name in dbg:
            dbgout[name] = nc.dram_tensor(name, shp, dt, kind="ExternalOutput").ap()

    S = Sched(nc)
    S.open()
    sb, ps = _namers(nc)
    final_toks = []

    def dbgdump(name, ap, key):
        if name in dbgout:
            final_toks.append(S.dma("sp", lambda e: e.dma_start(out=dbgout[name], in_=ap), reads=[key], writes=["dbg_" + name]))

    with ExitStack() as _es:
        modT = _es.enter_context(sb("modT", [128, 96], F32))
        A1 = _es.enter_context(sb("A1", [128, 16], F32))
        A2 = _es.enter_context(sb("A2", [128, 16], F32))
        identb = _es.enter_context(sb("identb", [128, 128], BF16))
        identf = _es.enter_context(sb("identf", [128, 128], F32))
        ngs = _es.enter_context(sb("ngs", [128, 16, 24], F32))
        slots = _es.enter_context(sb("slots", [128, 16, 2], I32))
        wab = _es.enter_context(sb("wab", [128, 16, 2], F32))
        global epsc
        epsc = _es.enter_context(sb("epsc", [128, 1], F32))
        S.op("dve", lambda e: e.memset(epsc[:], EPS), writes=["epsc"])
        S.dma("sp", lambda e: e.dma_start(out=identb[:], in_=io["identb"]), writes=["identb"])
        S.dma("sp", lambda e: e.dma_start(out=identf[:], in_=io["identf"]), writes=["identf"])

        with ExitStack() as _es:
            cT = _es.enter_context(sb("cT", [128, 16], F32))
            cact = _es.enter_context(sb("cact", [128, 16], BF16))
            badaT = _es.enter_context(sb("badaT", [128, 96], F32))
            n1gT = _es.enter_context(sb("n1gT", [128, 16], F32))
            n2gT = _es.enter_context(sb("n2gT", [128, 16], F32))
            wa = _es.enter_context(sb("wa", [128, 2, 16, 512], BF16))
            modps = _es.enter_context(ps("modps", [128, 96], F32))
            S.dma("sp", lambda e: e.dma_start(out=cT[:], in_=io["cT"]), writes=["cT"])
            S.dma("sp", lambda e: e.dma_start(out=badaT[:], in_=io["b_adaT"]), writes=["badaT"])
            S.dma("sp", lambda e: e.dma_start(out=n1gT[:], in_=io["n1gT"]), writes=["n1gT"])
            S.dma("sp", lambda e: e.dma_start(out=n2gT[:], in_=io["n2gT"]), writes=["n2gT"])
            S.op("act", lambda e: e.activation(out=cact[:], in_=cT[:], func=AF.Silu), reads=["cT"], writes=["cact"])
            if stop >= 7:
                zt = _es.enter_context(sb("zt", [128, RB, D], BF16))
                S.op("dve", lambda e: e.memset(zt[:], 0.0), writes=["zt"])
                for ex in range(64):
                    S.dma("sp", lambda e, ex=ex: e.dma_start(out=XS[ex * CAP:(ex + 1) * CAP, :].rearrange("(r p) d -> p r d", p=128), in_=zt[:]),
                          reads=["zt"], writes=["XS"])
            wada = io["w_ada"].rearrange("(k p) n -> p k n", p=128)
            for blk in range(24 if not os.environ.get("K_SKIPABC") else 0):
                b = blk % 2
                S.dma("pool", lambda e, b=b, blk=blk: e.dma_start(out=wa[:, b], in_=wada[:, :, blk * 512:(blk + 1) * 512]),
                      writes=[("wa", b)])
                for j in range(4):
                    col = blk * 4 + j
                    for k in range(16):
                        S.op("pe", lambda e, b=b, j=j, k=k, col=col: e.matmul(
                            modps[:, col:col + 1], lhsT=wa[:, b, k, j * 128:(j + 1) * 128], rhs=cact[:, k:k + 1],
                            start=(k == 0), stop=(k == 15)), reads=[("wa", b), "cact"], writes=["modps"])
            S.op("dve", lambda e: e.tensor_tensor(out=modT[:], in0=modps[:], in1=badaT[:], op=ALU.add),
                 reads=["modps", "badaT"], writes=["modT"])
            S.op("dve", lambda e: e.scalar_tensor_tensor(out=A1[:], in0=modT[:, 16:32], scalar=1.0, in1=n1gT[:],
                                                         op0=ALU.add, op1=ALU.mult), reads=["modT", "n1gT"], writes=["A1"])
            S.op("dve", lambda e: e.scalar_tensor_tensor(out=A2[:], in0=modT[:, 64:80], scalar=1.0, in1=n2gT[:],
                                                         op0=ALU.add, op1=ALU.mult), reads=["modT", "n2gT"], writes=["A2"])
            dbgdump("d_modT", modT[:], "modT")
            S.barrier()

        def rowbcast(dst, col0, key):
            with sb("rb_l", [128, 2, 128], F32) as rbl, ps("rb_ps", [128, 2, 512], F32) as rbps:
                for k in range(16):
                    lb = k % 2
                    S.op("dve", lambda e, k=k, lb=lb: e.tensor_copy(out=rbl[:, lb, :], in_=modT[:, col0 + k:col0 + k + 1].to_broadcast([128, 128])),
                         reads=["modT"], writes=[("rbl", lb)])
                    S.op("pe", lambda e, k=k, lb=lb: e.matmul(rbps[:, (k // 4) % 2, (k % 4) * 128:(k % 4 + 1) * 128], lhsT=rbl[:, lb, :], rhs=identf[:],
                                                              start=True, stop=True), reads=[("rbl", lb), "identf"], writes=[("rbps", (k // 4) % 2)])
                    if k % 4 == 3:
                        q = k // 4
                        S.op("act", lambda e, q=q: e.copy(out=dst[:, q * 512:(q + 1) * 512], in_=rbps[:, q % 2, :]),
                             reads=[("rbps", q % 2)], writes=[key])
                S.barrier()

        if stop >= 2 and not os.environ.get("K_SKIPABC"):
          with sb("hT", [128, 16, T], BF16) as hT:
            with ExitStack() as _es:
                xt = _es.enter_context(sb("xt", [128, 2, D], F32))
                xn = _es.enter_context(sb("xn", [128, 4, D], BF16))
                junk = _es.enter_context(sb("junk", [128, D], BF16))
                ss = _es.enter_context(sb("ss", [128, 16], F32))
                rstd = _es.enter_context(sb("rstd", [128, 16], F32))
                tp = _es.enter_context(ps("tpB", [128, 2, 1024], BF16))
                S.op("dve", lambda e: e.memset(ss[:], 0.0), writes=["ss"])
                for tg in range(4):
                    for i4 in range(4):
                        i = tg * 4 + i4
                        b = i % 2
                        S.dma("sp", lambda e, i=i, b=b: e.dma_start(out=xt[:, b], in_=io["x"][i * 128:(i + 1) * 128, :]), writes=[("xt", b)])
                        S.op("act", lambda e, i=i, b=b: e.activation(out=junk[:], in_=xt[:, b], func=AF.Square, accum_out=ss[:, i:i + 1]),
                             reads=[("xt", b), "ss"], writes=["junk", ("ss", i)])
                        S.op("act", lambda e, i=i: e.activation(out=rstd[:, i:i + 1], in_=ss[:, i:i + 1], func=AF.Sqrt, bias=epsc[:, 0:1], scale=1.0 / D),
                             reads=[("ss", i), "epsc"], writes=[("rstd", i)])
                        S.op("dve", lambda e, i=i: e.reciprocal(out=rstd[:, i:i + 1], in_=rstd[:, i:i + 1]), reads=[("rstd", i)], writes=[("rstd", i)])
                        S.op("dve", lambda e, i=i, i4=i4, b=b: e.tensor_scalar(out=xn[:, i4], in0=xt[:, b], scalar1=rstd[:, i:i + 1], scalar2=None,
                                                                               op0=ALU.mult), reads=[("xt", b), ("rstd", i)], writes=[("xn", i4)])
                    for k in range(16):
                        pb = k % 2
                        for i4 in range(4):
                            S.op("pe", lambda e, k=k, pb=pb, i4=i4: e.transpose(tp[:, pb, i4 * 128:(i4 + 1) * 128], xn[:, i4, k * 128:(k + 1) * 128], identb[:]),
                                 reads=[("xn", i4), "identb"], writes=[("tp", pb)])
                        S.op("act", lambda e, k=k, pb=pb, tg=tg: e.activation(out=hT[:, k, tg * 512:(tg + 1) * 512], in_=tp[:, pb, 0:512], func=AF.Identity,
                                                                              scale=A1[:, k:k + 1], bias=modT[:, k:k + 1]),
                             reads=[("tp", pb), "A1", "modT"], writes=[("hT", tg)])
                dbgdump("d_hT", hT[:], ("hT", 3))
                S.barrier()

            if stop >= 3:
                stage_C(nc, S, io, hT, PFM, PTM, ngs, dbgdump)
            S.barrier()

        if stop >= 4:
          with sb("ocatT", [128, 16, T], BF16) as ocatT:
            if not os.environ.get("K_SKIPD"):
                stage_D(nc, S, io, CST, PFM, PTM, ocatT, identb)
            if stop >= 5:
                stage_E(nc, S, io, PFM, PTM, ocatT, identb, ngs)
            dbgdump("d_ocatT", ocatT[:], "ocatT")
            if stop >= 6:
                with sb("g1row", [128, D], F32) as g1row:
                    rowbcast(g1row, 32, "g1row")
                    stage_F(nc, S, io, ocatT, g1row, X1)
            S.barrier()

        if stop >= 7:
            with sb("a2row", [128, D], F32) as a2row, sb("sh2row", [128, D], F32) as sh2row:
                S.op("dve", lambda e: e.tensor_copy(out=modT[:, 64:80], in_=A2[:]), reads=["A2", "modT"], writes=["modT"])
                rowbcast(a2row, 64, "a2row")
                rowbcast(sh2row, 48, "sh2row")
                stage_G(nc, S, io, X1, XS, a2row, sh2row, identf, slots, wab, dbgout, final_toks)
            S.barrier()
            dbgdump("d_slots", slots[:], "slots")
            dbgdump("d_wab", wab[:], "wab")
        if stop >= 8:
            stage_H(nc, S, io, XS, YS, identb)
            S.barrier()
        if stop >= 9:
            with sb("g2row", [128, D], F32) as g2row:
                rowbcast(g2row, 80, "g2row")
                final_toks += stage_I(nc, S, io, X1, YS, g2row, slots, wab, out)
        S.barrier()
        e = S.handles["sp"]
        waits = []
        for t in final_toks:
            S._need("sp", t, waits)
        for sid, val in waits:
            e.wait_ge(S._sem(sid), val)
    S.close()
    return nc


def stage_C(nc, S, io, hT, PFM, PTM, ngs, dbgdump):
    sb, ps = _namers(nc)
    win = io["w_in"].rearrange("(k p) n -> p k n", p=128)
    units = []
    for h in range(4):
        units.append((h * 256, "rotq", 2 * h))
    for h in range(4):
        units.append((1024 + h * 256, "rotk", 8 + 2 * h))
    for u in range(4):
        units.append((4096 + u * 256, "scale", 16 + 2 * u))
    units += [(5120, "copy", 24), (5376, "copy", 26), (5632, "copy", 28), (6144, "copy", 30)]
    with ExitStack() as _es:
        cos = _es.enter_context(sb("cos", [128, T], F32))
        sin = _es.enter_context(sb("sin", [128, T], F32))
        wf = _es.enter_context(sb("wf", [128, 2, 16, 256], BF16))
        stg = _es.enter_context(sb("stg", [128, 2, 2, T], BF16))
        f12 = _es.enter_context(sb("f12", [128, 2, 512], F32))
        tt = _es.enter_context(sb("tt", [128, 4, 512], F32))
        pfm = _es.enter_context(ps("pfm", [128, 2, 2, 512], F32))
        S.dma("sp", lambda e: e.dma_start(out=cos[:], in_=io["cos"]), writes=["cos"])
        S.dma("sp", lambda e: e.dma_start(out=sin[:], in_=io["sin"]), writes=["sin"])
        n = 0
        for ui, (c0, kind, ch) in enumerate(units):
            wb = ui % 2
            S.dma("pool", lambda e, wb=wb, c0=c0: e.dma_start(out=wf[:, wb], in_=win[:, :, c0:c0 + 256]), writes=[("wf", wb)])
            for tg in range(4):
                pb = n % 2
                n += 1
                for half in range(2):
                    for k in range(16):
                        S.op("pe", lambda e, wb=wb, pb=pb, half=half, k=k, tg=tg: e.matmul(
                            pfm[:, pb, half, :], lhsT=wf[:, wb, k, half * 128:(half + 1) * 128], rhs=hT[:, k, tg * 512:(tg + 1) * 512],
                            start=(k == 0), stop=(k == 15)), reads=[("wf", wb), ("hT", tg)], writes=[("pfm", pb)])
                tsl = slice(tg * 512, (tg + 1) * 512)
                if kind in ("copy", "scale"):
                    sc = 1.0 if kind == "copy" else 128.0 ** -0.5
                    for half in range(2):
                        S.op("act", lambda e, pb=pb, wb=wb, tsl=tsl, sc=sc, half=half: e.activation(out=stg[:, wb, half, tsl], in_=pfm[:, pb, half, :], func=AF.Copy, scale=sc),
                             reads=[("pfm", pb)], writes=[("stg", wb)])
                else:
                    sc = 1.0 if kind == "rotq" else 1.0 / 16.0
                    for half in range(2):
                        S.op("act", lambda e, pb=pb, sc=sc, half=half: e.activation(out=f12[:, half, :], in_=pfm[:, pb, half, :], func=AF.Copy, scale=sc),
                             reads=[("pfm", pb)], writes=["f12"])
                    S.op("dve", lambda e, tsl=tsl: e.tensor_tensor(out=tt[:, 0], in0=f12[:, 0], in1=cos[:, tsl], op=ALU.mult), reads=["f12", "cos"], writes=["tt0"])
                    S.op("dve", lambda e, tsl=tsl: e.tensor_tensor(out=tt[:, 1], in0=f12[:, 1], in1=sin[:, tsl], op=ALU.mult), reads=["f12", "sin"], writes=["tt1"])
                    S.op("pool", lambda e, tsl=tsl: e.tensor_tensor(out=tt[:, 2], in0=f12[:, 0], in1=sin[:, tsl], op=ALU.mult), reads=["f12", "sin"], writes=["tt2"])
                    S.op("pool", lambda e, tsl=tsl: e.tensor_tensor(out=tt[:, 3], in0=f12[:, 1], in1=cos[:, tsl], op=ALU.mult), reads=["f12", "cos"], writes=["tt3"])
                    S.op("dve", lambda e, wb=wb, tsl=tsl: e.tensor_tensor(out=stg[:, wb, 0, tsl], in0=tt[:, 0], in1=tt[:, 1], op=ALU.subtract),
                         reads=["tt0", "tt1"], writes=[("stg", wb)])
                    S.op("pool", lambda e, wb=wb, tsl=tsl: e.tensor_tensor(out=stg[:, wb, 1, tsl], in0=tt[:, 2], in1=tt[:, 3], op=ALU.add),
                         reads=["tt2", "tt3"], writes=[("stg", wb)])
            S.dma("sp", lambda e, wb=wb, ch=ch: e.dma_start(out=PFM[ch:ch + 2].rearrange("c p t -> p c t"), in_=stg[:, wb]),
                  reads=[("stg", wb)], writes=["PFM"])
        S.barrier()
    tunits = [([(2048, 512)], 0), ([(2560, 512)], 512), ([(3072, 512)], 1024), ([(3584, 512)], 1536),
              ([(5888, 256), (6400, 256)], 2048), ([(6656, 24)], None)]
    ptm_v = PTM.rearrange("(i p) c -> p i c", p=128)
    with ExitStack() as _es:
        wt = _es.enter_context(sb("wt", [128, 2, 16, 512], BF16))
        stgt = _es.enter_context(sb("stgt", [128, 2, 16, 512], BF16))
        ptm = _es.enter_context(ps("ptm", [128, 2, 512], F32))
        n = 0
        for ui, (srcs, dcol) in enumerate(tunits):
            wb = ui % 2
            off = 0
            for (c0, w) in srcs:
                S.dma("pool", lambda e, wb=wb, c0=c0, w=w, off=off: e.dma_start(out=wt[:, wb, :, off:off + w], in_=win[:, :, c0:c0 + w]),
                      writes=[("wt", wb)])
                off += w
            ncol = off
            for i in range(16):
                pb = n % 2
                n += 1
                for k in range(16):
                    S.op("pe", lambda e, wb=wb, pb=pb, i=i, k=k, ncol=ncol: e.matmul(
                        ptm[:, pb, 0:ncol], lhsT=hT[:, k, i * 128:(i + 1) * 128], rhs=wt[:, wb, k, 0:ncol],
                        start=(k == 0), stop=(k == 15)), reads=[("wt", wb), ("hT", i // 4)], writes=[("ptm", pb)])
                if dcol is None:
                    S.op("dve", lambda e, pb=pb, i=i: e.tensor_copy(out=ngs[:, i, :], in_=ptm[:, pb, 0:24]), reads=[("ptm", pb)], writes=["ngs"])
                else:
                    eng = "act" if i % 2 == 0 else "dve"
                    if eng == "act":
                        S.op("act", lambda e, pb=pb, wb=wb, i=i: e.copy(out=stgt[:, wb, i, :], in_=ptm[:, pb, :]), reads=[("ptm", pb)], writes=[("stgt", wb)])
                    else:
                        S.op("dve", lambda e, pb=pb, wb=wb, i=i: e.tensor_copy(out=stgt[:, wb, i, :], in_=ptm[:, pb, :]), reads=[("ptm", pb)], writes=[("stgt", wb)])
            if dcol is not None:
                S.dma("sp", lambda e, wb=wb, dcol=dcol: e.dma_start(out=ptm_v[:, :, dcol:dcol + 512], in_=stgt[:, wb]),
                      reads=[("stgt", wb)], writes=["PTM"])
        dbgdump("d_ngs", ngs[:], "ngs")
        S.barrier()


def stage_D(nc, S, io, CST, PFM, PTM, ocatT, identb):
    sb, ps = _namers(nc)
    ptm_v = PTM.rearrange("(i p) c -> p i c", p=128)
    with ExitStack() as _es:
        qT = _es.enter_context(sb("qT", [128, 2, T], BF16))
        kT = _es.enter_context(sb("kT", [128, 2, T], BF16))
        kz = _es.enter_context(sb("kz", [128, 16, 256], BF16))
        v = _es.enter_context(sb("v", [128, 16, 256], BF16))
        g = _es.enter_context(sb("g", [128, 16, 256], BF16))
        dm = _es.enter_context(sb("dm", [128, 4, 128], F32))
        zeta = _es.enter_context(sb("zeta", [128, 4], F32))
        qdec = _es.enter_context(sb("qdec", [128, 4], F32))
        gng = _es.enter_context(sb("gng", [128, 1024], F32))
        Sf = _es.enter_context(sb("Sf", [128, 2, 256], F32))
        Sb = _es.enter_context(sb("Sb", [128, 2, 256], BF16))
        sTm = _es.enter_context(sb("sTm", [128, 2, 128], BF16))
        o = _es.enter_context(sb("o", [128, 2, 256], F32))
        on = _es.enter_context(sb("on", [128, 2, 256], F32))
        sg = _es.enter_context(sb("sg", [128, 2, 256], F32))
        ob = _es.enter_context(sb("ob", [128, 2, 256], BF16))
        st = _es.enter_context(sb("st", [128, 2, 6], F32))
        junkd = _es.enter_context(sb("junkd", [128, 256], BF16))
        mv = _es.enter_context(sb("mv", [128, 2, 2], F32))
        rg = _es.enter_context(sb("rg", [128, 2], F32))
        sT_ps = _es.enter_context(ps("sT_ps", [128, 2, 512], F32))
        o_ps = _es.enter_context(ps("o_ps", [128, 2, 512], F32))
        kv_ps = _es.enter_context(ps("kv_ps", [128, 2, 256], F32))
        tpk = _es.enter_context(ps("tpk", [128, 2, 1024], BF16))
        S.dma("sp", lambda e: e.dma_start(out=dm[:], in_=io["dm"]), writes=["dm"])
        S.dma("sp", lambda e: e.dma_start(out=zeta[:], in_=io["zeta"]), writes=["zeta"])
        S.dma("sp", lambda e: e.dma_start(out=qdec[:], in_=io["qdec"]), writes=["qdec"])
        S.dma("sp", lambda e: e.dma_start(out=gng[:], in_=io["gng_row"]), writes=["gng"])
        for h in range(4):
            cd = CST["_cd"][h]
            S.dma("sp", lambda e, h=h: e.dma_start(out=qT[:], in_=PFM[2 * h:2 * h + 2].rearrange("c p t -> p c t")), reads=["PFM"], writes=["qT"])
            S.dma("sp", lambda e, h=h: e.dma_start(out=kT[:], in_=PFM[8 + 2 * h:10 + 2 * h].rearrange("c p t -> p c t")), reads=["PFM"], writes=["kT"])
            S.dma("sp", lambda e, h=h: e.dma_start(out=v[:], in_=ptm_v[:, :, h * 256:(h + 1) * 256]), reads=["PTM"], writes=["v"])
            S.dma("sp", lambda e, h=h: e.dma_start(out=g[:], in_=ptm_v[:, :, 1024 + h * 256:1024 + (h + 1) * 256]), reads=["PTM"], writes=["g"])
            for i in range(16):
                pb = i % 2
                for half in range(2):
                    S.op("pe", lambda e, i=i, pb=pb, half=half: e.transpose(tpk[:, pb, half * 128:(half + 1) * 128], kT[:, half, i * 128:(i + 1) * 128], identb[:]),
                         reads=["kT", "identb"], writes=[("tpk", pb)])
                S.op("act", lambda e, i=i, pb=pb, h=h: e.activation(out=kz[:, i, :].rearrange("p (a b) -> p a b", a=2), in_=tpk[:, pb, 0:256].rearrange("p (a b) -> p a b", a=2), func=AF.Identity,
                                                                    scale=zeta[:, h:h + 1]), reads=[("tpk", pb), "zeta"], writes=["kz"])
            S.op("dve", lambda e: e.memset(Sf[:], 0.0), writes=["Sf"])
            S.op("dve", lambda e: e.memset(Sb[:], 0.0), writes=["Sb"])
            for n in range(16):
                b = n % 2
                csl = slice(n * 128, (n + 1) * 128)
                for half in range(2):
                    S.op("pe", lambda e, b=b, half=half, csl=csl: e.matmul(sT_ps[:, b, 0:128], lhsT=kT[:, half, csl], rhs=qT[:, half, csl],
                                                                           start=(half == 0), stop=(half == 1)), reads=["kT", "qT"], writes=[("sT_ps", b)])
                S.op("dve", lambda e, b=b, h=h: e.tensor_tensor(out=sTm[:, b, :], in0=sT_ps[:, b, 0:128], in1=dm[:, h, :], op=ALU.mult),
                     reads=[("sT_ps", b), "dm"], writes=[("sTm", b)])
                S.op("pe", lambda e, b=b, n=n: e.matmul(o_ps[:, b, 0:256], lhsT=sTm[:, b, :], rhs=v[:, n, :], start=True, stop=(n == 0)),
                     reads=[("sTm", b), "v"], writes=[("o_ps", b)])
                if n > 0:
                    for half in range(2):
                        S.op("pe", lambda e, b=b, half=half, csl=csl: e.matmul(o_ps[:, b, 0:256], lhsT=qT[:, half, csl], rhs=Sb[:, half, :],
                                                                               start=False, stop=(half == 1)), reads=["qT", "Sb"], writes=[("o_ps", b)])
                if n < 15 and "state" not in os.environ.get("K_DSKIP", ""):
                    for half in range(2):
                        S.op("pe", lambda e, n=n, half=half: e.matmul(kv_ps[:, half, :], lhsT=kz[:, n, half * 128:(half + 1) * 128], rhs=v[:, n, :],
                                                                      start=True, stop=True), reads=["kz", "v"], writes=["kv_ps"])
                    S.op("dve", lambda e, cd=cd: e.scalar_tensor_tensor(out=Sf[:], in0=Sf[:], scalar=cd, in1=kv_ps[:], op0=ALU.mult, op1=ALU.add),
                         reads=["Sf", "kv_ps"], writes=["Sf"])
                    S.op("act", lambda e: e.copy(out=Sb[:], in_=Sf[:]), reads=["Sf"], writes=["Sb"])
                if "epi" in os.environ.get("K_DSKIP", ""):
                    continue
                S.op("dve", lambda e, b=b, h=h: e.tensor_scalar(out=o[:, b, :], in0=o_ps[:, b, 0:256], scalar1=qdec[:, h:h + 1], scalar2=None, op0=ALU.mult),
                     reads=[("o_ps", b), "qdec"], writes=[("o", b)])
                S.op("dve", lambda e, b=b: e.tensor_reduce(out=mv[:, b, 0:1], in_=o[:, b, :], axis=AX.X, op=ALU.add), reads=[("o", b)], writes=[("mv", b)])
                S.op("dve", lambda e, b=b: e.tensor_scalar(out=mv[:, b, 1:2], in0=mv[:, b, 0:1], scalar1=-1.0 / 256.0, scalar2=None, op0=ALU.mult),
                     reads=[("mv", b)], writes=[("mv1", b)])
                S.op("act", lambda e, b=b: e.activation(out=on[:, b, :], in_=o[:, b, :], func=AF.Identity, bias=mv[:, b, 1:2], scale=1.0),
                     reads=[("o", b), ("mv1", b)], writes=[("on", b)])
                S.op("act", lambda e, b=b: e.activation(out=junkd[:], in_=on[:, b, :], func=AF.Square, accum_out=st[:, b, 0:1]),
                     reads=[("on", b)], writes=["junkd", ("st", b)])
                S.op("act", lambda e, b=b: e.activation(out=rg[:, b:b + 1], in_=st[:, b, 0:1], func=AF.Sqrt, bias=epsc[:, 0:1], scale=1.0 / 256.0),
                     reads=[("st", b), "epsc"], writes=[("rg", b)])
                S.op("dve", lambda e, b=b: e.reciprocal(out=rg[:, b:b + 1], in_=rg[:, b:b + 1]), reads=[("rg", b)], writes=[("rg", b)])
                S.op("dve", lambda e, b=b, h=h: e.scalar_tensor_tensor(out=o[:, b, :], in0=on[:, b, :], scalar=rg[:, b:b + 1], in1=gng[:, h * 256:(h + 1) * 256],
                                                                       op0=ALU.mult, op1=ALU.mult), reads=[("on", b), ("rg", b), "gng"], writes=[("o", b)])
                S.op("act", lambda e, b=b, n=n: e.activation(out=sg[:, b, :], in_=g[:, n, :], func=AF.Silu), reads=["g"], writes=[("sg", b)])
                S.op("pool", lambda e, b=b: e.tensor_tensor(out=ob[:, b, :], in0=o[:, b, :], in1=sg[:, b, :], op=ALU.mult),
                     reads=[("o", b), ("sg", b)], writes=[("ob", b)])
                for half in range(2):
                    S.op("pe", lambda e, b=b, half=half: e.transpose(tpk[:, b, half * 128:(half + 1) * 128], ob[:, b, half * 128:(half + 1) * 128], identb[:]),
                         reads=[("ob", b), "identb"], writes=[("tpk", b)])
                S.op("act", lambda e, b=b, h=h, csl=csl: e.copy(out=ocatT[:, 2 * h:2 * h + 2, csl], in_=tpk[:, b, 0:256].rearrange("p (a b) -> p a b", a=2)), reads=[("tpk", b)], writes=["ocatT"])
        S.barrier()


def stage_E(nc, S, io, PFM, PTM, ocatT, identb, ngs):
    sb, ps = _namers(nc)
    ptm_v = PTM.rearrange("(i p) c -> p i c", p=128)
    with ExitStack() as _es:
        w1k = _es.enter_context(sb("w1k", [128, 32, 256], BF16))
        w1v = _es.enter_context(sb("w1v", [128, 32, 256], BF16))
        w2k = _es.enter_context(sb("w2k", [128, 2, 128], BF16))
        w2v = _es.enter_context(sb("w2v", [128, 2, 128], BF16))
        pek = _es.enter_context(sb("pek", [128, 32], BF16))
        pev = _es.enter_context(sb("pev", [128, 32], BF16))
        hb = _es.enter_context(sb("hb", [128, 2, 2], F32))
        caus = _es.enter_context(sb("caus", [128, 128], BF16))
        anti = _es.enter_context(sb("anti", [128, 128], BF16))
        cmask = _es.enter_context(sb("cmask", [128, T], BF16))
        overlap = _es.enter_context(sb("overlap", [128, 32], BF16))
        esel = _es.enter_context(sb("esel", [32, 16, 128], BF16))
        valid = _es.enter_context(sb("valid", [128, 16, 32], F32))
        ctab = _es.enter_context(sb("ctab", [128, 16, 32], F32))
        qT4 = _es.enter_context(sb("qT4", [128, 4, T], BF16))
        kcT2 = _es.enter_context(sb("kcT2", [128, 2, T], BF16))
        vcT2 = _es.enter_context(sb("vcT2", [128, 2, T], BF16))
        ksT = _es.enter_context(sb("ksT", [128, T], BF16))
        kwT = _es.enter_context(sb("kwT", [128, T], BF16))
        vsa = _es.enter_context(sb("vsa", [128, 16, 130], BF16))
        vwa = _es.enter_context(sb("vwa", [128, 16, 130], BF16))
        hid = _es.enter_context(sb("hid", [128, 4, 2, 128], BF16))
        kc16 = _es.enter_context(sb("kc16", [128, 2, 16, 130], BF16))
        kcmpT2 = _es.enter_context(sb("kcmpT2", [128, 2, 128], BF16))
        vcaug2 = _es.enter_context(sb("vcaug2", [128, 2, 162], BF16))
        ocmp = _es.enter_context(sb("ocmp", [128, 16, 4, 128], BF16))
        gts = _es.enter_context(sb("gts", [128, 16, 12], F32))
        selnegT = _es.enter_context(sb("selnegT", [32, T], BF16))
        _sk = os.environ.get("K_EPRO", "")
        if "c" not in _sk:
            for nm, t in [("caus", caus), ("anti", anti), ("cmask", cmask), ("overlap", overlap), ("esel", esel), ("valid", valid), ("ctab", ctab)]:
                S.dma("sp", lambda e, nm=nm, t=t: e.dma_start(out=t[:], in_=io[nm]), writes=[nm])
        if "w" not in _sk:
            for lh in range(2):
                S.dma("pool", lambda e, lh=lh: e.dma_start(out=w1k[:, lh * 16:(lh + 1) * 16, :], in_=io["w1_k"].rearrange("(l p) n -> p l n", p=128)[:, lh * 16:(lh + 1) * 16, :]), writes=["w1k"])
                S.dma("pool", lambda e, lh=lh: e.dma_start(out=w1v[:, lh * 16:(lh + 1) * 16, :], in_=io["w1_v"].rearrange("(l p) n -> p l n", p=128)[:, lh * 16:(lh + 1) * 16, :]), writes=["w1v"])
            S.dma("pool", lambda e: e.dma_start(out=w2k[:], in_=io["w2_k"].rearrange("(c p) n -> p c n", p=128)), writes=["w2k"])
            S.dma("pool", lambda e: e.dma_start(out=w2v[:], in_=io["w2_v"].rearrange("(c p) n -> p c n", p=128)), writes=["w2v"])
            S.dma("pool", lambda e: e.dma_start(out=pek[:], in_=io["peT_k"]), writes=["pek"])
            S.dma("pool", lambda e: e.dma_start(out=pev[:], in_=io["peT_v"]), writes=["pev"])

        with ps("hb_ps", [128, 2, 2], F32) as hb_ps:
            for kv, (w1, pe) in enumerate([(w1k, pek), (w1v, pev)]):
                for hc in range(2):
                    for l in range(32):
                        S.op("pe", lambda e, kv=kv, hc=hc, l=l, w1=w1, pe=pe: e.matmul(hb_ps[:, kv, hc:hc + 1], lhsT=w1[:, l, hc * 128:(hc + 1) * 128],
                                                                                     rhs=pe[:, l:l + 1], start=(l == 0), stop=(l == 31)),
                             reads=["w1k", "w1v", "pek", "pev"], writes=["hb_ps"])
            if "h" not in _sk:
                S.op("dve", lambda e: e.tensor_copy(out=hb[:], in_=hb_ps[:]), reads=["hb_ps"], writes=["hb"])
            S.barrier()

        for _ in range(int(os.environ.get("K_PENOP", "0"))):
            nc.tensor.wait_ge(S.sems["dve"], 0)
        S.op("dve", lambda e: e.memset(kc16[:], 0.0), writes=[("kc16", 0), ("kc16", 1)])
        S.op("dve", lambda e: e.memset(vcaug2[:], 0.0), writes=["vcaug"])
        with ExitStack() as _es2:
            hid_ps = _es2.enter_context(ps("hid_ps", [128, 4, 2, 128], F32))
            cmp_ps = _es2.enter_context(ps("cmp_ps", [128, 2, 512], F32))
            S.dma("sp", lambda e: e.dma_start(out=kcT2[:], in_=PFM[24:26].rearrange("c p t -> p c t")), reads=["PFM"], writes=["kcT2"])
            S.dma("sp", lambda e: e.dma_start(out=vcT2[:], in_=PFM[26:28].rearrange("c p t -> p c t")), reads=["PFM"], writes=["vcT2"])
            for u in range(4):
                kv, gg = u // 2, u % 2
                w1 = w1k if kv == 0 else w1v
                src = kcT2 if kv == 0 else vcT2
                nm = "kcT2" if kv == 0 else "vcT2"
                kb = u % 2
                S.op("dve" if u % 2 == 0 else "pool", lambda e, kb=kb, src=src, gg=gg: e.tensor_copy(
                    out=kc16[:, kb, :, 0:128], in_=src[:, gg, :].rearrange("p (c r) -> p r c", r=16)), reads=[nm], writes=[("kc16", kb)])
                for hc in range(2):
                    for l in range(32):
                        S.op("pe", lambda e, u=u, kb=kb, hc=hc, l=l, w1=w1: e.matmul(
                            hid_ps[:, u, hc, :], lhsT=w1[:, l, hc * 128:(hc + 1) * 128], rhs=kc16[:, kb, l % 16, (l // 16):(l // 16) + 128],
                            start=(l == 0), stop=(l == 31)), reads=["w1k", "w1v", ("kc16", kb)], writes=[("hid_ps", u // 2)])
                    S.op("act", lambda e, u=u, kv=kv, hc=hc: e.activation(out=hid[:, u, hc, :], in_=hid_ps[:, u, hc, :], func=AF.Silu,
                                                                     bias=hb[:, kv, hc:hc + 1]), reads=[("hid_ps", u // 2), "hb"], writes=["hid"])
            for gg in range(2):
                for hc in range(2):
                    S.op("pe", lambda e, gg=gg, hc=hc: e.matmul(cmp_ps[:, 0, gg * 128:(gg + 1) * 128], lhsT=w2k[:, hc, :], rhs=hid[:, gg, hc, :], start=(hc == 0), stop=(hc == 1)),
                         reads=["w2k", "hid"], writes=["cmp_ps0"])
            for gg in range(2):
                for hc in range(2):
                    S.op("pe", lambda e, gg=gg, hc=hc: e.matmul(cmp_ps[:, 1, gg * 128:(gg + 1) * 128], lhsT=hid[:, 2 + gg, hc, :], rhs=w2v[:, hc, :], start=(hc == 0), stop=(hc == 1)),
                         reads=["w2v", "hid"], writes=["cmp_ps1"])
            _cs = os.environ.get("K_CONS", "")
            if "A" not in _cs:
                S.op("act", lambda e: e.copy(out=kcmpT2[:], in_=cmp_ps[:, 0, 0:256].rearrange("p (a b) -> p a b", a=2)), reads=["cmp_ps0"], writes=["kcmpT"])
            if "V" not in _cs:
                S.op("dve", lambda e: e.tensor_copy(out=vcaug2[:, :, 0:128], in_=cmp_ps[:, 1, 0:256].rearrange("p (a b) -> p a b", a=2)), reads=["cmp_ps1", "vcaug"], writes=["vcaug"])
            if "M" not in _cs:
                S.op("dve", lambda e: e.memset(vcaug2[:, :, 128:129], 1.0), reads=["vcaug"], writes=["vcaug"])
            for gg in range(2 if "O" not in _cs else 0):
                S.op("dve", lambda e, gg=gg: e.tensor_copy(out=vcaug2[:, gg, 129:161], in_=overlap[:]), reads=["overlap", "vcaug"], writes=["vcaug"])
            S.barrier()
        if "cmp" in os.environ.get("K_ESKIP", ""):
            return
        for g in ([int(c) for c in os.environ["K_GSEL"]] if os.environ.get("K_GSEL") else range(2)):
            _ld = os.environ.get("K_ELD", "")
            if "q" not in _ld:
                S.dma("sp", lambda e, g=g: e.dma_start(out=qT4[:], in_=PFM[16 + 4 * g:20 + 4 * g].rearrange("c p t -> p c t")), reads=["PFM"], writes=["qT4"])
            for nm, t, ch in [("ksT", ksT, 28), ("kwT", kwT, 30)]:
                S.dma("sp", lambda e, t=t, ch=ch, g=g: e.dma_start(out=t[:], in_=PFM[ch + g]), reads=["PFM"], writes=[nm])
            if "a" not in _ld:
                S.dma("sp", lambda e, g=g: e.dma_start(out=vsa[:, :, 0:128], in_=ptm_v[:, :, 2048 + g * 128:2048 + (g + 1) * 128]), reads=["PTM"], writes=["vsa"])
                S.dma("sp", lambda e, g=g: e.dma_start(out=vwa[:, :, 0:128], in_=ptm_v[:, :, 2304 + g * 128:2304 + (g + 1) * 128]), reads=["PTM"], writes=["vwa"])
            if "m" not in _sk:
                S.op("dve", lambda e: e.memset(vsa[:, :, 128:130], 1.0), reads=["vsa"], writes=["vsa"])
                S.op("dve", lambda e: e.memset(vwa[:, :, 128:130], 1.0), reads=["vwa"], writes=["vwa"])
            if "e2a" in os.environ.get("K_ESKIP", ""):
                continue
            with ExitStack() as _es:
                sc_ps = _es.enter_context(ps("sc_ps", [128, 2, 512], F32))
                oc_ps = _es.enter_context(ps("oc_ps", [128, 4, 256], F32))
                tps = _es.enter_context(ps("tps", [32, 2, 128], BF16))
                pc = _es.enter_context(sb("pc", [128, 2, 512], BF16))
                rc = _es.enter_context(sb("rc", [128, 4], F32))
                imp = _es.enter_context(sb("imp", [128, 32], F32))
                score = _es.enter_context(sb("score", [128, 32], F32))
                sc2 = _es.enter_context(sb("sc2", [128, 32], F32))
                m8 = _es.enter_context(sb("m8", [128, 16], F32))
                selneg = _es.enter_context(sb("selneg", [128, 32], BF16))
                coef = _es.enter_context(sb("coef", [128, 4], F32))
                for qt in range(16):
                    b = qt % 2
                    qsl = slice(qt * 128, (qt + 1) * 128)
                    S.op("pe", lambda e, b=b, qsl=qsl: e.matmul(sc_ps[:, b, :].rearrange("p (h t) -> p h t", h=4), lhsT=kcmpT2[:, g, :], rhs=qT4[:, :, qsl],
                                                                start=True, stop=False), reads=["kcmpT", "qT4"], writes=[("sc_ps", b)])
                    S.op("pe", lambda e, b=b, qsl=qsl: e.matmul(sc_ps[:, b, :].rearrange("p (h t) -> p h t", h=4), lhsT=identb[:, :],
                                                                rhs=cmask[:, qsl].unsqueeze(1).to_broadcast([128, 4, 128]), start=False, stop=True),
                         reads=["identb", "cmask"], writes=[("sc_ps", b)])
                    S.op("act", lambda e, b=b: e.activation(out=pc[:, b, :], in_=sc_ps[:, b, :], func=AF.Exp), reads=[("sc_ps", b)], writes=[("pc", b)])
                    for h in range(4):
                        S.op("pe", lambda e, b=b, h=h: e.matmul(oc_ps[:, h, 0:162], lhsT=pc[:, b, h * 128:(h + 1) * 128], rhs=vcaug2[:, g, 0:162],
                                                                start=True, stop=True), reads=[("pc", b), "vcaug"], writes=["oc_ps"])
                    for hh in (0, 2):
                        S.op("dve", lambda e, hh=hh: e.tensor_scalar(out=rc[:, hh:hh + 2], in0=oc_ps[:, hh:hh + 2, 128], scalar1=1e-30, scalar2=None, op0=ALU.max),
                             reads=["oc_ps"], writes=["rc"])
                    S.op("dve", lambda e: e.reciprocal(out=rc[:], in_=rc[:]), reads=["rc"], writes=["rc"])
                    S.op("act", lambda e, qt=qt, g=g: e.activation(out=gts[:, qt, :], in_=ngs[:, qt, g * 12:(g + 1) * 12], func=AF.Sigmoid), reads=["ngs"], writes=["gts"])
                    for h in range(4):
                        if h == 0:
                            S.op("dve", lambda e: e.tensor_scalar(out=imp[:], in0=oc_ps[:, 0, 129:161], scalar1=rc[:, 0:1], scalar2=None, op0=ALU.mult),
                                 reads=["oc_ps", "rc"], writes=["imp"])
                        else:
                            S.op("dve", lambda e, h=h: e.scalar_tensor_tensor(out=imp[:], in0=oc_ps[:, h, 129:161], scalar=rc[:, h:h + 1], in1=imp[:],
                                                                              op0=ALU.mult, op1=ALU.add), reads=["oc_ps", "rc", "imp"], writes=["imp"])
                    S.op("dve", lambda e, qt=qt: e.tensor_tensor(out=coef[:], in0=rc[:], in1=gts[:, qt, 0:12:3], op=ALU.mult), reads=["rc", "gts"], writes=["coef"])
                    for h in range(4):
                        S.op("dve", lambda e, h=h, qt=qt: e.tensor_scalar(out=ocmp[:, qt, h, :], in0=oc_ps[:, h, 0:128], scalar1=coef[:, h:h + 1], scalar2=None,
                                                                          op0=ALU.mult), reads=["oc_ps", "coef"], writes=["ocmp"])
                    S.op("dve", lambda e, qt=qt: e.tensor_tensor(out=score[:], in0=imp[:], in1=valid[:, qt, :], op=ALU.mult), reads=["imp", "valid"], writes=["score"])
                    S.op("dve", lambda e, qt=qt: e.tensor_tensor(out=score[:], in0=score[:], in1=ctab[:, qt, :], op=ALU.add), reads=["score", "ctab"], writes=["score"])
                    S.op("dve", lambda e: e.max(out=m8[:, 0:8], in_=score[:]), reads=["score"], writes=["m8a"])
                    S.op("dve", lambda e: e.match_replace(out=sc2[:], in_to_replace=m8[:, 0:8], in_values=score[:], imm_value=-2.0),
                         reads=["score", "m8a"], writes=["sc2"])
                    S.op("dve", lambda e: e.max(out=m8[:, 8:16], in_=sc2[:]), reads=["sc2"], writes=["m8b"])
                    S.op("dve", lambda e: e.tensor_scalar(out=selneg[:], in0=score[:], scalar1=m8[:, 15:16], scalar2=NEG, op0=ALU.is_lt, op1=ALU.mult),
                         reads=["score", "m8b"], writes=["selneg"])
                    S.op("pe", lambda e, b=b: e.transpose(tps[:, 0, :], selneg[:], identb[:]), reads=["selneg", "identb"], writes=["tps"])
                    S.op("act", lambda e, b=b, qsl=qsl: e.copy(out=selnegT[:, qsl], in_=tps[:, 0, :]), reads=["tps"], writes=["selnegT"])
                S.barrier()
            if "e2b" in os.environ.get("K_ESKIP", ""):
                continue
            with ExitStack() as _es:
                ss_ps = _es.enter_context(ps("ss_ps", [128, 2, 512], F32))
                os_ps = _es.enter_context(ps("os_ps", [128, 4, 256], F32))
                ow_ps = _es.enter_context(ps("ow_ps", [128, 4, 256], F32))
                tpo = _es.enter_context(ps("tpo", [128, 4, 128], BF16))
                pp = _es.enter_context(sb("pp", [128, 2, 512], BF16))
                rs = _es.enter_context(sb("rs", [128, 4], F32))
                rw = _es.enter_context(sb("rw", [128, 4], F32))
                acc = _es.enter_context(sb("acc", [128, 4, 128], F32))
                ob4 = _es.enter_context(sb("ob4", [128, 4, 128], BF16))
                n = 0
                for qt in range(16):
                    qsl = slice(qt * 128, (qt + 1) * 128)
                    for br, (kTt, knm, va, vnm, o_ps, kts) in enumerate([
                            (ksT, "ksT", vsa, "vsa", os_ps, list(range(0, qt + 1))),
                            (kwT, "kwT", vwa, "vwa", ow_ps, list(range(max(0, qt - 4), qt + 1)))]):
                        opk = "os_ps" if br == 0 else "ow_ps"
                        for kt in kts:
                            b = n % 2
                            n += 1
                            ksl = slice(kt * 128, (kt + 1) * 128)
                            extra = []
                            if br == 0:
                                extra.append((esel[:, kt, :], selnegT[:, qsl].unsqueeze(1).to_broadcast([32, 4, 128]), ["esel", "selnegT"]))
                            if kt == qt:
                                extra.append((identb[:], caus[:].unsqueeze(1).to_broadcast([128, 4, 128]), ["identb", "caus"]))
                            if br == 1 and kt == qt - 4:
                                extra.append((identb[:], anti[:].unsqueeze(1).to_broadcast([128, 4, 128]), ["identb", "anti"]))
                            outv = ss_ps[:, b, :].rearrange("p (h t) -> p h t", h=4)
                            S.op("pe", lambda e, outv=outv, kTt=kTt, ksl=ksl, qsl=qsl, last=(len(extra) == 0): e.matmul(
                                outv, lhsT=kTt[:, ksl], rhs=qT4[:, :, qsl], start=True, stop=last), reads=[knm, "qT4"], writes=[("ss_ps", b)])
                            for xi, (l_, r_, rd) in enumerate(extra):
                                S.op("pe", lambda e, outv=outv, l_=l_, r_=r_, last=(xi == len(extra) - 1): e.matmul(outv, lhsT=l_, rhs=r_, start=False, stop=last),
                                     reads=rd, writes=[("ss_ps", b)])
                            S.op("act", lambda e, b=b: e.activation(out=pp[:, b, :], in_=ss_ps[:, b, :], func=AF.Exp), reads=[("ss_ps", b)], writes=[("pp", b)])
                            for h in range(4):
                                S.op("pe", lambda e, b=b, h=h, kt=kt, va=va, o_ps=o_ps, first=(kt == kts[0]), lastk=(kt == kts[-1]): e.matmul(
                                    o_ps[:, h, 0:130], lhsT=pp[:, b, h * 128:(h + 1) * 128], rhs=va[:, kt, :], start=(first and h % 2 == 0), stop=lastk, skip_group_check=True),
                                    reads=[("pp", b), vnm], writes=[opk])
                    for hh in (0, 2):
                        S.op("dve", lambda e, hh=hh: e.reciprocal(out=rs[:, hh:hh + 2], in_=os_ps[:, hh:hh + 2, 128]), reads=["os_ps"], writes=["rs"])
                        S.op("dve", lambda e, hh=hh: e.reciprocal(out=rw[:, hh:hh + 2], in_=ow_ps[:, hh:hh + 2, 128]), reads=["ow_ps"], writes=["rw"])
                    S.op("dve", lambda e, qt=qt: e.tensor_tensor(out=rs[:], in0=rs[:], in1=gts[:, qt, 1:12:3], op=ALU.mult), reads=["rs", "gts"], writes=["rs"])
                    S.op("dve", lambda e, qt=qt: e.tensor_tensor(out=rw[:], in0=rw[:], in1=gts[:, qt, 2:12:3], op=ALU.mult), reads=["rw", "gts"], writes=["rw"])
                    for h in range(4):
                        S.op("dve", lambda e, h=h: e.tensor_scalar(out=acc[:, h, :], in0=os_ps[:, h, 0:128], scalar1=rs[:, h:h + 1], scalar2=None, op0=ALU.mult),
                             reads=["os_ps", "rs"], writes=["acc"])
                        S.op("dve", lambda e, h=h: e.scalar_tensor_tensor(out=acc[:, h, :], in0=ow_ps[:, h, 0:128], scalar=rw[:, h:h + 1], in1=acc[:, h, :],
                                                                          op0=ALU.mult, op1=ALU.add), reads=["ow_ps", "rw", "acc"], writes=["acc"])
                    S.op("pool", lambda e, qt=qt: e.tensor_tensor(out=ob4[:], in0=acc[:], in1=ocmp[:, qt], op=ALU.add), reads=["acc", "ocmp"], writes=["ob4"])
                    for h in range(4):
                        S.op("pe", lambda e, h=h: e.transpose(tpo[:, h, :], ob4[:, h, :], identb[:]), reads=["ob4", "identb"], writes=["tpo"])
                    S.op("act", lambda e, g=g, qsl=qsl: e.copy(out=ocatT[:, 8 + 4 * g:12 + 4 * g, qsl], in_=tpo[:]), reads=["tpo"], writes=["ocatT"])
                S.barrier()
        S.barrier()


def stage_F(nc, S, io, ocatT, g1row, X1):
    sb, ps = _namers(nc)
    wout = io["w_out"].rearrange("(k p) n -> p k n", p=128)
    with ExitStack() as _es:
        wo = _es.enter_context(sb("wo", [128, 2, 16, 512], BF16))
        xr = _es.enter_context(sb("xr", [128, 2, 512], F32))
        x1t = _es.enter_context(sb("x1t", [128, 2, 512], F32))
        mx_ps = _es.enter_context(ps("mx_ps", [128, 2, 512], F32))
        n = 0
        for cb in range(4):
            wb = cb % 2
            csl = slice(cb * 512, (cb + 1) * 512)
            S.dma("pool", lambda e, wb=wb, csl=csl: e.dma_start(out=wo[:, wb], in_=wout[:, :, csl]), writes=[("wo", wb)])
            for i in range(16):
                b = n % 2
                n += 1
                isl = slice(i * 128, (i + 1) * 128)
                S.dma("sp", lambda e, b=b, isl=isl, csl=csl: e.dma_start(out=xr[:, b, :], in_=io["x"][isl, csl]), writes=[("xr", b)])
                for k in range(16):
                    S.op("pe", lambda e, b=b, wb=wb, k=k, isl=isl: e.matmul(mx_ps[:, b, :], lhsT=ocatT[:, k, isl], rhs=wo[:, wb, k, :],
                                                                            start=(k == 0), stop=(k == 15)), reads=["ocatT", ("wo", wb)], writes=[("mx_ps", b)])
                S.op("dve", lambda e, b=b, csl=csl: e.tensor_tensor(out=x1t[:, b, :], in0=mx_ps[:, b, :], in1=g1row[:, csl], op=ALU.mult),
                     reads=[("mx_ps", b), "g1row"], writes=[("x1t", b)])
                S.op("pool", lambda e, b=b: e.tensor_tensor(out=x1t[:, b, :], in0=x1t[:, b, :], in1=xr[:, b, :], op=ALU.add),
                     reads=[("x1t", b), ("xr", b)], writes=[("x1t", b)])
                S.dma("sp", lambda e, b=b, isl=isl, csl=csl: e.dma_start(out=X1[isl, csl], in_=x1t[:, b, :]), reads=[("x1t", b)], writes=["X1"])
        S.barrier()


def stage_G(nc, S, io, X1, XS, a2row, sh2row, identf, slots, wab, dbgout, final_toks):
    sb, ps = _namers(nc)
    with ExitStack() as _es:
        x1 = _es.enter_context(sb("x1", [128, 2, D], F32))
        h2f = _es.enter_context(sb("h2f", [128, D], F32))
        h2b = _es.enter_context(sb("h2b", [128, 2, D], BF16))
        junk = _es.enter_context(sb("junk2", [128, D], BF16))
        h2Tf = _es.enter_context(sb("h2Tf", [128, 16, 128], F32))
        wr = _es.enter_context(sb("wr", [128, 16, 72], F32))
        brt = _es.enter_context(sb("brt", [128, 72], F32))
        lstrict = _es.enter_context(sb("lstrict", [128, 128], BF16))
        ones = _es.enter_context(sb("ones", [128, 128], BF16))
        ebase = _es.enter_context(sb("ebase", [128, 64], F32))
        ss2 = _es.enter_context(sb("ss2", [128, 16], F32))
        rstd2 = _es.enter_context(sb("rstd2", [128, 16], F32))
        lg = _es.enter_context(sb("lg", [128, 72], F32))
        gm = _es.enter_context(sb("gm", [128, 4], F32))
        eg = _es.enter_context(sb("eg", [128, 8], F32))
        onehot = _es.enter_context(sb("onehot", [128, 8], F32))
        tmp88 = _es.enter_context(sb("tmp88", [128, 8, 8], F32))
        leg = _es.enter_context(sb("leg", [128, 8], F32))
        m8r = _es.enter_context(sb("m8r", [128, 8], F32))
        selloc = _es.enter_context(sb("selloc", [128, 8], F32))
        wl = _es.enter_context(sb("wl", [128, 8], F32))
        wfull = _es.enter_context(sb("wfull", [128, 64], F32))
        Ab = _es.enter_context(sb("Ab", [128, 64], BF16))
        Af = _es.enter_context(sb("Af", [128, 64], F32))
        tot = _es.enter_context(sb("tot", [128, 64], F32))
        cnt = _es.enter_context(sb("cnt", [128, 64], F32))
        key = _es.enter_context(sb("key", [128, 64], F32))
        m8k = _es.enter_context(sb("m8k", [128, 8], F32))
        slf = _es.enter_context(sb("slf", [128, 2], F32))
        eq = _es.enter_context(sb("eq", [128, 64], F32))
        tpf = _es.enter_context(ps("tpf", [128, 2, 4, 128], F32))
        lg_ps = _es.enter_context(ps("lg_ps", [128, 72], F32))
        cnt_ps = _es.enter_context(ps("cnt_ps", [128, 2, 64], F32))
        S.dma("sp", lambda e: e.dma_start(out=wr[:], in_=io["w_rt"].rearrange("(k p) n -> p k n", p=128)), writes=["wr"])
        for nm, t in [("b_rt", brt), ("lstrict", lstrict), ("ones", ones), ("ebase", ebase)]:
            S.dma("sp", lambda e, nm=nm, t=t: e.dma_start(out=t[:], in_=io[nm]), writes=[nm])
        S.op("dve", lambda e: e.memset(ss2[:], 0.0), writes=["ss2"])
        S.op("dve", lambda e: e.memset(tot[:], 0.0), writes=["tot"])
        S.op("dve", lambda e: e.memset(gm[:], 0.0), writes=["gm"])
        for i in range(16):
            b = i % 2
            isl = slice(i * 128, (i + 1) * 128)
            S.dma("sp", lambda e, b=b, isl=isl: e.dma_start(out=x1[:, b], in_=X1[isl, :]), reads=["X1"], writes=[("x1", b)])
            S.op("act", lambda e, b=b, i=i: e.activation(out=junk[:], in_=x1[:, b], func=AF.Square, accum_out=ss2[:, i:i + 1]),
                 reads=[("x1", b), "ss2"], writes=["junk", ("ss2", i)])
            S.op("act", lambda e, i=i: e.activation(out=rstd2[:, i:i + 1], in_=ss2[:, i:i + 1], func=AF.Sqrt, bias=epsc[:, 0:1], scale=1.0 / D),
                 reads=[("ss2", i), "epsc"], writes=[("rstd2", i)])
            S.op("dve", lambda e, i=i: e.reciprocal(out=rstd2[:, i:i + 1], in_=rstd2[:, i:i + 1]), reads=[("rstd2", i)], writes=[("rstd2", i)])
            S.op("dve", lambda e, b=b, i=i: e.scalar_tensor_tensor(out=h2f[:], in0=x1[:, b], scalar=rstd2[:, i:i + 1], in1=a2row[:], op0=ALU.mult, op1=ALU.mult),
                 reads=[("x1", b), ("rstd2", i), "a2row"], writes=["h2f"])
            S.op("pool", lambda e: e.tensor_tensor(out=h2f[:], in0=h2f[:], in1=sh2row[:], op=ALU.add), reads=["h2f", "sh2row"], writes=["h2f"])
            S.op("act", lambda e, b=b: e.copy(out=h2b[:, b], in_=h2f[:]), reads=["h2f"], writes=[("h2b", b)])
            for k4 in range(4):
                pb = k4 % 2
                for kk in range(4):
                    k = k4 * 4 + kk
                    S.op("pe", lambda e, pb=pb, kk=kk, k=k: e.transpose(tpf[:, pb, kk, :], h2f[:, k * 128:(k + 1) * 128], identf[:]),
                         reads=["h2f", "identf"], writes=[("tpf", pb)])
                S.op("dve", lambda e, pb=pb, k4=k4: e.tensor_copy(out=h2Tf[:, k4 * 4:(k4 + 1) * 4, :], in_=tpf[:, pb]), reads=[("tpf", pb)], writes=["h2Tf"])
            for k in range(16):
                S.op("pe", lambda e, k=k: e.matmul(lg_ps[:], lhsT=h2Tf[:, k, :], rhs=wr[:, k, :], start=(k == 0), stop=(k == 15)),
                     reads=["h2Tf", "wr"], writes=["lg_ps"])
            V = lambda fn, r, w: S.op("dve", fn, reads=r, writes=w)
            V(lambda e: e.tensor_tensor(out=lg[:], in0=lg_ps[:], in1=brt[:], op=ALU.add), ["lg_ps", "b_rt"], ["lg"])
            V(lambda e: e.tensor_reduce(out=gm[:, 0:1], in_=lg[:, 0:8], axis=AX.X, op=ALU.max), ["lg"], ["gm0"])
            V(lambda e: e.tensor_scalar(out=gm[:, 1:2], in0=gm[:, 0:1], scalar1=-1.0, scalar2=None, op0=ALU.mult), ["gm0"], ["gm1"])
            S.op("act", lambda e: e.activation(out=eg[:], in_=lg[:, 0:8], func=AF.Exp, bias=gm[:, 1:2], scale=1.0), reads=["lg", "gm1"], writes=["eg"])
            V(lambda e: e.tensor_reduce(out=gm[:, 2:3], in_=eg[:], axis=AX.X, op=ALU.add), ["eg"], ["gm2"])
            V(lambda e: e.tensor_scalar(out=onehot[:], in0=lg[:, 0:8], scalar1=gm[:, 0:1], scalar2=None, op0=ALU.is_ge), ["lg", "gm0"], ["onehot"])
            V(lambda e: e.tensor_tensor(out=tmp88[:], in0=lg[:, 8:72].rearrange("p (g j) -> p g j", g=8),
                                        in1=onehot[:].unsqueeze(2).to_broadcast([128, 8, 8]), op=ALU.mult), ["lg", "onehot"], ["tmp88"])
            V(lambda e: e.tensor_reduce(out=leg[:], in_=tmp88[:].rearrange("p g j -> p j g"), axis=AX.X, op=ALU.add), ["tmp88"], ["leg"])
            V(lambda e: e.max(out=m8r[:], in_=leg[:]), ["leg"], ["m8r"])
            V(lambda e: e.tensor_scalar(out=selloc[:], in0=leg[:], scalar1=m8r[:, 1:2], scalar2=None, op0=ALU.is_ge), ["leg", "m8r"], ["selloc"])
            V(lambda e: e.tensor_scalar(out=gm[:, 3:4], in0=m8r[:, 0:1], scalar1=-1.0, scalar2=None, op0=ALU.mult), ["m8r"], ["gm3"])
            S.op("act", lambda e: e.activation(out=wl[:], in_=leg[:], func=AF.Exp, bias=gm[:, 3:4], scale=1.0), reads=["leg", "gm3"], writes=["wl"])
            V(lambda e: e.tensor_tensor(out=wl[:], in0=wl[:], in1=selloc[:], op=ALU.mult), ["wl", "selloc"], ["wl"])
            V(lambda e: e.tensor_reduce(out=gm[:, 1:2], in_=wl[:], axis=AX.X, op=ALU.add), ["wl", "eg"], ["gm1"])
            V(lambda e: e.tensor_tensor(out=gm[:, 1:2], in0=gm[:, 1:2], in1=gm[:, 2:3], op=ALU.mult), ["gm1", "gm2"], ["gm1"])
            V(lambda e: e.reciprocal(out=gm[:, 1:2], in_=gm[:, 1:2]), ["gm1"], ["gm1"])
            V(lambda e: e.tensor_scalar(out=wl[:], in0=wl[:], scalar1=gm[:, 1:2], scalar2=None, op0=ALU.mult), ["wl", "gm1"], ["wl"])
            V(lambda e: e.tensor_tensor(out=wfull[:].rearrange("p (g j) -> p g j", g=8), in0=onehot[:].unsqueeze(2).to_broadcast([128, 8, 8]),
                                        in1=wl[:].unsqueeze(1).to_broadcast([128, 8, 8]), op=ALU.mult), ["onehot", "wl"], ["wfull"])
            V(lambda e: e.tensor_scalar(out=Af[:], in0=wfull[:], scalar1=0.0, scalar2=None, op0=ALU.is_gt), ["wfull"], ["Af"])
            V(lambda e: e.tensor_copy(out=Ab[:], in_=Af[:]), ["Af"], ["Ab"])
            S.op("pe", lambda e: e.matmul(cnt_ps[:, 0, :], lhsT=lstrict[:], rhs=Ab[:], start=True, stop=True), reads=["lstrict", "Ab"], writes=["cnt_ps"])
            S.op("pe", lambda e: e.matmul(cnt_ps[:, 1, :], lhsT=ones[:], rhs=Ab[:], start=True, stop=True), reads=["ones", "Ab"], writes=["cnt_ps"])
            V(lambda e: e.tensor_tensor(out=cnt[:], in0=cnt_ps[:, 0, :], in1=tot[:], op=ALU.add), ["cnt_ps", "tot"], ["cnt"])
            V(lambda e: e.tensor_tensor(out=tot[:], in0=cnt_ps[:, 1, :], in1=tot[:], op=ALU.add), ["cnt_ps", "tot"], ["tot"])
            V(lambda e: e.tensor_scalar(out=cnt[:], in0=cnt[:], scalar1=float(CAP - 1), scalar2=None, op0=ALU.min), ["cnt"], ["cnt"])
            V(lambda e: e.tensor_tensor(out=key[:], in0=cnt[:], in1=ebase[:], op=ALU.add), ["cnt", "ebase"], ["key"])
            V(lambda e: e.tensor_tensor(out=key[:], in0=key[:], in1=Af[:], op=ALU.mult), ["key", "Af"], ["key"])
            V(lambda e: e.max(out=m8k[:], in_=key[:]), ["key"], ["m8k"])
            V(lambda e: e.tensor_scalar(out=slf[:], in0=m8k[:, 0:2], scalar1=-1.0, scalar2=None, op0=ALU.add), ["m8k"], ["slf"])
            V(lambda e, i=i: e.tensor_copy(out=slots[:, i, :], in_=slf[:]), ["slf"], ["slots", ("slots", i)])
            for j in range(2):
                V(lambda e, j=j: e.tensor_scalar(out=eq[:], in0=key[:], scalar1=m8k[:, j:j + 1], scalar2=None, op0=ALU.is_equal), ["key", "m8k"], ["eq"])
                V(lambda e: e.tensor_tensor(out=eq[:], in0=eq[:], in1=wfull[:], op=ALU.mult), ["eq", "wfull"], ["eq"])
                V(lambda e, i=i, j=j: e.tensor_reduce(out=wab[:, i, j:j + 1], in_=eq[:], axis=AX.X, op=ALU.add), ["eq"], ["wab"])
            if "d_misc" in dbgout:
                final_toks.append(S.dma("sp", lambda e, i=i: e.dma_start(out=dbgout["d_misc"][:, i, :], in_=wfull[:]), reads=["wfull"], writes=["dbg_misc"]))
            for j in range(2):
                S.dma("pool", lambda e, b=b, i=i, j=j: e.indirect_dma_start(
                    out=XS, out_offset=bass.IndirectOffsetOnAxis(ap=slots[:, i, j:j + 1], axis=0), in_=h2b[:, b, :], in_offset=None),
                    reads=[("h2b", b), ("slots", i)], writes=["XS"])
        S.barrier()


def stage_H(nc, S, io, XS, YS, identb):
    sb, ps = _namers(nc)
    with ExitStack() as _es:
        wg = _es.enter_context(sb("wg", [128, 2, 16, 512], BF16))
        wu = _es.enter_context(sb("wu", [128, 2, 16, 512], BF16))
        wd = _es.enter_context(sb("wd", [128, 2, 4, D], BF16))
        xs = _es.enter_context(sb("xs", [128, 2, RB, D], BF16))
        xsT = _es.enter_context(sb("xsT", [128, 2, 16, CAP], BF16))
        sgh = _es.enter_context(sb("sgh", [128, 2, CAP], F32))
        aT = _es.enter_context(sb("aT", [128, 4, CAP], BF16))
        ysb = _es.enter_context(sb("ysb", [128, 2, D], BF16))
        tpx = _es.enter_context(ps("tpx", [128, 4, 1024], BF16))
        gu_ps = _es.enter_context(ps("gu_ps", [128, 2, 2, CAP], F32))
        y_ps = _es.enter_context(ps("y_ps", [128, 2, 512], F32))

        def load(ex):
            wb = ex % 2
            S.dma("pool", lambda e: e.dma_start(out=wg[:, wb], in_=io["w_gate"][ex].rearrange("(k p) n -> p k n", p=128)), writes=[("wg", wb)])
            S.dma("pool", lambda e: e.dma_start(out=wu[:, wb], in_=io["w_up"][ex].rearrange("(k p) n -> p k n", p=128)), writes=[("wu", wb)])
            S.dma("pool", lambda e: e.dma_start(out=wd[:, wb], in_=io["w_down"][ex].rearrange("(c p) n -> p c n", p=128)), writes=[("wd", wb)])
            S.dma("sp", lambda e: e.dma_start(out=xs[:, wb], in_=XS[ex * CAP:(ex + 1) * CAP, :].rearrange("(r p) d -> p r d", p=128)),
                  reads=["XS"], writes=[("xs", wb)])

        def transposes(ex):
            wb = ex % 2
            for r in range(RB):
                for k8 in range(2):
                    j = (r * 2 + k8) % 4
                    for kk in range(8):
                        k = k8 * 8 + kk
                        S.op("pe", lambda e, kk=kk, k=k: e.transpose(tpx[:, j, kk * 128:(kk + 1) * 128], xs[:, wb, r, k * 128:(k + 1) * 128], identb[:]),
                             reads=[("xs", wb), "identb"], writes=[("tpx", j)])
                    dst = xsT[:, wb, k8 * 8:(k8 + 1) * 8, r * 128:(r + 1) * 128]
                    src = tpx[:, j, :].rearrange("p (a b) -> p a b", a=8)
                    if j % 2 == 0:
                        S.op("act", lambda e, dst=dst, src=src: e.copy(out=dst, in_=src), reads=[("tpx", j)], writes=[("xsT", wb)])
                    else:
                        S.op("dve", lambda e, dst=dst, src=src: e.tensor_copy(out=dst, in_=src), reads=[("tpx", j)], writes=[("xsT", wb)])

        def gate_up(ex):
            wb = ex % 2
            for hc in range(4):
                gb = hc % 2
                for which, w in enumerate([wg, wu]):
                    for k in range(16):
                        S.op("pe", lambda e, which=which, w=w, k=k: e.matmul(
                            gu_ps[:, gb, which, :], lhsT=w[:, wb, k, hc * 128:(hc + 1) * 128], rhs=xsT[:, wb, k, :], start=(k == 0), stop=(k == 15)),
                            reads=[("wg", wb), ("wu", wb), ("xsT", wb)], writes=[("gu_ps", gb)])
                S.op("act", lambda e: e.activation(out=sgh[:, gb, :], in_=gu_ps[:, gb, 0, :], func=AF.Silu), reads=[("gu_ps", gb)], writes=[("sgh", gb)])
                S.op("dve", lambda e: e.tensor_tensor(out=aT[:, hc, :], in0=sgh[:, gb, :], in1=gu_ps[:, gb, 1, :], op=ALU.mult),
                     reads=[("sgh", gb), ("gu_ps", gb)], writes=["aT"])

        ny = [0]

        def down(ex):
            wb = ex % 2
            for r in range(RB):
                yb = ny[0] % 2
                ny[0] += 1
                for cb in range(4):
                    pb = cb % 2
                    for hc in range(4):
                        S.op("pe", lambda e, hc=hc: e.matmul(
                            y_ps[:, pb, :], lhsT=aT[:, hc, r * 128:(r + 1) * 128], rhs=wd[:, wb, hc, cb * 512:(cb + 1) * 512], start=(hc == 0), stop=(hc == 3)),
                            reads=["aT", ("wd", wb)], writes=[("y_ps", pb)])
                    if cb % 2 == 0:
                        S.op("act", lambda e: e.copy(out=ysb[:, yb, cb * 512:(cb + 1) * 512], in_=y_ps[:, pb, :]), reads=[("y_ps", pb)], writes=[("ysb", yb)])
                    else:
                        S.op("dve", lambda e: e.tensor_copy(out=ysb[:, yb, cb * 512:(cb + 1) * 512], in_=y_ps[:, pb, :]), reads=[("y_ps", pb)], writes=[("ysb", yb)])
                S.dma("sp", lambda e: e.dma_start(out=YS[ex * CAP + r * 128:ex * CAP + (r + 1) * 128, :], in_=ysb[:, yb]),
                      reads=[("ysb", yb)], writes=["YS"])

        load(0)
        transposes(0)
        for ex in range(64):
            if ex + 1 < 64:
                load(ex + 1)
            gate_up(ex)
            if ex + 1 < 64:
                transposes(ex + 1)
            down(ex)
        S.barrier()


def stage_I(nc, S, io, X1, YS, g2row, slots, wab, out):
    sb, ps = _namers(nc)
    toks = []
    with ExitStack() as _es:
        ya = _es.enter_context(sb("ya", [128, 2, D], BF16))
        yb_ = _es.enter_context(sb("yb_", [128, 2, D], BF16))
        x1i = _es.enter_context(sb("x1i", [128, 2, D], F32))
        mo = _es.enter_context(sb("mo", [128, D], F32))
        ot = _es.enter_context(sb("ot", [128, 2, D], F32))
        fg = _es.enter_context(sb("fg", [128, D], F32))
        junk = _es.enter_context(sb("junk3", [128, D], BF16))
        ss3 = _es.enter_context(sb("ss3", [128, 16], F32))
        rstd3 = _es.enter_context(sb("rstd3", [128, 16], F32))
        S.dma("sp", lambda e: e.dma_start(out=fg[:], in_=io["fg_row"]), writes=["fg"])
        S.op("dve", lambda e: e.memset(ss3[:], 0.0), writes=["ss3"])
        for i in range(16):
            b = i % 2
            isl = slice(i * 128, (i + 1) * 128)
            S.dma("pool", lambda e, b=b, i=i: e.indirect_dma_start(out=ya[:, b, :], out_offset=None, in_=YS,
                                                                   in_offset=bass.IndirectOffsetOnAxis(ap=slots[:, i, 0:1], axis=0)),
                  reads=["YS", "slots"], writes=[("ya", b)])
            S.dma("pool", lambda e, b=b, i=i: e.indirect_dma_start(out=yb_[:, b, :], out_offset=None, in_=YS,
                                                                   in_offset=bass.IndirectOffsetOnAxis(ap=slots[:, i, 1:2], axis=0)),
                  reads=["YS", "slots"], writes=[("yb", b)])
            S.dma("sp", lambda e, b=b, isl=isl: e.dma_start(out=x1i[:, b], in_=X1[isl, :]), reads=["X1"], writes=[("x1i", b)])
            S.op("dve", lambda e, b=b, i=i: e.tensor_scalar(out=mo[:], in0=ya[:, b], scalar1=wab[:, i, 0:1], scalar2=None, op0=ALU.mult),
                 reads=[("ya", b), "wab"], writes=["mo"])
            S.op("dve", lambda e, b=b, i=i: e.scalar_tensor_tensor(out=mo[:], in0=yb_[:, b], scalar=wab[:, i, 1:2], in1=mo[:], op0=ALU.mult, op1=ALU.add),
                 reads=[("yb", b), "wab", "mo"], writes=["mo"])
            S.op("pool", lambda e: e.tensor_tensor(out=mo[:], in0=mo[:], in1=g2row[:], op=ALU.mult), reads=["mo", "g2row"], writes=["mo"])
            S.op("pool", lambda e, b=b: e.tensor_tensor(out=mo[:], in0=mo[:], in1=x1i[:, b], op=ALU.add), reads=["mo", ("x1i", b)], writes=["mo"])
            S.op("act", lambda e, i=i: e.activation(out=junk[:], in_=mo[:], func=AF.Square, accum_out=ss3[:, i:i + 1]), reads=["mo", "ss3"], writes=["junk", ("ss3", i)])
            S.op("act", lambda e, i=i: e.activation(out=rstd3[:, i:i + 1], in_=ss3[:, i:i + 1], func=AF.Sqrt, bias=epsc[:, 0:1], scale=1.0 / D),
                 reads=[("ss3", i), "epsc"], writes=[("rstd3", i)])
            S.op("dve", lambda e, i=i: e.reciprocal(out=rstd3[:, i:i + 1], in_=rstd3[:, i:i + 1]), reads=[("rstd3", i)], writes=[("rstd3", i)])
            S.op("dve", lambda e, b=b, i=i: e.scalar_tensor_tensor(out=ot[:, b], in0=mo[:], scalar=rstd3[:, i:i + 1], in1=fg[:], op0=ALU.mult, op1=ALU.mult),
                 reads=["mo", ("rstd3", i), "fg"], writes=[("ot", b)])
            toks.append(S.dma("sp", lambda e, b=b, isl=isl: e.dma_start(out=out[isl, :], in_=ot[:, b]), reads=[("ot", b)], writes=["out"]))
        S.barrier()
    return toks


def host_inputs(inp, b):
    f = lambda a: np.ascontiguousarray(a, dtype=np.float32)
    m = {}
    m["x"] = f(inp["x"][b])
    m["cT"] = f(inp["c"][b].reshape(16, 128).T)
    m["w_ada"] = f(inp["w_ada"][0])
    m["b_adaT"] = f(inp["b_ada"][0].reshape(96, 128).T)
    m["n1gT"] = f(inp["norm1_g"][0].reshape(16, 128).T)
    m["n2gT"] = f(inp["norm2_g"][0].reshape(16, 128).T)
    m["fg_row"] = f(np.broadcast_to(inp["final_g"][None, :], (128, D)))
    m["w_in"] = f(inp["w_in"][0])
    m["gng_row"] = f(np.broadcast_to(inp["ret_gn_g"][0][None, :], (128, 1024)))
    m["peT_k"] = f(inp["cmp_pos_k"][0].T)
    m["w1_k"] = f(inp["cmp_w1_k"][0])
    m["w2_k"] = f(inp["cmp_w2_k"][0])
    m["peT_v"] = f(inp["cmp_pos_v"][0].T)
    m["w1_v"] = f(inp["cmp_w1_v"][0])
    m["w2_v"] = f(inp["cmp_w2_v"][0])
    m["w_out"] = f(inp["w_out"][0])
    m["w_rt"] = f(np.concatenate([inp["w_grp"][0], inp["w_exp"][0]], axis=1))
    m["b_rt"] = f(np.broadcast_to(np.concatenate([inp["b_grp"][0], inp["b_exp"][0]])[None, :], (128, 72)))
    m["w_gate"] = f(inp["w_gate"][0])
    m["w_up"] = f(inp["w_up"][0])
    m["w_down"] = f(inp["w_down"][0])
    return m


def kernel(**inputs):
    inp = {k: np.asarray(v) for k, v in inputs.items()}
    nc = build_nc()
    consts = {k: v for k, v in make_consts().items() if not k.startswith("_")}
    shared = None
    in_maps = []
    for b in range(8):
        m = host_inputs(inp, b)
        if shared is None:
            shared = {k: m[k] for k in m if k not in ("x", "cT")}
        else:
            for k in shared:
                m[k] = shared[k]
        m.update(consts)
        in_maps.append(m)
    res = run_bass_kernel_spmd(nc, in_maps, core_ids=list(range(8)))
    return np.stack([np.asarray(r["out"], dtype=np.float32).reshape(T, D) for r in res.results], axis=0)
```

```python
import os
from contextlib import ExitStack
import numpy as np
import ml_dtypes
import concourse.bass as bass
import concourse.mybir as mybir
from concourse.bass_utils import run_bass_kernel_spmd

F32 = mybir.dt.float32
BF16 = mybir.dt.bfloat16
I32 = mybir.dt.int32
AF = mybir.ActivationFunctionType
ALU = mybir.AluOpType
AX = mybir.AxisListType

D = 2048
T = 2048
NT = 16
KC = 16
PROJ = 6680
CAP = 256
RB = CAP // 128
NEG = -30000.0
EPS = 1e-6

ENGS = ("pe", "act", "dve", "pool", "sp")


_UNIQ = [0]


def _namers(nc):
    def sb(name, shp, dt):
        _UNIQ[0] += 1
        return nc.sbuf_tensor("s%d_%s" % (_UNIQ[0], name), shp, dt)

    def ps(name, shp, dt):
        _UNIQ[0] += 1
        return nc.psum_tensor("p%d_%s" % (_UNIQ[0], name), shp, dt)
    return sb, ps


class Sched:
    NDMA = 24

    def __init__(self, nc):
        self.nc = nc
        self.handles = dict(pe=nc.tensor, act=nc.scalar, dve=nc.vector, pool=nc.gpsimd, sp=nc.sync)
        self.cnt = {e: 0 for e in ENGS}
        self.waited = {e: {} for e in ENGS}
        self.lastw = {}
        self.readers = {}
        self.sems = {}
        self.dma_sems = []
        self.dma_n = 0
        self.dma_q = [0, 0]
        self.dma_last = {}
        self._stack = []

    def open(self):
        for e in ENGS:
            cm = self.nc.semaphore("sem_" + e)
            self.sems[e] = cm.__enter__()
            self._stack.append(cm)
        for i in range(self.NDMA):
            cm = self.nc.semaphore("semd%d" % i)
            self.dma_sems.append(cm.__enter__())
            self._stack.append(cm)

    def close(self):
        for cm in reversed(self._stack):
            cm.__exit__(None, None, None)

    def _need(self, eng, tok, waits):
        if tok is None:
            return
        sem_id, val, src = tok
        if src == "pe" and eng == "pe":
            return
        w = self.waited[eng]
        if w.get(sem_id, 0) >= val:
            return
        w[sem_id] = val
        waits.append((sem_id, val))

    def _deps(self, eng, reads, writes):
        waits = []
        for b in reads:
            self._need(eng, self.lastw.get(b), waits)
        for b in writes:
            self._need(eng, self.lastw.get(b), waits)
            for t in self.readers.get(b, ()):
                self._need(eng, t, waits)
        return waits

    def _commit(self, tok, reads, writes):
        for b in reads:
            self.readers.setdefault(b, []).append(tok)
        for b in writes:
            self.lastw[b] = tok
            self.readers[b] = []

    def op(self, eng, fn, reads=(), writes=()):
        waits = self._deps(eng, reads, writes)
        self.cnt[eng] += 1
        tok = ("E" + eng, self.cnt[eng], eng)
        self._emit1(eng, waits, fn, ("E" + eng, 1))
        self._commit(tok, reads, writes)
        return tok

    def dma(self, eng, fn, reads=(), writes=()):
        half = self.NDMA // 2
        qi = 0 if eng == "pool" else 1
        j = self.dma_q[qi]
        self.dma_q[qi] += 1
        s = qi * half + (j % half)
        sid = "D%d" % s
        prev = self.dma_last.get(s, 0)
        waits = self._deps(eng, reads, writes)
        if prev and self.waited[eng].get(sid, 0) < prev:
            self.waited[eng][sid] = prev
            waits.append((sid, prev))
        val = prev + 16
        self.dma_last[s] = val
        tok = (sid, val, "dma")
        self._emit1(eng, waits, fn, (sid, 16))
        self._commit(tok, reads, writes)
        return tok

    def _emit1(self, engname, waits, fn, inc):
        e = self.handles[engname]
        if os.environ.get("K_TRACE"):
            print("TR", engname, self.cnt[engname], waits, inc, flush=True)
        for sid, val in waits:
            e.wait_ge(self._sem(sid), val)
        if fn is not None:
            ins = fn(e)
            ins.then_inc(self._sem(inc[0]), inc[1])

    def _sem(self, sid):
        if sid[0] == "E":
            return self.sems[sid[1:]]
        return self.dma_sems[int(sid[1:])]

    def barrier(self, engs=ENGS):
        toks = [("E" + e, self.cnt[e], e) for e in ENGS if self.cnt[e] > 0]
        toks += [("D%d" % s, v, "dma") for s, v in self.dma_last.items()]
        for e in engs:
            waits = []
            for t in toks:
                self._need(e, t, waits)
            self._emit1(e, waits, None, None)


def make_consts():
    bf = ml_dtypes.bfloat16
    c = {}
    half = 128
    inv = (10000.0 ** (-np.arange(half, dtype=np.float32) / half)).astype(np.float32)
    pos = np.arange(T, dtype=np.float32)
    ang = (inv[:, None] * pos[None, :]).astype(np.float32)
    c["cos"] = np.cos(ang).astype(np.float32)
    c["sin"] = np.sin(ang).astype(np.float32)
    c["identb"] = np.eye(128, dtype=np.float32).astype(bf)
    c["identf"] = np.eye(128, dtype=np.float32)
    H = 4
    lg = np.log1p(-np.exp2(-5.0 - np.arange(H, dtype=np.float64)))
    m = np.arange(128, dtype=np.float64)
    dm = np.zeros((128, H, 128), np.float32)
    for h in range(H):
        val = np.exp(lg[h] * (-m - 1.0))
        dm[:, h, :] = np.where(m[None, :] >= m[:, None], val[:, None], 0.0)
    c["dm"] = dm
    c["zeta"] = np.exp(lg[None, :] * (127.0 - m)[:, None]).astype(np.float32)
    c["qdec"] = np.exp(lg[None, :] * (m + 1.0)[:, None]).astype(np.float32)
    c["_cd"] = [float(np.exp(lg[h] * 128.0)) for h in range(H)]
    j = np.arange(128)
    caus = np.where(j[:, None] <= j[None, :], 0.0, NEG).astype(np.float32)
    anti = np.where(j[:, None] > j[None, :], 0.0, NEG).astype(np.float32)
    c["caus"] = caus.astype(bf)
    c["anti"] = anti.astype(bf)
    cc = np.arange(128)
    tt = np.arange(T)
    c["cmask"] = np.where(16 * cc[:, None] + 31 <= tt[None, :], 0.0, NEG).astype(np.float32).astype(bf)
    ss = np.arange(32)
    ov = ((16 * cc[:, None] < 64 * ss[None, :] + 64) & (16 * cc[:, None] + 32 > 64 * ss[None, :])).astype(np.float32)
    c["overlap"] = ov.astype(bf)
    es = np.zeros((32, 16, 128), np.float32)
    for kt in range(16):
        for p in range(128):
            es[2 * kt + p // 64, kt, p] = 1.0
    c["esel"] = es.astype(bf)
    cur = tt // 64
    valid = (ss[None, :] <= cur[:, None])
    forced = (ss[None, :] == 0) | (ss[None, :] == cur[:, None]) | (ss[None, :] == cur[:, None] - 1)
    ctab = np.where(valid, np.where(forced, 1e4, 0.0), -1.0).astype(np.float32)
    c["valid"] = np.ascontiguousarray(valid.astype(np.float32).reshape(16, 128, 32).transpose(1, 0, 2))
    c["ctab"] = np.ascontiguousarray(ctab.reshape(16, 128, 32).transpose(1, 0, 2))
    c["lstrict"] = (j[:, None] < j[None, :]).astype(np.float32).astype(bf)
    c["ones"] = np.ones((128, 128), np.float32).astype(bf)
    c["ebase"] = np.tile((np.arange(64, dtype=np.float32) * CAP + 1.0)[None, :], (128, 1))
    return c


CONST_SPECS = [
    ("cos", [128, T], F32), ("sin", [128, T], F32), ("identb", [128, 128], BF16), ("identf", [128, 128], F32),
    ("dm", [128, 4, 128], F32), ("zeta", [128, 4], F32), ("qdec", [128, 4], F32),
    ("caus", [128, 128], BF16), ("anti", [128, 128], BF16), ("cmask", [128, T], BF16), ("overlap", [128, 32], BF16),
    ("esel", [32, 16, 128], BF16), ("valid", [128, 16, 32], F32), ("ctab", [128, 16, 32], F32),
    ("lstrict", [128, 128], BF16), ("ones", [128, 128], BF16), ("ebase", [128, 64], F32),
]

IN_SPECS = [
    ("x", [T, D], F32), ("cT", [128, 16], F32), ("w_ada", [D, 6 * D], F32), ("b_adaT", [128, 96], F32),
    ("n1gT", [128, 16], F32), ("n2gT", [128, 16], F32), ("fg_row", [128, D], F32), ("w_in", [D, PROJ], F32),
    ("gng_row", [128, 1024], F32), ("peT_k", [128, 32], F32), ("w1_k", [4096, 256], F32), ("w2_k", [256, 128], F32),
    ("peT_v", [128, 32], F32), ("w1_v", [4096, 256], F32), ("w2_v", [256, 128], F32), ("w_out", [D, D], F32),
    ("w_rt", [D, 72], F32), ("b_rt", [128, 72], F32), ("w_gate", [64, D, 512], F32), ("w_up", [64, D, 512], F32),
    ("w_down", [64, 512, D], F32),
]


def build_nc(stop=99, dbg=()):
    CST = make_consts()
    nc = bass.Bass("TRN2", target_bir_lowering=False)
    io = {}
    for name, shp, dt in IN_SPECS + CONST_SPECS:
        if stop < 8 and name in ("w_gate", "w_up", "w_down"):
            shp = [1] + shp[1:]
        io[name] = nc.dram_tensor(name, shp, dt, kind="ExternalInput").ap()
    out = nc.dram_tensor("out", [T, D], F32, kind="ExternalOutput").ap()

    def scratch(name, shp, dt):
        kind = "ExternalOutput" if name in dbg else "Internal"
        return nc.dram_tensor(name, shp, dt, kind=kind).ap()

    PFM = scratch("PFM", [32, 128, T], BF16)
    PTM = scratch("PTM", [T, 2560], BF16)
    X1 = scratch("X1", [T, D], F32)
    XS = scratch("XS", [64 * CAP, D], BF16)
    YS = scratch("YS", [64 * CAP, D], BF16)
    dbgout = {}
    for name, shp, dt in [("d_modT", [128, 96], F32), ("d_hT", [128, 16, T], BF16), ("d_ocatT", [128, 16, T], BF16),
                          ("d_ngs", [128, 16, 24], F32), ("d_slots", [128, 16, 2], I32), ("d_wab", [128, 16, 2], F32),
                          ("d_misc", [128, 16, 64], F32)]:
        if name in dbg:
            dbgout[name] = nc.dram_tensor(name, shp, dt, kind="ExternalOutput").ap()

    S = Sched(nc)
    S.open()
    sb, ps = _namers(nc)
    final_toks = []

    def dbgdump(name, ap, key):
        if name in dbgout:
            final_toks.append(S.dma("sp", lambda e: e.dma_start(out=dbgout[name], in_=ap), reads=[key], writes=["dbg_" + name]))

    with ExitStack() as _es:
        modT = _es.enter_context(sb("modT", [128, 96], F32))
        A1 = _es.enter_context(sb("A1", [128, 16], F32))
        A2 = _es.enter_context(sb("A2", [128, 16], F32))
        identb = _es.enter_context(sb("identb", [128, 128], BF16))
        identf = _es.enter_context(sb("identf", [128, 128], F32))
        ngs = _es.enter_context(sb("ngs", [128, 16, 24], F32))
        slots = _es.enter_context(sb("slots", [128, 16, 2], I32))
        wab = _es.enter_context(sb("wab", [128, 16, 2], F32))
        global epsc
        epsc = _es.enter_context(sb("epsc", [128, 1], F32))
        S.op("dve", lambda e: e.memset(epsc[:], EPS), writes=["epsc"])
        S.dma("sp", lambda e: e.dma_start(out=identb[:], in_=io["identb"]), writes=["identb"])
        S.dma("sp", lambda e: e.dma_start(out=identf[:], in_=io["identf"]), writes=["identf"])

        with ExitStack() as _es:
            cT = _es.enter_context(sb("cT", [128, 16], F32))
            cact = _es.enter_context(sb("cact", [128, 16], BF16))
            badaT = _es.enter_context(sb("badaT", [128, 96], F32))
            n1gT = _es.enter_context(sb("n1gT", [128, 16], F32))
            n2gT = _es.enter_context(sb("n2gT", [128, 16], F32))
            wa = _es.enter_context(sb("wa", [128, 2, 16, 512], BF16))
            modps = _es.enter_context(ps("modps", [128, 96], F32))
            S.dma("sp", lambda e: e.dma_start(out=cT[:], in_=io["cT"]), writes=["cT"])
            S.dma("sp", lambda e: e.dma_start(out=badaT[:], in_=io["b_adaT"]), writes=["badaT"])
            S.dma("sp", lambda e: e.dma_start(out=n1gT[:], in_=io["n1gT"]), writes=["n1gT"])
            S.dma("sp", lambda e: e.dma_start(out=n2gT[:], in_=io["n2gT"]), writes=["n2gT"])
            S.op("act", lambda e: e.activation(out=cact[:], in_=cT[:], func=AF.Silu), reads=["cT"], writes=["cact"])
            if stop >= 7:
                zt = _es.enter_context(sb("zt", [128, RB, D], BF16))
                S.op("dve", lambda e: e.memset(zt[:], 0.0), writes=["zt"])
                for ex in range(64):
                    S.dma("sp", lambda e, ex=ex: e.dma_start(out=XS[ex * CAP:(ex + 1) * CAP, :].rearrange("(r p) d -> p r d", p=128), in_=zt[:]),
                          reads=["zt"], writes=["XS"])
            wada = io["w_ada"].rearrange("(k p) n -> p k n", p=128)
            for blk in range(24 if not os.environ.get("K_SKIPABC") else 0):
                b = blk % 2
                S.dma("pool", lambda e, b=b, blk=blk: e.dma_start(out=wa[:, b], in_=wada[:, :, blk * 512:(blk + 1) * 512]),
                      writes=[("wa", b)])
                for j in range(4):
                    col = blk * 4 + j
                    for k in range(16):
                        S.op("pe", lambda e, b=b, j=j, k=k, col=col: e.matmul(
                            modps[:, col:col + 1], lhsT=wa[:, b, k, j * 128:(j + 1) * 128], rhs=cact[:, k:k + 1],
                            start=(k == 0), stop=(k == 15)), reads=[("wa", b), "cact"], writes=["modps"])
            S.op("dve", lambda e: e.tensor_tensor(out=modT[:], in0=modps[:], in1=badaT[:], op=ALU.add),
                 reads=["modps", "badaT"], writes=["modT"])
            S.op("dve", lambda e: e.scalar_tensor_tensor(out=A1[:], in0=modT[:, 16:32], scalar=1.0, in1=n1gT[:],
                                                         op0=ALU.add, op1=ALU.mult), reads=["modT", "n1gT"], writes=["A1"])
            S.op("dve", lambda e: e.scalar_tensor_tensor(out=A2[:], in0=modT[:, 64:80], scalar=1.0, in1=n2gT[:],
                                                         op0=ALU.add, op1=ALU.mult), reads=["modT", "n2gT"], writes=["A2"])
            dbgdump("d_modT", modT[:], "modT")
            S.barrier()

        def rowbcast(dst, col0, key):
            with sb("rb_l", [128, 2, 128], F32) as rbl, ps("rb_ps", [128, 2, 512], F32) as rbps:
                for k in range(16):
                    lb = k % 2
                    S.op("dve", lambda e, k=k, lb=lb: e.tensor_copy(out=rbl[:, lb, :], in_=modT[:, col0 + k:col0 + k + 1].to_broadcast([128, 128])),
                         reads=["modT"], writes=[("rbl", lb)])
                    S.op("pe", lambda e, k=k, lb=lb: e.matmul(rbps[:, (k // 4) % 2, (k % 4) * 128:(k % 4 + 1) * 128], lhsT=rbl[:, lb, :], rhs=identf[:],
                                                              start=True, stop=True), reads=[("rbl", lb), "identf"], writes=[("rbps", (k // 4) % 2)])
                    if k % 4 == 3:
                        q = k // 4
                        S.op("act", lambda e, q=q: e.copy(out=dst[:, q * 512:(q + 1) * 512], in_=rbps[:, q % 2, :]),
                             reads=[("rbps", q % 2)], writes=[key])
                S.barrier()

        if stop >= 2 and not os.environ.get("K_SKIPABC"):
          with sb("hT", [128, 16, T], BF16) as hT:
            with ExitStack() as _es:
                xt = _es.enter_context(sb("xt", [128, 2, D], F32))
                xn = _es.enter_context(sb("xn", [128, 4, D], BF16))
                junk = _es.enter_context(sb("junk", [128, D], BF16))
                ss = _es.enter_context(sb("ss", [128, 16], F32))
                rstd = _es.enter_context(sb("rstd", [128, 16], F32))
                tp = _es.enter_context(ps("tpB", [128, 2, 1024], BF16))
                S.op("dve", lambda e: e.memset(ss[:], 0.0), writes=["ss"])
                for tg in range(4):
                    for i4 in range(4):
                        i = tg * 4 + i4
                        b = i % 2
                        S.dma("sp", lambda e, i=i, b=b: e.dma_start(out=xt[:, b], in_=io["x"][i * 128:(i + 1) * 128, :]), writes=[("xt", b)])
                        S.op("act", lambda e, i=i, b=b: e.activation(out=junk[:], in_=xt[:, b], func=AF.Square, accum_out=ss[:, i:i + 1]),
                             reads=[("xt", b), "ss"], writes=["junk", ("ss", i)])
                        S.op("act", lambda e, i=i: e.activation(out=rstd[:, i:i + 1], in_=ss[:, i:i + 1], func=AF.Sqrt, bias=epsc[:, 0:1], scale=1.0 / D),
                             reads=[("ss", i), "epsc"], writes=[("rstd", i)])
                        S.op("dve", lambda e, i=i: e.reciprocal(out=rstd[:, i:i + 1], in_=rstd[:, i:i + 1]), reads=[("rstd", i)], writes=[("rstd", i)])
                        S.op("dve", lambda e, i=i, i4=i4, b=b: e.tensor_scalar(out=xn[:, i4], in0=xt[:, b], scalar1=rstd[:, i:i + 1], scalar2=None,
                                                                               op0=ALU.mult), reads=[("xt", b), ("rstd", i)], writes=[("xn", i4)])
                    for k in range(16):
                        pb = k % 2
                        for i4 in range(4):
                            S.op("pe", lambda e, k=k, pb=pb, i4=i4: e.transpose(tp[:, pb, i4 * 128:(i4 + 1) * 128], xn[:, i4, k * 128:(k + 1) * 128], identb[:]),
                                 reads=[("xn", i4), "identb"], writes=[("tp", pb)])
                        S.op("act", lambda e, k=k, pb=pb, tg=tg: e.activation(out=hT[:, k, tg * 512:(tg + 1) * 512], in_=tp[:, pb, 0:512], func=AF.Identity,
                                                                              scale=A1[:, k:k + 1], bias=modT[:, k:k + 1]),
                             reads=[("tp", pb), "A1", "modT"], writes=[("hT", tg)])
                dbgdump("d_hT", hT[:], ("hT", 3))
                S.barrier()

            if stop >= 3:
                stage_C(nc, S, io, hT, PFM, PTM, ngs, dbgdump)
            S.barrier()

        if stop >= 4:
          with sb("ocatT", [128, 16, T], BF16) as ocatT:
            if not os.environ.get("K_SKIPD"):
                stage_D(nc, S, io, CST, PFM, PTM, ocatT, identb)
            if stop >= 5:
                stage_E(nc, S, io, PFM, PTM, ocatT, identb, ngs)
            dbgdump("d_ocatT", ocatT[:], "ocatT")
            if stop >= 6:
                with sb("g1row", [128, D], F32) as g1row:
                    rowbcast(g1row, 32, "g1row")
                    stage_F(nc, S, io, ocatT, g1row, X1)
            S.barrier()

        if stop >= 7:
            with sb("a2row", [128, D], F32) as a2row, sb("sh2row", [128, D], F32) as sh2row:
                S.op("dve", lambda e: e.tensor_copy(out=modT[:, 64:80], in_=A2[:]), reads=["A2", "modT"], writes=["modT"])
                rowbcast(a2row, 64, "a2row")
                rowbcast(sh2row, 48, "sh2row")
                stage_G(nc, S, io, X1, XS, a2row, sh2row, identf, slots, wab, dbgout, final_toks)
            S.barrier()
            dbgdump("d_slots", slots[:], "slots")
            dbgdump("d_wab", wab[:], "wab")
        if stop >= 8:
            stage_H(nc, S, io, XS, YS, identb)
            S.barrier()
        if stop >= 9:
            with sb("g2row", [128, D], F32) as g2row:
                rowbcast(g2row, 80, "g2row")
                final_toks += stage_I(nc, S, io, X1, YS, g2row, slots, wab, out)
        S.barrier()
        e = S.handles["sp"]
        waits = []
        for t in final_toks:
            S._need("sp", t, waits)
        for sid, val in waits:
            e.wait_ge(S._sem(sid), val)
    S.close()
    return nc


def stage_C(nc, S, io, hT, PFM, PTM, ngs, dbgdump):
    sb, ps = _namers(nc)
    win = io["w_in"].rearrange("(k p) n -> p k n", p=128)
    units = []
    for h in range(4):
        units.append((h * 256, "rotq", 2 * h))
    for h in range(4):
        units.append((1024 + h * 256, "rotk", 8 + 2 * h))
    for u in range(4):
        units.append((4096 + u * 256, "scale", 16 + 2 * u))
    units += [(5120, "copy", 24), (5376, "copy", 26), (5632, "copy", 28), (6144, "copy", 30)]
    with ExitStack() as _es:
        cos = _es.enter_context(sb("cos", [128, T], F32))
        sin = _es.enter_context(sb("sin", [128, T], F32))
        wf = _es.enter_context(sb("wf", [128, 2, 16, 256], BF16))
        stg = _es.enter_context(sb("stg", [128, 2, 2, T], BF16))
        f12 = _es.enter_context(sb("f12", [128, 2, 512], F32))
        tt = _es.enter_context(sb("tt", [128, 4, 512], F32))
        pfm = _es.enter_context(ps("pfm", [128, 2, 2, 512], F32))
        S.dma("sp", lambda e: e.dma_start(out=cos[:], in_=io["cos"]), writes=["cos"])
        S.dma("sp", lambda e: e.dma_start(out=sin[:], in_=io["sin"]), writes=["sin"])
        n = 0
        for ui, (c0, kind, ch) in enumerate(units):
            wb = ui % 2
            S.dma("pool", lambda e, wb=wb, c0=c0: e.dma_start(out=wf[:, wb], in_=win[:, :, c0:c0 + 256]), writes=[("wf", wb)])
            for tg in range(4):
                pb = n % 2
                n += 1
                for half in range(2):
                    for k in range(16):
                        S.op("pe", lambda e, wb=wb, pb=pb, half=half, k=k, tg=tg: e.matmul(
                            pfm[:, pb, half, :], lhsT=wf[:, wb, k, half * 128:(half + 1) * 128], rhs=hT[:, k, tg * 512:(tg + 1) * 512],
                            start=(k == 0), stop=(k == 15)), reads=[("wf", wb), ("hT", tg)], writes=[("pfm", pb)])
                tsl = slice(tg * 512, (tg + 1) * 512)
                if kind in ("copy", "scale"):
                    sc = 1.0 if kind == "copy" else 128.0 ** -0.5
                    for half in range(2):
                        S.op("act", lambda e, pb=pb, wb=wb, tsl=tsl, sc=sc, half=half: e.activation(out=stg[:, wb, half, tsl], in_=pfm[:, pb, half, :], func=AF.Copy, scale=sc),
                             reads=[("pfm", pb)], writes=[("stg", wb)])
                else:
                    sc = 1.0 if kind == "rotq" else 1.0 / 16.0
                    for half in range(2):
                        S.op("act", lambda e, pb=pb, sc=sc, half=half: e.activation(out=f12[:, half, :], in_=pfm[:, pb, half, :], func=AF.Copy, scale=sc),
                             reads=[("pfm", pb)], writes=["f12"])
                    S.op("dve", lambda e, tsl=tsl: e.tensor_tensor(out=tt[:, 0], in0=f12[:, 0], in1=cos[:, tsl], op=ALU.mult), reads=["f12", "cos"], writes=["tt0"])
                    S.op("dve", lambda e, tsl=tsl: e.tensor_tensor(out=tt[:, 1], in0=f12[:, 1], in1=sin[:, tsl], op=ALU.mult), reads=["f12", "sin"], writes=["tt1"])
                    S.op("pool", lambda e, tsl=tsl: e.tensor_tensor(out=tt[:, 2], in0=f12[:, 0], in1=sin[:, tsl], op=ALU.mult), reads=["f12", "sin"], writes=["tt2"])
                    S.op("pool", lambda e, tsl=tsl: e.tensor_tensor(out=tt[:, 3], in0=f12[:, 1], in1=cos[:, tsl], op=ALU.mult), reads=["f12", "cos"], writes=["tt3"])
                    S.op("dve", lambda e, wb=wb, tsl=tsl: e.tensor_tensor(out=stg[:, wb, 0, tsl], in0=tt[:, 0], in1=tt[:, 1], op=ALU.subtract),
                         reads=["tt0", "tt1"], writes=[("stg", wb)])
                    S.op("pool", lambda e, wb=wb, tsl=tsl: e.tensor_tensor(out=stg[:, wb, 1, tsl], in0=tt[:, 2], in1=tt[:, 3], op=ALU.add),
                         reads=["tt2", "tt3"], writes=[("stg", wb)])
            S.dma("sp", lambda e, wb=wb, ch=ch: e.dma_start(out=PFM[ch:ch + 2].rearrange("c p t -> p c t"), in_=stg[:, wb]),
                  reads=[("stg", wb)], writes=["PFM"])
        S.barrier()
    tunits = [([(2048, 512)], 0), ([(2560, 512)], 512), ([(3072, 512)], 1024), ([(3584, 512)], 1536),
              ([(5888, 256), (6400, 256)], 2048), ([(6656, 24)], None)]
    ptm_v = PTM.rearrange("(i p) c -> p i c", p=128)
    with ExitStack() as _es:
        wt = _es.enter_context(sb("wt", [128, 2, 16, 512], BF16))
        stgt = _es.enter_context(sb("stgt", [128, 2, 16, 512], BF16))
        ptm = _es.enter_context(ps("ptm", [128, 2, 512], F32))
        n = 0
        for ui, (srcs, dcol) in enumerate(tunits):
            wb = ui % 2
            off = 0
            for (c0, w) in srcs:
                S.dma("pool", lambda e, wb=wb, c0=c0, w=w, off=off: e.dma_start(out=wt[:, wb, :, off:off + w], in_=win[:, :, c0:c0 + w]),
                      writes=[("wt", wb)])
                off += w
            ncol = off
            for i in range(16):
                pb = n % 2
                n += 1
                for k in range(16):
                    S.op("pe", lambda e, wb=wb, pb=pb, i=i, k=k, ncol=ncol: e.matmul(
                        ptm[:, pb, 0:ncol], lhsT=hT[:, k, i * 128:(i + 1) * 128], rhs=wt[:, wb, k, 0:ncol],
                        start=(k == 0), stop=(k == 15)), reads=[("wt", wb), ("hT", i // 4)], writes=[("ptm", pb)])
                if dcol is None:
                    S.op("dve", lambda e, pb=pb, i=i: e.tensor_copy(out=ngs[:, i, :], in_=ptm[:, pb, 0:24]), reads=[("ptm", pb)], writes=["ngs"])
                else:
                    eng = "act" if i % 2 == 0 else "dve"
                    if eng == "act":
                        S.op("act", lambda e, pb=pb, wb=wb, i=i: e.copy(out=stgt[:, wb, i, :], in_=ptm[:, pb, :]), reads=[("ptm", pb)], writes=[("stgt", wb)])
                    else:
                        S.op("dve", lambda e, pb=pb, wb=wb, i=i: e.tensor_copy(out=stgt[:, wb, i, :], in_=ptm[:, pb, :]), reads=[("ptm", pb)], writes=[("stgt", wb)])
            if dcol is not None:
                S.dma("sp", lambda e, wb=wb, dcol=dcol: e.dma_start(out=ptm_v[:, :, dcol:dcol + 512], in_=stgt[:, wb]),
                      reads=[("stgt", wb)], writes=["PTM"])
        dbgdump("d_ngs", ngs[:], "ngs")
        S.barrier()


def stage_D(nc, S, io, CST, PFM, PTM, ocatT, identb):
    sb, ps = _namers(nc)
    ptm_v = PTM.rearrange("(i p) c -> p i c", p=128)
    with ExitStack() as _es:
        qT = _es.enter_context(sb("qT", [128, 2, T], BF16))
        kT = _es.enter_context(sb("kT", [128, 2, T], BF16))
        kz = _es.enter_context(sb("kz", [128, 16, 256], BF16))
        v = _es.enter_context(sb("v", [128, 16, 256], BF16))
        g = _es.enter_context(sb("g", [128, 16, 256], BF16))
        dm = _es.enter_context(sb("dm", [128, 4, 128], F32))
        zeta = _es.enter_context(sb("zeta", [128, 4], F32))
        qdec = _es.enter_context(sb("qdec", [128, 4], F32))
        gng = _es.enter_context(sb("gng", [128, 1024], F32))
        Sf = _es.enter_context(sb("Sf", [128, 2, 256], F32))
        Sb = _es.enter_context(sb("Sb", [128, 2, 256], BF16))
        sTm = _es.enter_context(sb("sTm", [128, 2, 128], BF16))
        o = _es.enter_context(sb("o", [128, 2, 256], F32))
        on = _es.enter_context(sb("on", [128, 2, 256], F32))
        sg = _es.enter_context(sb("sg", [128, 2, 256], F32))
        ob = _es.enter_context(sb("ob", [128, 2, 256], BF16))
        st = _es.enter_context(sb("st", [128, 2, 6], F32))
        junkd = _es.enter_context(sb("junkd", [128, 256], BF16))
        mv = _es.enter_context(sb("mv", [128, 2, 2], F32))
        rg = _es.enter_context(sb("rg", [128, 2], F32))
        sT_ps = _es.enter_context(ps("sT_ps", [128, 2, 512], F32))
        o_ps = _es.enter_context(ps("o_ps", [128, 2, 512], F32))
        kv_ps = _es.enter_context(ps("kv_ps", [128, 2, 256], F32))
        tpk = _es.enter_context(ps("tpk", [128, 2, 1024], BF16))
        S.dma("sp", lambda e: e.dma_start(out=dm[:], in_=io["dm"]), writes=["dm"])
        S.dma("sp", lambda e: e.dma_start(out=zeta[:], in_=io["zeta"]), writes=["zeta"])
        S.dma("sp", lambda e: e.dma_start(out=qdec[:], in_=io["qdec"]), writes=["qdec"])
        S.dma("sp", lambda e: e.dma_start(out=gng[:], in_=io["gng_row"]), writes=["gng"])
        for h in range(4):
            cd = CST["_cd"][h]
            S.dma("sp", lambda e, h=h: e.dma_start(out=qT[:], in_=PFM[2 * h:2 * h + 2].rearrange("c p t -> p c t")), reads=["PFM"], writes=["qT"])
            S.dma("sp", lambda e, h=h: e.dma_start(out=kT[:], in_=PFM[8 + 2 * h:10 + 2 * h].rearrange("c p t -> p c t")), reads=["PFM"], writes=["kT"])
            S.dma("sp", lambda e, h=h: e.dma_start(out=v[:], in_=ptm_v[:, :, h * 256:(h + 1) * 256]), reads=["PTM"], writes=["v"])
            S.dma("sp", lambda e, h=h: e.dma_start(out=g[:], in_=ptm_v[:, :, 1024 + h * 256:1024 + (h + 1) * 256]), reads=["PTM"], writes=["g"])
            for i in range(16):
                pb = i % 2
                for half in range(2):
                    S.op("pe", lambda e, i=i, pb=pb, half=half: e.transpose(tpk[:, pb, half * 128:(half + 1) * 128], kT[:, half, i * 128:(i + 1) * 128], identb[:]),
                         reads=["kT", "identb"], writes=[("tpk", pb)])
                S.op("act", lambda e, i=i, pb=pb, h=h: e.activation(out=kz[:, i, :].rearrange("p (a b) -> p a b", a=2), in_=tpk[:, pb, 0:256].rearrange("p (a b) -> p a b", a=2), func=AF.Identity,
                                                                    scale=zeta[:, h:h + 1]), reads=[("tpk", pb), "zeta"], writes=["kz"])
            S.op("dve", lambda e: e.memset(Sf[:], 0.0), writes=["Sf"])
            S.op("dve", lambda e: e.memset(Sb[:], 0.0), writes=["Sb"])
            for n in range(16):
                b = n % 2
                csl = slice(n * 128, (n + 1) * 128)
                for half in range(2):
                    S.op("pe", lambda e, b=b, half=half, csl=csl: e.matmul(sT_ps[:, b, 0:128], lhsT=kT[:, half, csl], rhs=qT[:, half, csl],
                                                                           start=(half == 0), stop=(half == 1)), reads=["kT", "qT"], writes=[("sT_ps", b)])
                S.op("dve", lambda e, b=b, h=h: e.tensor_tensor(out=sTm[:, b, :], in0=sT_ps[:, b, 0:128], in1=dm[:, h, :], op=ALU.mult),
                     reads=[("sT_ps", b), "dm"], writes=[("sTm", b)])
                S.op("pe", lambda e, b=b, n=n: e.matmul(o_ps[:, b, 0:256], lhsT=sTm[:, b, :], rhs=v[:, n, :], start=True, stop=(n == 0)),
                     reads=[("sTm", b), "v"], writes=[("o_ps", b)])
                if n > 0:
                    for half in range(2):
                        S.op("pe", lambda e, b=b, half=half, csl=csl: e.matmul(o_ps[:, b, 0:256], lhsT=qT[:, half, csl], rhs=Sb[:, half, :],
                                                                               start=False, stop=(half == 1)), reads=["qT", "Sb"], writes=[("o_ps", b)])
                if n < 15 and "state" not in os.environ.get("K_DSKIP", ""):
                    for half in range(2):
                        S.op("pe", lambda e, n=n, half=half: e.matmul(kv_ps[:, half, :], lhsT=kz[:, n, half * 128:(half + 1) * 128], rhs=v[:, n, :],
                                                                      start=True, stop=True), reads=["kz", "v"], writes=["kv_ps"])
                    S.op("dve", lambda e, cd=cd: e.scalar_tensor_tensor(out=Sf[:], in0=Sf[:], scalar=cd, in1=kv_ps[:], op0=ALU.mult, op1=ALU.add),
                         reads=["Sf", "kv_ps"], writes=["Sf"])
                    S.op("act", lambda e: e.copy(out=Sb[:], in_=Sf[:]), reads=["Sf"], writes=["Sb"])
                if "epi" in os.environ.get("K_DSKIP", ""):
                    continue
                S.op("dve", lambda e, b=b, h=h: e.tensor_scalar(out=o[:, b, :], in0=o_ps[:, b, 0:256], scalar1=qdec[:, h:h + 1], scalar2=None, op0=ALU.mult),
                     reads=[("o_ps", b), "qdec"], writes=[("o", b)])
                S.op("dve", lambda e, b=b: e.tensor_reduce(out=mv[:, b, 0:1], in_=o[:, b, :], axis=AX.X, op=ALU.add), reads=[("o", b)], writes=[("mv", b)])
                S.op("dve", lambda e, b=b: e.tensor_scalar(out=mv[:, b, 1:2], in0=mv[:, b, 0:1], scalar1=-1.0 / 256.0, scalar2=None, op0=ALU.mult),
                     reads=[("mv", b)], writes=[("mv1", b)])
                S.op("act", lambda e, b=b: e.activation(out=on[:, b, :], in_=o[:, b, :], func=AF.Identity, bias=mv[:, b, 1:2], scale=1.0),
                     reads=[("o", b), ("mv1", b)], writes=[("on", b)])
                S.op("act", lambda e, b=b: e.activation(out=junkd[:], in_=on[:, b, :], func=AF.Square, accum_out=st[:, b, 0:1]),
                     reads=[("on", b)], writes=["junkd", ("st", b)])
                S.op("act", lambda e, b=b: e.activation(out=rg[:, b:b + 1], in_=st[:, b, 0:1], func=AF.Sqrt, bias=epsc[:, 0:1], scale=1.0 / 256.0),
                     reads=[("st", b), "epsc"], writes=[("rg", b)])
                S.op("dve", lambda e, b=b: e.reciprocal(out=rg[:, b:b + 1], in_=rg[:, b:b + 1]), reads=[("rg", b)], writes=[("rg", b)])
                S.op("dve", lambda e, b=b, h=h: e.scalar_tensor_tensor(out=o[:, b, :], in0=on[:, b, :], scalar=rg[:, b:b + 1], in1=gng[:, h * 256:(h + 1) * 256],
                                                                       op0=ALU.mult, op1=ALU.mult), reads=[("on", b), ("rg", b), "gng"], writes=[("o", b)])
                S.op("act", lambda e, b=b, n=n: e.activation(out=sg[:, b, :], in_=g[:, n, :], func=AF.Silu), reads=["g"], writes=[("sg", b)])
                S.op("pool", lambda e, b=b: e.tensor_tensor(out=ob[:, b, :], in0=o[:, b, :], in1=sg[:, b, :], op=ALU.mult),
                     reads=[("o", b), ("sg", b)], writes=[("ob", b)])
                for half in range(2):
                    S.op("pe", lambda e, b=b, half=half: e.transpose(tpk[:, b, half * 128:(half + 1) * 128], ob[:, b, half * 128:(half + 1) * 128], identb[:]),
                         reads=[("ob", b), "identb"], writes=[("tpk", b)])
                S.op("act", lambda e, b=b, h=h, csl=csl: e.copy(out=ocatT[:, 2 * h:2 * h + 2, csl], in_=tpk[:, b, 0:256].rearrange("p (a b) -> p a b", a=2)), reads=[("tpk", b)], writes=["ocatT"])
        S.barrier()


def stage_E(nc, S, io, PFM, PTM, ocatT, identb, ngs):
    sb, ps = _namers(nc)
    ptm_v = PTM.rearrange("(i p) c -> p i c", p=128)
    with ExitStack() as _es:
        w1k = _es.enter_context(sb("w1k", [128, 32, 256], BF16))
        w1v = _es.enter_context(sb("w1v", [128, 32, 256], BF16))
        w2k = _es.enter_context(sb("w2k", [128, 2, 128], BF16))
        w2v = _es.enter_context(sb("w2v", [128, 2, 128], BF16))
        pek = _es.enter_context(sb("pek", [128, 32], BF16))
        pev = _es.enter_context(sb("pev", [128, 32], BF16))
        hb = _es.enter_context(sb("hb", [128, 2, 2], F32))
        caus = _es.enter_context(sb("caus", [128, 128], BF16))
        anti = _es.enter_context(sb("anti", [128, 128], BF16))
        cmask = _es.enter_context(sb("cmask", [128, T], BF16))
        overlap = _es.enter_context(sb("overlap", [128, 32], BF16))
        esel = _es.enter_context(sb("esel", [32, 16, 128], BF16))
        valid = _es.enter_context(sb("valid", [128, 16, 32], F32))
        ctab = _es.enter_context(sb("ctab", [128, 16, 32], F32))
        qT4 = _es.enter_context(sb("qT4", [128, 4, T], BF16))
        kcT2 = _es.enter_context(sb("kcT2", [128, 2, T], BF16))
        vcT2 = _es.enter_context(sb("vcT2", [128, 2, T], BF16))
        ksT = _es.enter_context(sb("ksT", [128, T], BF16))
        kwT = _es.enter_context(sb("kwT", [128, T], BF16))
        vsa = _es.enter_context(sb("vsa", [128, 16, 130], BF16))
        vwa = _es.enter_context(sb("vwa", [128, 16, 130], BF16))
        hid = _es.enter_context(sb("hid", [128, 4, 2, 128], BF16))
        kc16 = _es.enter_context(sb("kc16", [128, 2, 16, 130], BF16))
        kcmpT2 = _es.enter_context(sb("kcmpT2", [128, 2, 128], BF16))
        vcaug2 = _es.enter_context(sb("vcaug2", [128, 2, 162], BF16))
        ocmp = _es.enter_context(sb("ocmp", [128, 16, 4, 128], BF16))
        gts = _es.enter_context(sb("gts", [128, 16, 12], F32))
        selnegT = _es.enter_context(sb("selnegT", [32, T], BF16))
        _sk = os.environ.get("K_EPRO", "")
        if "c" not in _sk:
            for nm, t in [("caus", caus), ("anti", anti), ("cmask", cmask), ("overlap", overlap), ("esel", esel), ("valid", valid), ("ctab", ctab)]:
                S.dma("sp", lambda e, nm=nm, t=t: e.dma_start(out=t[:], in_=io[nm]), writes=[nm])
        if "w" not in _sk:
            for lh in range(2):
                S.dma("pool", lambda e, lh=lh: e.dma_start(out=w1k[:, lh * 16:(lh + 1) * 16, :], in_=io["w1_k"].rearrange("(l p) n -> p l n", p=128)[:, lh * 16:(lh + 1) * 16, :]), writes=["w1k"])
                S.dma("pool", lambda e, lh=lh: e.dma_start(out=w1v[:, lh * 16:(lh + 1) * 16, :], in_=io["w1_v"].rearrange("(l p) n -> p l n", p=128)[:, lh * 16:(lh + 1) * 16, :]), writes=["w1v"])
            S.dma("pool", lambda e: e.dma_start(out=w2k[:], in_=io["w2_k"].rearrange("(c p) n -> p c n", p=128)), writes=["w2k"])
            S.dma("pool", lambda e: e.dma_start(out=w2v[:], in_=io["w2_v"].rearrange("(c p) n -> p c n", p=128)), writes=["w2v"])
            S.dma("pool", lambda e: e.dma_start(out=pek[:], in_=io["peT_k"]), writes=["pek"])
            S.dma("pool", lambda e: e.dma_start(out=pev[:], in_=io["peT_v"]), writes=["pev"])

        with ps("hb_ps", [128, 2, 2], F32) as hb_ps:
            for kv, (w1, pe) in enumerate([(w1k, pek), (w1v, pev)]):
                for hc in range(2):
                    for l in range(32):
                        S.op("pe", lambda e, kv=kv, hc=hc, l=l, w1=w1, pe=pe: e.matmul(hb_ps[:, kv, hc:hc + 1], lhsT=w1[:, l, hc * 128:(hc + 1) * 128],
                                                                                     rhs=pe[:, l:l + 1], start=(l == 0), stop=(l == 31)),
                             reads=["w1k", "w1v", "pek", "pev"], writes=["hb_ps"])
            if "h" not in _sk:
                S.op("dve", lambda e: e.tensor_copy(out=hb[:], in_=hb_ps[:]), reads=["hb_ps"], writes=["hb"])
            S.barrier()

        for _ in range(int(os.environ.get("K_PENOP", "0"))):
            nc.tensor.wait_ge(S.sems["dve"], 0)
        S.op("dve", lambda e: e.memset(kc16[:], 0.0), writes=[("kc16", 0), ("kc16", 1)])
        S.op("dve", lambda e: e.memset(vcaug2[:], 0.0), writes=["vcaug"])
        with ExitStack() as _es2:
            hid_ps = _es2.enter_context(ps("hid_ps", [128, 4, 2, 128], F32))
            cmp_ps = _es2.enter_context(ps("cmp_ps", [128, 2, 512], F32))
            S.dma("sp", lambda e: e.dma_start(out=kcT2[:], in_=PFM[24:26].rearrange("c p t -> p c t")), reads=["PFM"], writes=["kcT2"])
            S.dma("sp", lambda e: e.dma_start(out=vcT2[:], in_=PFM[26:28].rearrange("c p t -> p c t")), reads=["PFM"], writes=["vcT2"])
            for u in range(4):
                kv, gg = u // 2, u % 2
                w1 = w1k if kv == 0 else w1v
                src = kcT2 if kv == 0 else vcT2
                nm = "kcT2" if kv == 0 else "vcT2"
                kb = u % 2
                S.op("dve" if u % 2 == 0 else "pool", lambda e, kb=kb, src=src, gg=gg: e.tensor_copy(
                    out=kc16[:, kb, :, 0:128], in_=src[:, gg, :].rearrange("p (c r) -> p r c", r=16)), reads=[nm], writes=[("kc16", kb)])
                for hc in range(2):
                    for l in range(32):
                        S.op("pe", lambda e, u=u, kb=kb, hc=hc, l=l, w1=w1: e.matmul(
                            hid_ps[:, u, hc, :], lhsT=w1[:, l, hc * 128:(hc + 1) * 128], rhs=kc16[:, kb, l % 16, (l // 16):(l // 16) + 128],
                            start=(l == 0), stop=(l == 31)), reads=["w1k", "w1v", ("kc16", kb)], writes=[("hid_ps", u // 2)])
                    S.op("act", lambda e, u=u, kv=kv, hc=hc: e.activation(out=hid[:, u, hc, :], in_=hid_ps[:, u, hc, :], func=AF.Silu,
                                                                     bias=hb[:, kv, hc:hc + 1]), reads=[("hid_ps", u // 2), "hb"], writes=["hid"])
            for gg in range(2):
                for hc in range(2):
                    S.op("pe", lambda e, gg=gg, hc=hc: e.matmul(cmp_ps[:, 0, gg * 128:(gg + 1) * 128], lhsT=w2k[:, hc, :], rhs=hid[:, gg, hc, :], start=(hc == 0), stop=(hc == 1)),
                         reads=["w2k", "hid"], writes=["cmp_ps0"])
            for gg in range(2):
                for hc in range(2):
                    S.op("pe", lambda e, gg=gg, hc=hc: e.matmul(cmp_ps[:, 1, gg * 128:(gg + 1) * 128], lhsT=hid[:, 2 + gg, hc, :], rhs=w2v[:, hc, :], start=(hc == 0), stop=(hc == 1)),
                         reads=["w2v", "hid"], writes=["cmp_ps1"])
            _cs = os.environ.get("K_CONS", "")
            if "A" not in _cs:
                S.op("act", lambda e: e.copy(out=kcmpT2[:], in_=cmp_ps[:, 0, 0:256].rearrange("p (a b) -> p a b", a=2)), reads=["cmp_ps0"], writes=["kcmpT"])
            if "V" not in _cs:
                S.op("dve", lambda e: e.tensor_copy(out=vcaug2[:, :, 0:128], in_=cmp_ps[:, 1, 0:256].rearrange("p (a b) -> p a b", a=2)), reads=["cmp_ps1", "vcaug"], writes=["vcaug"])
            if "M" not in _cs:
                S.op("dve", lambda e: e.memset(vcaug2[:, :, 128:129], 1.0), reads=["vcaug"], writes=["vcaug"])
            for gg in range(2 if "O" not in _cs else 0):
                S.op("dve", lambda e, gg=gg: e.tensor_copy(out=vcaug2[:, gg, 129:161], in_=overlap[:]), reads=["overlap", "vcaug"], writes=["vcaug"])
            S.barrier()
        if "cmp" in os.environ.get("K_ESKIP", ""):
            return
        for g in ([int(c) for c in os.environ["K_GSEL"]] if os.environ.get("K_GSEL") else range(2)):
            _ld = os.environ.get("K_ELD", "")
            if "q" not in _ld:
                S.dma("sp", lambda e, g=g: e.dma_start(out=qT4[:], in_=PFM[16 + 4 * g:20 + 4 * g].rearrange("c p t -> p c t")), reads=["PFM"], writes=["qT4"])
            for nm, t, ch in [("ksT", ksT, 28), ("kwT", kwT, 30)]:
                S.dma("sp", lambda e, t=t, ch=ch, g=g: e.dma_start(out=t[:], in_=PFM[ch + g]), reads=["PFM"], writes=[nm])
            if "a" not in _ld:
                S.dma("sp", lambda e, g=g: e.dma_start(out=vsa[:, :, 0:128], in_=ptm_v[:, :, 2048 + g * 128:2048 + (g + 1) * 128]), reads=["PTM"], writes=["vsa"])
                S.dma("sp", lambda e, g=g: e.dma_start(out=vwa[:, :, 0:128], in_=ptm_v[:, :, 2304 + g * 128:2304 + (g + 1) * 128]), reads=["PTM"], writes=["vwa"])
            if "m" not in _sk:
                S.op("dve", lambda e: e.memset(vsa[:, :, 128:130], 1.0), reads=["vsa"], writes=["vsa"])
                S.op("dve", lambda e: e.memset(vwa[:, :, 128:130], 1.0), reads=["vwa"], writes=["vwa"])
            if "e2a" in os.environ.get("K_ESKIP", ""):
                continue
            with ExitStack() as _es:
                sc_ps = _es.enter_context(ps("sc_ps", [128, 2, 512], F32))
                oc_ps = _es.enter_context(ps("oc_ps", [128, 4, 256], F32))
                tps = _es.enter_context(ps("tps", [32, 2, 128], BF16))
                pc = _es.enter_context(sb("pc", [128, 2, 512], BF16))
                rc = _es.enter_context(sb("rc", [128, 4], F32))
                imp = _es.enter_context(sb("imp", [128, 32], F32))
                score = _es.enter_context(sb("score", [128, 32], F32))
                sc2 = _es.enter_context(sb("sc2", [128, 32], F32))
                m8 = _es.enter_context(sb("m8", [128, 16], F32))
                selneg = _es.enter_context(sb("selneg", [128, 32], BF16))
                coef = _es.enter_context(sb("coef", [128, 4], F32))
                for qt in range(16):
                    b = qt % 2
                    qsl = slice(qt * 128, (qt + 1) * 128)
                    S.op("pe", lambda e, b=b, qsl=qsl: e.matmul(sc_ps[:, b, :].rearrange("p (h t) -> p h t", h=4), lhsT=kcmpT2[:, g, :], rhs=qT4[:, :, qsl],
                                                                start=True, stop=False), reads=["kcmpT", "qT4"], writes=[("sc_ps", b)])
                    S.op("pe", lambda e, b=b, qsl=qsl: e.matmul(sc_ps[:, b, :].rearrange("p (h t) -> p h t", h=4), lhsT=identb[:, :],
                                                                rhs=cmask[:, qsl].unsqueeze(1).to_broadcast([128, 4, 128]), start=False, stop=True),
                         reads=["identb", "cmask"], writes=[("sc_ps", b)])
                    S.op("act", lambda e, b=b: e.activation(out=pc[:, b, :], in_=sc_ps[:, b, :], func=AF.Exp), reads=[("sc_ps", b)], writes=[("pc", b)])
                    for h in range(4):
                        S.op("pe", lambda e, b=b, h=h: e.matmul(oc_ps[:, h, 0:162], lhsT=pc[:, b, h * 128:(h + 1) * 128], rhs=vcaug2[:, g, 0:162],
                                                                start=True, stop=True), reads=[("pc", b), "vcaug"], writes=["oc_ps"])
                    for hh in (0, 2):
                        S.op("dve", lambda e, hh=hh: e.tensor_scalar(out=rc[:, hh:hh + 2], in0=oc_ps[:, hh:hh + 2, 128], scalar1=1e-30, scalar2=None, op0=ALU.max),
                             reads=["oc_ps"], writes=["rc"])
                    S.op("dve", lambda e: e.reciprocal(out=rc[:], in_=rc[:]), reads=["rc"], writes=["rc"])
                    S.op("act", lambda e, qt=qt, g=g: e.activation(out=gts[:, qt, :], in_=ngs[:, qt, g * 12:(g + 1) * 12], func=AF.Sigmoid), reads=["ngs"], writes=["gts"])
                    for h in range(4):
                        if h == 0:
                            S.op("dve", lambda e: e.tensor_scalar(out=imp[:], in0=oc_ps[:, 0, 129:161], scalar1=rc[:, 0:1], scalar2=None, op0=ALU.mult),
                                 reads=["oc_ps", "rc"], writes=["imp"])
                        else:
                            S.op("dve", lambda e, h=h: e.scalar_tensor_tensor(out=imp[:], in0=oc_ps[:, h, 129:161], scalar=rc[:, h:h + 1], in1=imp[:],
                                                                              op0=ALU.mult, op1=ALU.add), reads=["oc_ps", "rc", "imp"], writes=["imp"])
                    S.op("dve", lambda e, qt=qt: e.tensor_tensor(out=coef[:], in0=rc[:], in1=gts[:, qt, 0:12:3], op=ALU.mult), reads=["rc", "gts"], writes=["coef"])
                    for h in range(4):
                        S.op("dve", lambda e, h=h, qt=qt: e.tensor_scalar(out=ocmp[:, qt, h, :], in0=oc_ps[:, h, 0:128], scalar1=coef[:, h:h + 1], scalar2=None,
                                                                          op0=ALU.mult), reads=["oc_ps", "coef"], writes=["ocmp"])
                    S.op("dve", lambda e, qt=qt: e.tensor_tensor(out=score[:], in0=imp[:], in1=valid[:, qt, :], op=ALU.mult), reads=["imp", "valid"], writes=["score"])
                    S.op("dve", lambda e, qt=qt: e.tensor_tensor(out=score[:], in0=score[:], in1=ctab[:, qt, :], op=ALU.add), reads=["score", "ctab"], writes=["score"])
                    S.op("dve", lambda e: e.max(out=m8[:, 0:8], in_=score[:]), reads=["score"], writes=["m8a"])
                    S.op("dve", lambda e: e.match_replace(out=sc2[:], in_to_replace=m8[:, 0:8], in_values=score[:], imm_value=-2.0),
                         reads=["score", "m8a"], writes=["sc2"])
                    S.op("dve", lambda e: e.max(out=m8[:, 8:16], in_=sc2[:]), reads=["sc2"], writes=["m8b"])
                    S.op("dve", lambda e: e.tensor_scalar(out=selneg[:], in0=score[:], scalar1=m8[:, 15:16], scalar2=NEG, op0=ALU.is_lt, op1=ALU.mult),
                         reads=["score", "m8b"], writes=["selneg"])
                    S.op("pe", lambda e, b=b: e.transpose(tps[:, 0, :], selneg[:], identb[:]), reads=["selneg", "identb"], writes=["tps"])
                    S.op("act", lambda e, b=b, qsl=qsl: e.copy(out=selnegT[:, qsl], in_=tps[:, 0, :]), reads=["tps"], writes=["selnegT"])
                S.barrier()
            if "e2b" in os.environ.get("K_ESKIP", ""):
                continue
            with ExitStack() as _es:
                ss_ps = _es.enter_context(ps("ss_ps", [128, 2, 512], F32))
                os_ps = _es.enter_context(ps("os_ps", [128, 4, 256], F32))
                ow_ps = _es.enter_context(ps("ow_ps", [128, 4, 256], F32))
                tpo = _es.enter_context(ps("tpo", [128, 4, 128], BF16))
                pp = _es.enter_context(sb("pp", [128, 2, 512], BF16))
                rs = _es.enter_context(sb("rs", [128, 4], F32))
                rw = _es.enter_context(sb("rw", [128, 4], F32))
                acc = _es.enter_context(sb("acc", [128, 4, 128], F32))
                ob4 = _es.enter_context(sb("ob4", [128, 4, 128], BF16))
                n = 0
                for qt in range(16):
                    qsl = slice(qt * 128, (qt + 1) * 128)
                    for br, (kTt, knm, va, vnm, o_ps, kts) in enumerate([
                            (ksT, "ksT", vsa, "vsa", os_ps, list(range(0, qt + 1))),
                            (kwT, "kwT", vwa, "vwa", ow_ps, list(range(max(0, qt - 4), qt + 1)))]):
                        opk = "os_ps" if br == 0 else "ow_ps"
                        for kt in kts:
                            b = n % 2
                            n += 1
                            ksl = slice(kt * 128, (kt + 1) * 128)
                            extra = []
                            if br == 0:
                                extra.append((esel[:, kt, :], selnegT[:, qsl].unsqueeze(1).to_broadcast([32, 4, 128]), ["esel", "selnegT"]))
                            if kt == qt:
                                extra.append((identb[:], caus[:].unsqueeze(1).to_broadcast([128, 4, 128]), ["identb", "caus"]))
                            if br == 1 and kt == qt - 4:
                                extra.append((identb[:], anti[:].unsqueeze(1).to_broadcast([128, 4, 128]), ["identb", "anti"]))
                            outv = ss_ps[:, b, :].rearrange("p (h t) -> p h t", h=4)
                            S.op("pe", lambda e, outv=outv, kTt=kTt, ksl=ksl, qsl=qsl, last=(len(extra) == 0): e.matmul(
                                outv, lhsT=kTt[:, ksl], rhs=qT4[:, :, qsl], start=True, stop=last), reads=[knm, "qT4"], writes=[("ss_ps", b)])
                            for xi, (l_, r_, rd) in enumerate(extra):
                                S.op("pe", lambda e, outv=outv, l_=l_, r_=r_, last=(xi == len(extra) - 1): e.matmul(outv, lhsT=l_, rhs=r_, start=False, stop=last),
                                     reads=rd, writes=[("ss_ps", b)])
                            S.op("act", lambda e, b=b: e.activation(out=pp[:, b, :], in_=ss_ps[:, b, :], func=AF.Exp), reads=[("ss_ps", b)], writes=[("pp", b)])
                            for h in range(4):
                                S.op("pe", lambda e, b=b, h=h, kt=kt, va=va, o_ps=o_ps, first=(kt == kts[0]), lastk=(kt == kts[-1]): e.matmul(
                                    o_ps[:, h, 0:130], lhsT=pp[:, b, h * 128:(h + 1) * 128], rhs=va[:, kt, :], start=(first and h % 2 == 0), stop=lastk, skip_group_check=True),
                                    reads=[("pp", b), vnm], writes=[opk])
                    for hh in (0, 2):
                        S.op("dve", lambda e, hh=hh: e.reciprocal(out=rs[:, hh:hh + 2], in_=os_ps[:, hh:hh + 2, 128]), reads=["os_ps"], writes=["rs"])
                        S.op("dve", lambda e, hh=hh: e.reciprocal(out=rw[:, hh:hh + 2], in_=ow_ps[:, hh:hh + 2, 128]), reads=["ow_ps"], writes=["rw"])
                    S.op("dve", lambda e, qt=qt: e.tensor_tensor(out=rs[:], in0=rs[:], in1=gts[:, qt, 1:12:3], op=ALU.mult), reads=["rs", "gts"], writes=["rs"])
                    S.op("dve", lambda e, qt=qt: e.tensor_tensor(out=rw[:], in0=rw[:], in1=gts[:, qt, 2:12:3], op=ALU.mult), reads=["rw", "gts"], writes=["rw"])
                    for h in range(4):
                        S.op("dve", lambda e, h=h: e.tensor_scalar(out=acc[:, h, :], in0=os_ps[:, h, 0:128], scalar1=rs[:, h:h + 1], scalar2=None, op0=ALU.mult),
                             reads=["os_ps", "rs"], writes=["acc"])
                        S.op("dve", lambda e, h=h: e.scalar_tensor_tensor(out=acc[:, h, :], in0=ow_ps[:, h, 0:128], scalar=rw[:, h:h + 1], in1=acc[:, h, :],
                                                                          op0=ALU.mult, op1=ALU.add), reads=["ow_ps", "rw", "acc"], writes=["acc"])
                    S.op("pool", lambda e, qt=qt: e.tensor_tensor(out=ob4[:], in0=acc[:], in1=ocmp[:, qt], op=ALU.add), reads=["acc", "ocmp"], writes=["ob4"])
                    for h in range(4):
                        S.op("pe", lambda e, h=h: e.transpose(tpo[:, h, :], ob4[:, h, :], identb[:]), reads=["ob4", "identb"], writes=["tpo"])
                    S.op("act", lambda e, g=g, qsl=qsl: e.copy(out=ocatT[:, 8 + 4 * g:12 + 4 * g, qsl], in_=tpo[:]), reads=["tpo"], writes=["ocatT"])
                S.barrier()
        S.barrier()


def stage_F(nc, S, io, ocatT, g1row, X1):
    sb, ps = _namers(nc)
    wout = io["w_out"].rearrange("(k p) n -> p k n", p=128)
    with ExitStack() as _es:
        wo = _es.enter_context(sb("wo", [128, 2, 16, 512], BF16))
        xr = _es.enter_context(sb("xr", [128, 2, 512], F32))
        x1t = _es.enter_context(sb("x1t", [128, 2, 512], F32))
        mx_ps = _es.enter_context(ps("mx_ps", [128, 2, 512], F32))
        n = 0
        for cb in range(4):
            wb = cb % 2
            csl = slice(cb * 512, (cb + 1) * 512)
            S.dma("pool", lambda e, wb=wb, csl=csl: e.dma_start(out=wo[:, wb], in_=wout[:, :, csl]), writes=[("wo", wb)])
            for i in range(16):
                b = n % 2
                n += 1
                isl = slice(i * 128, (i + 1) * 128)
                S.dma("sp", lambda e, b=b, isl=isl, csl=csl: e.dma_start(out=xr[:, b, :], in_=io["x"][isl, csl]), writes=[("xr", b)])
                for k in range(16):
                    S.op("pe", lambda e, b=b, wb=wb, k=k, isl=isl: e.matmul(mx_ps[:, b, :], lhsT=ocatT[:, k, isl], rhs=wo[:, wb, k, :],
                                                                            start=(k == 0), stop=(k == 15)), reads=["ocatT", ("wo", wb)], writes=[("mx_ps", b)])
                S.op("dve", lambda e, b=b, csl=csl: e.tensor_tensor(out=x1t[:, b, :], in0=mx_ps[:, b, :], in1=g1row[:, csl], op=ALU.mult),
                     reads=[("mx_ps", b), "g1row"], writes=[("x1t", b)])
                S.op("pool", lambda e, b=b: e.tensor_tensor(out=x1t[:, b, :], in0=x1t[:, b, :], in1=xr[:, b, :], op=ALU.add),
                     reads=[("x1t", b), ("xr", b)], writes=[("x1t", b)])
                S.dma("sp", lambda e, b=b, isl=isl, csl=csl: e.dma_start(out=X1[isl, csl], in_=x1t[:, b, :]), reads=[("x1t", b)], writes=["X1"])
        S.barrier()


def stage_G(nc, S, io, X1, XS, a2row, sh2row, identf, slots, wab, dbgout, final_toks):
    sb, ps = _namers(nc)
    with ExitStack() as _es:
        A_ = _es.enter_context
        x1 = A_(sb("x1", [128, 2, D], F32)); h2f = A_(sb("h2f", [128, D], F32)); h2b = A_(sb("h2b", [128, 16, D], BF16))
        junk = A_(sb("junk2", [128, D], BF16)); h2Tf = A_(sb("h2Tf", [128, 16, 128], F32))
        wr = A_(sb("wr", [128, 16, 72], F32)); brt = A_(sb("brt", [128, 72], F32))
        lstrict = A_(sb("lstrict", [128, 128], BF16)); ones = A_(sb("ones", [128, 128], BF16)); ebase = A_(sb("ebase", [128, 64], F32))
        ss2 = A_(sb("ss2", [128, 16], F32)); rstd2 = A_(sb("rstd2", [128, 16], F32))
        lg = A_(sb("lg", [128, 16, 72], F32)); gm0 = A_(sb("gm0", [128, 16], F32)); dg = A_(sb("dg", [128, 16, 8], F32))
        eg = A_(sb("eg", [128, 16, 8], F32)); gsum = A_(sb("gsum", [128, 16], F32)); onehot = A_(sb("onehot", [128, 16, 8], F32))
        tmp = A_(sb("tmp88", [128, 16, 8, 8], F32)); leg = A_(sb("leg", [128, 16, 8], F32)); m8r = A_(sb("m8r", [128, 16, 8], F32))
        selloc = A_(sb("selloc", [128, 16, 8], F32)); wl = A_(sb("wl", [128, 16, 8], F32)); den = A_(sb("den", [128, 16], F32))
        wfull = A_(sb("wfull", [128, 16, 64], F32)); Af = A_(sb("Af", [128, 16, 64], F32)); Ab = A_(sb("Ab", [128, 16, 64], BF16))
        tot = A_(sb("tot", [128, 64], F32)); cnt = A_(sb("cnt", [128, 16, 64], F32)); key = A_(sb("key", [128, 16, 64], F32))
        m8k = A_(sb("m8k", [128, 16, 8], F32)); slf = A_(sb("slf", [128, 16, 2], F32)); eq = A_(sb("eq", [128, 16, 64], F32))
        tpf = A_(ps("tpf", [128, 2, 4, 128], F32)); lg_ps = A_(ps("lg_ps", [128, 2, 512], F32)); cnt_ps = A_(ps("cnt_ps", [128, 2, 512], F32))
        S.dma("sp", lambda e: e.dma_start(out=wr[:], in_=io["w_rt"].rearrange("(k p) n -> p k n", p=128)), writes=["wr"])
        for nm, t in [("b_rt", brt), ("lstrict", lstrict), ("ones", ones), ("ebase", ebase)]:
            S.dma("sp", lambda e, nm=nm, t=t: e.dma_start(out=t[:], in_=io[nm]), writes=[nm])
        S.op("dve", lambda e: e.memset(ss2[:], 0.0), writes=["ss2"])
        S.op("dve", lambda e: e.memset(tot[:], 0.0), writes=["tot"])
        V = lambda fn, r, w: S.op("dve", fn, reads=r, writes=w)
        for i in range(16):
            b = i % 2
            isl = slice(i * 128, (i + 1) * 128)
            S.dma("sp", lambda e, b=b, isl=isl: e.dma_start(out=x1[:, b], in_=X1[isl, :]), reads=["X1"], writes=[("x1", b)])
            S.op("act", lambda e, b=b, i=i: e.activation(out=junk[:], in_=x1[:, b], func=AF.Square, accum_out=ss2[:, i:i + 1]),
                 reads=[("x1", b), "ss2"], writes=["junk", ("ss2", i)])
            S.op("act", lambda e, i=i: e.activation(out=rstd2[:, i:i + 1], in_=ss2[:, i:i + 1], func=AF.Sqrt, bias=epsc[:, 0:1], scale=1.0 / D),
                 reads=[("ss2", i), "epsc"], writes=[("rstd2", i)])
            V(lambda e, i=i: e.reciprocal(out=rstd2[:, i:i + 1], in_=rstd2[:, i:i + 1]), [("rstd2", i)], [("rstd2", i)])
            V(lambda e, b=b, i=i: e.scalar_tensor_tensor(out=h2f[:], in0=x1[:, b], scalar=rstd2[:, i:i + 1], in1=a2row[:], op0=ALU.mult, op1=ALU.mult),
              [("x1", b), ("rstd2", i), "a2row"], ["h2f"])
            S.op("pool", lambda e: e.tensor_tensor(out=h2f[:], in0=h2f[:], in1=sh2row[:], op=ALU.add), reads=["h2f", "sh2row"], writes=["h2f"])
            S.op("act", lambda e, i=i: e.copy(out=h2b[:, i], in_=h2f[:]), reads=["h2f"], writes=[("h2b", i)])
            for k4 in range(4):
                pb = k4 % 2
                for kk in range(4):
                    k = k4 * 4 + kk
                    S.op("pe", lambda e, pb=pb, kk=kk, k=k: e.transpose(tpf[:, pb, kk, :], h2f[:, k * 128:(k + 1) * 128], identf[:]),
                         reads=["h2f", "identf"], writes=[("tpf", pb)])
                V(lambda e, pb=pb, k4=k4: e.tensor_copy(out=h2Tf[:, k4 * 4:(k4 + 1) * 4, :], in_=tpf[:, pb]), [("tpf", pb)], ["h2Tf"])
            for k in range(16):
                S.op("pe", lambda e, k=k, b=b: e.matmul(lg_ps[:, b, 0:72], lhsT=h2Tf[:, k, :], rhs=wr[:, k, :], start=(k == 0), stop=(k == 15)),
                     reads=["h2Tf", "wr"], writes=[("lg_ps", b)])
            V(lambda e, i=i, b=b: e.tensor_tensor(out=lg[:, i, :], in0=lg_ps[:, b, 0:72], in1=brt[:], op=ALU.add), [("lg_ps", b), "b_rt"], ["lg"])
        B8 = lambda ap: ap.unsqueeze(2).to_broadcast([128, 16, 8])
        V(lambda e: e.tensor_reduce(out=gm0[:], in_=lg[:, :, 0:8], axis=AX.X, op=ALU.max), ["lg"], ["gm0"])
        V(lambda e: e.tensor_tensor(out=dg[:], in0=lg[:, :, 0:8], in1=B8(gm0[:]), op=ALU.subtract), ["lg", "gm0"], ["dg"])
        S.op("act", lambda e: e.activation(out=eg[:], in_=dg[:], func=AF.Exp), reads=["dg"], writes=["eg"])
        V(lambda e: e.tensor_reduce(out=gsum[:], in_=eg[:], axis=AX.X, op=ALU.add), ["eg"], ["gsum"])
        V(lambda e: e.tensor_scalar(out=onehot[:], in0=dg[:], scalar1=0.0, scalar2=None, op0=ALU.is_ge), ["dg"], ["onehot"])
        V(lambda e: e.tensor_tensor(out=tmp[:], in0=lg[:, :, 8:72].rearrange("p i (g j) -> p i g j", g=8),
                                    in1=onehot[:].unsqueeze(3).to_broadcast([128, 16, 8, 8]), op=ALU.mult), ["lg", "onehot"], ["tmp"])
        V(lambda e: e.tensor_reduce(out=leg[:], in_=tmp[:].rearrange("p i g j -> p i j g"), axis=AX.X, op=ALU.add), ["tmp"], ["leg"])
        for i in range(16):
            V(lambda e, i=i: e.max(out=m8r[:, i, :], in_=leg[:, i, :]), ["leg"], ["m8r"])
        V(lambda e: e.tensor_tensor(out=selloc[:], in0=leg[:], in1=m8r[:, :, 1:2].to_broadcast([128, 16, 8]), op=ALU.is_ge), ["leg", "m8r"], ["selloc"])
        V(lambda e: e.tensor_tensor(out=dg[:], in0=leg[:], in1=m8r[:, :, 0:1].to_broadcast([128, 16, 8]), op=ALU.subtract), ["leg", "m8r", "eg", "onehot"], ["dg2"])
        S.op("act", lambda e: e.activation(out=wl[:], in_=dg[:], func=AF.Exp), reads=["dg2"], writes=["wl"])
        V(lambda e: e.tensor_tensor(out=wl[:], in0=wl[:], in1=selloc[:], op=ALU.mult), ["wl", "selloc"], ["wl"])
        V(lambda e: e.tensor_reduce(out=den[:], in_=wl[:], axis=AX.X, op=ALU.add), ["wl"], ["den"])
        V(lambda e: e.tensor_tensor(out=den[:], in0=den[:], in1=gsum[:], op=ALU.mult), ["den", "gsum"], ["den"])
        V(lambda e: e.reciprocal(out=den[:], in_=den[:]), ["den"], ["den"])
        V(lambda e: e.tensor_tensor(out=wl[:], in0=wl[:], in1=B8(den[:]), op=ALU.mult), ["wl", "den"], ["wl"])
        V(lambda e: e.tensor_tensor(out=wfull[:].rearrange("p i (g j) -> p i g j", g=8), in0=onehot[:].unsqueeze(3).to_broadcast([128, 16, 8, 8]),
                                    in1=wl[:].unsqueeze(2).to_broadcast([128, 16, 8, 8]), op=ALU.mult), ["onehot", "wl"], ["wfull"])
        V(lambda e: e.tensor_scalar(out=Af[:], in0=wfull[:], scalar1=0.0, scalar2=None, op0=ALU.is_gt), ["wfull"], ["Af"])
        V(lambda e: e.tensor_copy(out=Ab[:], in_=Af[:]), ["Af"], ["Ab"])
        for i in range(16):
            b = i % 2
            S.op("pe", lambda e, i=i, b=b: e.matmul(cnt_ps[:, b, 0:64], lhsT=lstrict[:], rhs=Ab[:, i, :], start=True, stop=True),
                 reads=["lstrict", "Ab"], writes=[("cnt_ps", b)])
            S.op("pe", lambda e, i=i, b=b: e.matmul(cnt_ps[:, b, 64:128], lhsT=ones[:], rhs=Ab[:, i, :], start=True, stop=True),
                 reads=["ones", "Ab"], writes=[("cnt_ps", b)])
            V(lambda e, i=i, b=b: e.tensor_tensor(out=cnt[:, i, :], in0=cnt_ps[:, b, 0:64], in1=tot[:], op=ALU.add), [("cnt_ps", b), "tot"], ["cnt"])
            V(lambda e, b=b: e.tensor_tensor(out=tot[:], in0=cnt_ps[:, b, 64:128], in1=tot[:], op=ALU.add), [("cnt_ps", b), "tot"], ["tot"])
        V(lambda e: e.tensor_scalar(out=cnt[:], in0=cnt[:], scalar1=float(CAP - 1), scalar2=None, op0=ALU.min), ["cnt"], ["cnt"])
        V(lambda e: e.tensor_tensor(out=key[:], in0=cnt[:], in1=ebase[:].unsqueeze(1).to_broadcast([128, 16, 64]), op=ALU.add), ["cnt", "ebase"], ["key"])
        V(lambda e: e.tensor_tensor(out=key[:], in0=key[:], in1=Af[:], op=ALU.mult), ["key", "Af"], ["key"])
        for i in range(16):
            V(lambda e, i=i: e.max(out=m8k[:, i, :], in_=key[:, i, :]), ["key"], ["m8k"])
        V(lambda e: e.tensor_scalar(out=slf[:], in0=m8k[:, :, 0:2], scalar1=-1.0, scalar2=None, op0=ALU.add), ["m8k"], ["slf"])
        V(lambda e: e.tensor_copy(out=slots[:], in_=slf[:]), ["slf"], ["slots"])
        for j in range(2):
            V(lambda e, j=j: e.tensor_tensor(out=eq[:], in0=key[:], in1=m8k[:, :, j:j + 1].to_broadcast([128, 16, 64]), op=ALU.is_equal), ["key", "m8k"], ["eq"])
            V(lambda e: e.tensor_tensor(out=eq[:], in0=eq[:], in1=wfull[:], op=ALU.mult), ["eq", "wfull"], ["eq"])
            V(lambda e, j=j: e.tensor_reduce(out=wab[:, :, j], in_=eq[:], axis=AX.X, op=ALU.add), ["eq"], ["wab"])
        for i in range(16):
            for j in range(2):
                S.dma("pool", lambda e, i=i, j=j: e.indirect_dma_start(
                    out=XS, out_offset=bass.IndirectOffsetOnAxis(ap=slots[:, i, j:j + 1], axis=0), in_=h2b[:, i, :], in_offset=None),
                    reads=[("h2b", i), "slots"], writes=["XS"])
        S.barrier()


def stage_H(nc, S, io, XS, YS, identb):
    sb, ps = _namers(nc)
    with ExitStack() as _es:
        wg = _es.enter_context(sb("wg", [128, 2, 16, 512], BF16))
        wu = _es.enter_context(sb("wu", [128, 2, 16, 512], BF16))
        wd = _es.enter_context(sb("wd", [128, 2, 4, D], BF16))
        xs = _es.enter_context(sb("xs", [128, 2, RB, D], BF16))
        xsT = _es.enter_context(sb("xsT", [128, 2, 16, CAP], BF16))
        sgh = _es.enter_context(sb("sgh", [128, 2, CAP], F32))
        aT = _es.enter_context(sb("aT", [128, 4, CAP], BF16))
        ysb = _es.enter_context(sb("ysb", [128, 2, D], BF16))
        tpx = _es.enter_context(ps("tpx", [128, 4, 1024], BF16))
        gu_ps = _es.enter_context(ps("gu_ps", [128, 2, 2, CAP], F32))
        y_ps = _es.enter_context(ps("y_ps", [128, 2, 512], F32))

        def load(ex):
            wb = ex % 2
            S.dma("pool", lambda e: e.dma_start(out=wg[:, wb], in_=io["w_gate"][ex].rearrange("(k p) n -> p k n", p=128)), writes=[("wg", wb)])
            S.dma("pool", lambda e: e.dma_start(out=wu[:, wb], in_=io["w_up"][ex].rearrange("(k p) n -> p k n", p=128)), writes=[("wu", wb)])
            S.dma("pool", lambda e: e.dma_start(out=wd[:, wb], in_=io["w_down"][ex].rearrange("(c p) n -> p c n", p=128)), writes=[("wd", wb)])
            S.dma("sp", lambda e: e.dma_start(out=xs[:, wb], in_=XS[ex * CAP:(ex + 1) * CAP, :].rearrange("(r p) d -> p r d", p=128)),
                  reads=["XS"], writes=[("xs", wb)])

        def transposes(ex):
            wb = ex % 2
            for r in range(RB):
                for k8 in range(2):
                    j = (r * 2 + k8) % 4
                    for kk in range(8):
                        k = k8 * 8 + kk
                        S.op("pe", lambda e, kk=kk, k=k: e.transpose(tpx[:, j, kk * 128:(kk + 1) * 128], xs[:, wb, r, k * 128:(k + 1) * 128], identb[:]),
                             reads=[("xs", wb), "identb"], writes=[("tpx", j)])
                    dst = xsT[:, wb, k8 * 8:(k8 + 1) * 8, r * 128:(r + 1) * 128]
                    src = tpx[:, j, :].rearrange("p (a b) -> p a b", a=8)
                    if j % 2 == 0:
                        S.op("act", lambda e, dst=dst, src=src: e.copy(out=dst, in_=src), reads=[("tpx", j)], writes=[("xsT", wb)])
                    else:
                        S.op("dve", lambda e, dst=dst, src=src: e.tensor_copy(out=dst, in_=src), reads=[("tpx", j)], writes=[("xsT", wb)])

        def gate_up(ex):
            wb = ex % 2
            for hc in range(4):
                gb = hc % 2
                for which, w in enumerate([wg, wu]):
                    for k in range(16):
                        S.op("pe", lambda e, which=which, w=w, k=k: e.matmul(
                            gu_ps[:, gb, which, :], lhsT=w[:, wb, k, hc * 128:(hc + 1) * 128], rhs=xsT[:, wb, k, :], start=(k == 0), stop=(k == 15)),
                            reads=[("wg", wb), ("wu", wb), ("xsT", wb)], writes=[("gu_ps", gb)])
                S.op("act", lambda e: e.activation(out=sgh[:, gb, :], in_=gu_ps[:, gb, 0, :], func=AF.Silu), reads=[("gu_ps", gb)], writes=[("sgh", gb)])
                S.op("dve", lambda e: e.tensor_tensor(out=aT[:, hc, :], in0=sgh[:, gb, :], in1=gu_ps[:, gb, 1, :], op=ALU.mult),
                     reads=[("sgh", gb), ("gu_ps", gb)], writes=["aT"])

        ny = [0]

        def down(ex):
            wb = ex % 2
            for r in range(RB):
                yb = ny[0] % 2
                ny[0] += 1
                for cb in range(4):
                    pb = cb % 2
                    for hc in range(4):
                        S.op("pe", lambda e, hc=hc: e.matmul(
                            y_ps[:, pb, :], lhsT=aT[:, hc, r * 128:(r + 1) * 128], rhs=wd[:, wb, hc, cb * 512:(cb + 1) * 512], start=(hc == 0), stop=(hc == 3)),
                            reads=["aT", ("wd", wb)], writes=[("y_ps", pb)])
                    if cb % 2 == 0:
                        S.op("act", lambda e: e.copy(out=ysb[:, yb, cb * 512:(cb + 1) * 512], in_=y_ps[:, pb, :]), reads=[("y_ps", pb)], writes=[("ysb", yb)])
                    else:
                        S.op("dve", lambda e: e.tensor_copy(out=ysb[:, yb, cb * 512:(cb + 1) * 512], in_=y_ps[:, pb, :]), reads=[("y_ps", pb)], writes=[("ysb", yb)])
                S.dma("sp", lambda e: e.dma_start(out=YS[ex * CAP + r * 128:ex * CAP + (r + 1) * 128, :], in_=ysb[:, yb]),
                      reads=[("ysb", yb)], writes=["YS"])

        load(0)
        transposes(0)
        for ex in range(64):
            if ex + 1 < 64:
                load(ex + 1)
            gate_up(ex)
            if ex + 1 < 64:
                transposes(ex + 1)
            down(ex)
        S.barrier()


def stage_I(nc, S, io, X1, YS, g2row, slots, wab, out):
    sb, ps = _namers(nc)
    toks = []
    with ExitStack() as _es:
        ya = _es.enter_context(sb("ya", [128, 2, D], BF16))
        yb_ = _es.enter_context(sb("yb_", [128, 2, D], BF16))
        x1i = _es.enter_context(sb("x1i", [128, 2, D], F32))
        mo = _es.enter_context(sb("mo", [128, D], F32))
        ot = _es.enter_context(sb("ot", [128, 2, D], F32))
        fg = _es.enter_context(sb("fg", [128, D], F32))
        junk = _es.enter_context(sb("junk3", [128, D], BF16))
        ss3 = _es.enter_context(sb("ss3", [128, 16], F32))
        rstd3 = _es.enter_context(sb("rstd3", [128, 16], F32))
        S.dma("sp", lambda e: e.dma_start(out=fg[:], in_=io["fg_row"]), writes=["fg"])
        S.op("dve", lambda e: e.memset(ss3[:], 0.0), writes=["ss3"])
        for i in range(16):
            b = i % 2
            isl = slice(i * 128, (i + 1) * 128)
            S.dma("pool", lambda e, b=b, i=i: e.indirect_dma_start(out=ya[:, b, :], out_offset=None, in_=YS,
                                                                   in_offset=bass.IndirectOffsetOnAxis(ap=slots[:, i, 0:1], axis=0)),
                  reads=["YS", "slots"], writes=[("ya", b)])
            S.dma("pool", lambda e, b=b, i=i: e.indirect_dma_start(out=yb_[:, b, :], out_offset=None, in_=YS,
                                                                   in_offset=bass.IndirectOffsetOnAxis(ap=slots[:, i, 1:2], axis=0)),
                  reads=["YS", "slots"], writes=[("yb", b)])
            S.dma("sp", lambda e, b=b, isl=isl: e.dma_start(out=x1i[:, b], in_=X1[isl, :]), reads=["X1"], writes=[("x1i", b)])
            S.op("dve", lambda e, b=b, i=i: e.tensor_scalar(out=mo[:], in0=ya[:, b], scalar1=wab[:, i, 0:1], scalar2=None, op0=ALU.mult),
                 reads=[("ya", b), "wab"], writes=["mo"])
            S.op("dve", lambda e, b=b, i=i: e.scalar_tensor_tensor(out=mo[:], in0=yb_[:, b], scalar=wab[:, i, 1:2], in1=mo[:], op0=ALU.mult, op1=ALU.add),
                 reads=[("yb", b), "wab", "mo"], writes=["mo"])
            S.op("pool", lambda e: e.tensor_tensor(out=mo[:], in0=mo[:], in1=g2row[:], op=ALU.mult), reads=["mo", "g2row"], writes=["mo"])
            S.op("pool", lambda e, b=b: e.tensor_tensor(out=mo[:], in0=mo[:], in1=x1i[:, b], op=ALU.add), reads=["mo", ("x1i", b)], writes=["mo"])
            S.op("act", lambda e, i=i: e.activation(out=junk[:], in_=mo[:], func=AF.Square, accum_out=ss3[:, i:i + 1]), reads=["mo", "ss3"], writes=["junk", ("ss3", i)])
            S.op("act", lambda e, i=i: e.activation(out=rstd3[:, i:i + 1], in_=ss3[:, i:i + 1], func=AF.Sqrt, bias=epsc[:, 0:1], scale=1.0 / D),
                 reads=[("ss3", i), "epsc"], writes=[("rstd3", i)])
            S.op("dve", lambda e, i=i: e.reciprocal(out=rstd3[:, i:i + 1], in_=rstd3[:, i:i + 1]), reads=[("rstd3", i)], writes=[("rstd3", i)])
            S.op("dve", lambda e, b=b, i=i: e.scalar_tensor_tensor(out=ot[:, b], in0=mo[:], scalar=rstd3[:, i:i + 1], in1=fg[:], op0=ALU.mult, op1=ALU.mult),
                 reads=["mo", ("rstd3", i), "fg"], writes=[("ot", b)])
            toks.append(S.dma("sp", lambda e, b=b, isl=isl: e.dma_start(out=out[isl, :], in_=ot[:, b]), reads=[("ot", b)], writes=["out"]))
        S.barrier()
    return toks


def host_inputs(inp, b):
    f = lambda a: np.ascontiguousarray(a, dtype=np.float32)
    m = {}
    m["x"] = f(inp["x"][b])
    m["cT"] = f(inp["c"][b].reshape(16, 128).T)
    m["w_ada"] = f(inp["w_ada"][0])
    m["b_adaT"] = f(inp["b_ada"][0].reshape(96, 128).T)
    m["n1gT"] = f(inp["norm1_g"][0].reshape(16, 128).T)
    m["n2gT"] = f(inp["norm2_g"][0].reshape(16, 128).T)
    m["fg_row"] = f(np.broadcast_to(inp["final_g"][None, :], (128, D)))
    m["w_in"] = f(inp["w_in"][0])
    m["gng_row"] = f(np.broadcast_to(inp["ret_gn_g"][0][None, :], (128, 1024)))
    m["peT_k"] = f(inp["cmp_pos_k"][0].T)
    m["w1_k"] = f(inp["cmp_w1_k"][0])
    m["w2_k"] = f(inp["cmp_w2_k"][0])
    m["peT_v"] = f(inp["cmp_pos_v"][0].T)
    m["w1_v"] = f(inp["cmp_w1_v"][0])
    m["w2_v"] = f(inp["cmp_w2_v"][0])
    m["w_out"] = f(inp["w_out"][0])
    m["w_rt"] = f(np.concatenate([inp["w_grp"][0], inp["w_exp"][0]], axis=1))
    m["b_rt"] = f(np.broadcast_to(np.concatenate([inp["b_grp"][0], inp["b_exp"][0]])[None, :], (128, 72)))
    m["w_gate"] = f(inp["w_gate"][0])
    m["w_up"] = f(inp["w_up"][0])
    m["w_down"] = f(inp["w_down"][0])
    return m


def kernel(**inputs):
    inp = {k: np.asarray(v) for k, v in inputs.items()}
    nc = build_nc()
    consts = {k: v for k, v in make_consts().items() if not k.startswith("_")}
    shared = None
    in_maps = []
    for b in range(8):
        m = host_inputs(inp, b)
        if shared is None:
            shared = {k: m[k] for k in m if k not in ("x", "cT")}
        else:
            for k in shared:
                m[k] = shared[k]
        m.update(consts)
        in_maps.append(m)
    res = run_bass_kernel_spmd(nc, in_maps, core_ids=list(range(8)))
    return np.stack([np.asarray(r["out"], dtype=np.float32).reshape(T, D) for r in res.results], axis=0)
```

```python
import os
from contextlib import ExitStack
import numpy as np
import ml_dtypes
import concourse.bass as bass
import concourse.mybir as mybir
from concourse.bass_utils import run_bass_kernel_spmd

F32 = mybir.dt.float32
BF16 = mybir.dt.bfloat16
I32 = mybir.dt.int32
AF = mybir.ActivationFunctionType
ALU = mybir.AluOpType
AX = mybir.AxisListType

D = 2048
T = 2048
NT = 16
KC = 16
PROJ = 6680
CAP = 256
RB = CAP // 128
NEG = -30000.0
EPS = 1e-6

ENGS = ("pe", "act", "dve", "pool", "sp")


_UNIQ = [0]


def _namers(nc):
    def sb(name, shp, dt):
        _UNIQ[0] += 1
        return nc.sbuf_tensor("s%d_%s" % (_UNIQ[0], name), shp, dt)

    def ps(name, shp, dt):
        _UNIQ[0] += 1
        return nc.psum_tensor("p%d_%s" % (_UNIQ[0], name), shp, dt)
    return sb, ps


class Sched:
    NDMA = 24

    def __init__(self, nc):
        self.nc = nc
        self.handles = dict(pe=nc.tensor, act=nc.scalar, dve=nc.vector, pool=nc.gpsimd, sp=nc.sync)
        self.cnt = {e: 0 for e in ENGS}
        self.waited = {e: {} for e in ENGS}
        self.lastw = {}
        self.readers = {}
        self.sems = {}
        self.dma_sems = []
        self.dma_n = 0
        self.dma_q = [0, 0]
        self.dma_last = {}
        self._stack = []

    def open(self):
        for e in ENGS:
            cm = self.nc.semaphore("sem_" + e)
            self.sems[e] = cm.__enter__()
            self._stack.append(cm)
        for i in range(self.NDMA):
            cm = self.nc.semaphore("semd%d" % i)
            self.dma_sems.append(cm.__enter__())
            self._stack.append(cm)

    def close(self):
        for cm in reversed(self._stack):
            cm.__exit__(None, None, None)

    def _need(self, eng, tok, waits):
        if tok is None:
            return
        sem_id, val, src = tok
        if src == "pe" and eng == "pe":
            return
        w = self.waited[eng]
        if w.get(sem_id, 0) >= val:
            return
        w[sem_id] = val
        waits.append((sem_id, val))

    def _deps(self, eng, reads, writes):
        waits = []
        for b in reads:
            self._need(eng, self.lastw.get(b), waits)
        for b in writes:
            self._need(eng, self.lastw.get(b), waits)
            for t in self.readers.get(b, ()):
                self._need(eng, t, waits)
        return waits

    def _commit(self, tok, reads, writes):
        for b in reads:
            self.readers.setdefault(b, []).append(tok)
        for b in writes:
            self.lastw[b] = tok
            self.readers[b] = []

    def op(self, eng, fn, reads=(), writes=()):
        waits = self._deps(eng, reads, writes)
        self.cnt[eng] += 1
        tok = ("E" + eng, self.cnt[eng], eng)
        self._emit1(eng, waits, fn, ("E" + eng, 1))
        self._commit(tok, reads, writes)
        return tok

    def dma(self, eng, fn, reads=(), writes=()):
        half = self.NDMA // 2
        qi = 0 if eng == "pool" else 1
        j = self.dma_q[qi]
        self.dma_q[qi] += 1
        s = qi * half + (j % half)
        sid = "D%d" % s
        prev = self.dma_last.get(s, 0)
        waits = self._deps(eng, reads, writes)
        if prev and self.waited[eng].get(sid, 0) < prev:
            self.waited[eng][sid] = prev
            waits.append((sid, prev))
        val = prev + 16
        self.dma_last[s] = val
        tok = (sid, val, "dma")
        self._emit1(eng, waits, fn, (sid, 16))
        self._commit(tok, reads, writes)
        return tok

    def _emit1(self, engname, waits, fn, inc):
        e = self.handles[engname]
        if os.environ.get("K_TRACE"):
            print("TR", engname, self.cnt[engname], waits, inc, flush=True)
        for sid, val in waits:
            e.wait_ge(self._sem(sid), val)
        if fn is not None:
            ins = fn(e)
            ins.then_inc(self._sem(inc[0]), inc[1])

    def _sem(self, sid):
        if sid[0] == "E":
            return self.sems[sid[1:]]
        return self.dma_sems[int(sid[1:])]

    def barrier(self, engs=ENGS):
        toks = [("E" + e, self.cnt[e], e) for e in ENGS if self.cnt[e] > 0]
        toks += [("D%d" % s, v, "dma") for s, v in self.dma_last.items()]
        for e in engs:
            waits = []
            for t in toks:
                self._need(e, t, waits)
            self._emit1(e, waits, None, None)


def make_consts():
    bf = ml_dtypes.bfloat16
    c = {}
    half = 128
    inv = (10000.0 ** (-np.arange(half, dtype=np.float32) / half)).astype(np.float32)
    pos = np.arange(T, dtype=np.float32)
    ang = (inv[:, None] * pos[None, :]).astype(np.float32)
    c["cos"] = np.cos(ang).astype(np.float32)
    c["sin"] = np.sin(ang).astype(np.float32)
    c["identb"] = np.eye(128, dtype=np.float32).astype(bf)
    c["identf"] = np.eye(128, dtype=np.float32)
    H = 4
    lg = np.log1p(-np.exp2(-5.0 - np.arange(H, dtype=np.float64)))
    m = np.arange(128, dtype=np.float64)
    dm = np.zeros((128, H, 128), np.float32)
    for h in range(H):
        val = np.exp(lg[h] * (-m - 1.0))
        dm[:, h, :] = np.where(m[None, :] >= m[:, None], val[:, None], 0.0)
    c["dm"] = dm
    c["zeta"] = np.exp(lg[None, :] * (127.0 - m)[:, None]).astype(np.float32)
    c["qdec"] = np.exp(lg[None, :] * (m + 1.0)[:, None]).astype(np.float32)
    c["_cd"] = [float(np.exp(lg[h] * 128.0)) for h in range(H)]
    j = np.arange(128)
    caus = np.where(j[:, None] <= j[None, :], 0.0, NEG).astype(np.float32)
    anti = np.where(j[:, None] > j[None, :], 0.0, NEG).astype(np.float32)
    c["caus"] = caus.astype(bf)
    c["anti"] = anti.astype(bf)
    cc = np.arange(128)
    tt = np.arange(T)
    c["cmask"] = np.where(16 * cc[:, None] + 31 <= tt[None, :], 0.0, NEG).astype(np.float32).astype(bf)
    ss = np.arange(32)
    ov = ((16 * cc[:, None] < 64 * ss[None, :] + 64) & (16 * cc[:, None] + 32 > 64 * ss[None, :])).astype(np.float32)
    c["overlap"] = ov.astype(bf)
    es = np.zeros((32, 16, 128), np.float32)
    for kt in range(16):
        for p in range(128):
            es[2 * kt + p // 64, kt, p] = 1.0
    c["esel"] = es.astype(bf)
    cur = tt // 64
    valid = (ss[None, :] <= cur[:, None])
    forced = (ss[None, :] == 0) | (ss[None, :] == cur[:, None]) | (ss[None, :] == cur[:, None] - 1)
    ctab = np.where(valid, np.where(forced, 1e4, 0.0), -1.0).astype(np.float32)
    c["valid"] = np.ascontiguousarray(valid.astype(np.float32).reshape(16, 128, 32).transpose(1, 0, 2))
    c["ctab"] = np.ascontiguousarray(ctab.reshape(16, 128, 32).transpose(1, 0, 2))
    c["lstrict"] = (j[:, None] < j[None, :]).astype(np.float32).astype(bf)
    c["ones"] = np.ones((128, 128), np.float32).astype(bf)
    c["ebase"] = np.tile((np.arange(64, dtype=np.float32) * CAP + 1.0)[None, :], (128, 1))
    return c


CONST_SPECS = [
    ("cos", [128, T], F32), ("sin", [128, T], F32), ("identb", [128, 128], BF16), ("identf", [128, 128], F32),
    ("dm", [128, 4, 128], F32), ("zeta", [128, 4], F32), ("qdec", [128, 4], F32),
    ("caus", [128, 128], BF16), ("anti", [128, 128], BF16), ("cmask", [128, T], BF16), ("overlap", [128, 32], BF16),
    ("esel", [32, 16, 128], BF16), ("valid", [128, 16, 32], F32), ("ctab", [128, 16, 32], F32),
    ("lstrict", [128, 128], BF16), ("ones", [128, 128], BF16), ("ebase", [128, 64], F32),
]

IN_SPECS = [
    ("x", [T, D], F32), ("cT", [128, 16], F32), ("w_ada", [D, 6 * D], F32), ("b_adaT", [128, 96], F32),
    ("n1gT", [128, 16], F32), ("n2gT", [128, 16], F32), ("fg_row", [128, D], F32), ("w_in", [D, PROJ], F32),
    ("gng_row", [128, 1024], F32), ("peT_k", [128, 32], F32), ("w1_k", [4096, 256], F32), ("w2_k", [256, 128], F32),
    ("peT_v", [128, 32], F32), ("w1_v", [4096, 256], F32), ("w2_v", [256, 128], F32), ("w_out", [D, D], F32),
    ("w_rt", [D, 72], F32), ("b_rt", [128, 72], F32), ("w_gate", [64, D, 512], F32), ("w_up", [64, D, 512], F32),
    ("w_down", [64, 512, D], F32),
]


def build_nc(stop=99, dbg=()):
    CST = make_consts()
    nc = bass.Bass("TRN2", target_bir_lowering=False)
    io = {}
    for name, shp, dt in IN_SPECS + CONST_SPECS:
        if stop < 8 and name in ("w_gate", "w_up", "w_down"):
            shp = [1] + shp[1:]
        io[name] = nc.dram_tensor(name, shp, dt, kind="ExternalInput").ap()
    out = nc.dram_tensor("out", [T, D], F32, kind="ExternalOutput").ap()

    def scratch(name, shp, dt):
        kind = "ExternalOutput" if name in dbg else "Internal"
        return nc.dram_tensor(name, shp, dt, kind=kind).ap()

    PFM = scratch("PFM", [32, 128, T], BF16)
    PTM = scratch("PTM", [T, 2560], BF16)
    X1 = scratch("X1", [T, D], F32)
    XS = scratch("XS", [64 * CAP, D], BF16)
    YS = scratch("YS", [64 * CAP, D], BF16)
    dbgout = {}
    for name, shp, dt in [("d_modT", [128, 96], F32), ("d_hT", [128, 16, T], BF16), ("d_ocatT", [128, 16, T], BF16),
                          ("d_ngs", [128, 16, 24], F32), ("d_slots", [128, 16, 2], I32), ("d_wab", [128, 16, 2], F32),
                          ("d_misc", [128, 16, 64], F32)]:
        if name in dbg:
            dbgout[name] = nc.dram_tensor(name, shp, dt, kind="ExternalOutput").ap()

    S = Sched(nc)
    S.open()
    sb, ps = _namers(nc)
    final_toks = []

    def dbgdump(name, ap, key):
        if name in dbgout:
            final_toks.append(S.dma("sp", lambda e: e.dma_start(out=dbgout[name], in_=ap), reads=[key], writes=["dbg_" + name]))

    with ExitStack() as _es:
        modT = _es.enter_context(sb("modT", [128, 96], F32))
        A1 = _es.enter_context(sb("A1", [128, 16], F32))
        A2 = _es.enter_context(sb("A2", [128, 16], F32))
        identb = _es.enter_context(sb("identb", [128, 128], BF16))
        identf = _es.enter_context(sb("identf", [128, 128], F32))
        ngs = _es.enter_context(sb("ngs", [128, 16, 24], F32))
        slots = _es.enter_context(sb("slots", [128, 16, 2], I32))
        wab = _es.enter_context(sb("wab", [128, 16, 2], F32))
        global epsc
        epsc = _es.enter_context(sb("epsc", [128, 1], F32))
        S.op("dve", lambda e: e.memset(epsc[:], EPS), writes=["epsc"])
        S.dma("sp", lambda e: e.dma_start(out=identb[:], in_=io["identb"]), writes=["identb"])
        S.dma("sp", lambda e: e.dma_start(out=identf[:], in_=io["identf"]), writes=["identf"])

        with ExitStack() as _es:
            cT = _es.enter_context(sb("cT", [128, 16], F32))
            cact = _es.enter_context(sb("cact", [128, 16], BF16))
            badaT = _es.enter_context(sb("badaT", [128, 96], F32))
            n1gT = _es.enter_context(sb("n1gT", [128, 16], F32))
            n2gT = _es.enter_context(sb("n2gT", [128, 16], F32))
            wa = _es.enter_context(sb("wa", [128, 2, 16, 512], BF16))
            modps = _es.enter_context(ps("modps", [128, 96], F32))
            S.dma("sp", lambda e: e.dma_start(out=cT[:], in_=io["cT"]), writes=["cT"])
            S.dma("sp", lambda e: e.dma_start(out=badaT[:], in_=io["b_adaT"]), writes=["badaT"])
            S.dma("sp", lambda e: e.dma_start(out=n1gT[:], in_=io["n1gT"]), writes=["n1gT"])
            S.dma("sp", lambda e: e.dma_start(out=n2gT[:], in_=io["n2gT"]), writes=["n2gT"])
            S.op("act", lambda e: e.activation(out=cact[:], in_=cT[:], func=AF.Silu), reads=["cT"], writes=["cact"])
            if stop >= 7:
                zt = _es.enter_context(sb("zt", [128, RB, D], BF16))
                S.op("dve", lambda e: e.memset(zt[:], 0.0), writes=["zt"])
                for ex in range(64):
                    S.dma("sp", lambda e, ex=ex: e.dma_start(out=XS[ex * CAP:(ex + 1) * CAP, :].rearrange("(r p) d -> p r d", p=128), in_=zt[:]),
                          reads=["zt"], writes=["XS"])
            wada = io["w_ada"].rearrange("(k p) n -> p k n", p=128)
            for blk in range(24 if not os.environ.get("K_SKIPABC") else 0):
                b = blk % 2
                S.dma("pool", lambda e, b=b, blk=blk: e.dma_start(out=wa[:, b], in_=wada[:, :, blk * 512:(blk + 1) * 512]),
                      writes=[("wa", b)])
                for j in range(4):
                    col = blk * 4 + j
                    for k in range(16):
                        S.op("pe", lambda e, b=b, j=j, k=k, col=col: e.matmul(
                            modps[:, col:col + 1], lhsT=wa[:, b, k, j * 128:(j + 1) * 128], rhs=cact[:, k:k + 1],
                            start=(k == 0), stop=(k == 15)), reads=[("wa", b), "cact"], writes=["modps"])
            S.op("dve", lambda e: e.tensor_tensor(out=modT[:], in0=modps[:], in1=badaT[:], op=ALU.add),
                 reads=["modps", "badaT"], writes=["modT"])
            S.op("dve", lambda e: e.scalar_tensor_tensor(out=A1[:], in0=modT[:, 16:32], scalar=1.0, in1=n1gT[:],
                                                         op0=ALU.add, op1=ALU.mult), reads=["modT", "n1gT"], writes=["A1"])
            S.op("dve", lambda e: e.scalar_tensor_tensor(out=A2[:], in0=modT[:, 64:80], scalar=1.0, in1=n2gT[:],
                                                         op0=ALU.add, op1=ALU.mult), reads=["modT", "n2gT"], writes=["A2"])
            dbgdump("d_modT", modT[:], "modT")
            S.barrier()

        def rowbcast(dst, col0, key):
            with sb("rb_l", [128, 2, 128], F32) as rbl, ps("rb_ps", [128, 2, 512], F32) as rbps:
                for k in range(16):
                    lb = k % 2
                    S.op("dve", lambda e, k=k, lb=lb: e.tensor_copy(out=rbl[:, lb, :], in_=modT[:, col0 + k:col0 + k + 1].to_broadcast([128, 128])),
                         reads=["modT"], writes=[("rbl", lb)])
                    S.op("pe", lambda e, k=k, lb=lb: e.matmul(rbps[:, (k // 4) % 2, (k % 4) * 128:(k % 4 + 1) * 128], lhsT=rbl[:, lb, :], rhs=identf[:],
                                                              start=True, stop=True), reads=[("rbl", lb), "identf"], writes=[("rbps", (k // 4) % 2)])
                    if k % 4 == 3:
                        q = k // 4
                        S.op("act", lambda e, q=q: e.copy(out=dst[:, q * 512:(q + 1) * 512], in_=rbps[:, q % 2, :]),
                             reads=[("rbps", q % 2)], writes=[key])
                S.barrier()

        if stop >= 2 and not os.environ.get("K_SKIPABC"):
          with sb("hT", [128, 16, T], BF16) as hT:
            with ExitStack() as _es:
                xt = _es.enter_context(sb("xt", [128, 2, D], F32))
                xn = _es.enter_context(sb("xn", [128, 4, D], BF16))
                junk = _es.enter_context(sb("junk", [128, D], BF16))
                ss = _es.enter_context(sb("ss", [128, 16], F32))
                rstd = _es.enter_context(sb("rstd", [128, 16], F32))
                tp = _es.enter_context(ps("tpB", [128, 2, 1024], BF16))
                S.op("dve", lambda e: e.memset(ss[:], 0.0), writes=["ss"])
                for tg in range(4):
                    for i4 in range(4):
                        i = tg * 4 + i4
                        b = i % 2
                        S.dma("sp", lambda e, i=i, b=b: e.dma_start(out=xt[:, b], in_=io["x"][i * 128:(i + 1) * 128, :]), writes=[("xt", b)])
                        S.op("act", lambda e, i=i, b=b: e.activation(out=junk[:], in_=xt[:, b], func=AF.Square, accum_out=ss[:, i:i + 1]),
                             reads=[("xt", b), "ss"], writes=["junk", ("ss", i)])
                        S.op("act", lambda e, i=i: e.activation(out=rstd[:, i:i + 1], in_=ss[:, i:i + 1], func=AF.Sqrt, bias=epsc[:, 0:1], scale=1.0 / D),
                             reads=[("ss", i), "epsc"], writes=[("rstd", i)])
                        S.op("dve", lambda e, i=i: e.reciprocal(out=rstd[:, i:i + 1], in_=rstd[:, i:i + 1]), reads=[("rstd", i)], writes=[("rstd", i)])
                        S.op("dve", lambda e, i=i, i4=i4, b=b: e.tensor_scalar(out=xn[:, i4], in0=xt[:, b], scalar1=rstd[:, i:i + 1], scalar2=None,
                                                                               op0=ALU.mult), reads=[("xt", b), ("rstd", i)], writes=[("xn", i4)])
                    for k in range(16):
                        pb = k % 2
                        for i4 in range(4):
                            S.op("pe", lambda e, k=k, pb=pb, i4=i4: e.transpose(tp[:, pb, i4 * 128:(i4 + 1) * 128], xn[:, i4, k * 128:(k + 1) * 128], identb[:]),
                                 reads=[("xn", i4), "identb"], writes=[("tp", pb)])
                        S.op("act", lambda e, k=k, pb=pb, tg=tg: e.activation(out=hT[:, k, tg * 512:(tg + 1) * 512], in_=tp[:, pb, 0:512], func=AF.Identity,
                                                                              scale=A1[:, k:k + 1], bias=modT[:, k:k + 1]),
                             reads=[("tp", pb), "A1", "modT"], writes=[("hT", tg)])
                dbgdump("d_hT", hT[:], ("hT", 3))
                S.barrier()

            if stop >= 3:
                stage_C(nc, S, io, hT, PFM, PTM, ngs, dbgdump)
            S.barrier()

        if stop >= 4:
          with sb("ocatT", [128, 16, T], BF16) as ocatT:
            if not os.environ.get("K_SKIPD"):
                stage_D(nc, S, io, CST, PFM, PTM, ocatT, identb)
            if stop >= 5:
                stage_E(nc, S, io, PFM, PTM, ocatT, identb, ngs)
            dbgdump("d_ocatT", ocatT[:], "ocatT")
            if stop >= 6:
                with sb("g1row", [128, D], F32) as g1row:
                    rowbcast(g1row, 32, "g1row")
                    stage_F(nc, S, io, ocatT, g1row, X1)
            S.barrier()

        if stop >= 7:
            with sb("a2row", [128, D], F32) as a2row, sb("sh2row", [128, D], F32) as sh2row:
                S.op("dve", lambda e: e.tensor_copy(out=modT[:, 64:80], in_=A2[:]), reads=["A2", "modT"], writes=["modT"])
                rowbcast(a2row, 64, "a2row")
                rowbcast(sh2row, 48, "sh2row")
                stage_G(nc, S, io, X1, XS, a2row, sh2row, identf, slots, wab, dbgout, final_toks)
            S.barrier()
            dbgdump("d_slots", slots[:], "slots")
            dbgdump("d_wab", wab[:], "wab")
        if stop >= 8:
            stage_H(nc, S, io, XS, YS, identb)
            S.barrier()
        if stop >= 9:
            with sb("g2row", [128, D], F32) as g2row:
                rowbcast(g2row, 80, "g2row")
                final_toks += stage_I(nc, S, io, X1, YS, g2row, slots, wab, out)
        S.barrier()
        e = S.handles["sp"]
        waits = []
        for t in final_toks:
            S._need("sp", t, waits)
        for sid, val in waits:
            e.wait_ge(S._sem(sid), val)
    S.close()
    return nc


def stage_C(nc, S, io, hT, PFM, PTM, ngs, dbgdump):
    sb, ps = _namers(nc)
    win = io["w_in"].rearrange("(k p) n -> p k n", p=128)
    units = []
    for h in range(4):
        units.append((h * 256, "rotq", 2 * h))
    for h in range(4):
        units.append((1024 + h * 256, "rotk", 8 + 2 * h))
    for u in range(4):
        units.append((4096 + u * 256, "scale", 16 + 2 * u))
    units += [(5120, "copy", 24), (5376, "copy", 26), (5632, "copy", 28), (6144, "copy", 30)]
    with ExitStack() as _es:
        cos = _es.enter_context(sb("cos", [128, T], F32))
        sin = _es.enter_context(sb("sin", [128, T], F32))
        wf = _es.enter_context(sb("wf", [128, 2, 16, 256], BF16))
        stg = _es.enter_context(sb("stg", [128, 2, 2, T], BF16))
        f12 = _es.enter_context(sb("f12", [128, 2, 512], F32))
        tt = _es.enter_context(sb("tt", [128, 4, 512], F32))
        pfm = _es.enter_context(ps("pfm", [128, 2, 2, 512], F32))
        S.dma("sp", lambda e: e.dma_start(out=cos[:], in_=io["cos"]), writes=["cos"])
        S.dma("sp", lambda e: e.dma_start(out=sin[:], in_=io["sin"]), writes=["sin"])
        n = 0
        for ui, (c0, kind, ch) in enumerate(units):
            wb = ui % 2
            S.dma("pool", lambda e, wb=wb, c0=c0: e.dma_start(out=wf[:, wb], in_=win[:, :, c0:c0 + 256]), writes=[("wf", wb)])
            for tg in range(4):
                pb = n % 2
                n += 1
                for half in range(2):
                    for k in range(16):
                        S.op("pe", lambda e, wb=wb, pb=pb, half=half, k=k, tg=tg: e.matmul(
                            pfm[:, pb, half, :], lhsT=wf[:, wb, k, half * 128:(half + 1) * 128], rhs=hT[:, k, tg * 512:(tg + 1) * 512],
                            start=(k == 0), stop=(k == 15)), reads=[("wf", wb), ("hT", tg)], writes=[("pfm", pb)])
                tsl = slice(tg * 512, (tg + 1) * 512)
                if kind in ("copy", "scale"):
                    sc = 1.0 if kind == "copy" else 128.0 ** -0.5
                    for half in range(2):
                        S.op("act", lambda e, pb=pb, wb=wb, tsl=tsl, sc=sc, half=half: e.activation(out=stg[:, wb, half, tsl], in_=pfm[:, pb, half, :], func=AF.Copy, scale=sc),
                             reads=[("pfm", pb)], writes=[("stg", wb)])
                else:
                    sc = 1.0 if kind == "rotq" else 1.0 / 16.0
                    for half in range(2):
                        S.op("act", lambda e, pb=pb, sc=sc, half=half: e.activation(out=f12[:, half, :], in_=pfm[:, pb, half, :], func=AF.Copy, scale=sc),
                             reads=[("pfm", pb)], writes=["f12"])
                    S.op("dve", lambda e, tsl=tsl: e.tensor_tensor(out=tt[:, 0], in0=f12[:, 0], in1=cos[:, tsl], op=ALU.mult), reads=["f12", "cos"], writes=["tt0"])
                    S.op("dve", lambda e, tsl=tsl: e.tensor_tensor(out=tt[:, 1], in0=f12[:, 1], in1=sin[:, tsl], op=ALU.mult), reads=["f12", "sin"], writes=["tt1"])
                    S.op("pool", lambda e, tsl=tsl: e.tensor_tensor(out=tt[:, 2], in0=f12[:, 0], in1=sin[:, tsl], op=ALU.mult), reads=["f12", "sin"], writes=["tt2"])
                    S.op("pool", lambda e, tsl=tsl: e.tensor_tensor(out=tt[:, 3], in0=f12[:, 1], in1=cos[:, tsl], op=ALU.mult), reads=["f12", "cos"], writes=["tt3"])
                    S.op("dve", lambda e, wb=wb, tsl=tsl: e.tensor_tensor(out=stg[:, wb, 0, tsl], in0=tt[:, 0], in1=tt[:, 1], op=ALU.subtract),
                         reads=["tt0", "tt1"], writes=[("stg", wb)])
                    S.op("pool", lambda e, wb=wb, tsl=tsl: e.tensor_tensor(out=stg[:, wb, 1, tsl], in0=tt[:, 2], in1=tt[:, 3], op=ALU.add),
                         reads=["tt2", "tt3"], writes=[("stg", wb)])
            S.dma("sp", lambda e, wb=wb, ch=ch: e.dma_start(out=PFM[ch:ch + 2].rearrange("c p t -> p c t"), in_=stg[:, wb]),
                  reads=[("stg", wb)], writes=["PFM"])
        S.barrier()
    tunits = [([(2048, 512)], 0), ([(2560, 512)], 512), ([(3072, 512)], 1024), ([(3584, 512)], 1536),
              ([(5888, 256), (6400, 256)], 2048), ([(6656, 24)], None)]
    ptm_v = PTM.rearrange("(i p) c -> p i c", p=128)
    with ExitStack() as _es:
        wt = _es.enter_context(sb("wt", [128, 2, 16, 512], BF16))
        stgt = _es.enter_context(sb("stgt", [128, 2, 16, 512], BF16))
        ptm = _es.enter_context(ps("ptm", [128, 2, 512], F32))
        n = 0
        for ui, (srcs, dcol) in enumerate(tunits):
            wb = ui % 2
            off = 0
            for (c0, w) in srcs:
                S.dma("pool", lambda e, wb=wb, c0=c0, w=w, off=off: e.dma_start(out=wt[:, wb, :, off:off + w], in_=win[:, :, c0:c0 + w]),
                      writes=[("wt", wb)])
                off += w
            ncol = off
            for i in range(16):
                pb = n % 2
                n += 1
                for k in range(16):
                    S.op("pe", lambda e, wb=wb, pb=pb, i=i, k=k, ncol=ncol: e.matmul(
                        ptm[:, pb, 0:ncol], lhsT=hT[:, k, i * 128:(i + 1) * 128], rhs=wt[:, wb, k, 0:ncol],
                        start=(k == 0), stop=(k == 15)), reads=[("wt", wb), ("hT", i // 4)], writes=[("ptm", pb)])
                if dcol is None:
                    S.op("dve", lambda e, pb=pb, i=i: e.tensor_copy(out=ngs[:, i, :], in_=ptm[:, pb, 0:24]), reads=[("ptm", pb)], writes=["ngs"])
                else:
                    eng = "act" if i % 2 == 0 else "dve"
                    if eng == "act":
                        S.op("act", lambda e, pb=pb, wb=wb, i=i: e.copy(out=stgt[:, wb, i, :], in_=ptm[:, pb, :]), reads=[("ptm", pb)], writes=[("stgt", wb)])
                    else:
                        S.op("dve", lambda e, pb=pb, wb=wb, i=i: e.tensor_copy(out=stgt[:, wb, i, :], in_=ptm[:, pb, :]), reads=[("ptm", pb)], writes=[("stgt", wb)])
            if dcol is not None:
                S.dma("sp", lambda e, wb=wb, dcol=dcol: e.dma_start(out=ptm_v[:, :, dcol:dcol + 512], in_=stgt[:, wb]),
                      reads=[("stgt", wb)], writes=["PTM"])
        dbgdump("d_ngs", ngs[:], "ngs")
        S.barrier()


def stage_D(nc, S, io, CST, PFM, PTM, ocatT, identb):
    sb, ps = _namers(nc)
    ptm_v = PTM.rearrange("(i p) c -> p i c", p=128)
    with ExitStack() as _es:
        qT = _es.enter_context(sb("qT", [128, 2, T], BF16))
        kT = _es.enter_context(sb("kT", [128, 2, T], BF16))
        kz = _es.enter_context(sb("kz", [128, 16, 256], BF16))
        v = _es.enter_context(sb("v", [128, 16, 256], BF16))
        g = _es.enter_context(sb("g", [128, 16, 256], BF16))
        dm = _es.enter_context(sb("dm", [128, 4, 128], F32))
        zeta = _es.enter_context(sb("zeta", [128, 4], F32))
        qdec = _es.enter_context(sb("qdec", [128, 4], F32))
        gng = _es.enter_context(sb("gng", [128, 1024], F32))
        Sf = _es.enter_context(sb("Sf", [128, 2, 256], F32))
        Sb = _es.enter_context(sb("Sb", [128, 2, 256], BF16))
        sTm = _es.enter_context(sb("sTm", [128, 2, 128], BF16))
        o = _es.enter_context(sb("o", [128, 2, 256], F32))
        on = _es.enter_context(sb("on", [128, 2, 256], F32))
        sg = _es.enter_context(sb("sg", [128, 2, 256], F32))
        ob = _es.enter_context(sb("ob", [128, 2, 256], BF16))
        st = _es.enter_context(sb("st", [128, 2, 6], F32))
        junkd = _es.enter_context(sb("junkd", [128, 256], BF16))
        mv = _es.enter_context(sb("mv", [128, 2, 2], F32))
        rg = _es.enter_context(sb("rg", [128, 2], F32))
        sT_ps = _es.enter_context(ps("sT_ps", [128, 2, 512], F32))
        o_ps = _es.enter_context(ps("o_ps", [128, 2, 512], F32))
        kv_ps = _es.enter_context(ps("kv_ps", [128, 2, 256], F32))
        tpk = _es.enter_context(ps("tpk", [128, 2, 1024], BF16))
        S.dma("sp", lambda e: e.dma_start(out=dm[:], in_=io["dm"]), writes=["dm"])
        S.dma("sp", lambda e: e.dma_start(out=zeta[:], in_=io["zeta"]), writes=["zeta"])
        S.dma("sp", lambda e: e.dma_start(out=qdec[:], in_=io["qdec"]), writes=["qdec"])
        S.dma("sp", lambda e: e.dma_start(out=gng[:], in_=io["gng_row"]), writes=["gng"])
        for h in range(4):
            cd = CST["_cd"][h]
            S.dma("sp", lambda e, h=h: e.dma_start(out=qT[:], in_=PFM[2 * h:2 * h + 2].rearrange("c p t -> p c t")), reads=["PFM"], writes=["qT"])
            S.dma("sp", lambda e, h=h: e.dma_start(out=kT[:], in_=PFM[8 + 2 * h:10 + 2 * h].rearrange("c p t -> p c t")), reads=["PFM"], writes=["kT"])
            S.dma("sp", lambda e, h=h: e.dma_start(out=v[:], in_=ptm_v[:, :, h * 256:(h + 1) * 256]), reads=["PTM"], writes=["v"])
            S.dma("sp", lambda e, h=h: e.dma_start(out=g[:], in_=ptm_v[:, :, 1024 + h * 256:1024 + (h + 1) * 256]), reads=["PTM"], writes=["g"])
            for i in range(16):
                pb = i % 2
                for half in range(2):
                    S.op("pe", lambda e, i=i, pb=pb, half=half: e.transpose(tpk[:, pb, half * 128:(half + 1) * 128], kT[:, half, i * 128:(i + 1) * 128], identb[:]),
                         reads=["kT", "identb"], writes=[("tpk", pb)])
                S.op("act", lambda e, i=i, pb=pb, h=h: e.activation(out=kz[:, i, :].rearrange("p (a b) -> p a b", a=2), in_=tpk[:, pb, 0:256].rearrange("p (a b) -> p a b", a=2), func=AF.Identity,
                                                                    scale=zeta[:, h:h + 1]), reads=[("tpk", pb), "zeta"], writes=["kz"])
            S.op("dve", lambda e: e.memset(Sf[:], 0.0), writes=["Sf"])
            S.op("dve", lambda e: e.memset(Sb[:], 0.0), writes=["Sb"])
            for n in range(16):
                b = n % 2
                csl = slice(n * 128, (n + 1) * 128)
                for half in range(2):
                    S.op("pe", lambda e, b=b, half=half, csl=csl: e.matmul(sT_ps[:, b, 0:128], lhsT=kT[:, half, csl], rhs=qT[:, half, csl],
                                                                           start=(half == 0), stop=(half == 1)), reads=["kT", "qT"], writes=[("sT_ps", b)])
                S.op("dve", lambda e, b=b, h=h: e.tensor_tensor(out=sTm[:, b, :], in0=sT_ps[:, b, 0:128], in1=dm[:, h, :], op=ALU.mult),
                     reads=[("sT_ps", b), "dm"], writes=[("sTm", b)])
                S.op("pe", lambda e, b=b, n=n: e.matmul(o_ps[:, b, 0:256], lhsT=sTm[:, b, :], rhs=v[:, n, :], start=True, stop=(n == 0)),
                     reads=[("sTm", b), "v"], writes=[("o_ps", b)])
                if n > 0:
                    for half in range(2):
                        S.op("pe", lambda e, b=b, half=half, csl=csl: e.matmul(o_ps[:, b, 0:256], lhsT=qT[:, half, csl], rhs=Sb[:, half, :],
                                                                               start=False, stop=(half == 1)), reads=["qT", "Sb"], writes=[("o_ps", b)])
                if n < 15 and "state" not in os.environ.get("K_DSKIP", ""):
                    for half in range(2):
                        S.op("pe", lambda e, n=n, half=half: e.matmul(kv_ps[:, half, :], lhsT=kz[:, n, half * 128:(half + 1) * 128], rhs=v[:, n, :],
                                                                      start=True, stop=True), reads=["kz", "v"], writes=["kv_ps"])
                    S.op("dve", lambda e, cd=cd: e.scalar_tensor_tensor(out=Sf[:], in0=Sf[:], scalar=cd, in1=kv_ps[:], op0=ALU.mult, op1=ALU.add),
                         reads=["Sf", "kv_ps"], writes=["Sf"])
                    S.op("act", lambda e: e.copy(out=Sb[:], in_=Sf[:]), reads=["Sf"], writes=["Sb"])
                if "epi" in os.environ.get("K_DSKIP", ""):
                    continue
                S.op("dve", lambda e, b=b, h=h: e.tensor_scalar(out=o[:, b, :], in0=o_ps[:, b, 0:256], scalar1=qdec[:, h:h + 1], scalar2=None, op0=ALU.mult),
                     reads=[("o_ps", b), "qdec"], writes=[("o", b)])
                S.op("dve", lambda e, b=b: e.tensor_reduce(out=mv[:, b, 0:1], in_=o[:, b, :], axis=AX.X, op=ALU.add), reads=[("o", b)], writes=[("mv", b)])
                S.op("dve", lambda e, b=b: e.tensor_scalar(out=mv[:, b, 1:2], in0=mv[:, b, 0:1], scalar1=-1.0 / 256.0, scalar2=None, op0=ALU.mult),
                     reads=[("mv", b)], writes=[("mv1", b)])
                S.op("act", lambda e, b=b: e.activation(out=on[:, b, :], in_=o[:, b, :], func=AF.Identity, bias=mv[:, b, 1:2], scale=1.0),
                     reads=[("o", b), ("mv1", b)], writes=[("on", b)])
                S.op("act", lambda e, b=b: e.activation(out=junkd[:], in_=on[:, b, :], func=AF.Square, accum_out=st[:, b, 0:1]),
                     reads=[("on", b)], writes=["junkd", ("st", b)])
                S.op("act", lambda e, b=b: e.activation(out=rg[:, b:b + 1], in_=st[:, b, 0:1], func=AF.Sqrt, bias=epsc[:, 0:1], scale=1.0 / 256.0),
                     reads=[("st", b), "epsc"], writes=[("rg", b)])
                S.op("dve", lambda e, b=b: e.reciprocal(out=rg[:, b:b + 1], in_=rg[:, b:b + 1]), reads=[("rg", b)], writes=[("rg", b)])
                S.op("dve", lambda e, b=b, h=h: e.scalar_tensor_tensor(out=o[:, b, :], in0=on[:, b, :], scalar=rg[:, b:b + 1], in1=gng[:, h * 256:(h + 1) * 256],
                                                                       op0=ALU.mult, op1=ALU.mult), reads=[("on", b), ("rg", b), "gng"], writes=[("o", b)])
                S.op("act", lambda e, b=b, n=n: e.activation(out=sg[:, b, :], in_=g[:, n, :], func=AF.Silu), reads=["g"], writes=[("sg", b)])
                S.op("pool", lambda e, b=b: e.tensor_tensor(out=ob[:, b, :], in0=o[:, b, :], in1=sg[:, b, :], op=ALU.mult),
                     reads=[("o", b), ("sg", b)], writes=[("ob", b)])
                for half in range(2):
                    S.op("pe", lambda e, b=b, half=half: e.transpose(tpk[:, b, half * 128:(half + 1) * 128], ob[:, b, half * 128:(half + 1) * 128], identb[:]),
                         reads=[("ob", b), "identb"], writes=[("tpk", b)])
                S.op("act", lambda e, b=b, h=h, csl=csl: e.copy(out=ocatT[:, 2 * h:2 * h + 2, csl], in_=tpk[:, b, 0:256].rearrange("p (a b) -> p a b", a=2)), reads=[("tpk", b)], writes=["ocatT"])
        S.barrier()


def stage_E(nc, S, io, PFM, PTM, ocatT, identb, ngs):
    sb, ps = _namers(nc)
    ptm_v = PTM.rearrange("(i p) c -> p i c", p=128)
    with ExitStack() as _es:
        w1k = _es.enter_context(sb("w1k", [128, 32, 256], BF16))
        w1v = _es.enter_context(sb("w1v", [128, 32, 256], BF16))
        w2k = _es.enter_context(sb("w2k", [128, 2, 128], BF16))
        w2v = _es.enter_context(sb("w2v", [128, 2, 128], BF16))
        pek = _es.enter_context(sb("pek", [128, 32], BF16))
        pev = _es.enter_context(sb("pev", [128, 32], BF16))
        hb = _es.enter_context(sb("hb", [128, 2, 2], F32))
        caus = _es.enter_context(sb("caus", [128, 128], BF16))
        anti = _es.enter_context(sb("anti", [128, 128], BF16))
        cmask = _es.enter_context(sb("cmask", [128, T], BF16))
        overlap = _es.enter_context(sb("overlap", [128, 32], BF16))
        esel = _es.enter_context(sb("esel", [32, 16, 128], BF16))
        valid = _es.enter_context(sb("valid", [128, 16, 32], F32))
        ctab = _es.enter_context(sb("ctab", [128, 16, 32], F32))
        qT4 = _es.enter_context(sb("qT4", [128, 4, T], BF16))
        kcT2 = _es.enter_context(sb("kcT2", [128, 2, T], BF16))
        vcT2 = _es.enter_context(sb("vcT2", [128, 2, T], BF16))
        ksT = _es.enter_context(sb("ksT", [128, T], BF16))
        kwT = _es.enter_context(sb("kwT", [128, T], BF16))
        vsa = _es.enter_context(sb("vsa", [128, 16, 130], BF16))
        vwa = _es.enter_context(sb("vwa", [128, 16, 130], BF16))
        hid = _es.enter_context(sb("hid", [128, 4, 2, 128], BF16))
        kc16 = _es.enter_context(sb("kc16", [128, 2, 16, 130], BF16))
        kcmpT2 = _es.enter_context(sb("kcmpT2", [128, 2, 128], BF16))
        vcaug2 = _es.enter_context(sb("vcaug2", [128, 2, 162], BF16))
        ocmp = _es.enter_context(sb("ocmp", [128, 16, 4, 128], BF16))
        gts = _es.enter_context(sb("gts", [128, 16, 12], F32))
        selnegT = _es.enter_context(sb("selnegT", [32, T], BF16))
        _sk = os.environ.get("K_EPRO", "")
        if "c" not in _sk:
            for nm, t in [("caus", caus), ("anti", anti), ("cmask", cmask), ("overlap", overlap), ("esel", esel), ("valid", valid), ("ctab", ctab)]:
                S.dma("sp", lambda e, nm=nm, t=t: e.dma_start(out=t[:], in_=io[nm]), writes=[nm])
        if "w" not in _sk:
            for lh in range(2):
                S.dma("pool", lambda e, lh=lh: e.dma_start(out=w1k[:, lh * 16:(lh + 1) * 16, :], in_=io["w1_k"].rearrange("(l p) n -> p l n", p=128)[:, lh * 16:(lh + 1) * 16, :]), writes=["w1k"])
                S.dma("pool", lambda e, lh=lh: e.dma_start(out=w1v[:, lh * 16:(lh + 1) * 16, :], in_=io["w1_v"].rearrange("(l p) n -> p l n", p=128)[:, lh * 16:(lh + 1) * 16, :]), writes=["w1v"])
            S.dma("pool", lambda e: e.dma_start(out=w2k[:], in_=io["w2_k"].rearrange("(c p) n -> p c n", p=128)), writes=["w2k"])
            S.dma("pool", lambda e: e.dma_start(out=w2v[:], in_=io["w2_v"].rearrange("(c p) n -> p c n", p=128)), writes=["w2v"])
            S.dma("pool", lambda e: e.dma_start(out=pek[:], in_=io["peT_k"]), writes=["pek"])
            S.dma("pool", lambda e: e.dma_start(out=pev[:], in_=io["peT_v"]), writes=["pev"])

        with ps("hb_ps", [128, 2, 2], F32) as hb_ps:
            for kv, (w1, pe) in enumerate([(w1k, pek), (w1v, pev)]):
                for hc in range(2):
                    for l in range(32):
                        S.op("pe", lambda e, kv=kv, hc=hc, l=l, w1=w1, pe=pe: e.matmul(hb_ps[:, kv, hc:hc + 1], lhsT=w1[:, l, hc * 128:(hc + 1) * 128],
                                                                                     rhs=pe[:, l:l + 1], start=(l == 0), stop=(l == 31)),
                             reads=["w1k", "w1v", "pek", "pev"], writes=["hb_ps"])
            if "h" not in _sk:
                S.op("dve", lambda e: e.tensor_copy(out=hb[:], in_=hb_ps[:]), reads=["hb_ps"], writes=["hb"])
            S.barrier()

        for _ in range(int(os.environ.get("K_PENOP", "0"))):
            nc.tensor.wait_ge(S.sems["dve"], 0)
        S.op("dve", lambda e: e.memset(kc16[:], 0.0), writes=[("kc16", 0), ("kc16", 1)])
        S.op("dve", lambda e: e.memset(vcaug2[:], 0.0), writes=["vcaug"])
        with ExitStack() as _es2:
            hid_ps = _es2.enter_context(ps("hid_ps", [128, 4, 2, 128], F32))
            cmp_ps = _es2.enter_context(ps("cmp_ps", [128, 2, 512], F32))
            S.dma("sp", lambda e: e.dma_start(out=kcT2[:], in_=PFM[24:26].rearrange("c p t -> p c t")), reads=["PFM"], writes=["kcT2"])
            S.dma("sp", lambda e: e.dma_start(out=vcT2[:], in_=PFM[26:28].rearrange("c p t -> p c t")), reads=["PFM"], writes=["vcT2"])
            for u in range(4):
                kv, gg = u // 2, u % 2
                w1 = w1k if kv == 0 else w1v
                src = kcT2 if kv == 0 else vcT2
                nm = "kcT2" if kv == 0 else "vcT2"
                kb = u % 2
                S.op("dve" if u % 2 == 0 else "pool", lambda e, kb=kb, src=src, gg=gg: e.tensor_copy(
                    out=kc16[:, kb, :, 0:128], in_=src[:, gg, :].rearrange("p (c r) -> p r c", r=16)), reads=[nm], writes=[("kc16", kb)])
                for hc in range(2):
                    for l in range(32):
                        S.op("pe", lambda e, u=u, kb=kb, hc=hc, l=l, w1=w1: e.matmul(
                            hid_ps[:, u, hc, :], lhsT=w1[:, l, hc * 128:(hc + 1) * 128], rhs=kc16[:, kb, l % 16, (l // 16):(l // 16) + 128],
                            start=(l == 0), stop=(l == 31)), reads=["w1k", "w1v", ("kc16", kb)], writes=[("hid_ps", u // 2)])
                    S.op("act", lambda e, u=u, kv=kv, hc=hc: e.activation(out=hid[:, u, hc, :], in_=hid_ps[:, u, hc, :], func=AF.Silu,
                                                                     bias=hb[:, kv, hc:hc + 1]), reads=[("hid_ps", u // 2), "hb"], writes=["hid"])
            for gg in range(2):
                for hc in range(2):
                    S.op("pe", lambda e, gg=gg, hc=hc: e.matmul(cmp_ps[:, 0, gg * 128:(gg + 1) * 128], lhsT=w2k[:, hc, :], rhs=hid[:, gg, hc, :], start=(hc == 0), stop=(hc == 1)),
                         reads=["w2k", "hid"], writes=["cmp_ps0"])
            for gg in range(2):
                for hc in range(2):
                    S.op("pe", lambda e, gg=gg, hc=hc: e.matmul(cmp_ps[:, 1, gg * 128:(gg + 1) * 128], lhsT=hid[:, 2 + gg, hc, :], rhs=w2v[:, hc, :], start=(hc == 0), stop=(hc == 1)),
                         reads=["w2v", "hid"], writes=["cmp_ps1"])
            _cs = os.environ.get("K_CONS", "")
            if "A" not in _cs:
                S.op("act", lambda e: e.copy(out=kcmpT2[:], in_=cmp_ps[:, 0, 0:256].rearrange("p (a b) -> p a b", a=2)), reads=["cmp_ps0"], writes=["kcmpT"])
            if "V" not in _cs:
                S.op("dve", lambda e: e.tensor_copy(out=vcaug2[:, :, 0:128], in_=cmp_ps[:, 1, 0:256].rearrange("p (a b) -> p a b", a=2)), reads=["cmp_ps1", "vcaug"], writes=["vcaug"])
            if "M" not in _cs:
                S.op("dve", lambda e: e.memset(vcaug2[:, :, 128:129], 1.0), reads=["vcaug"], writes=["vcaug"])
            for gg in range(2 if "O" not in _cs else 0):
                S.op("dve", lambda e, gg=gg: e.tensor_copy(out=vcaug2[:, gg, 129:161], in_=overlap[:]), reads=["overlap", "vcaug"], writes=["vcaug"])
            S.barrier()
        if "cmp" in os.environ.get("K_ESKIP", ""):
            return
        for g in ([int(c) for c in os.environ["K_GSEL"]] if os.environ.get("K_GSEL") else range(2)):
            _ld = os.environ.get("K_ELD", "")
            if "q" not in _ld:
                S.dma("sp", lambda e, g=g: e.dma_start(out=qT4[:], in_=PFM[16 + 4 * g:20 + 4 * g].rearrange("c p t -> p c t")), reads=["PFM"], writes=["qT4"])
            for nm, t, ch in [("ksT", ksT, 28), ("kwT", kwT, 30)]:
                S.dma("sp", lambda e, t=t, ch=ch, g=g: e.dma_start(out=t[:], in_=PFM[ch + g]), reads=["PFM"], writes=[nm])
            if "a" not in _ld:
                S.dma("sp", lambda e, g=g: e.dma_start(out=vsa[:, :, 0:128], in_=ptm_v[:, :, 2048 + g * 128:2048 + (g + 1) * 128]), reads=["PTM"], writes=["vsa"])
                S.dma("sp", lambda e, g=g: e.dma_start(out=vwa[:, :, 0:128], in_=ptm_v[:, :, 2304 + g * 128:2304 + (g + 1) * 128]), reads=["PTM"], writes=["vwa"])
            if "m" not in _sk:
                S.op("dve", lambda e: e.memset(vsa[:, :, 128:130], 1.0), reads=["vsa"], writes=["vsa"])
                S.op("dve", lambda e: e.memset(vwa[:, :, 128:130], 1.0), reads=["vwa"], writes=["vwa"])
            if "e2a" in os.environ.get("K_ESKIP", ""):
                continue
            with ExitStack() as _es:
                sc_ps = _es.enter_context(ps("sc_ps", [128, 2, 512], F32))
                oc_ps = _es.enter_context(ps("oc_ps", [128, 4, 256], F32))
                tps = _es.enter_context(ps("tps", [32, 2, 128], BF16))
                pc = _es.enter_context(sb("pc", [128, 2, 512], BF16))
                rc = _es.enter_context(sb("rc", [128, 4], F32))
                imp = _es.enter_context(sb("imp", [128, 32], F32))
                score = _es.enter_context(sb("score", [128, 32], F32))
                sc2 = _es.enter_context(sb("sc2", [128, 32], F32))
                m8 = _es.enter_context(sb("m8", [128, 16], F32))
                selneg = _es.enter_context(sb("selneg", [128, 32], BF16))
                coef = _es.enter_context(sb("coef", [128, 4], F32))
                for qt in range(16):
                    b = qt % 2
                    qsl = slice(qt * 128, (qt + 1) * 128)
                    S.op("pe", lambda e, b=b, qsl=qsl: e.matmul(sc_ps[:, b, :].rearrange("p (h t) -> p h t", h=4), lhsT=kcmpT2[:, g, :], rhs=qT4[:, :, qsl],
                                                                start=True, stop=False), reads=["kcmpT", "qT4"], writes=[("sc_ps", b)])
                    S.op("pe", lambda e, b=b, qsl=qsl: e.matmul(sc_ps[:, b, :].rearrange("p (h t) -> p h t", h=4), lhsT=identb[:, :],
                                                                rhs=cmask[:, qsl].unsqueeze(1).to_broadcast([128, 4, 128]), start=False, stop=True),
                         reads=["identb", "cmask"], writes=[("sc_ps", b)])
                    S.op("act", lambda e, b=b: e.activation(out=pc[:, b, :], in_=sc_ps[:, b, :], func=AF.Exp), reads=[("sc_ps", b)], writes=[("pc", b)])
                    for h in range(4):
                        S.op("pe", lambda e, b=b, h=h: e.matmul(oc_ps[:, h, 0:162], lhsT=pc[:, b, h * 128:(h + 1) * 128], rhs=vcaug2[:, g, 0:162],
                                                                start=True, stop=True), reads=[("pc", b), "vcaug"], writes=["oc_ps"])
                    for hh in (0, 2):
                        S.op("dve", lambda e, hh=hh: e.tensor_scalar(out=rc[:, hh:hh + 2], in0=oc_ps[:, hh:hh + 2, 128], scalar1=1e-30, scalar2=None, op0=ALU.max),
                             reads=["oc_ps"], writes=["rc"])
                    S.op("dve", lambda e: e.reciprocal(out=rc[:], in_=rc[:]), reads=["rc"], writes=["rc"])
                    S.op("act", lambda e, qt=qt, g=g: e.activation(out=gts[:, qt, :], in_=ngs[:, qt, g * 12:(g + 1) * 12], func=AF.Sigmoid), reads=["ngs"], writes=["gts"])
                    for h in range(4):
                        if h == 0:
                            S.op("dve", lambda e: e.tensor_scalar(out=imp[:], in0=oc_ps[:, 0, 129:161], scalar1=rc[:, 0:1], scalar2=None, op0=ALU.mult),
                                 reads=["oc_ps", "rc"], writes=["imp"])
                        else:
                            S.op("dve", lambda e, h=h: e.scalar_tensor_tensor(out=imp[:], in0=oc_ps[:, h, 129:161], scalar=rc[:, h:h + 1], in1=imp[:],
                                                                              op0=ALU.mult, op1=ALU.add), reads=["oc_ps", "rc", "imp"], writes=["imp"])
                    S.op("dve", lambda e, qt=qt: e.tensor_tensor(out=coef[:], in0=rc[:], in1=gts[:, qt, 0:12:3], op=ALU.mult), reads=["rc", "gts"], writes=["coef"])
                    for h in range(4):
                        S.op("dve", lambda e, h=h, qt=qt: e.tensor_scalar(out=ocmp[:, qt, h, :], in0=oc_ps[:, h, 0:128], scalar1=coef[:, h:h + 1], scalar2=None,
                                                                          op0=ALU.mult), reads=["oc_ps", "coef"], writes=["ocmp"])
                    S.op("dve", lambda e, qt=qt: e.tensor_tensor(out=score[:], in0=imp[:], in1=valid[:, qt, :], op=ALU.mult), reads=["imp", "valid"], writes=["score"])
                    S.op("dve", lambda e, qt=qt: e.tensor_tensor(out=score[:], in0=score[:], in1=ctab[:, qt, :], op=ALU.add), reads=["score", "ctab"], writes=["score"])
                    S.op("dve", lambda e: e.max(out=m8[:, 0:8], in_=score[:]), reads=["score"], writes=["m8a"])
                    S.op("dve", lambda e: e.match_replace(out=sc2[:], in_to_replace=m8[:, 0:8], in_values=score[:], imm_value=-2.0),
                         reads=["score", "m8a"], writes=["sc2"])
                    S.op("dve", lambda e: e.max(out=m8[:, 8:16], in_=sc2[:]), reads=["sc2"], writes=["m8b"])
                    S.op("dve", lambda e: e.tensor_scalar(out=selneg[:], in0=score[:], scalar1=m8[:, 15:16], scalar2=NEG, op0=ALU.is_lt, op1=ALU.mult),
                         reads=["score", "m8b"], writes=["selneg"])
                    S.op("pe", lambda e, b=b: e.transpose(tps[:, 0, :], selneg[:], identb[:]), reads=["selneg", "identb"], writes=["tps"])
                    S.op("act", lambda e, b=b, qsl=qsl: e.copy(out=selnegT[:, qsl], in_=tps[:, 0, :]), reads=["tps"], writes=["selnegT"])
                S.barrier()
            if "e2b" in os.environ.get("K_ESKIP", ""):
                continue
            with ExitStack() as _es:
                ss_ps = _es.enter_context(ps("ss_ps", [128, 2, 512], F32))
                os_ps = _es.enter_context(ps("os_ps", [128, 4, 256], F32))
                ow_ps = _es.enter_context(ps("ow_ps", [128, 4, 256], F32))
                tpo = _es.enter_context(ps("tpo", [128, 4, 128], BF16))
                pp = _es.enter_context(sb("pp", [128, 2, 512], BF16))
                rs = _es.enter_context(sb("rs", [128, 4], F32))
                rw = _es.enter_context(sb("rw", [128, 4], F32))
                acc = _es.enter_context(sb("acc", [128, 4, 128], F32))
                ob4 = _es.enter_context(sb("ob4", [128, 4, 128], BF16))
                n = 0
                for qt in range(16):
                    qsl = slice(qt * 128, (qt + 1) * 128)
                    for br, (kTt, knm, va, vnm, o_ps, kts) in enumerate([
                            (ksT, "ksT", vsa, "vsa", os_ps, list(range(0, qt + 1))),
                            (kwT, "kwT", vwa, "vwa", ow_ps, list(range(max(0, qt - 4), qt + 1)))]):
                        opk = "os_ps" if br == 0 else "ow_ps"
                        for kt in kts:
                            b = n % 2
                            n += 1
                            ksl = slice(kt * 128, (kt + 1) * 128)
                            extra = []
                            if br == 0:
                                extra.append((esel[:, kt, :], selnegT[:, qsl].unsqueeze(1).to_broadcast([32, 4, 128]), ["esel", "selnegT"]))
                            if kt == qt:
                                extra.append((identb[:], caus[:].unsqueeze(1).to_broadcast([128, 4, 128]), ["identb", "caus"]))
                            if br == 1 and kt == qt - 4:
                                extra.append((identb[:], anti[:].unsqueeze(1).to_broadcast([128, 4, 128]), ["identb", "anti"]))
                            outv = ss_ps[:, b, :].rearrange("p (h t) -> p h t", h=4)
                            S.op("pe", lambda e, outv=outv, kTt=kTt, ksl=ksl, qsl=qsl, last=(len(extra) == 0): e.matmul(
                                outv, lhsT=kTt[:, ksl], rhs=qT4[:, :, qsl], start=True, stop=last), reads=[knm, "qT4"], writes=[("ss_ps", b)])
                            for xi, (l_, r_, rd) in enumerate(extra):
                                S.op("pe", lambda e, outv=outv, l_=l_, r_=r_, last=(xi == len(extra) - 1): e.matmul(outv, lhsT=l_, rhs=r_, start=False, stop=last),
                                     reads=rd, writes=[("ss_ps", b)])
                            S.op("act", lambda e, b=b: e.activation(out=pp[:, b, :], in_=ss_ps[:, b, :], func=AF.Exp), reads=[("ss_ps", b)], writes=[("pp", b)])
                            for h in range(4):
                                S.op("pe", lambda e, b=b, h=h, kt=kt, va=va, o_ps=o_ps, first=(kt == kts[0]), lastk=(kt == kts[-1]): e.matmul(
                                    o_ps[:, h, 0:130], lhsT=pp[:, b, h * 128:(h + 1) * 128], rhs=va[:, kt, :], start=(first and h % 2 == 0), stop=lastk, skip_group_check=True),
                                    reads=[("pp", b), vnm], writes=[opk])
                    for hh in (0, 2):
                        S.op("dve", lambda e, hh=hh: e.reciprocal(out=rs[:, hh:hh + 2], in_=os_ps[:, hh:hh + 2, 128]), reads=["os_ps"], writes=["rs"])
                        S.op("dve", lambda e, hh=hh: e.reciprocal(out=rw[:, hh:hh + 2], in_=ow_ps[:, hh:hh + 2, 128]), reads=["ow_ps"], writes=["rw"])
                    S.op("dve", lambda e, qt=qt: e.tensor_tensor(out=rs[:], in0=rs[:], in1=gts[:, qt, 1:12:3], op=ALU.mult), reads=["rs", "gts"], writes=["rs"])
                    S.op("dve", lambda e, qt=qt: e.tensor_tensor(out=rw[:], in0=rw[:], in1=gts[:, qt, 2:12:3], op=ALU.mult), reads=["rw", "gts"], writes=["rw"])
                    for h in range(4):
                        S.op("dve", lambda e, h=h: e.tensor_scalar(out=acc[:, h, :], in0=os_ps[:, h, 0:128], scalar1=rs[:, h:h + 1], scalar2=None, op0=ALU.mult),
                             reads=["os_ps", "rs"], writes=["acc"])
                        S.op("dve", lambda e, h=h: e.scalar_tensor_tensor(out=acc[:, h, :], in0=ow_ps[:, h, 0:128], scalar=rw[:, h:h + 1], in1=acc[:, h, :],
                                                                          op0=ALU.mult, op1=ALU.add), reads=["ow_ps", "rw", "acc"], writes=["acc"])
                    S.op("pool", lambda e, qt=qt: e.tensor_tensor(out=ob4[:], in0=acc[:], in1=ocmp[:, qt], op=ALU.add), reads=["acc", "ocmp"], writes=["ob4"])
                    for h in range(4):
                        S.op("pe", lambda e, h=h: e.transpose(tpo[:, h, :], ob4[:, h, :], identb[:]), reads=["ob4", "identb"], writes=["tpo"])
                    S.op("act", lambda e, g=g, qsl=qsl: e.copy(out=ocatT[:, 8 + 4 * g:12 + 4 * g, qsl], in_=tpo[:]), reads=["tpo"], writes=["ocatT"])
                S.barrier()
        S.barrier()


def stage_F(nc, S, io, ocatT, g1row, X1):
    sb, ps = _namers(nc)
    wout = io["w_out"].rearrange("(k p) n -> p k n", p=128)
    with ExitStack() as _es:
        wo = _es.enter_context(sb("wo", [128, 2, 16, 512], BF16))
        xr = _es.enter_context(sb("xr", [128, 2, 512], F32))
        x1t = _es.enter_context(sb("x1t", [128, 2, 512], F32))
        mx_ps = _es.enter_context(ps("mx_ps", [128, 2, 512], F32))
        n = 0
        for cb in range(4):
            wb = cb % 2
            csl = slice(cb * 512, (cb + 1) * 512)
            S.dma("pool", lambda e, wb=wb, csl=csl: e.dma_start(out=wo[:, wb], in_=wout[:, :, csl]), writes=[("wo", wb)])
            for i in range(16):
                b = n % 2
                n += 1
                isl = slice(i * 128, (i + 1) * 128)
                S.dma("sp", lambda e, b=b, isl=isl, csl=csl: e.dma_start(out=xr[:, b, :], in_=io["x"][isl, csl]), writes=[("xr", b)])
                for k in range(16):
                    S.op("pe", lambda e, b=b, wb=wb, k=k, isl=isl: e.matmul(mx_ps[:, b, :], lhsT=ocatT[:, k, isl], rhs=wo[:, wb, k, :],
                                                                            start=(k == 0), stop=(k == 15)), reads=["ocatT", ("wo", wb)], writes=[("mx_ps", b)])
                S.op("dve", lambda e, b=b, csl=csl: e.tensor_tensor(out=x1t[:, b, :], in0=mx_ps[:, b, :], in1=g1row[:, csl], op=ALU.mult),
                     reads=[("mx_ps", b), "g1row"], writes=[("x1t", b)])
                S.op("pool", lambda e, b=b: e.tensor_tensor(out=x1t[:, b, :], in0=x1t[:, b, :], in1=xr[:, b, :], op=ALU.add),
                     reads=[("x1t", b), ("xr", b)], writes=[("x1t", b)])
                S.dma("sp", lambda e, b=b, isl=isl, csl=csl: e.dma_start(out=X1[isl, csl], in_=x1t[:, b, :]), reads=[("x1t", b)], writes=["X1"])
        S.barrier()


def stage_G(nc, S, io, X1, XS, a2row, sh2row, identf, slots, wab, dbgout, final_toks):
    sb, ps = _namers(nc)
    with ExitStack() as _es:
        A_ = _es.enter_context
        x1 = A_(sb("x1", [128, 2, D], F32)); h2f = A_(sb("h2f", [128, 2, D], F32)); h2b = A_(sb("h2b", [128, 16, D], BF16))
        junk = A_(sb("junk2", [128, D], BF16)); h2Tf = A_(sb("h2Tf", [128, 2, 16, 128], F32))
        wr = A_(sb("wr", [128, 16, 72], F32)); brt = A_(sb("brt", [128, 72], F32))
        lstrict = A_(sb("lstrict", [128, 128], BF16)); ones = A_(sb("ones", [128, 128], BF16)); ebase = A_(sb("ebase", [128, 64], F32))
        ss2 = A_(sb("ss2", [128, 16], F32)); rstd2 = A_(sb("rstd2", [128, 16], F32))
        lg = A_(sb("lg", [128, 16, 72], F32)); gm0 = A_(sb("gm0", [128, 16], F32)); dg = A_(sb("dg", [128, 16, 8], F32))
        eg = A_(sb("eg", [128, 16, 8], F32)); gsum = A_(sb("gsum", [128, 16], F32)); onehot = A_(sb("onehot", [128, 16, 8], F32))
        tmp = A_(sb("tmp88", [128, 16, 8, 8], F32)); leg = A_(sb("leg", [128, 16, 8], F32)); m8r = A_(sb("m8r", [128, 16, 8], F32))
        selloc = A_(sb("selloc", [128, 16, 8], F32)); wl = A_(sb("wl", [128, 16, 8], F32)); den = A_(sb("den", [128, 16], F32))
        wfull = A_(sb("wfull", [128, 16, 64], F32)); Af = A_(sb("Af", [128, 16, 64], F32)); Ab = A_(sb("Ab", [128, 16, 64], BF16))
        tot = A_(sb("tot", [128, 64], F32)); cnt = A_(sb("cnt", [128, 16, 64], F32)); key = A_(sb("key", [128, 16, 64], F32))
        m8k = A_(sb("m8k", [128, 16, 8], F32)); slf = A_(sb("slf", [128, 16, 2], F32)); eq = A_(sb("eq", [128, 16, 64], F32))
        tpf = A_(ps("tpf", [128, 2, 4, 128], F32)); lg_ps = A_(ps("lg_ps", [128, 2, 512], F32)); cnt_ps = A_(ps("cnt_ps", [128, 2, 512], F32))
        S.dma("sp", lambda e: e.dma_start(out=wr[:], in_=io["w_rt"].rearrange("(k p) n -> p k n", p=128)), writes=["wr"])
        for nm, t in [("b_rt", brt), ("lstrict", lstrict), ("ones", ones), ("ebase", ebase)]:
            S.dma("sp", lambda e, nm=nm, t=t: e.dma_start(out=t[:], in_=io[nm]), writes=[nm])
        S.op("dve", lambda e: e.memset(ss2[:], 0.0), writes=["ss2"])
        S.op("dve", lambda e: e.memset(tot[:], 0.0), writes=["tot"])
        V = lambda fn, r, w: S.op("dve", fn, reads=r, writes=w)
        for i in range(16):
            b = i % 2
            isl = slice(i * 128, (i + 1) * 128)
            S.dma("sp", lambda e, b=b, isl=isl: e.dma_start(out=x1[:, b], in_=X1[isl, :]), reads=["X1"], writes=[("x1", b)])
            S.op("act", lambda e, b=b, i=i: e.activation(out=junk[:], in_=x1[:, b], func=AF.Square, accum_out=ss2[:, i:i + 1]),
                 reads=[("x1", b), "ss2"], writes=["junk", ("ss2", i)])
            S.op("act", lambda e, i=i: e.activation(out=rstd2[:, i:i + 1], in_=ss2[:, i:i + 1], func=AF.Sqrt, bias=epsc[:, 0:1], scale=1.0 / D),
                 reads=[("ss2", i), "epsc"], writes=[("rstd2", i)])
            V(lambda e, i=i: e.reciprocal(out=rstd2[:, i:i + 1], in_=rstd2[:, i:i + 1]), [("rstd2", i)], [("rstd2", i)])
            V(lambda e, b=b, i=i: e.scalar_tensor_tensor(out=h2f[:, b], in0=x1[:, b], scalar=rstd2[:, i:i + 1], in1=a2row[:], op0=ALU.mult, op1=ALU.mult),
              [("x1", b), ("rstd2", i), "a2row"], [("h2f", b)])
            S.op("pool", lambda e, b=b: e.tensor_tensor(out=h2f[:, b], in0=h2f[:, b], in1=sh2row[:], op=ALU.add), reads=[("h2f", b), "sh2row"], writes=[("h2f", b)])
            S.op("act", lambda e, i=i, b=b: e.copy(out=h2b[:, i], in_=h2f[:, b]), reads=[("h2f", b)], writes=[("h2b", i)])
            for k4 in range(4):
                pb = k4 % 2
                for kk in range(4):
                    k = k4 * 4 + kk
                    S.op("pe", lambda e, pb=pb, kk=kk, k=k, b=b: e.transpose(tpf[:, pb, kk, :], h2f[:, b, k * 128:(k + 1) * 128], identf[:]),
                         reads=[("h2f", b), "identf"], writes=[("tpf", pb)])
                V(lambda e, pb=pb, k4=k4, b=b: e.tensor_copy(out=h2Tf[:, b, k4 * 4:(k4 + 1) * 4, :], in_=tpf[:, pb]), [("tpf", pb)], [("h2Tf", b)])
            for k in range(16):
                S.op("pe", lambda e, k=k, b=b: e.matmul(lg_ps[:, b, 0:72], lhsT=h2Tf[:, b, k, :], rhs=wr[:, k, :], start=(k == 0), stop=(k == 15)),
                     reads=[("h2Tf", b), "wr"], writes=[("lg_ps", b)])
            V(lambda e, i=i, b=b: e.tensor_tensor(out=lg[:, i, :], in0=lg_ps[:, b, 0:72], in1=brt[:], op=ALU.add), [("lg_ps", b), "b_rt"], ["lg"])
        B8 = lambda ap: ap.unsqueeze(2).to_broadcast([128, 16, 8])
        V(lambda e: e.tensor_reduce(out=gm0[:], in_=lg[:, :, 0:8], axis=AX.X, op=ALU.max), ["lg"], ["gm0"])
        V(lambda e: e.tensor_tensor(out=dg[:], in0=lg[:, :, 0:8], in1=B8(gm0[:]), op=ALU.subtract), ["lg", "gm0"], ["dg"])
        S.op("act", lambda e: e.activation(out=eg[:], in_=dg[:], func=AF.Exp), reads=["dg"], writes=["eg"])
        V(lambda e: e.tensor_reduce(out=gsum[:], in_=eg[:], axis=AX.X, op=ALU.add), ["eg"], ["gsum"])
        V(lambda e: e.tensor_scalar(out=onehot[:], in0=dg[:], scalar1=0.0, scalar2=None, op0=ALU.is_ge), ["dg"], ["onehot"])
        V(lambda e: e.tensor_tensor(out=tmp[:], in0=lg[:, :, 8:72].rearrange("p i (g j) -> p i g j", g=8),
                                    in1=onehot[:].unsqueeze(3).to_broadcast([128, 16, 8, 8]), op=ALU.mult), ["lg", "onehot"], ["tmp"])
        V(lambda e: e.tensor_reduce(out=leg[:], in_=tmp[:].rearrange("p i g j -> p i j g"), axis=AX.X, op=ALU.add), ["tmp"], ["leg"])
        for i in range(16):
            V(lambda e, i=i: e.max(out=m8r[:, i, :], in_=leg[:, i, :]), ["leg"], ["m8r"])
        V(lambda e: e.tensor_tensor(out=selloc[:], in0=leg[:], in1=m8r[:, :, 1:2].to_broadcast([128, 16, 8]), op=ALU.is_ge), ["leg", "m8r"], ["selloc"])
        V(lambda e: e.tensor_tensor(out=dg[:], in0=leg[:], in1=m8r[:, :, 0:1].to_broadcast([128, 16, 8]), op=ALU.subtract), ["leg", "m8r", "eg", "onehot"], ["dg2"])
        S.op("act", lambda e: e.activation(out=wl[:], in_=dg[:], func=AF.Exp), reads=["dg2"], writes=["wl"])
        V(lambda e: e.tensor_tensor(out=wl[:], in0=wl[:], in1=selloc[:], op=ALU.mult), ["wl", "selloc"], ["wl"])
        V(lambda e: e.tensor_reduce(out=den[:], in_=wl[:], axis=AX.X, op=ALU.add), ["wl"], ["den"])
        V(lambda e: e.tensor_tensor(out=den[:], in0=den[:], in1=gsum[:], op=ALU.mult), ["den", "gsum"], ["den"])
        V(lambda e: e.reciprocal(out=den[:], in_=den[:]), ["den"], ["den"])
        V(lambda e: e.tensor_tensor(out=wl[:], in0=wl[:], in1=B8(den[:]), op=ALU.mult), ["wl", "den"], ["wl"])
        V(lambda e: e.tensor_tensor(out=wfull[:].rearrange("p i (g j) -> p i g j", g=8), in0=onehot[:].unsqueeze(3).to_broadcast([128, 16, 8, 8]),
                                    in1=wl[:].unsqueeze(2).to_broadcast([128, 16, 8, 8]), op=ALU.mult), ["onehot", "wl"], ["wfull"])
        V(lambda e: e.tensor_scalar(out=Af[:], in0=wfull[:], scalar1=0.0, scalar2=None, op0=ALU.is_gt), ["wfull"], ["Af"])
        V(lambda e: e.tensor_copy(out=Ab[:], in_=Af[:]), ["Af"], ["Ab"])
        for i in range(16):
            b = i % 2
            S.op("pe", lambda e, i=i, b=b: e.matmul(cnt_ps[:, b, 0:64], lhsT=lstrict[:], rhs=Ab[:, i, :], start=True, stop=True),
                 reads=["lstrict", "Ab"], writes=[("cnt_ps", b)])
            S.op("pe", lambda e, i=i, b=b: e.matmul(cnt_ps[:, b, 64:128], lhsT=ones[:], rhs=Ab[:, i, :], start=True, stop=True),
                 reads=["ones", "Ab"], writes=[("cnt_ps", b)])
            V(lambda e, i=i, b=b: e.tensor_tensor(out=cnt[:, i, :], in0=cnt_ps[:, b, 0:64], in1=tot[:], op=ALU.add), [("cnt_ps", b), "tot"], ["cnt"])
            V(lambda e, b=b: e.tensor_tensor(out=tot[:], in0=cnt_ps[:, b, 64:128], in1=tot[:], op=ALU.add), [("cnt_ps", b), "tot"], ["tot"])
        V(lambda e: e.tensor_scalar(out=cnt[:], in0=cnt[:], scalar1=float(CAP - 1), scalar2=None, op0=ALU.min), ["cnt"], ["cnt"])
        V(lambda e: e.tensor_tensor(out=key[:], in0=cnt[:], in1=ebase[:].unsqueeze(1).to_broadcast([128, 16, 64]), op=ALU.add), ["cnt", "ebase"], ["key"])
        V(lambda e: e.tensor_tensor(out=key[:], in0=key[:], in1=Af[:], op=ALU.mult), ["key", "Af"], ["key"])
        for i in range(16):
            V(lambda e, i=i: e.max(out=m8k[:, i, :], in_=key[:, i, :]), ["key"], ["m8k"])
        V(lambda e: e.tensor_scalar(out=slf[:], in0=m8k[:, :, 0:2], scalar1=-1.0, scalar2=None, op0=ALU.add), ["m8k"], ["slf"])
        V(lambda e: e.tensor_copy(out=slots[:], in_=slf[:]), ["slf"], ["slots"])
        for j in range(2):
            V(lambda e, j=j: e.tensor_tensor(out=eq[:], in0=key[:], in1=m8k[:, :, j:j + 1].to_broadcast([128, 16, 64]), op=ALU.is_equal), ["key", "m8k"], ["eq"])
            V(lambda e: e.tensor_tensor(out=eq[:], in0=eq[:], in1=wfull[:], op=ALU.mult), ["eq", "wfull"], ["eq"])
            V(lambda e, j=j: e.tensor_reduce(out=wab[:, :, j], in_=eq[:], axis=AX.X, op=ALU.add), ["eq"], ["wab"])
        for i in range(16):
            for j in range(2):
                S.dma("pool", lambda e, i=i, j=j: e.indirect_dma_start(
                    out=XS, out_offset=bass.IndirectOffsetOnAxis(ap=slots[:, i, j:j + 1], axis=0), in_=h2b[:, i, :], in_offset=None),
                    reads=[("h2b", i), "slots"], writes=["XS"])
        S.barrier()


def stage_H(nc, S, io, XS, YS, identb):
    sb, ps = _namers(nc)
    with ExitStack() as _es:
        wg = _es.enter_context(sb("wg", [128, 2, 16, 512], BF16))
        wu = _es.enter_context(sb("wu", [128, 2, 16, 512], BF16))
        wd = _es.enter_context(sb("wd", [128, 2, 4, D], BF16))
        xs = _es.enter_context(sb("xs", [128, 2, RB, D], BF16))
        xsT = _es.enter_context(sb("xsT", [128, 2, 16, CAP], BF16))
        sgh = _es.enter_context(sb("sgh", [128, 2, CAP], F32))
        aT = _es.enter_context(sb("aT", [128, 4, CAP], BF16))
        ysb = _es.enter_context(sb("ysb", [128, 2, D], BF16))
        tpx = _es.enter_context(ps("tpx", [128, 4, 1024], BF16))
        gu_ps = _es.enter_context(ps("gu_ps", [128, 2, 2, CAP], F32))
        y_ps = _es.enter_context(ps("y_ps", [128, 2, 512], F32))

        def load(ex):
            wb = ex % 2
            S.dma("pool", lambda e: e.dma_start(out=wg[:, wb], in_=io["w_gate"][ex].rearrange("(k p) n -> p k n", p=128)), writes=[("wg", wb)])
            S.dma("pool", lambda e: e.dma_start(out=wu[:, wb], in_=io["w_up"][ex].rearrange("(k p) n -> p k n", p=128)), writes=[("wu", wb)])
            S.dma("pool", lambda e: e.dma_start(out=wd[:, wb], in_=io["w_down"][ex].rearrange("(c p) n -> p c n", p=128)), writes=[("wd", wb)])
            S.dma("sp", lambda e: e.dma_start(out=xs[:, wb], in_=XS[ex * CAP:(ex + 1) * CAP, :].rearrange("(r p) d -> p r d", p=128)),
                  reads=["XS"], writes=[("xs", wb)])

        def transposes(ex):
            wb = ex % 2
            for r in range(RB):
                for k8 in range(2):
                    j = (r * 2 + k8) % 4
                    for kk in range(8):
                        k = k8 * 8 + kk
                        S.op("pe", lambda e, kk=kk, k=k: e.transpose(tpx[:, j, kk * 128:(kk + 1) * 128], xs[:, wb, r, k * 128:(k + 1) * 128], identb[:]),
                             reads=[("xs", wb), "identb"], writes=[("tpx", j)])
                    dst = xsT[:, wb, k8 * 8:(k8 + 1) * 8, r * 128:(r + 1) * 128]
                    src = tpx[:, j, :].rearrange("p (a b) -> p a b", a=8)
                    if j % 2 == 0:
                        S.op("act", lambda e, dst=dst, src=src: e.copy(out=dst, in_=src), reads=[("tpx", j)], writes=[("xsT", wb)])
                    else:
                        S.op("dve", lambda e, dst=dst, src=src: e.tensor_copy(out=dst, in_=src), reads=[("tpx", j)], writes=[("xsT", wb)])

        def gate_up(ex):
            wb = ex % 2
            for hc in range(4):
                gb = hc % 2
                for which, w in enumerate([wg, wu]):
                    for k in range(16):
                        S.op("pe", lambda e, which=which, w=w, k=k: e.matmul(
                            gu_ps[:, gb, which, :], lhsT=w[:, wb, k, hc * 128:(hc + 1) * 128], rhs=xsT[:, wb, k, :], start=(k == 0), stop=(k == 15)),
                            reads=[("wg", wb), ("wu", wb), ("xsT", wb)], writes=[("gu_ps", gb)])
                S.op("act", lambda e: e.activation(out=sgh[:, gb, :], in_=gu_ps[:, gb, 0, :], func=AF.Silu), reads=[("gu_ps", gb)], writes=[("sgh", gb)])
                S.op("dve", lambda e: e.tensor_tensor(out=aT[:, hc, :], in0=sgh[:, gb, :], in1=gu_ps[:, gb, 1, :], op=ALU.mult),
                     reads=[("sgh", gb), ("gu_ps", gb)], writes=["aT"])

        ny = [0]

        def down(ex):
            wb = ex % 2
            for r in range(RB):
                yb = ny[0] % 2
                ny[0] += 1
                for cb in range(4):
                    pb = cb % 2
                    for hc in range(4):
                        S.op("pe", lambda e, hc=hc: e.matmul(
                            y_ps[:, pb, :], lhsT=aT[:, hc, r * 128:(r + 1) * 128], rhs=wd[:, wb, hc, cb * 512:(cb + 1) * 512], start=(hc == 0), stop=(hc == 3)),
                            reads=["aT", ("wd", wb)], writes=[("y_ps", pb)])
                    if cb % 2 == 0:
                        S.op("act", lambda e: e.copy(out=ysb[:, yb, cb * 512:(cb + 1) * 512], in_=y_ps[:, pb, :]), reads=[("y_ps", pb)], writes=[("ysb", yb)])
                    else:
                        S.op("dve", lambda e: e.tensor_copy(out=ysb[:, yb, cb * 512:(cb + 1) * 512], in_=y_ps[:, pb, :]), reads=[("y_ps", pb)], writes=[("ysb", yb)])
                S.dma("sp", lambda e: e.dma_start(out=YS[ex * CAP + r * 128:ex * CAP + (r + 1) * 128, :], in_=ysb[:, yb]),
                      reads=[("ysb", yb)], writes=["YS"])

        load(0)
        transposes(0)
        for ex in range(64):
            if ex + 1 < 64:
                load(ex + 1)
            gate_up(ex)
            if ex + 1 < 64:
                transposes(ex + 1)
            down(ex)
        S.barrier()


def stage_I(nc, S, io, X1, YS, g2row, slots, wab, out):
    sb, ps = _namers(nc)
    toks = []
    with ExitStack() as _es:
        ya = _es.enter_context(sb("ya", [128, 2, D], BF16))
        yb_ = _es.enter_context(sb("yb_", [128, 2, D], BF16))
        x1i = _es.enter_context(sb("x1i", [128, 2, D], F32))
        mo = _es.enter_context(sb("mo", [128, 2, D], F32))
        ot = _es.enter_context(sb("ot", [128, 2, D], F32))
        fg = _es.enter_context(sb("fg", [128, D], F32))
        junk = _es.enter_context(sb("junk3", [128, D], BF16))
        ss3 = _es.enter_context(sb("ss3", [128, 16], F32))
        rstd3 = _es.enter_context(sb("rstd3", [128, 16], F32))
        S.dma("sp", lambda e: e.dma_start(out=fg[:], in_=io["fg_row"]), writes=["fg"])
        S.op("dve", lambda e: e.memset(ss3[:], 0.0), writes=["ss3"])
        for i in range(16):
            b = i % 2
            isl = slice(i * 128, (i + 1) * 128)
            S.dma("pool", lambda e, b=b, i=i: e.indirect_dma_start(out=ya[:, b, :], out_offset=None, in_=YS,
                                                                   in_offset=bass.IndirectOffsetOnAxis(ap=slots[:, i, 0:1], axis=0)),
                  reads=["YS", "slots"], writes=[("ya", b)])
            S.dma("pool", lambda e, b=b, i=i: e.indirect_dma_start(out=yb_[:, b, :], out_offset=None, in_=YS,
                                                                   in_offset=bass.IndirectOffsetOnAxis(ap=slots[:, i, 1:2], axis=0)),
                  reads=["YS", "slots"], writes=[("yb", b)])
            S.dma("sp", lambda e, b=b, isl=isl: e.dma_start(out=x1i[:, b], in_=X1[isl, :]), reads=["X1"], writes=[("x1i", b)])
            S.op("dve", lambda e, b=b, i=i: e.tensor_scalar(out=mo[:, b], in0=ya[:, b], scalar1=wab[:, i, 0:1], scalar2=None, op0=ALU.mult),
                 reads=[("ya", b), "wab"], writes=[("mo", b)])
            S.op("dve", lambda e, b=b, i=i: e.scalar_tensor_tensor(out=mo[:, b], in0=yb_[:, b], scalar=wab[:, i, 1:2], in1=mo[:, b], op0=ALU.mult, op1=ALU.add),
                 reads=[("yb", b), "wab", ("mo", b)], writes=[("mo", b)])
            S.op("pool", lambda e, b=b: e.tensor_tensor(out=mo[:, b], in0=mo[:, b], in1=g2row[:], op=ALU.mult), reads=[("mo", b), "g2row"], writes=[("mo", b)])
            S.op("pool", lambda e, b=b: e.tensor_tensor(out=mo[:, b], in0=mo[:, b], in1=x1i[:, b], op=ALU.add), reads=[("mo", b), ("x1i", b)], writes=[("mo", b)])
            S.op("act", lambda e, i=i, b=b: e.activation(out=junk[:], in_=mo[:, b], func=AF.Square, accum_out=ss3[:, i:i + 1]), reads=[("mo", b), "ss3"], writes=["junk", ("ss3", i)])
            S.op("act", lambda e, i=i: e.activation(out=rstd3[:, i:i + 1], in_=ss3[:, i:i + 1], func=AF.Sqrt, bias=epsc[:, 0:1], scale=1.0 / D),
                 reads=[("ss3", i), "epsc"], writes=[("rstd3", i)])
            S.op("dve", lambda e, i=i: e.reciprocal(out=rstd3[:, i:i + 1], in_=rstd3[:, i:i + 1]), reads=[("rstd3", i)], writes=[("rstd3", i)])
            S.op("dve", lambda e, b=b, i=i: e.scalar_tensor_tensor(out=ot[:, b], in0=mo[:, b], scalar=rstd3[:, i:i + 1], in1=fg[:], op0=ALU.mult, op1=ALU.mult),
                 reads=[("mo", b), ("rstd3", i), "fg"], writes=[("ot", b)])
            toks.append(S.dma("sp", lambda e, b=b, isl=isl: e.dma_start(out=out[isl, :], in_=ot[:, b]), reads=[("ot", b)], writes=["out"]))
        S.barrier()
    return toks


def host_inputs(inp, b):
    f = lambda a: np.ascontiguousarray(a, dtype=np.float32)
    m = {}
    m["x"] = f(inp["x"][b])
    m["cT"] = f(inp["c"][b].reshape(16, 128).T)
    m["w_ada"] = f(inp["w_ada"][0])
    m["b_adaT"] = f(inp["b_ada"][0].reshape(96, 128).T)
    m["n1gT"] = f(inp["norm1_g"][0].reshape(16, 128).T)
    m["n2gT"] = f(inp["norm2_g"][0].reshape(16, 128).T)
    m["fg_row"] = f(np.broadcast_to(inp["final_g"][None, :], (128, D)))
    m["w_in"] = f(inp["w_in"][0])
    m["gng_row"] = f(np.broadcast_to(inp["ret_gn_g"][0][None, :], (128, 1024)))
    m["peT_k"] = f(inp["cmp_pos_k"][0].T)
    m["w1_k"] = f(inp["cmp_w1_k"][0])
    m["w2_k"] = f(inp["cmp_w2_k"][0])
    m["peT_v"] = f(inp["cmp_pos_v"][0].T)
    m["w1_v"] = f(inp["cmp_w1_v"][0])
    m["w2_v"] = f(inp["cmp_w2_v"][0])
    m["w_out"] = f(inp["w_out"][0])
    m["w_rt"] = f(np.concatenate([inp["w_grp"][0], inp["w_exp"][0]], axis=1))
    m["b_rt"] = f(np.broadcast_to(np.concatenate([inp["b_grp"][0], inp["b_exp"][0]])[None, :], (128, 72)))
    m["w_gate"] = f(inp["w_gate"][0])
    m["w_up"] = f(inp["w_up"][0])
    m["w_down"] = f(inp["w_down"][0])
    return m


def kernel(**inputs):
    inp = {k: np.asarray(v) for k, v in inputs.items()}
    nc = build_nc()
    consts = {k: v for k, v in make_consts().items() if not k.startswith("_")}
    shared = None
    in_maps = []
    for b in range(8):
        m = host_inputs(inp, b)
        if shared is None:
            shared = {k: m[k] for k in m if k not in ("x", "cT")}
        else:
            for k in shared:
                m[k] = shared[k]
        m.update(consts)
        in_maps.append(m)
    res = run_bass_kernel_spmd(nc, in_maps, core_ids=list(range(8)))
    return np.stack([np.asarray(r["out"], dtype=np.float32).reshape(T, D) for r in res.results], axis=0)
```

```python
import os
from contextlib import ExitStack
import numpy as np
import ml_dtypes
import concourse.bass as bass
import concourse.mybir as mybir
from concourse.bass_utils import run_bass_kernel_spmd

F32 = mybir.dt.float32
BF16 = mybir.dt.bfloat16
I32 = mybir.dt.int32
AF = mybir.ActivationFunctionType
ALU = mybir.AluOpType
AX = mybir.AxisListType

D = 2048
T = 2048
NT = 16
KC = 16
PROJ = 6680
CAP = 256
RB = CAP // 128
NEG = -30000.0
EPS = 1e-6

ENGS = ("pe", "act", "dve", "pool", "sp")


_UNIQ = [0]


def _namers(nc):
    def sb(name, shp, dt):
        _UNIQ[0] += 1
        return nc.sbuf_tensor("s%d_%s" % (_UNIQ[0], name), shp, dt)

    def ps(name, shp, dt):
        _UNIQ[0] += 1
        return nc.psum_tensor("p%d_%s" % (_UNIQ[0], name), shp, dt)
    return sb, ps


class Sched:
    NDMA = 24

    def __init__(self, nc):
        self.nc = nc
        self.handles = dict(pe=nc.tensor, act=nc.scalar, dve=nc.vector, pool=nc.gpsimd, sp=nc.sync)
        self.cnt = {e: 0 for e in ENGS}
        self.waited = {e: {} for e in ENGS}
        self.lastw = {}
        self.readers = {}
        self.sems = {}
        self.dma_sems = []
        self.dma_n = 0
        self.dma_q = [0, 0]
        self.dma_last = {}
        self._stack = []

    def open(self):
        for e in ENGS:
            cm = self.nc.semaphore("sem_" + e)
            self.sems[e] = cm.__enter__()
            self._stack.append(cm)
        for i in range(self.NDMA):
            cm = self.nc.semaphore("semd%d" % i)
            self.dma_sems.append(cm.__enter__())
            self._stack.append(cm)

    def close(self):
        for cm in reversed(self._stack):
            cm.__exit__(None, None, None)

    def _need(self, eng, tok, waits):
        if tok is None:
            return
        sem_id, val, src = tok
        if src == "pe" and eng == "pe":
            return
        w = self.waited[eng]
        if w.get(sem_id, 0) >= val:
            return
        w[sem_id] = val
        waits.append((sem_id, val))

    def _deps(self, eng, reads, writes):
        waits = []
        for b in reads:
            self._need(eng, self.lastw.get(b), waits)
        for b in writes:
            self._need(eng, self.lastw.get(b), waits)
            for t in self.readers.get(b, ()):
                self._need(eng, t, waits)
        return waits

    def _commit(self, tok, reads, writes):
        for b in reads:
            self.readers.setdefault(b, []).append(tok)
        for b in writes:
            self.lastw[b] = tok
            self.readers[b] = []

    def op(self, eng, fn, reads=(), writes=()):
        waits = self._deps(eng, reads, writes)
        self.cnt[eng] += 1
        tok = ("E" + eng, self.cnt[eng], eng)
        self._emit1(eng, waits, fn, ("E" + eng, 1))
        self._commit(tok, reads, writes)
        return tok

    def dma(self, eng, fn, reads=(), writes=()):
        half = self.NDMA // 2
        qi = 0 if eng == "pool" else 1
        j = self.dma_q[qi]
        self.dma_q[qi] += 1
        s = qi * half + (j % half)
        sid = "D%d" % s
        prev = self.dma_last.get(s, 0)
        waits = self._deps(eng, reads, writes)
        if prev and self.waited[eng].get(sid, 0) < prev:
            self.waited[eng][sid] = prev
            waits.append((sid, prev))
        val = prev + 16
        self.dma_last[s] = val
        tok = (sid, val, "dma")
        self._emit1(eng, waits, fn, (sid, 16))
        self._commit(tok, reads, writes)
        return tok

    def _emit1(self, engname, waits, fn, inc):
        e = self.handles[engname]
        if os.environ.get("K_TRACE"):
            print("TR", engname, self.cnt[engname], waits, inc, flush=True)
        for sid, val in waits:
            e.wait_ge(self._sem(sid), val)
        if fn is not None:
            ins = fn(e)
            ins.then_inc(self._sem(inc[0]), inc[1])

    def _sem(self, sid):
        if sid[0] == "E":
            return self.sems[sid[1:]]
        return self.dma_sems[int(sid[1:])]

    def barrier(self, engs=ENGS):
        toks = [("E" + e, self.cnt[e], e) for e in ENGS if self.cnt[e] > 0]
        toks += [("D%d" % s, v, "dma") for s, v in self.dma_last.items()]
        for e in engs:
            waits = []
            for t in toks:
                self._need(e, t, waits)
            self._emit1(e, waits, None, None)


def make_consts():
    bf = ml_dtypes.bfloat16
    c = {}
    half = 128
    inv = (10000.0 ** (-np.arange(half, dtype=np.float32) / half)).astype(np.float32)
    pos = np.arange(T, dtype=np.float32)
    ang = (inv[:, None] * pos[None, :]).astype(np.float32)
    c["cos"] = np.cos(ang).astype(np.float32)
    c["sin"] = np.sin(ang).astype(np.float32)
    c["identb"] = np.eye(128, dtype=np.float32).astype(bf)
    c["identf"] = np.eye(128, dtype=np.float32)
    H = 4
    lg = np.log1p(-np.exp2(-5.0 - np.arange(H, dtype=np.float64)))
    m = np.arange(128, dtype=np.float64)
    dm = np.zeros((128, H, 128), np.float32)
    for h in range(H):
        val = np.exp(lg[h] * (-m - 1.0))
        dm[:, h, :] = np.where(m[None, :] >= m[:, None], val[:, None], 0.0)
    c["dm"] = dm
    c["zeta"] = np.exp(lg[None, :] * (127.0 - m)[:, None]).astype(np.float32)
    c["qdec"] = np.exp(lg[None, :] * (m + 1.0)[:, None]).astype(np.float32)
    c["_cd"] = [float(np.exp(lg[h] * 128.0)) for h in range(H)]
    j = np.arange(128)
    caus = np.where(j[:, None] <= j[None, :], 0.0, NEG).astype(np.float32)
    anti = np.where(j[:, None] > j[None, :], 0.0, NEG).astype(np.float32)
    c["caus"] = caus.astype(bf)
    c["anti"] = anti.astype(bf)
    cc = np.arange(128)
    tt = np.arange(T)
    c["cmask"] = np.where(16 * cc[:, None] + 31 <= tt[None, :], 0.0, NEG).astype(np.float32).astype(bf)
    ss = np.arange(32)
    ov = ((16 * cc[:, None] < 64 * ss[None, :] + 64) & (16 * cc[:, None] + 32 > 64 * ss[None, :])).astype(np.float32)
    c["overlap"] = ov.astype(bf)
    es = np.zeros((32, 16, 128), np.float32)
    for kt in range(16):
        for p in range(128):
            es[2 * kt + p // 64, kt, p] = 1.0
    c["esel"] = es.astype(bf)
    cur = tt // 64
    valid = (ss[None, :] <= cur[:, None])
    forced = (ss[None, :] == 0) | (ss[None, :] == cur[:, None]) | (ss[None, :] == cur[:, None] - 1)
    ctab = np.where(valid, np.where(forced, 1e4, 0.0), -1.0).astype(np.float32)
    c["valid"] = np.ascontiguousarray(valid.astype(np.float32).reshape(16, 128, 32).transpose(1, 0, 2))
    c["ctab"] = np.ascontiguousarray(ctab.reshape(16, 128, 32).transpose(1, 0, 2))
    c["lstrict"] = (j[:, None] < j[None, :]).astype(np.float32).astype(bf)
    c["ones"] = np.ones((128, 128), np.float32).astype(bf)
    c["ebase"] = np.tile((np.arange(64, dtype=np.float32) * CAP + 1.0)[None, :], (128, 1))
    return c


CONST_SPECS = [
    ("cos", [128, T], F32), ("sin", [128, T], F32), ("identb", [128, 128], BF16), ("identf", [128, 128], F32),
    ("dm", [128, 4, 128], F32), ("zeta", [128, 4], F32), ("qdec", [128, 4], F32),
    ("caus", [128, 128], BF16), ("anti", [128, 128], BF16), ("cmask", [128, T], BF16), ("overlap", [128, 32], BF16),
    ("esel", [32, 16, 128], BF16), ("valid", [128, 16, 32], F32), ("ctab", [128, 16, 32], F32),
    ("lstrict", [128, 128], BF16), ("ones", [128, 128], BF16), ("ebase", [128, 64], F32),
]

IN_SPECS = [
    ("x", [T, D], F32), ("cT", [128, 16], F32), ("w_ada", [D, 6 * D], F32), ("b_adaT", [128, 96], F32),
    ("n1gT", [128, 16], F32), ("n2gT", [128, 16], F32), ("fg_row", [128, D], F32), ("w_in", [D, PROJ], F32),
    ("gng_row", [128, 1024], F32), ("peT_k", [128, 32], F32), ("w1_k", [4096, 256], F32), ("w2_k", [256, 128], F32),
    ("peT_v", [128, 32], F32), ("w1_v", [4096, 256], F32), ("w2_v", [256, 128], F32), ("w_out", [D, D], F32),
    ("w_rt", [D, 72], F32), ("b_rt", [128, 72], F32), ("w_gate", [64, D, 512], F32), ("w_up", [64, D, 512], F32),
    ("w_down", [64, 512, D], F32),
]


def build_nc(stop=99, dbg=()):
    CST = make_consts()
    nc = bass.Bass("TRN2", target_bir_lowering=False)
    io = {}
    for name, shp, dt in IN_SPECS + CONST_SPECS:
        if stop < 8 and name in ("w_gate", "w_up", "w_down"):
            shp = [1] + shp[1:]
        io[name] = nc.dram_tensor(name, shp, dt, kind="ExternalInput").ap()
    out = nc.dram_tensor("out", [T, D], F32, kind="ExternalOutput").ap()

    def scratch(name, shp, dt):
        kind = "ExternalOutput" if name in dbg else "Internal"
        return nc.dram_tensor(name, shp, dt, kind=kind).ap()

    PFM = scratch("PFM", [32, 128, T], BF16)
    PTM = scratch("PTM", [T, 2560], BF16)
    X1 = scratch("X1", [T, D], F32)
    XS = scratch("XS", [64 * CAP, D], BF16)
    YS = scratch("YS", [64 * CAP, D], BF16)
    dbgout = {}
    for name, shp, dt in [("d_modT", [128, 96], F32), ("d_hT", [128, 16, T], BF16), ("d_ocatT", [128, 16, T], BF16),
                          ("d_ngs", [128, 16, 24], F32), ("d_slots", [128, 16, 2], I32), ("d_wab", [128, 16, 2], F32),
                          ("d_misc", [128, 16, 64], F32)]:
        if name in dbg:
            dbgout[name] = nc.dram_tensor(name, shp, dt, kind="ExternalOutput").ap()

    S = Sched(nc)
    S.open()
    sb, ps = _namers(nc)
    final_toks = []

    def dbgdump(name, ap, key):
        if name in dbgout:
            final_toks.append(S.dma("sp", lambda e: e.dma_start(out=dbgout[name], in_=ap), reads=[key], writes=["dbg_" + name]))

    with ExitStack() as _es:
        modT = _es.enter_context(sb("modT", [128, 96], F32))
        A1 = _es.enter_context(sb("A1", [128, 16], F32))
        A2 = _es.enter_context(sb("A2", [128, 16], F32))
        identb = _es.enter_context(sb("identb", [128, 128], BF16))
        identf = _es.enter_context(sb("identf", [128, 128], F32))
        ngs = _es.enter_context(sb("ngs", [128, 16, 24], F32))
        slots = _es.enter_context(sb("slots", [128, 16, 2], I32))
        wab = _es.enter_context(sb("wab", [128, 16, 2], F32))
        global epsc
        epsc = _es.enter_context(sb("epsc", [128, 1], F32))
        S.op("dve", lambda e: e.memset(epsc[:], EPS), writes=["epsc"])
        S.dma("sp", lambda e: e.dma_start(out=identb[:], in_=io["identb"]), writes=["identb"])
        S.dma("sp", lambda e: e.dma_start(out=identf[:], in_=io["identf"]), writes=["identf"])

        with ExitStack() as _es:
            cT = _es.enter_context(sb("cT", [128, 16], F32))
            cact = _es.enter_context(sb("cact", [128, 16], BF16))
            badaT = _es.enter_context(sb("badaT", [128, 96], F32))
            n1gT = _es.enter_context(sb("n1gT", [128, 16], F32))
            n2gT = _es.enter_context(sb("n2gT", [128, 16], F32))
            wa = _es.enter_context(sb("wa", [128, 2, 16, 512], BF16))
            modps = _es.enter_context(ps("modps", [128, 96], F32))
            S.dma("sp", lambda e: e.dma_start(out=cT[:], in_=io["cT"]), writes=["cT"])
            S.dma("sp", lambda e: e.dma_start(out=badaT[:], in_=io["b_adaT"]), writes=["badaT"])
            S.dma("sp", lambda e: e.dma_start(out=n1gT[:], in_=io["n1gT"]), writes=["n1gT"])
            S.dma("sp", lambda e: e.dma_start(out=n2gT[:], in_=io["n2gT"]), writes=["n2gT"])
            S.op("act", lambda e: e.activation(out=cact[:], in_=cT[:], func=AF.Silu), reads=["cT"], writes=["cact"])
            if stop >= 7:
                zt = _es.enter_context(sb("zt", [128, RB, D], BF16))
                S.op("dve", lambda e: e.memset(zt[:], 0.0), writes=["zt"])
                for ex in range(64):
                    S.dma("sp", lambda e, ex=ex: e.dma_start(out=XS[ex * CAP:(ex + 1) * CAP, :].rearrange("(r p) d -> p r d", p=128), in_=zt[:]),
                          reads=["zt"], writes=["XS"])
            wada = io["w_ada"].rearrange("(k p) n -> p k n", p=128)
            for blk in range(24 if not os.environ.get("K_SKIPABC") else 0):
                b = blk % 2
                S.dma("pool", lambda e, b=b, blk=blk: e.dma_start(out=wa[:, b], in_=wada[:, :, blk * 512:(blk + 1) * 512]),
                      writes=[("wa", b)])
                for j in range(4):
                    col = blk * 4 + j
                    for k in range(16):
                        S.op("pe", lambda e, b=b, j=j, k=k, col=col: e.matmul(
                            modps[:, col:col + 1], lhsT=wa[:, b, k, j * 128:(j + 1) * 128], rhs=cact[:, k:k + 1],
                            start=(k == 0), stop=(k == 15)), reads=[("wa", b), "cact"], writes=["modps"])
            S.op("dve", lambda e: e.tensor_tensor(out=modT[:], in0=modps[:], in1=badaT[:], op=ALU.add),
                 reads=["modps", "badaT"], writes=["modT"])
            S.op("dve", lambda e: e.scalar_tensor_tensor(out=A1[:], in0=modT[:, 16:32], scalar=1.0, in1=n1gT[:],
                                                         op0=ALU.add, op1=ALU.mult), reads=["modT", "n1gT"], writes=["A1"])
            S.op("dve", lambda e: e.scalar_tensor_tensor(out=A2[:], in0=modT[:, 64:80], scalar=1.0, in1=n2gT[:],
                                                         op0=ALU.add, op1=ALU.mult), reads=["modT", "n2gT"], writes=["A2"])
            dbgdump("d_modT", modT[:], "modT")
            S.barrier()

        def rowbcast(dst, col0, key):
            with sb("rb_l", [128, 2, 128], F32) as rbl, ps("rb_ps", [128, 2, 512], F32) as rbps:
                for k in range(16):
                    lb = k % 2
                    S.op("dve", lambda e, k=k, lb=lb: e.tensor_copy(out=rbl[:, lb, :], in_=modT[:, col0 + k:col0 + k + 1].to_broadcast([128, 128])),
                         reads=["modT"], writes=[("rbl", lb)])
                    S.op("pe", lambda e, k=k, lb=lb: e.matmul(rbps[:, (k // 4) % 2, (k % 4) * 128:(k % 4 + 1) * 128], lhsT=rbl[:, lb, :], rhs=identf[:],
                                                              start=True, stop=True), reads=[("rbl", lb), "identf"], writes=[("rbps", (k // 4) % 2)])
                    if k % 4 == 3:
                        q = k // 4
                        S.op("act", lambda e, q=q: e.copy(out=dst[:, q * 512:(q + 1) * 512], in_=rbps[:, q % 2, :]),
                             reads=[("rbps", q % 2)], writes=[key])
                S.barrier()

        if stop >= 2 and not os.environ.get("K_SKIPABC"):
          with sb("hT", [128, 16, T], BF16) as hT:
            with ExitStack() as _es:
                xt = _es.enter_context(sb("xt", [128, 2, D], F32))
                xn = _es.enter_context(sb("xn", [128, 4, D], BF16))
                junk = _es.enter_context(sb("junk", [128, D], BF16))
                ss = _es.enter_context(sb("ss", [128, 16], F32))
                rstd = _es.enter_context(sb("rstd", [128, 16], F32))
                tp = _es.enter_context(ps("tpB", [128, 2, 1024], BF16))
                S.op("dve", lambda e: e.memset(ss[:], 0.0), writes=["ss"])
                for tg in range(4):
                    for i4 in range(4):
                        i = tg * 4 + i4
                        b = i % 2
                        S.dma("sp", lambda e, i=i, b=b: e.dma_start(out=xt[:, b], in_=io["x"][i * 128:(i + 1) * 128, :]), writes=[("xt", b)])
                        S.op("act", lambda e, i=i, b=b: e.activation(out=junk[:], in_=xt[:, b], func=AF.Square, accum_out=ss[:, i:i + 1]),
                             reads=[("xt", b), "ss"], writes=["junk", ("ss", i)])
                        S.op("act", lambda e, i=i: e.activation(out=rstd[:, i:i + 1], in_=ss[:, i:i + 1], func=AF.Sqrt, bias=epsc[:, 0:1], scale=1.0 / D),
                             reads=[("ss", i), "epsc"], writes=[("rstd", i)])
                        S.op("dve", lambda e, i=i: e.reciprocal(out=rstd[:, i:i + 1], in_=rstd[:, i:i + 1]), reads=[("rstd", i)], writes=[("rstd", i)])
                        S.op("dve", lambda e, i=i, i4=i4, b=b: e.tensor_scalar(out=xn[:, i4], in0=xt[:, b], scalar1=rstd[:, i:i + 1], scalar2=None,
                                                                               op0=ALU.mult), reads=[("xt", b), ("rstd", i)], writes=[("xn", i4)])
                    for k in range(16):
                        pb = k % 2
                        for i4 in range(4):
                            S.op("pe", lambda e, k=k, pb=pb, i4=i4: e.transpose(tp[:, pb, i4 * 128:(i4 + 1) * 128], xn[:, i4, k * 128:(k + 1) * 128], identb[:]),
                                 reads=[("xn", i4), "identb"], writes=[("tp", pb)])
                        S.op("act", lambda e, k=k, pb=pb, tg=tg: e.activation(out=hT[:, k, tg * 512:(tg + 1) * 512], in_=tp[:, pb, 0:512], func=AF.Identity,
                                                                              scale=A1[:, k:k + 1], bias=modT[:, k:k + 1]),
                             reads=[("tp", pb), "A1", "modT"], writes=[("hT", tg)])
                dbgdump("d_hT", hT[:], ("hT", 3))
                S.barrier()

            if stop >= 3:
                stage_C(nc, S, io, hT, PFM, PTM, ngs, dbgdump)
            S.barrier()

        if stop >= 4:
          with sb("ocatT", [128, 16, T], BF16) as ocatT:
            if not os.environ.get("K_SKIPD"):
                stage_D(nc, S, io, CST, PFM, PTM, ocatT, identb)
            if stop >= 5:
                stage_E(nc, S, io, PFM, PTM, ocatT, identb, ngs)
            dbgdump("d_ocatT", ocatT[:], "ocatT")
            if stop >= 6:
                with sb("g1row", [128, D], F32) as g1row:
                    rowbcast(g1row, 32, "g1row")
                    stage_F(nc, S, io, ocatT, g1row, X1)
            S.barrier()

        if stop >= 7:
            with sb("a2row", [128, D], F32) as a2row, sb("sh2row", [128, D], F32) as sh2row:
                S.op("dve", lambda e: e.tensor_copy(out=modT[:, 64:80], in_=A2[:]), reads=["A2", "modT"], writes=["modT"])
                rowbcast(a2row, 64, "a2row")
                rowbcast(sh2row, 48, "sh2row")
                stage_G(nc, S, io, X1, XS, a2row, sh2row, identf, slots, wab, dbgout, final_toks)
            S.barrier()
            dbgdump("d_slots", slots[:], "slots")
            dbgdump("d_wab", wab[:], "wab")
        if stop >= 8:
            stage_H(nc, S, io, XS, YS, identb)
            S.barrier()
        if stop >= 9:
            with sb("g2row", [128, D], F32) as g2row:
                rowbcast(g2row, 80, "g2row")
                final_toks += stage_I(nc, S, io, X1, YS, g2row, slots, wab, out)
        S.barrier()
        e = S.handles["sp"]
        waits = []
        for t in final_toks:
            S._need("sp", t, waits)
        for sid, val in waits:
            e.wait_ge(S._sem(sid), val)
    S.close()
    return nc


def stage_C(nc, S, io, hT, PFM, PTM, ngs, dbgdump):
    sb, ps = _namers(nc)
    win = io["w_in"].rearrange("(k p) n -> p k n", p=128)
    units = []
    for h in range(4):
        units.append((h * 256, "rotq", 2 * h))
    for h in range(4):
        units.append((1024 + h * 256, "rotk", 8 + 2 * h))
    for u in range(4):
        units.append((4096 + u * 256, "scale", 16 + 2 * u))
    units += [(5120, "copy", 24), (5376, "copy", 26), (5632, "copy", 28), (6144, "copy", 30)]
    with ExitStack() as _es:
        cos = _es.enter_context(sb("cos", [128, T], F32))
        sin = _es.enter_context(sb("sin", [128, T], F32))
        wf = _es.enter_context(sb("wf", [128, 2, 16, 256], BF16))
        stg = _es.enter_context(sb("stg", [128, 2, 2, T], BF16))
        f12 = _es.enter_context(sb("f12", [128, 2, 512], F32))
        tt = _es.enter_context(sb("tt", [128, 4, 512], F32))
        pfm = _es.enter_context(ps("pfm", [128, 2, 2, 512], F32))
        S.dma("sp", lambda e: e.dma_start(out=cos[:], in_=io["cos"]), writes=["cos"])
        S.dma("sp", lambda e: e.dma_start(out=sin[:], in_=io["sin"]), writes=["sin"])
        n = 0
        for ui, (c0, kind, ch) in enumerate(units):
            wb = ui % 2
            S.dma("pool", lambda e, wb=wb, c0=c0: e.dma_start(out=wf[:, wb], in_=win[:, :, c0:c0 + 256]), writes=[("wf", wb)])
            for tg in range(4):
                pb = n % 2
                n += 1
                for half in range(2):
                    for k in range(16):
                        S.op("pe", lambda e, wb=wb, pb=pb, half=half, k=k, tg=tg: e.matmul(
                            pfm[:, pb, half, :], lhsT=wf[:, wb, k, half * 128:(half + 1) * 128], rhs=hT[:, k, tg * 512:(tg + 1) * 512],
                            start=(k == 0), stop=(k == 15)), reads=[("wf", wb), ("hT", tg)], writes=[("pfm", pb)])
                tsl = slice(tg * 512, (tg + 1) * 512)
                if kind in ("copy", "scale"):
                    sc = 1.0 if kind == "copy" else 128.0 ** -0.5
                    for half in range(2):
                        S.op("act", lambda e, pb=pb, wb=wb, tsl=tsl, sc=sc, half=half: e.activation(out=stg[:, wb, half, tsl], in_=pfm[:, pb, half, :], func=AF.Copy, scale=sc),
                             reads=[("pfm", pb)], writes=[("stg", wb)])
                else:
                    sc = 1.0 if kind == "rotq" else 1.0 / 16.0
                    for half in range(2):
                        S.op("act", lambda e, pb=pb, sc=sc, half=half: e.activation(out=f12[:, half, :], in_=pfm[:, pb, half, :], func=AF.Copy, scale=sc),
                             reads=[("pfm", pb)], writes=["f12"])
                    S.op("dve", lambda e, tsl=tsl: e.tensor_tensor(out=tt[:, 0], in0=f12[:, 0], in1=cos[:, tsl], op=ALU.mult), reads=["f12", "cos"], writes=["tt0"])
                    S.op("dve", lambda e, tsl=tsl: e.tensor_tensor(out=tt[:, 1], in0=f12[:, 1], in1=sin[:, tsl], op=ALU.mult), reads=["f12", "sin"], writes=["tt1"])
                    S.op("pool", lambda e, tsl=tsl: e.tensor_tensor(out=tt[:, 2], in0=f12[:, 0], in1=sin[:, tsl], op=ALU.mult), reads=["f12", "sin"], writes=["tt2"])
                    S.op("pool", lambda e, tsl=tsl: e.tensor_tensor(out=tt[:, 3], in0=f12[:, 1], in1=cos[:, tsl], op=ALU.mult), reads=["f12", "cos"], writes=["tt3"])
                    S.op("dve", lambda e, wb=wb, tsl=tsl: e.tensor_tensor(out=stg[:, wb, 0, tsl], in0=tt[:, 0], in1=tt[:, 1], op=ALU.subtract),
                         reads=["tt0", "tt1"], writes=[("stg", wb)])
                    S.op("pool", lambda e, wb=wb, tsl=tsl: e.tensor_tensor(out=stg[:, wb, 1, tsl], in0=tt[:, 2], in1=tt[:, 3], op=ALU.add),
                         reads=["tt2", "tt3"], writes=[("stg", wb)])
            S.dma("sp", lambda e, wb=wb, ch=ch: e.dma_start(out=PFM[ch:ch + 2].rearrange("c p t -> p c t"), in_=stg[:, wb]),
                  reads=[("stg", wb)], writes=["PFM"])
        S.barrier()
    tunits = [([(2048, 512)], 0), ([(2560, 512)], 512), ([(3072, 512)], 1024), ([(3584, 512)], 1536),
              ([(5888, 256), (6400, 256)], 2048), ([(6656, 24)], None)]
    ptm_v = PTM.rearrange("(i p) c -> p i c", p=128)
    with ExitStack() as _es:
        wt = _es.enter_context(sb("wt", [128, 2, 16, 512], BF16))
        stgt = _es.enter_context(sb("stgt", [128, 2, 16, 512], BF16))
        ptm = _es.enter_context(ps("ptm", [128, 2, 512], F32))
        n = 0
        for ui, (srcs, dcol) in enumerate(tunits):
            wb = ui % 2
            off = 0
            for (c0, w) in srcs:
                S.dma("pool", lambda e, wb=wb, c0=c0, w=w, off=off: e.dma_start(out=wt[:, wb, :, off:off + w], in_=win[:, :, c0:c0 + w]),
                      writes=[("wt", wb)])
                off += w
            ncol = off
            for i in range(16):
                pb = n % 2
                n += 1
                for k in range(16):
                    S.op("pe", lambda e, wb=wb, pb=pb, i=i, k=k, ncol=ncol: e.matmul(
                        ptm[:, pb, 0:ncol], lhsT=hT[:, k, i * 128:(i + 1) * 128], rhs=wt[:, wb, k, 0:ncol],
                        start=(k == 0), stop=(k == 15)), reads=[("wt", wb), ("hT", i // 4)], writes=[("ptm", pb)])
                if dcol is None:
                    S.op("dve", lambda e, pb=pb, i=i: e.tensor_copy(out=ngs[:, i, :], in_=ptm[:, pb, 0:24]), reads=[("ptm", pb)], writes=["ngs"])
                else:
                    eng = "act" if i % 2 == 0 else "dve"
                    if eng == "act":
                        S.op("act", lambda e, pb=pb, wb=wb, i=i: e.copy(out=stgt[:, wb, i, :], in_=ptm[:, pb, :]), reads=[("ptm", pb)], writes=[("stgt", wb)])
                    else:
                        S.op("dve", lambda e, pb=pb, wb=wb, i=i: e.tensor_copy(out=stgt[:, wb, i, :], in_=ptm[:, pb, :]), reads=[("ptm", pb)], writes=[("stgt", wb)])
            if dcol is not None:
                S.dma("sp", lambda e, wb=wb, dcol=dcol: e.dma_start(out=ptm_v[:, :, dcol:dcol + 512], in_=stgt[:, wb]),
                      reads=[("stgt", wb)], writes=["PTM"])
        dbgdump("d_ngs", ngs[:], "ngs")
        S.barrier()


def stage_D(nc, S, io, CST, PFM, PTM, ocatT, identb):
    sb, ps = _namers(nc)
    ptm_v = PTM.rearrange("(i p) c -> p i c", p=128)
    with ExitStack() as _es:
        A_ = _es.enter_context
        qT = A_(sb("qT", [128, 2, 2, T], BF16)); kT = A_(sb("kT", [128, 2, 2, T], BF16)); kz = A_(sb("kz", [128, 2, 16, 256], BF16))
        v = A_(sb("v", [128, 2, 16, 256], BF16)); g = A_(sb("g", [128, 2, 16, 256], BF16))
        dm = A_(sb("dm", [128, 4, 128], F32)); zeta = A_(sb("zeta", [128, 4], F32)); qdec = A_(sb("qdec", [128, 4], F32))
        gng = A_(sb("gng", [128, 1024], F32))
        Sf = A_(sb("Sf", [128, 2, 2, 256], F32)); Sb = A_(sb("Sb", [128, 2, 2, 256], BF16))
        sTm = A_(sb("sTm", [128, 2, 128], BF16)); o = A_(sb("o", [128, 2, 256], F32)); on = A_(sb("on", [128, 2, 256], F32))
        sg = A_(sb("sg", [128, 2, 256], F32)); ob = A_(sb("ob", [128, 2, 256], BF16)); st = A_(sb("st", [128, 2, 6], F32))
        junkd = A_(sb("junkd", [128, 2, 256], BF16)); mv = A_(sb("mv", [128, 2, 2], F32)); rg = A_(sb("rg", [128, 2], F32))
        sT_ps = A_(ps("sT_ps", [128, 2, 512], F32)); o_ps = A_(ps("o_ps", [128, 2, 512], F32))
        kv_ps = A_(ps("kv_ps", [128, 2, 512], F32)); tpk = A_(ps("tpk", [128, 2, 1024], BF16))
        S.dma("sp", lambda e: e.dma_start(out=dm[:], in_=io["dm"]), writes=["dm"])
        S.dma("sp", lambda e: e.dma_start(out=zeta[:], in_=io["zeta"]), writes=["zeta"])
        S.dma("sp", lambda e: e.dma_start(out=qdec[:], in_=io["qdec"]), writes=["qdec"])
        S.dma("sp", lambda e: e.dma_start(out=gng[:], in_=io["gng_row"]), writes=["gng"])

        def chunk(h, b, n):
            cd = CST["_cd"][h]
            csl = slice(n * 128, (n + 1) * 128)
            for half in range(2):
                S.op("pe", lambda e, half=half: e.matmul(sT_ps[:, b, 0:128], lhsT=kT[:, b, half, csl], rhs=qT[:, b, half, csl],
                                                         start=(half == 0), stop=(half == 1)), reads=[("kT", b), ("qT", b)], writes=[("sT_ps", b)])
            S.op("dve", lambda e: e.tensor_tensor(out=sTm[:, b, :], in0=sT_ps[:, b, 0:128], in1=dm[:, h, :], op=ALU.mult),
                 reads=[("sT_ps", b), "dm"], writes=[("sTm", b)])
            S.op("pe", lambda e: e.matmul(o_ps[:, b, 0:256], lhsT=sTm[:, b, :], rhs=v[:, b, n, :], start=True, stop=(n == 0)),
                 reads=[("sTm", b), ("v", b)], writes=[("o_ps", b)])
            if n > 0:
                for half in range(2):
                    S.op("pe", lambda e, half=half: e.matmul(o_ps[:, b, 0:256], lhsT=qT[:, b, half, csl], rhs=Sb[:, b, half, :],
                                                             start=False, stop=(half == 1)), reads=[("qT", b), ("Sb", b)], writes=[("o_ps", b)])
            if n < 15:
                for half in range(2):
                    S.op("pe", lambda e, half=half: e.matmul(kv_ps[:, b, half * 256:(half + 1) * 256], lhsT=kz[:, b, n, half * 128:(half + 1) * 128], rhs=v[:, b, n, :],
                                                             start=True, stop=True), reads=[("kz", b), ("v", b)], writes=[("kv_ps", b)])
                S.op("dve", lambda e: e.scalar_tensor_tensor(out=Sf[:, b], in0=Sf[:, b], scalar=cd, in1=kv_ps[:, b, :].rearrange("p (a c) -> p a c", a=2),
                                                             op0=ALU.mult, op1=ALU.add), reads=[("Sf", b), ("kv_ps", b)], writes=[("Sf", b)])
                S.op("act", lambda e: e.copy(out=Sb[:, b], in_=Sf[:, b]), reads=[("Sf", b)], writes=[("Sb", b)])
            S.op("dve", lambda e: e.tensor_scalar(out=o[:, b, :], in0=o_ps[:, b, 0:256], scalar1=qdec[:, h:h + 1], scalar2=None, op0=ALU.mult),
                 reads=[("o_ps", b), "qdec"], writes=[("o", b)])
            S.op("dve", lambda e: e.tensor_reduce(out=mv[:, b, 0:1], in_=o[:, b, :], axis=AX.X, op=ALU.add), reads=[("o", b)], writes=[("mv", b)])
            S.op("dve", lambda e: e.tensor_scalar(out=mv[:, b, 1:2], in0=mv[:, b, 0:1], scalar1=-1.0 / 256.0, scalar2=None, op0=ALU.mult),
                 reads=[("mv", b)], writes=[("mv1", b)])
            S.op("act", lambda e: e.activation(out=on[:, b, :], in_=o[:, b, :], func=AF.Identity, bias=mv[:, b, 1:2], scale=1.0),
                 reads=[("o", b), ("mv1", b)], writes=[("on", b)])
            S.op("act", lambda e: e.activation(out=junkd[:, b, :], in_=on[:, b, :], func=AF.Square, accum_out=st[:, b, 0:1]),
                 reads=[("on", b)], writes=[("junkd", b), ("st", b)])
            S.op("act", lambda e: e.activation(out=rg[:, b:b + 1], in_=st[:, b, 0:1], func=AF.Sqrt, bias=epsc[:, 0:1], scale=1.0 / 256.0),
                 reads=[("st", b), "epsc"], writes=[("rg", b)])
            S.op("dve", lambda e: e.reciprocal(out=rg[:, b:b + 1], in_=rg[:, b:b + 1]), reads=[("rg", b)], writes=[("rg", b)])
            S.op("dve", lambda e: e.scalar_tensor_tensor(out=o[:, b, :], in0=on[:, b, :], scalar=rg[:, b:b + 1], in1=gng[:, h * 256:(h + 1) * 256],
                                                         op0=ALU.mult, op1=ALU.mult), reads=[("on", b), ("rg", b), "gng"], writes=[("o", b)])
            S.op("act", lambda e: e.activation(out=sg[:, b, :], in_=g[:, b, n, :], func=AF.Silu), reads=[("g", b)], writes=[("sg", b)])
            S.op("pool", lambda e: e.tensor_tensor(out=ob[:, b, :], in0=o[:, b, :], in1=sg[:, b, :], op=ALU.mult),
                 reads=[("o", b), ("sg", b)], writes=[("ob", b)])

        def tail(h, b, n):
            csl = slice(n * 128, (n + 1) * 128)
            for half in range(2):
                S.op("pe", lambda e, half=half: e.transpose(tpk[:, b, half * 128:(half + 1) * 128], ob[:, b, half * 128:(half + 1) * 128], identb[:]),
                     reads=[("ob", b), "identb"], writes=[("tpk", b)])
            S.op("act", lambda e: e.copy(out=ocatT[:, 2 * h:2 * h + 2, csl], in_=tpk[:, b, 0:256].rearrange("p (a c) -> p a c", a=2)),
                 reads=[("tpk", b)], writes=["ocatT"])

        for hp in range(2):
            for b in range(2):
                h = 2 * hp + b
                S.dma("sp", lambda e, h=h, b=b: e.dma_start(out=qT[:, b], in_=PFM[2 * h:2 * h + 2].rearrange("c p t -> p c t")), reads=["PFM"], writes=[("qT", b)])
                S.dma("sp", lambda e, h=h, b=b: e.dma_start(out=kT[:, b], in_=PFM[8 + 2 * h:10 + 2 * h].rearrange("c p t -> p c t")), reads=["PFM"], writes=[("kT", b)])
                S.dma("sp", lambda e, h=h, b=b: e.dma_start(out=v[:, b], in_=ptm_v[:, :, h * 256:(h + 1) * 256]), reads=["PTM"], writes=[("v", b)])
                S.dma("sp", lambda e, h=h, b=b: e.dma_start(out=g[:, b], in_=ptm_v[:, :, 1024 + h * 256:1024 + (h + 1) * 256]), reads=["PTM"], writes=[("g", b)])
            for b in range(2):
                h = 2 * hp + b
                for i in range(16):
                    for half in range(2):
                        S.op("pe", lambda e, i=i, b=b, half=half: e.transpose(tpk[:, b, half * 128:(half + 1) * 128], kT[:, b, half, i * 128:(i + 1) * 128], identb[:]),
                             reads=[("kT", b), "identb"], writes=[("tpk", b)])
                    S.op("act", lambda e, i=i, b=b, h=h: e.activation(out=kz[:, b, i, :].rearrange("p (a c) -> p a c", a=2),
                                                                    in_=tpk[:, b, 0:256].rearrange("p (a c) -> p a c", a=2), func=AF.Identity, scale=zeta[:, h:h + 1]),
                         reads=[("tpk", b), "zeta"], writes=[("kz", b)])
                S.op("dve", lambda e, b=b: e.memset(Sf[:, b], 0.0), writes=[("Sf", b)])
                S.op("dve", lambda e, b=b: e.memset(Sb[:, b], 0.0), writes=[("Sb", b)])
            pend = None
            for n in range(16):
                for b in range(2):
                    chunk(2 * hp + b, b, n)
                    if pend is not None:
                        tail(*pend)
                    pend = (2 * hp + b, b, n)
            tail(*pend)
        S.barrier()


def stage_E(nc, S, io, PFM, PTM, ocatT, identb, ngs):
    sb, ps = _namers(nc)
    ptm_v = PTM.rearrange("(i p) c -> p i c", p=128)
    with ExitStack() as _es:
        w1k = _es.enter_context(sb("w1k", [128, 32, 256], BF16))
        w1v = _es.enter_context(sb("w1v", [128, 32, 256], BF16))
        w2k = _es.enter_context(sb("w2k", [128, 2, 128], BF16))
        w2v = _es.enter_context(sb("w2v", [128, 2, 128], BF16))
        pek = _es.enter_context(sb("pek", [128, 32], BF16))
        pev = _es.enter_context(sb("pev", [128, 32], BF16))
        hb = _es.enter_context(sb("hb", [128, 2, 2], F32))
        caus = _es.enter_context(sb("caus", [128, 128], BF16))
        anti = _es.enter_context(sb("anti", [128, 128], BF16))
        cmask = _es.enter_context(sb("cmask", [128, T], BF16))
        overlap = _es.enter_context(sb("overlap", [128, 32], BF16))
        esel = _es.enter_context(sb("esel", [32, 16, 128], BF16))
        valid = _es.enter_context(sb("valid", [128, 16, 32], F32))
        ctab = _es.enter_context(sb("ctab", [128, 16, 32], F32))
        qT4 = _es.enter_context(sb("qT4", [128, 4, T], BF16))
        kcT2 = _es.enter_context(sb("kcT2", [128, 2, T], BF16))
        vcT2 = _es.enter_context(sb("vcT2", [128, 2, T], BF16))
        ksT = _es.enter_context(sb("ksT", [128, T], BF16))
        kwT = _es.enter_context(sb("kwT", [128, T], BF16))
        vsa = _es.enter_context(sb("vsa", [128, 16, 130], BF16))
        vwa = _es.enter_context(sb("vwa", [128, 16, 130], BF16))
        hid = _es.enter_context(sb("hid", [128, 4, 2, 128], BF16))
        kc16 = _es.enter_context(sb("kc16", [128, 2, 16, 130], BF16))
        kcmpT2 = _es.enter_context(sb("kcmpT2", [128, 2, 128], BF16))
        vcaug2 = _es.enter_context(sb("vcaug2", [128, 2, 162], BF16))
        ocmp = _es.enter_context(sb("ocmp", [128, 16, 4, 128], BF16))
        gts = _es.enter_context(sb("gts", [128, 16, 12], F32))
        selnegT = _es.enter_context(sb("selnegT", [32, T], BF16))
        _sk = os.environ.get("K_EPRO", "")
        if "c" not in _sk:
            for nm, t in [("caus", caus), ("anti", anti), ("cmask", cmask), ("overlap", overlap), ("esel", esel), ("valid", valid), ("ctab", ctab)]:
                S.dma("sp", lambda e, nm=nm, t=t: e.dma_start(out=t[:], in_=io[nm]), writes=[nm])
        if "w" not in _sk:
            for lh in range(2):
                S.dma("pool", lambda e, lh=lh: e.dma_start(out=w1k[:, lh * 16:(lh + 1) * 16, :], in_=io["w1_k"].rearrange("(l p) n -> p l n", p=128)[:, lh * 16:(lh + 1) * 16, :]), writes=["w1k"])
                S.dma("pool", lambda e, lh=lh: e.dma_start(out=w1v[:, lh * 16:(lh + 1) * 16, :], in_=io["w1_v"].rearrange("(l p) n -> p l n", p=128)[:, lh * 16:(lh + 1) * 16, :]), writes=["w1v"])
            S.dma("pool", lambda e: e.dma_start(out=w2k[:], in_=io["w2_k"].rearrange("(c p) n -> p c n", p=128)), writes=["w2k"])
            S.dma("pool", lambda e: e.dma_start(out=w2v[:], in_=io["w2_v"].rearrange("(c p) n -> p c n", p=128)), writes=["w2v"])
            S.dma("pool", lambda e: e.dma_start(out=pek[:], in_=io["peT_k"]), writes=["pek"])
            S.dma("pool", lambda e: e.dma_start(out=pev[:], in_=io["peT_v"]), writes=["pev"])

        with ps("hb_ps", [128, 2, 2], F32) as hb_ps:
            for kv, (w1, pe) in enumerate([(w1k, pek), (w1v, pev)]):
                for hc in range(2):
                    for l in range(32):
                        S.op("pe", lambda e, kv=kv, hc=hc, l=l, w1=w1, pe=pe: e.matmul(hb_ps[:, kv, hc:hc + 1], lhsT=w1[:, l, hc * 128:(hc + 1) * 128],
                                                                                     rhs=pe[:, l:l + 1], start=(l == 0), stop=(l == 31)),
                             reads=["w1k", "w1v", "pek", "pev"], writes=["hb_ps"])
            if "h" not in _sk:
                S.op("dve", lambda e: e.tensor_copy(out=hb[:], in_=hb_ps[:]), reads=["hb_ps"], writes=["hb"])
            S.barrier()

        for _ in range(int(os.environ.get("K_PENOP", "0"))):
            nc.tensor.wait_ge(S.sems["dve"], 0)
        S.op("dve", lambda e: e.memset(kc16[:], 0.0), writes=[("kc16", 0), ("kc16", 1)])
        S.op("dve", lambda e: e.memset(vcaug2[:], 0.0), writes=["vcaug"])
        with ExitStack() as _es2:
            hid_ps = _es2.enter_context(ps("hid_ps", [128, 4, 2, 128], F32))
            cmp_ps = _es2.enter_context(ps("cmp_ps", [128, 2, 512], F32))
            S.dma("sp", lambda e: e.dma_start(out=kcT2[:], in_=PFM[24:26].rearrange("c p t -> p c t")), reads=["PFM"], writes=["kcT2"])
            S.dma("sp", lambda e: e.dma_start(out=vcT2[:], in_=PFM[26:28].rearrange("c p t -> p c t")), reads=["PFM"], writes=["vcT2"])
            for u in range(4):
                kv, gg = u // 2, u % 2
                w1 = w1k if kv == 0 else w1v
                src = kcT2 if kv == 0 else vcT2
                nm = "kcT2" if kv == 0 else "vcT2"
                kb = u % 2
                S.op("dve" if u % 2 == 0 else "pool", lambda e, kb=kb, src=src, gg=gg: e.tensor_copy(
                    out=kc16[:, kb, :, 0:128], in_=src[:, gg, :].rearrange("p (c r) -> p r c", r=16)), reads=[nm], writes=[("kc16", kb)])
                for hc in range(2):
                    for l in range(32):
                        S.op("pe", lambda e, u=u, kb=kb, hc=hc, l=l, w1=w1: e.matmul(
                            hid_ps[:, u, hc, :], lhsT=w1[:, l, hc * 128:(hc + 1) * 128], rhs=kc16[:, kb, l % 16, (l // 16):(l // 16) + 128],
                            start=(l == 0), stop=(l == 31)), reads=["w1k", "w1v", ("kc16", kb)], writes=[("hid_ps", u // 2)])
                    S.op("act", lambda e, u=u, kv=kv, hc=hc: e.activation(out=hid[:, u, hc, :], in_=hid_ps[:, u, hc, :], func=AF.Silu,
                                                                     bias=hb[:, kv, hc:hc + 1]), reads=[("hid_ps", u // 2), "hb"], writes=["hid"])
            for gg in range(2):
                for hc in range(2):
                    S.op("pe", lambda e, gg=gg, hc=hc: e.matmul(cmp_ps[:, 0, gg * 128:(gg + 1) * 128], lhsT=w2k[:, hc, :], rhs=hid[:, gg, hc, :], start=(hc == 0), stop=(hc == 1)),
                         reads=["w2k", "hid"], writes=["cmp_ps0"])
            for gg in range(2):
                for hc in range(2):
                    S.op("pe", lambda e, gg=gg, hc=hc: e.matmul(cmp_ps[:, 1, gg * 128:(gg + 1) * 128], lhsT=hid[:, 2 + gg, hc, :], rhs=w2v[:, hc, :], start=(hc == 0), stop=(hc == 1)),
                         reads=["w2v", "hid"], writes=["cmp_ps1"])
            _cs = os.environ.get("K_CONS", "")
            if "A" not in _cs:
                S.op("act", lambda e: e.copy(out=kcmpT2[:], in_=cmp_ps[:, 0, 0:256].rearrange("p (a b) -> p a b", a=2)), reads=["cmp_ps0"], writes=["kcmpT"])
            if "V" not in _cs:
                S.op("dve", lambda e: e.tensor_copy(out=vcaug2[:, :, 0:128], in_=cmp_ps[:, 1, 0:256].rearrange("p (a b) -> p a b", a=2)), reads=["cmp_ps1", "vcaug"], writes=["vcaug"])
            if "M" not in _cs:
                S.op("dve", lambda e: e.memset(vcaug2[:, :, 128:129], 1.0), reads=["vcaug"], writes=["vcaug"])
            for gg in range(2 if "O" not in _cs else 0):
                S.op("dve", lambda e, gg=gg: e.tensor_copy(out=vcaug2[:, gg, 129:161], in_=overlap[:]), reads=["overlap", "vcaug"], writes=["vcaug"])
            S.barrier()
        if "cmp" in os.environ.get("K_ESKIP", ""):
            return
        for g in ([int(c) for c in os.environ["K_GSEL"]] if os.environ.get("K_GSEL") else range(2)):
            _ld = os.environ.get("K_ELD", "")
            if "q" not in _ld:
                S.dma("sp", lambda e, g=g: e.dma_start(out=qT4[:], in_=PFM[16 + 4 * g:20 + 4 * g].rearrange("c p t -> p c t")), reads=["PFM"], writes=["qT4"])
            for nm, t, ch in [("ksT", ksT, 28), ("kwT", kwT, 30)]:
                S.dma("sp", lambda e, t=t, ch=ch, g=g: e.dma_start(out=t[:], in_=PFM[ch + g]), reads=["PFM"], writes=[nm])
            if "a" not in _ld:
                S.dma("sp", lambda e, g=g: e.dma_start(out=vsa[:, :, 0:128], in_=ptm_v[:, :, 2048 + g * 128:2048 + (g + 1) * 128]), reads=["PTM"], writes=["vsa"])
                S.dma("sp", lambda e, g=g: e.dma_start(out=vwa[:, :, 0:128], in_=ptm_v[:, :, 2304 + g * 128:2304 + (g + 1) * 128]), reads=["PTM"], writes=["vwa"])
            if "m" not in _sk:
                S.op("dve", lambda e: e.memset(vsa[:, :, 128:130], 1.0), reads=["vsa"], writes=["vsa"])
                S.op("dve", lambda e: e.memset(vwa[:, :, 128:130], 1.0), reads=["vwa"], writes=["vwa"])
            if "e2a" in os.environ.get("K_ESKIP", ""):
                continue
            with ExitStack() as _es:
                sc_ps = _es.enter_context(ps("sc_ps", [128, 2, 512], F32))
                oc_ps = _es.enter_context(ps("oc_ps", [128, 4, 256], F32))
                tps = _es.enter_context(ps("tps", [32, 2, 128], BF16))
                pc = _es.enter_context(sb("pc", [128, 2, 512], BF16))
                rc = _es.enter_context(sb("rc", [128, 4], F32))
                imp = _es.enter_context(sb("imp", [128, 32], F32))
                score = _es.enter_context(sb("score", [128, 32], F32))
                sc2 = _es.enter_context(sb("sc2", [128, 32], F32))
                m8 = _es.enter_context(sb("m8", [128, 16], F32))
                selneg = _es.enter_context(sb("selneg", [128, 32], BF16))
                coef = _es.enter_context(sb("coef", [128, 4], F32))
                for qt in range(16):
                    b = qt % 2
                    qsl = slice(qt * 128, (qt + 1) * 128)
                    S.op("pe", lambda e, b=b, qsl=qsl: e.matmul(sc_ps[:, b, :].rearrange("p (h t) -> p h t", h=4), lhsT=kcmpT2[:, g, :], rhs=qT4[:, :, qsl],
                                                                start=True, stop=False), reads=["kcmpT", "qT4"], writes=[("sc_ps", b)])
                    S.op("pe", lambda e, b=b, qsl=qsl: e.matmul(sc_ps[:, b, :].rearrange("p (h t) -> p h t", h=4), lhsT=identb[:, :],
                                                                rhs=cmask[:, qsl].unsqueeze(1).to_broadcast([128, 4, 128]), start=False, stop=True),
                         reads=["identb", "cmask"], writes=[("sc_ps", b)])
                    S.op("act", lambda e, b=b: e.activation(out=pc[:, b, :], in_=sc_ps[:, b, :], func=AF.Exp), reads=[("sc_ps", b)], writes=[("pc", b)])
                    for h in range(4):
                        S.op("pe", lambda e, b=b, h=h: e.matmul(oc_ps[:, h, 0:162], lhsT=pc[:, b, h * 128:(h + 1) * 128], rhs=vcaug2[:, g, 0:162],
                                                                start=True, stop=True), reads=[("pc", b), "vcaug"], writes=["oc_ps"])
                    for hh in (0, 2):
                        S.op("dve", lambda e, hh=hh: e.tensor_scalar(out=rc[:, hh:hh + 2], in0=oc_ps[:, hh:hh + 2, 128], scalar1=1e-30, scalar2=None, op0=ALU.max),
                             reads=["oc_ps"], writes=["rc"])
                    S.op("dve", lambda e: e.reciprocal(out=rc[:], in_=rc[:]), reads=["rc"], writes=["rc"])
                    S.op("act", lambda e, qt=qt, g=g: e.activation(out=gts[:, qt, :], in_=ngs[:, qt, g * 12:(g + 1) * 12], func=AF.Sigmoid), reads=["ngs"], writes=["gts"])
                    for h in range(4):
                        if h == 0:
                            S.op("dve", lambda e: e.tensor_scalar(out=imp[:], in0=oc_ps[:, 0, 129:161], scalar1=rc[:, 0:1], scalar2=None, op0=ALU.mult),
                                 reads=["oc_ps", "rc"], writes=["imp"])
                        else:
                            S.op("dve", lambda e, h=h: e.scalar_tensor_tensor(out=imp[:], in0=oc_ps[:, h, 129:161], scalar=rc[:, h:h + 1], in1=imp[:],
                                                                              op0=ALU.mult, op1=ALU.add), reads=["oc_ps", "rc", "imp"], writes=["imp"])
                    S.op("dve", lambda e, qt=qt: e.tensor_tensor(out=coef[:], in0=rc[:], in1=gts[:, qt, 0:12:3], op=ALU.mult), reads=["rc", "gts"], writes=["coef"])
                    for h in range(4):
                        S.op("dve", lambda e, h=h, qt=qt: e.tensor_scalar(out=ocmp[:, qt, h, :], in0=oc_ps[:, h, 0:128], scalar1=coef[:, h:h + 1], scalar2=None,
                                                                          op0=ALU.mult), reads=["oc_ps", "coef"], writes=["ocmp"])
                    S.op("dve", lambda e, qt=qt: e.tensor_tensor(out=score[:], in0=imp[:], in1=valid[:, qt, :], op=ALU.mult), reads=["imp", "valid"], writes=["score"])
                    S.op("dve", lambda e, qt=qt: e.tensor_tensor(out=score[:], in0=score[:], in1=ctab[:, qt, :], op=ALU.add), reads=["score", "ctab"], writes=["score"])
                    S.op("dve", lambda e: e.max(out=m8[:, 0:8], in_=score[:]), reads=["score"], writes=["m8a"])
                    S.op("dve", lambda e: e.match_replace(out=sc2[:], in_to_replace=m8[:, 0:8], in_values=score[:], imm_value=-2.0),
                         reads=["score", "m8a"], writes=["sc2"])
                    S.op("dve", lambda e: e.max(out=m8[:, 8:16], in_=sc2[:]), reads=["sc2"], writes=["m8b"])
                    S.op("dve", lambda e: e.tensor_scalar(out=selneg[:], in0=score[:], scalar1=m8[:, 15:16], scalar2=NEG, op0=ALU.is_lt, op1=ALU.mult),
                         reads=["score", "m8b"], writes=["selneg"])
                    S.op("pe", lambda e, b=b: e.transpose(tps[:, 0, :], selneg[:], identb[:]), reads=["selneg", "identb"], writes=["tps"])
                    S.op("act", lambda e, b=b, qsl=qsl: e.copy(out=selnegT[:, qsl], in_=tps[:, 0, :]), reads=["tps"], writes=["selnegT"])
                S.barrier()
            if "e2b" in os.environ.get("K_ESKIP", ""):
                continue
            with ExitStack() as _es:
                ss_ps = _es.enter_context(ps("ss_ps", [128, 2, 512], F32))
                os_ps = _es.enter_context(ps("os_ps", [128, 4, 256], F32))
                ow_ps = _es.enter_context(ps("ow_ps", [128, 4, 256], F32))
                tpo = _es.enter_context(ps("tpo", [128, 4, 128], BF16))
                pp = _es.enter_context(sb("pp", [128, 2, 512], BF16))
                rs = _es.enter_context(sb("rs", [128, 4], F32))
                rw = _es.enter_context(sb("rw", [128, 4], F32))
                acc = _es.enter_context(sb("acc", [128, 4, 128], F32))
                ob4 = _es.enter_context(sb("ob4", [128, 4, 128], BF16))
                n = 0
                for qt in range(16):
                    qsl = slice(qt * 128, (qt + 1) * 128)
                    for br, (kTt, knm, va, vnm, o_ps, kts) in enumerate([
                            (ksT, "ksT", vsa, "vsa", os_ps, list(range(0, qt + 1))),
                            (kwT, "kwT", vwa, "vwa", ow_ps, list(range(max(0, qt - 4), qt + 1)))]):
                        opk = "os_ps" if br == 0 else "ow_ps"
                        for kt in kts:
                            b = n % 2
                            n += 1
                            ksl = slice(kt * 128, (kt + 1) * 128)
                            extra = []
                            if br == 0:
                                extra.append((esel[:, kt, :], selnegT[:, qsl].unsqueeze(1).to_broadcast([32, 4, 128]), ["esel", "selnegT"]))
                            if kt == qt:
                                extra.append((identb[:], caus[:].unsqueeze(1).to_broadcast([128, 4, 128]), ["identb", "caus"]))
                            if br == 1 and kt == qt - 4:
                                extra.append((identb[:], anti[:].unsqueeze(1).to_broadcast([128, 4, 128]), ["identb", "anti"]))
                            outv = ss_ps[:, b, :].rearrange("p (h t) -> p h t", h=4)
                            S.op("pe", lambda e, outv=outv, kTt=kTt, ksl=ksl, qsl=qsl, last=(len(extra) == 0): e.matmul(
                                outv, lhsT=kTt[:, ksl], rhs=qT4[:, :, qsl], start=True, stop=last), reads=[knm, "qT4"], writes=[("ss_ps", b)])
                            for xi, (l_, r_, rd) in enumerate(extra):
                                S.op("pe", lambda e, outv=outv, l_=l_, r_=r_, last=(xi == len(extra) - 1): e.matmul(outv, lhsT=l_, rhs=r_, start=False, stop=last),
                                     reads=rd, writes=[("ss_ps", b)])
                            S.op("act", lambda e, b=b: e.activation(out=pp[:, b, :], in_=ss_ps[:, b, :], func=AF.Exp), reads=[("ss_ps", b)], writes=[("pp", b)])
                            for h in range(4):
                                S.op("pe", lambda e, b=b, h=h, kt=kt, va=va, o_ps=o_ps, first=(kt == kts[0]), lastk=(kt == kts[-1]): e.matmul(
                                    o_ps[:, h, 0:130], lhsT=pp[:, b, h * 128:(h + 1) * 128], rhs=va[:, kt, :], start=(first and h % 2 == 0), stop=lastk, skip_group_check=True),
                                    reads=[("pp", b), vnm], writes=[opk])
                    for hh in (0, 2):
                        S.op("dve", lambda e, hh=hh: e.reciprocal(out=rs[:, hh:hh + 2], in_=os_ps[:, hh:hh + 2, 128]), reads=["os_ps"], writes=["rs"])
                        S.op("dve", lambda e, hh=hh: e.reciprocal(out=rw[:, hh:hh + 2], in_=ow_ps[:, hh:hh + 2, 128]), reads=["ow_ps"], writes=["rw"])
                    S.op("dve", lambda e, qt=qt: e.tensor_tensor(out=rs[:], in0=rs[:], in1=gts[:, qt, 1:12:3], op=ALU.mult), reads=["rs", "gts"], writes=["rs"])
                    S.op("dve", lambda e, qt=qt: e.tensor_tensor(out=rw[:], in0=rw[:], in1=gts[:, qt, 2:12:3], op=ALU.mult), reads=["rw", "gts"], writes=["rw"])
                    for h in range(4):
                        S.op("dve", lambda e, h=h: e.tensor_scalar(out=acc[:, h, :], in0=os_ps[:, h, 0:128], scalar1=rs[:, h:h + 1], scalar2=None, op0=ALU.mult),
                             reads=["os_ps", "rs"], writes=["acc"])
                        S.op("dve", lambda e, h=h: e.scalar_tensor_tensor(out=acc[:, h, :], in0=ow_ps[:, h, 0:128], scalar=rw[:, h:h + 1], in1=acc[:, h, :],
                                                                          op0=ALU.mult, op1=ALU.add), reads=["ow_ps", "rw", "acc"], writes=["acc"])
                    S.op("pool", lambda e, qt=qt: e.tensor_tensor(out=ob4[:], in0=acc[:], in1=ocmp[:, qt], op=ALU.add), reads=["acc", "ocmp"], writes=["ob4"])
                    for h in range(4):
                        S.op("pe", lambda e, h=h: e.transpose(tpo[:, h, :], ob4[:, h, :], identb[:]), reads=["ob4", "identb"], writes=["tpo"])
                    S.op("act", lambda e, g=g, qsl=qsl: e.copy(out=ocatT[:, 8 + 4 * g:12 + 4 * g, qsl], in_=tpo[:]), reads=["tpo"], writes=["ocatT"])
                S.barrier()
        S.barrier()


def stage_F(nc, S, io, ocatT, g1row, X1):
    sb, ps = _namers(nc)
    wout = io["w_out"].rearrange("(k p) n -> p k n", p=128)
    with ExitStack() as _es:
        wo = _es.enter_context(sb("wo", [128, 2, 16, 512], BF16))
        xr = _es.enter_context(sb("xr", [128, 2, 512], F32))
        x1t = _es.enter_context(sb("x1t", [128, 2, 512], F32))
        mx_ps = _es.enter_context(ps("mx_ps", [128, 2, 512], F32))
        n = 0
        for cb in range(4):
            wb = cb % 2
            csl = slice(cb * 512, (cb + 1) * 512)
            S.dma("pool", lambda e, wb=wb, csl=csl: e.dma_start(out=wo[:, wb], in_=wout[:, :, csl]), writes=[("wo", wb)])
            for i in range(16):
                b = n % 2
                n += 1
                isl = slice(i * 128, (i + 1) * 128)
                S.dma("sp", lambda e, b=b, isl=isl, csl=csl: e.dma_start(out=xr[:, b, :], in_=io["x"][isl, csl]), writes=[("xr", b)])
                for k in range(16):
                    S.op("pe", lambda e, b=b, wb=wb, k=k, isl=isl: e.matmul(mx_ps[:, b, :], lhsT=ocatT[:, k, isl], rhs=wo[:, wb, k, :],
                                                                            start=(k == 0), stop=(k == 15)), reads=["ocatT", ("wo", wb)], writes=[("mx_ps", b)])
                S.op("dve", lambda e, b=b, csl=csl: e.tensor_tensor(out=x1t[:, b, :], in0=mx_ps[:, b, :], in1=g1row[:, csl], op=ALU.mult),
                     reads=[("mx_ps", b), "g1row"], writes=[("x1t", b)])
                S.op("pool", lambda e, b=b: e.tensor_tensor(out=x1t[:, b, :], in0=x1t[:, b, :], in1=xr[:, b, :], op=ALU.add),
                     reads=[("x1t", b), ("xr", b)], writes=[("x1t", b)])
                S.dma("sp", lambda e, b=b, isl=isl, csl=csl: e.dma_start(out=X1[isl, csl], in_=x1t[:, b, :]), reads=[("x1t", b)], writes=["X1"])
        S.barrier()


def stage_G(nc, S, io, X1, XS, a2row, sh2row, identf, slots, wab, dbgout, final_toks):
    sb, ps = _namers(nc)
    with ExitStack() as _es:
        A_ = _es.enter_context
        x1 = A_(sb("x1", [128, 2, D], F32)); h2f = A_(sb("h2f", [128, 2, D], F32)); h2b = A_(sb("h2b", [128, 16, D], BF16))
        junk = A_(sb("junk2", [128, D], BF16)); h2Tf = A_(sb("h2Tf", [128, 2, 16, 128], F32))
        wr = A_(sb("wr", [128, 16, 72], F32)); brt = A_(sb("brt", [128, 72], F32))
        lstrict = A_(sb("lstrict", [128, 128], BF16)); ones = A_(sb("ones", [128, 128], BF16)); ebase = A_(sb("ebase", [128, 64], F32))
        ss2 = A_(sb("ss2", [128, 16], F32)); rstd2 = A_(sb("rstd2", [128, 16], F32))
        lg = A_(sb("lg", [128, 16, 72], F32)); gm0 = A_(sb("gm0", [128, 16], F32)); dg = A_(sb("dg", [128, 16, 8], F32))
        eg = A_(sb("eg", [128, 16, 8], F32)); gsum = A_(sb("gsum", [128, 16], F32)); onehot = A_(sb("onehot", [128, 16, 8], F32))
        tmp = A_(sb("tmp88", [128, 16, 8, 8], F32)); leg = A_(sb("leg", [128, 16, 8], F32)); m8r = A_(sb("m8r", [128, 16, 8], F32))
        selloc = A_(sb("selloc", [128, 16, 8], F32)); wl = A_(sb("wl", [128, 16, 8], F32)); den = A_(sb("den", [128, 16], F32))
        wfull = A_(sb("wfull", [128, 16, 64], F32)); Af = A_(sb("Af", [128, 16, 64], F32)); Ab = A_(sb("Ab", [128, 16, 64], BF16))
        tot = A_(sb("tot", [128, 64], F32)); cnt = A_(sb("cnt", [128, 16, 64], F32)); key = A_(sb("key", [128, 16, 64], F32))
        m8k = A_(sb("m8k", [128, 16, 8], F32)); slf = A_(sb("slf", [128, 16, 2], F32)); eq = A_(sb("eq", [128, 16, 64], F32))
        tpf = A_(ps("tpf", [128, 2, 4, 128], F32)); lg_ps = A_(ps("lg_ps", [128, 2, 512], F32)); cnt_ps = A_(ps("cnt_ps", [128, 2, 512], F32))
        S.dma("sp", lambda e: e.dma_start(out=wr[:], in_=io["w_rt"].rearrange("(k p) n -> p k n", p=128)), writes=["wr"])
        for nm, t in [("b_rt", brt), ("lstrict", lstrict), ("ones", ones), ("ebase", ebase)]:
            S.dma("sp", lambda e, nm=nm, t=t: e.dma_start(out=t[:], in_=io[nm]), writes=[nm])
        S.op("dve", lambda e: e.memset(ss2[:], 0.0), writes=["ss2"])
        S.op("dve", lambda e: e.memset(tot[:], 0.0), writes=["tot"])
        V = lambda fn, r, w: S.op("dve", fn, reads=r, writes=w)
        for i in range(16):
            b = i % 2
            isl = slice(i * 128, (i + 1) * 128)
            S.dma("sp", lambda e, b=b, isl=isl: e.dma_start(out=x1[:, b], in_=X1[isl, :]), reads=["X1"], writes=[("x1", b)])
            S.op("act", lambda e, b=b, i=i: e.activation(out=junk[:], in_=x1[:, b], func=AF.Square, accum_out=ss2[:, i:i + 1]),
                 reads=[("x1", b), "ss2"], writes=["junk", ("ss2", i)])
            S.op("act", lambda e, i=i: e.activation(out=rstd2[:, i:i + 1], in_=ss2[:, i:i + 1], func=AF.Sqrt, bias=epsc[:, 0:1], scale=1.0 / D),
                 reads=[("ss2", i), "epsc"], writes=[("rstd2", i)])
            V(lambda e, i=i: e.reciprocal(out=rstd2[:, i:i + 1], in_=rstd2[:, i:i + 1]), [("rstd2", i)], [("rstd2", i)])
            V(lambda e, b=b, i=i: e.scalar_tensor_tensor(out=h2f[:, b], in0=x1[:, b], scalar=rstd2[:, i:i + 1], in1=a2row[:], op0=ALU.mult, op1=ALU.mult),
              [("x1", b), ("rstd2", i), "a2row"], [("h2f", b)])
            S.op("pool", lambda e, b=b: e.tensor_tensor(out=h2f[:, b], in0=h2f[:, b], in1=sh2row[:], op=ALU.add), reads=[("h2f", b), "sh2row"], writes=[("h2f", b)])
            S.op("act", lambda e, i=i, b=b: e.copy(out=h2b[:, i], in_=h2f[:, b]), reads=[("h2f", b)], writes=[("h2b", i)])
            for k4 in range(4):
                pb = k4 % 2
                for kk in range(4):
                    k = k4 * 4 + kk
                    S.op("pe", lambda e, pb=pb, kk=kk, k=k, b=b: e.transpose(tpf[:, pb, kk, :], h2f[:, b, k * 128:(k + 1) * 128], identf[:]),
                         reads=[("h2f", b), "identf"], writes=[("tpf", pb)])
                V(lambda e, pb=pb, k4=k4, b=b: e.tensor_copy(out=h2Tf[:, b, k4 * 4:(k4 + 1) * 4, :], in_=tpf[:, pb]), [("tpf", pb)], [("h2Tf", b)])
            for k in range(16):
                S.op("pe", lambda e, k=k, b=b: e.matmul(lg_ps[:, b, 0:72], lhsT=h2Tf[:, b, k, :], rhs=wr[:, k, :], start=(k == 0), stop=(k == 15)),
                     reads=[("h2Tf", b), "wr"], writes=[("lg_ps", b)])
            V(lambda e, i=i, b=b: e.tensor_tensor(out=lg[:, i, :], in0=lg_ps[:, b, 0:72], in1=brt[:], op=ALU.add), [("lg_ps", b), "b_rt"], ["lg"])
        B8 = lambda ap: ap.unsqueeze(2).to_broadcast([128, 16, 8])
        V(lambda e: e.tensor_reduce(out=gm0[:], in_=lg[:, :, 0:8], axis=AX.X, op=ALU.max), ["lg"], ["gm0"])
        V(lambda e: e.tensor_tensor(out=dg[:], in0=lg[:, :, 0:8], in1=B8(gm0[:]), op=ALU.subtract), ["lg", "gm0"], ["dg"])
        S.op("act", lambda e: e.activation(out=eg[:], in_=dg[:], func=AF.Exp), reads=["dg"], writes=["eg"])
        V(lambda e: e.tensor_reduce(out=gsum[:], in_=eg[:], axis=AX.X, op=ALU.add), ["eg"], ["gsum"])
        V(lambda e: e.tensor_scalar(out=onehot[:], in0=dg[:], scalar1=0.0, scalar2=None, op0=ALU.is_ge), ["dg"], ["onehot"])
        V(lambda e: e.tensor_tensor(out=tmp[:], in0=lg[:, :, 8:72].rearrange("p i (g j) -> p i g j", g=8),
                                    in1=onehot[:].unsqueeze(3).to_broadcast([128, 16, 8, 8]), op=ALU.mult), ["lg", "onehot"], ["tmp"])
        V(lambda e: e.tensor_reduce(out=leg[:], in_=tmp[:].rearrange("p i g j -> p i j g"), axis=AX.X, op=ALU.add), ["tmp"], ["leg"])
        for i in range(16):
            V(lambda e, i=i: e.max(out=m8r[:, i, :], in_=leg[:, i, :]), ["leg"], ["m8r"])
        V(lambda e: e.tensor_tensor(out=selloc[:], in0=leg[:], in1=m8r[:, :, 1:2].to_broadcast([128, 16, 8]), op=ALU.is_ge), ["leg", "m8r"], ["selloc"])
        V(lambda e: e.tensor_tensor(out=dg[:], in0=leg[:], in1=m8r[:, :, 0:1].to_broadcast([128, 16, 8]), op=ALU.subtract), ["leg", "m8r", "eg", "onehot"], ["dg2"])
        S.op("act", lambda e: e.activation(out=wl[:], in_=dg[:], func=AF.Exp), reads=["dg2"], writes=["wl"])
        V(lambda e: e.tensor_tensor(out=wl[:], in0=wl[:], in1=selloc[:], op=ALU.mult), ["wl", "selloc"], ["wl"])
        V(lambda e: e.tensor_reduce(out=den[:], in_=wl[:], axis=AX.X, op=ALU.add), ["wl"], ["den"])
        V(lambda e: e.tensor_tensor(out=den[:], in0=den[:], in1=gsum[:], op=ALU.mult), ["den", "gsum"], ["den"])
        V(lambda e: e.reciprocal(out=den[:], in_=den[:]), ["den"], ["den"])
        V(lambda e: e.tensor_tensor(out=wl[:], in0=wl[:], in1=B8(den[:]), op=ALU.mult), ["wl", "den"], ["wl"])
        V(lambda e: e.tensor_tensor(out=wfull[:].rearrange("p i (g j) -> p i g j", g=8), in0=onehot[:].unsqueeze(3).to_broadcast([128, 16, 8, 8]),
                                    in1=wl[:].unsqueeze(2).to_broadcast([128, 16, 8, 8]), op=ALU.mult), ["onehot", "wl"], ["wfull"])
        V(lambda e: e.tensor_scalar(out=Af[:], in0=wfull[:], scalar1=0.0, scalar2=None, op0=ALU.is_gt), ["wfull"], ["Af"])
        V(lambda e: e.tensor_copy(out=Ab[:], in_=Af[:]), ["Af"], ["Ab"])
        for i in range(16):
            b = i % 2
            S.op("pe", lambda e, i=i, b=b: e.matmul(cnt_ps[:, b, 0:64], lhsT=lstrict[:], rhs=Ab[:, i, :], start=True, stop=True),
                 reads=["lstrict", "Ab"], writes=[("cnt_ps", b)])
            S.op("pe", lambda e, i=i, b=b: e.matmul(cnt_ps[:, b, 64:128], lhsT=ones[:], rhs=Ab[:, i, :], start=True, stop=True),
                 reads=["ones", "Ab"], writes=[("cnt_ps", b)])
            V(lambda e, i=i, b=b: e.tensor_tensor(out=cnt[:, i, :], in0=cnt_ps[:, b, 0:64], in1=tot[:], op=ALU.add), [("cnt_ps", b), "tot"], ["cnt"])
            V(lambda e, b=b: e.tensor_tensor(out=tot[:], in0=cnt_ps[:, b, 64:128], in1=tot[:], op=ALU.add), [("cnt_ps", b), "tot"], ["tot"])
        V(lambda e: e.tensor_scalar(out=cnt[:], in0=cnt[:], scalar1=float(CAP - 1), scalar2=None, op0=ALU.min), ["cnt"], ["cnt"])
        V(lambda e: e.tensor_tensor(out=key[:], in0=cnt[:], in1=ebase[:].unsqueeze(1).to_broadcast([128, 16, 64]), op=ALU.add), ["cnt", "ebase"], ["key"])
        V(lambda e: e.tensor_tensor(out=key[:], in0=key[:], in1=Af[:], op=ALU.mult), ["key", "Af"], ["key"])
        for i in range(16):
            V(lambda e, i=i: e.max(out=m8k[:, i, :], in_=key[:, i, :]), ["key"], ["m8k"])
        V(lambda e: e.tensor_scalar(out=slf[:], in0=m8k[:, :, 0:2], scalar1=-1.0, scalar2=None, op0=ALU.add), ["m8k"], ["slf"])
        V(lambda e: e.tensor_copy(out=slots[:], in_=slf[:]), ["slf"], ["slots"])
        for j in range(2):
            V(lambda e, j=j: e.tensor_tensor(out=eq[:], in0=key[:], in1=m8k[:, :, j:j + 1].to_broadcast([128, 16, 64]), op=ALU.is_equal), ["key", "m8k"], ["eq"])
            V(lambda e: e.tensor_tensor(out=eq[:], in0=eq[:], in1=wfull[:], op=ALU.mult), ["eq", "wfull"], ["eq"])
            V(lambda e, j=j: e.tensor_reduce(out=wab[:, :, j], in_=eq[:], axis=AX.X, op=ALU.add), ["eq"], ["wab"])
        for i in range(16):
            for j in range(2):
                S.dma("pool", lambda e, i=i, j=j: e.indirect_dma_start(
                    out=XS, out_offset=bass.IndirectOffsetOnAxis(ap=slots[:, i, j:j + 1], axis=0), in_=h2b[:, i, :], in_offset=None),
                    reads=[("h2b", i), "slots"], writes=["XS"])
        S.barrier()


def stage_H(nc, S, io, XS, YS, identb):
    sb, ps = _namers(nc)
    with ExitStack() as _es:
        wg = _es.enter_context(sb("wg", [128, 2, 16, 512], BF16))
        wu = _es.enter_context(sb("wu", [128, 2, 16, 512], BF16))
        wd = _es.enter_context(sb("wd", [128, 2, 4, D], BF16))
        xs = _es.enter_context(sb("xs", [128, 2, RB, D], BF16))
        xsT = _es.enter_context(sb("xsT", [128, 2, 16, CAP], BF16))
        sgh = _es.enter_context(sb("sgh", [128, 2, CAP], F32))
        aT = _es.enter_context(sb("aT", [128, 4, CAP], BF16))
        ysb = _es.enter_context(sb("ysb", [128, 2, D], BF16))
        tpx = _es.enter_context(ps("tpx", [128, 4, 1024], BF16))
        gu_ps = _es.enter_context(ps("gu_ps", [128, 2, 2, CAP], F32))
        y_ps = _es.enter_context(ps("y_ps", [128, 2, 512], F32))

        def load(ex):
            wb = ex % 2
            S.dma("pool", lambda e: e.dma_start(out=wg[:, wb], in_=io["w_gate"][ex].rearrange("(k p) n -> p k n", p=128)), writes=[("wg", wb)])
            S.dma("pool", lambda e: e.dma_start(out=wu[:, wb], in_=io["w_up"][ex].rearrange("(k p) n -> p k n", p=128)), writes=[("wu", wb)])
            S.dma("pool", lambda e: e.dma_start(out=wd[:, wb], in_=io["w_down"][ex].rearrange("(c p) n -> p c n", p=128)), writes=[("wd", wb)])
            S.dma("sp", lambda e: e.dma_start(out=xs[:, wb], in_=XS[ex * CAP:(ex + 1) * CAP, :].rearrange("(r p) d -> p r d", p=128)),
                  reads=["XS"], writes=[("xs", wb)])

        def transposes(ex):
            wb = ex % 2
            for r in range(RB):
                for k8 in range(2):
                    j = (r * 2 + k8) % 4
                    for kk in range(8):
                        k = k8 * 8 + kk
                        S.op("pe", lambda e, kk=kk, k=k: e.transpose(tpx[:, j, kk * 128:(kk + 1) * 128], xs[:, wb, r, k * 128:(k + 1) * 128], identb[:]),
                             reads=[("xs", wb), "identb"], writes=[("tpx", j)])
                    dst = xsT[:, wb, k8 * 8:(k8 + 1) * 8, r * 128:(r + 1) * 128]
                    src = tpx[:, j, :].rearrange("p (a b) -> p a b", a=8)
                    if j % 2 == 0:
                        S.op("act", lambda e, dst=dst, src=src: e.copy(out=dst, in_=src), reads=[("tpx", j)], writes=[("xsT", wb)])
                    else:
                        S.op("dve", lambda e, dst=dst, src=src: e.tensor_copy(out=dst, in_=src), reads=[("tpx", j)], writes=[("xsT", wb)])

        def gate_up(ex):
            wb = ex % 2
            for hc in range(4):
                gb = hc % 2
                for which, w in enumerate([wg, wu]):
                    for k in range(16):
                        S.op("pe", lambda e, which=which, w=w, k=k: e.matmul(
                            gu_ps[:, gb, which, :], lhsT=w[:, wb, k, hc * 128:(hc + 1) * 128], rhs=xsT[:, wb, k, :], start=(k == 0), stop=(k == 15)),
                            reads=[("wg", wb), ("wu", wb), ("xsT", wb)], writes=[("gu_ps", gb)])
                S.op("act", lambda e: e.activation(out=sgh[:, gb, :], in_=gu_ps[:, gb, 0, :], func=AF.Silu), reads=[("gu_ps", gb)], writes=[("sgh", gb)])
                S.op("dve", lambda e: e.tensor_tensor(out=aT[:, hc, :], in0=sgh[:, gb, :], in1=gu_ps[:, gb, 1, :], op=ALU.mult),
                     reads=[("sgh", gb), ("gu_ps", gb)], writes=["aT"])

        ny = [0]

        def down(ex):
            wb = ex % 2
            for r in range(RB):
                yb = ny[0] % 2
                ny[0] += 1
                for cb in range(4):
                    pb = cb % 2
                    for hc in range(4):
                        S.op("pe", lambda e, hc=hc: e.matmul(
                            y_ps[:, pb, :], lhsT=aT[:, hc, r * 128:(r + 1) * 128], rhs=wd[:, wb, hc, cb * 512:(cb + 1) * 512], start=(hc == 0), stop=(hc == 3)),
                            reads=["aT", ("wd", wb)], writes=[("y_ps", pb)])
                    if cb % 2 == 0:
                        S.op("act", lambda e: e.copy(out=ysb[:, yb, cb * 512:(cb + 1) * 512], in_=y_ps[:, pb, :]), reads=[("y_ps", pb)], writes=[("ysb", yb)])
                    else:
                        S.op("dve", lambda e: e.tensor_copy(out=ysb[:, yb, cb * 512:(cb + 1) * 512], in_=y_ps[:, pb, :]), reads=[("y_ps", pb)], writes=[("ysb", yb)])
                S.dma("sp", lambda e: e.dma_start(out=YS[ex * CAP + r * 128:ex * CAP + (r + 1) * 128, :], in_=ysb[:, yb]),
                      reads=[("ysb", yb)], writes=["YS"])

        load(0)
        transposes(0)
        for ex in range(64):
            if ex + 1 < 64:
                load(ex + 1)
            gate_up(ex)
            if ex + 1 < 64:
                transposes(ex + 1)
            down(ex)
        S.barrier()


def stage_I(nc, S, io, X1, YS, g2row, slots, wab, out):
    sb, ps = _namers(nc)
    toks = []
    with ExitStack() as _es:
        ya = _es.enter_context(sb("ya", [128, 2, D], BF16))
        yb_ = _es.enter_context(sb("yb_", [128, 2, D], BF16))
        x1i = _es.enter_context(sb("x1i", [128, 2, D], F32))
        mo = _es.enter_context(sb("mo", [128, 2, D], F32))
        ot = _es.enter_context(sb("ot", [128, 2, D], F32))
        fg = _es.enter_context(sb("fg", [128, D], F32))
        junk = _es.enter_context(sb("junk3", [128, D], BF16))
        ss3 = _es.enter_context(sb("ss3", [128, 16], F32))
        rstd3 = _es.enter_context(sb("rstd3", [128, 16], F32))
        S.dma("sp", lambda e: e.dma_start(out=fg[:], in_=io["fg_row"]), writes=["fg"])
        S.op("dve", lambda e: e.memset(ss3[:], 0.0), writes=["ss3"])
        for i in range(16):
            b = i % 2
            isl = slice(i * 128, (i + 1) * 128)
            S.dma("pool", lambda e, b=b, i=i: e.indirect_dma_start(out=ya[:, b, :], out_offset=None, in_=YS,
                                                                   in_offset=bass.IndirectOffsetOnAxis(ap=slots[:, i, 0:1], axis=0)),
                  reads=["YS", "slots"], writes=[("ya", b)])
            S.dma("pool", lambda e, b=b, i=i: e.indirect_dma_start(out=yb_[:, b, :], out_offset=None, in_=YS,
                                                                   in_offset=bass.IndirectOffsetOnAxis(ap=slots[:, i, 1:2], axis=0)),
                  reads=["YS", "slots"], writes=[("yb", b)])
            S.dma("sp", lambda e, b=b, isl=isl: e.dma_start(out=x1i[:, b], in_=X1[isl, :]), reads=["X1"], writes=[("x1i", b)])
            S.op("dve", lambda e, b=b, i=i: e.tensor_scalar(out=mo[:, b], in0=ya[:, b], scalar1=wab[:, i, 0:1], scalar2=None, op0=ALU.mult),
                 reads=[("ya", b), "wab"], writes=[("mo", b)])
            S.op("dve", lambda e, b=b, i=i: e.scalar_tensor_tensor(out=mo[:, b], in0=yb_[:, b], scalar=wab[:, i, 1:2], in1=mo[:, b], op0=ALU.mult, op1=ALU.add),
                 reads=[("yb", b), "wab", ("mo", b)], writes=[("mo", b)])
            S.op("pool", lambda e, b=b: e.tensor_tensor(out=mo[:, b], in0=mo[:, b], in1=g2row[:], op=ALU.mult), reads=[("mo", b), "g2row"], writes=[("mo", b)])
            S.op("pool", lambda e, b=b: e.tensor_tensor(out=mo[:, b], in0=mo[:, b], in1=x1i[:, b], op=ALU.add), reads=[("mo", b), ("x1i", b)], writes=[("mo", b)])
            S.op("act", lambda e, i=i, b=b: e.activation(out=junk[:], in_=mo[:, b], func=AF.Square, accum_out=ss3[:, i:i + 1]), reads=[("mo", b), "ss3"], writes=["junk", ("ss3", i)])
            S.op("act", lambda e, i=i: e.activation(out=rstd3[:, i:i + 1], in_=ss3[:, i:i + 1], func=AF.Sqrt, bias=epsc[:, 0:1], scale=1.0 / D),
                 reads=[("ss3", i), "epsc"], writes=[("rstd3", i)])
            S.op("dve", lambda e, i=i: e.reciprocal(out=rstd3[:, i:i + 1], in_=rstd3[:, i:i + 1]), reads=[("rstd3", i)], writes=[("rstd3", i)])
            S.op("dve", lambda e, b=b, i=i: e.scalar_tensor_tensor(out=ot[:, b], in0=mo[:, b], scalar=rstd3[:, i:i + 1], in1=fg[:], op0=ALU.mult, op1=ALU.mult),
                 reads=[("mo", b), ("rstd3", i), "fg"], writes=[("ot", b)])
            toks.append(S.dma("sp", lambda e, b=b, isl=isl: e.dma_start(out=out[isl, :], in_=ot[:, b]), reads=[("ot", b)], writes=["out"]))
        S.barrier()
    return toks


def host_inputs(inp, b):
    f = lambda a: np.ascontiguousarray(a, dtype=np.float32)
    m = {}
    m["x"] = f(inp["x"][b])
    m["cT"] = f(inp["c"][b].reshape(16, 128).T)
    m["w_ada"] = f(inp["w_ada"][0])
    m["b_adaT"] = f(inp["b_ada"][0].reshape(96, 128).T)
    m["n1gT"] = f(inp["norm1_g"][0].reshape(16, 128).T)
    m["n2gT"] = f(inp["norm2_g"][0].reshape(16, 128).T)
    m["fg_row"] = f(np.broadcast_to(inp["final_g"][None, :], (128, D)))
    m["w_in"] = f(inp["w_in"][0])
    m["gng_row"] = f(np.broadcast_to(inp["ret_gn_g"][0][None, :], (128, 1024)))
    m["peT_k"] = f(inp["cmp_pos_k"][0].T)
    m["w1_k"] = f(inp["cmp_w1_k"][0])
    m["w2_k"] = f(inp["cmp_w2_k"][0])
    m["peT_v"] = f(inp["cmp_pos_v"][0].T)
    m["w1_v"] = f(inp["cmp_w1_v"][0])
    m["w2_v"] = f(inp["cmp_w2_v"][0])
    m["w_out"] = f(inp["w_out"][0])
    m["w_rt"] = f(np.concatenate([inp["w_grp"][0], inp["w_exp"][0]], axis=1))
    m["b_rt"] = f(np.broadcast_to(np.concatenate([inp["b_grp"][0], inp["b_exp"][0]])[None, :], (128, 72)))
    m["w_gate"] = f(inp["w_gate"][0])
    m["w_up"] = f(inp["w_up"][0])
    m["w_down"] = f(inp["w_down"][0])
    return m


def kernel(**inputs):
    inp = {k: np.asarray(v) for k, v in inputs.items()}
    nc = build_nc()
    consts = {k: v for k, v in make_consts().items() if not k.startswith("_")}
    shared = None
    in_maps = []
    for b in range(8):
        m = host_inputs(inp, b)
        if shared is None:
            shared = {k: m[k] for k in m if k not in ("x", "cT")}
        else:
            for k in shared:
                m[k] = shared[k]
        m.update(consts)
        in_maps.append(m)
    res = run_bass_kernel_spmd(nc, in_maps, core_ids=list(range(8)))
    return np.stack([np.asarray(r["out"], dtype=np.float32).reshape(T, D) for r in res.results], axis=0)
```
